# Optimizing a Trainium2 kernel written in Bass

```python
import jax
import jax.numpy as jnp
from jax import lax
import numpy as np

D_MODEL = 2048
BATCH = 16
SEQ = 2048
DEPTH = 4

N_GROUPS = 4
GROUP_WIDTH = D_MODEL // N_GROUPS
HEAD_DIM = 64
N_HEADS = GROUP_WIDTH // HEAD_DIM
D_FF = ((8 * D_MODEL + 3 * 256 - 1) // (3 * 256)) * 256

RET_CHUNK = 128
HGRN_CHUNK = 32
MLSTM_CHUNK = 64
CONV_WIDTH = 4
ROPE_BASE = 10000.0
RWKV_W_LORA = 64
RWKV_A_LORA = 64
RWKV_V_LORA = 32
RWKV_G_LORA = 128
RWKV_DECAY_SCALE = 0.6065306597126334
RWKV_LN_EPS = 64e-5
NORM_EPS = 1e-6

RET_SPLIT = (GROUP_WIDTH,) * 4
HGRN_SPLIT = (GROUP_WIDTH,) * 4
MLSTM_SPLIT = (GROUP_WIDTH,) * 4 + (N_HEADS, N_HEADS)
RWKV_SPLIT = (GROUP_WIDTH,) * 3 + (RWKV_W_LORA, RWKV_A_LORA, RWKV_G_LORA)
GROUP_COLS = (sum(RET_SPLIT), sum(HGRN_SPLIT), sum(MLSTM_SPLIT), sum(RWKV_SPLIT))
N_IN = sum(GROUP_COLS)

kernel_name = 'hybrid_parallel_groups_trunk'


def split_cols(t, sizes):
    return jnp.split(t, [int(i) for i in np.cumsum(sizes)[:-1]], axis=-1)


def rms_norm(x, w):
    xf = x.astype(jnp.float32)
    y = xf * lax.rsqrt(jnp.mean(xf * xf, axis=-1, keepdims=True) + NORM_EPS)
    return (y * w.astype(jnp.float32)).astype(x.dtype)


def to_heads(t):
    return t.reshape(t.shape[0], t.shape[1], N_HEADS, HEAD_DIM).transpose(0, 2, 1, 3)


def from_heads(t):
    return t.transpose(0, 2, 1, 3)


def head_rms(t, w=None):
    y = t * lax.rsqrt(jnp.mean(t * t, axis=-1, keepdims=True) + NORM_EPS)
    if w is not None:
        y = y * w.reshape(N_HEADS, HEAD_DIM)
    return y.reshape(t.shape[0], t.shape[1], -1)


def to_chunks(t, c):
    b, h, s = t.shape[:3]
    return jnp.moveaxis(t.reshape(b, h, s // c, c, *t.shape[3:]), 2, 0)


def from_chunks(t):
    t = jnp.moveaxis(t, 0, 2)
    return t.reshape(t.shape[0], t.shape[1], -1, t.shape[-1])


def rotary(t, pos):
    half = HEAD_DIM // 2
    inv_freq = ROPE_BASE ** (-jnp.arange(half, dtype=jnp.float32) / half)
    ang = pos[:, None] * inv_freq[None, :]
    cos, sin = jnp.cos(ang), jnp.sin(ang)
    t1, t2 = t[..., :half], t[..., half:]
    return jnp.concatenate([t1 * cos - t2 * sin, t1 * sin + t2 * cos], axis=-1)


def token_shift(t):
    return jnp.pad(t, ((0, 0), (1, 0), (0, 0)))[:, :-1]


def causal_dwconv(t, w, b):
    seq = t.shape[1]
    tp = jnp.pad(t, ((0, 0), (CONV_WIDTH - 1, 0), (0, 0)))
    out = b + tp[:, 0:seq] * w[0]
    for j in range(1, CONV_WIDTH):
        out = out + tp[:, j:j + seq] * w[j]
    return out


def chunked_retention(q, k, v):
    c = RET_CHUNK
    b, h, s, d = q.shape
    log_gamma = jnp.log1p(-jnp.exp2(-5.0 - jnp.arange(h, dtype=jnp.float32)))
    idx = jnp.arange(c, dtype=jnp.float32)
    rel = idx[:, None] - idx[None, :]
    intra = jnp.where(rel >= 0, jnp.exp(log_gamma[:, None, None] * jnp.maximum(rel, 0.0)), 0.0)
    q_dec = jnp.exp(log_gamma[:, None] * (idx + 1.0))[:, :, None]
    k_dec = jnp.exp(log_gamma[:, None] * (c - 1.0 - idx))[:, :, None]
    chunk_dec = jnp.exp(log_gamma * c)[:, None, None]

    def step(state, inp):
        qc, kc, vc = inp
        scores = jnp.einsum('bhtd,bhsd->bhts', qc, kc) * intra
        out = (jnp.einsum('bhts,bhse->bhte', scores, vc)
               + jnp.einsum('bhtd,bhde->bhte', qc * q_dec, state))
        state = chunk_dec * state + jnp.einsum('bhsd,bhse->bhde', kc * k_dec, vc)
        return state, out

    _, o = lax.scan(step, jnp.zeros((b, h, d, d), q.dtype),
                    (to_chunks(q, c), to_chunks(k, c), to_chunks(v, c)))
    return from_chunks(o)


def retention_group(p):
    q, k, v, g = split_cols(p, RET_SPLIT)
    pos = jnp.arange(p.shape[1], dtype=jnp.float32)
    qh = rotary(to_heads(q), pos)
    kh = rotary(to_heads(k), pos) * HEAD_DIM ** -0.5
    o = chunked_retention(qh, kh, to_heads(v))
    return head_rms(from_heads(o)) * jax.nn.silu(g)


def chunked_hgrn2(q, k, v, log_f):
    c = HGRN_CHUNK
    b, h, s, dk = q.shape
    dv = v.shape[-1]
    causal = jnp.tril(jnp.ones((c, c), dtype=bool))[:, :, None]

    def step(state, inp):
        qc, kc, vc, gc = inp
        cum = jnp.cumsum(gc, axis=2)
        pair = jnp.where(causal,
                         jnp.exp(jnp.minimum(cum[:, :, :, None, :] - cum[:, :, None, :, :], 0.0)), 0.0)
        scores = jnp.einsum('bhtd,bhsd,bhtsd->bhts', qc, kc, pair)
        out = (jnp.einsum('bhts,bhse->bhte', scores, vc)
               + jnp.einsum('bhtd,bhde->bhte', qc * jnp.exp(cum), state))
        cum_end = cum[:, :, -1:, :]
        state = (jnp.exp(cum_end)[:, :, 0, :, None] * state
                 + jnp.einsum('bhsd,bhse->bhde', kc * jnp.exp(cum_end - cum), vc))
        return state, out

    _, o = lax.scan(step, jnp.zeros((b, h, dk, dv), q.dtype),
                    (to_chunks(q, c), to_chunks(k, c), to_chunks(v, c), to_chunks(log_f, c)))
    return from_chunks(o)


def hgrn2_group(p, lb, norm_w):
    q, f, i, g = split_cols(p, HGRN_SPLIT)
    log_f = jnp.logaddexp(jnp.log(lb), jnp.log1p(-lb) + jax.nn.log_sigmoid(f))
    k = (1.0 - lb) * jax.nn.sigmoid(-f)
    o = chunked_hgrn2(to_heads(jax.nn.silu(q)) * HEAD_DIM ** -0.5, to_heads(k), to_heads(i), to_heads(log_f))
    return head_rms(from_heads(o), norm_w) * jax.nn.sigmoid(g)


def chunked_mlstm(q, k, v, log_i, log_f):
    c = MLSTM_CHUNK
    b, h, s, dk = q.shape
    dv = v.shape[-1]
    causal = jnp.tril(jnp.ones((c, c), dtype=bool))

    def step(carry, inp):
        cmem, nvec, m = carry
        qc, kc, vc, ic, fc = inp
        cum = jnp.cumsum(fc, axis=-1)
        logd = jnp.where(causal, cum[..., :, None] - cum[..., None, :] + ic[..., None, :], -jnp.inf)
        log_inter = cum + m[..., None]
        m_t = jnp.maximum(jnp.max(logd, axis=-1), log_inter)
        scores = jnp.einsum('bhtd,bhsd->bhts', qc, kc) * jnp.exp(logd - m_t[..., None])
        w_inter = jnp.exp(log_inter - m_t)
        num = (jnp.einsum('bhts,bhse->bhte', scores, vc)
               + w_inter[..., None] * jnp.einsum('bhtd,bhde->bhte', qc, cmem))
        den = jnp.sum(scores, axis=-1) + w_inter * jnp.einsum('bhtd,bhd->bht', qc, nvec)
        h_out = num / jnp.maximum(jnp.abs(den), jnp.exp(-m_t))[..., None]
        cum_end = cum[..., -1]
        log_w = cum_end[..., None] - cum + ic
        m_new = jnp.maximum(cum_end + m, jnp.max(log_w, axis=-1))
        decay = jnp.exp(cum_end + m - m_new)
        kw = kc * jnp.exp(log_w - m_new[..., None])[..., None]
        cmem = decay[..., None, None] * cmem + jnp.einsum('bhsd,bhse->bhde', kw, vc)
        nvec = decay[..., None] * nvec + jnp.sum(kw, axis=2)
        return (cmem, nvec, m_new), h_out

    init = (jnp.zeros((b, h, dk, dv), q.dtype), jnp.zeros((b, h, dk), q.dtype), jnp.zeros((b, h), q.dtype))
    _, o = lax.scan(step, init, (to_chunks(q, c), to_chunks(k, c), to_chunks(v, c),
                                 to_chunks(log_i, c), to_chunks(log_f, c)))
    return from_chunks(o)


def mlstm_group(p, conv_w, conv_b, i_bias, f_bias, norm_w):
    q, k, v, o, ig, fg = split_cols(p, MLSTM_SPLIT)
    qk = jax.nn.silu(causal_dwconv(jnp.concatenate([q, k], axis=-1), conv_w, conv_b))
    q, k = jnp.split(qk, 2, axis=-1)
    log_i = jnp.swapaxes(ig + i_bias, 1, 2)
    log_f = jnp.swapaxes(jax.nn.log_sigmoid(fg + f_bias), 1, 2)
    hm = chunked_mlstm(to_heads(q), to_heads(k) * HEAD_DIM ** -0.5, to_heads(v), log_i, log_f)
    return jax.nn.sigmoid(o) * head_rms(from_heads(hm), norm_w)


def rwkv7_scan(r, log_w, k, v, kk, a):
    b, s, h, d = r.shape
    xs = tuple(jnp.swapaxes(t, 0, 1) for t in (r, log_w, k, v, kk, a))

    def step(state, inp):
        rt, lwt, kt, vt, kkt, at = inp
        state = (state * jnp.exp(lwt)[:, :, None, :]
                 - jnp.einsum('bhvk,bhk->bhv', state, kkt)[..., None] * (at * kkt)[:, :, None, :]
                 + vt[..., None] * kt[:, :, None, :])
        return state, jnp.einsum('bhvk,bhk->bhv', state, rt)

    _, y = lax.scan(step, jnp.zeros((b, h, d, d), r.dtype), xs)
    return jnp.swapaxes(y, 0, 1)


def rwkv7_group(p, v_first, mu, w0, w_up, a0, a_up, g_up, k_k, k_a, r_k, ln_w, ln_b, vmix):
    b, s, _ = p.shape
    p = p + (token_shift(p) - p) * mu
    r, k, v, wd, ad, gd = split_cols(p, RWKV_SPLIT)
    log_w = -RWKV_DECAY_SCALE * jax.nn.sigmoid(w0 + jnp.tanh(wd) @ w_up)
    a = jax.nn.sigmoid(a0 + ad @ a_up)
    g = jax.nn.sigmoid(gd) @ g_up
    if vmix is None:
        v_first = v
    else:
        v0, v_down, v_up = vmix
        v = v + (v_first - v) * jax.nn.sigmoid(v0 + (v @ v_down) @ v_up)

    def hs(t):
        return t.reshape(b, s, N_HEADS, HEAD_DIM)

    kk = hs(k * k_k)
    kk = kk / jnp.maximum(jnp.sqrt(jnp.sum(kk * kk, axis=-1, keepdims=True)), 1e-12)
    k = k * (1.0 + (a - 1.0) * k_a)
    y = rwkv7_scan(hs(r), hs(log_w), hs(k), hs(v), kk, hs(a))
    mean = jnp.mean(y, axis=-1, keepdims=True)
    var = jnp.mean(jnp.square(y - mean), axis=-1, keepdims=True)
    y = ((y - mean) * lax.rsqrt(var + RWKV_LN_EPS)).reshape(b, s, -1) * ln_w + ln_b
    bonus = (jnp.sum(hs(r * k * r_k), axis=-1, keepdims=True) * hs(v)).reshape(b, s, -1)
    return (y + bonus) * g, v_first


def setup_inputs(seed: int = 0) -> dict:
    key = jax.random.key(seed)
    keys = jax.random.split(key, 31)
    counter = iter(range(31))
    G, H, L = GROUP_WIDTH, N_HEADS, DEPTH

    def nrm(shape, scale):
        return scale * jax.random.normal(keys[next(counter)], shape, jnp.float32)

    def gain(shape):
        return 1.0 + nrm(shape, 0.05)

    return {
        'x': nrm((BATCH, SEQ, D_MODEL), 1.0),
        'w_in': nrm((L, D_MODEL, N_IN), D_MODEL ** -0.5),
        'w_out': nrm((L, D_MODEL, D_MODEL), D_MODEL ** -0.5),
        'norm_pre_mix': gain((L, D_MODEL)),
        'norm_post_mix': gain((L, D_MODEL)),
        'norm_pre_ffn': gain((L, D_MODEL)),
        'norm_post_ffn': gain((L, D_MODEL)),
        'w_ffn_gate': nrm((L, D_MODEL, D_FF), D_MODEL ** -0.5),
        'w_ffn_up': nrm((L, D_MODEL, D_FF), D_MODEL ** -0.5),
        'w_ffn_down': nrm((L, D_FF, D_MODEL), D_FF ** -0.5),
        'hgrn_lb_logits': nrm((L, G), 0.5),
        'hgrn_norm_w': gain((L, G)),
        'mlstm_conv_w': nrm((L, CONV_WIDTH, 2 * G), CONV_WIDTH ** -0.5),
        'mlstm_conv_b': nrm((L, 2 * G), 0.02),
        'mlstm_i_bias': nrm((L, H), 0.1) - 1.0,
        'mlstm_f_bias': nrm((L, H), 0.5) + 3.0,
        'mlstm_norm_w': gain((L, G)),
        'rwkv_mu': jax.random.uniform(keys[next(counter)], (L, GROUP_COLS[3]), jnp.float32),
        'rwkv_w0': nrm((L, G), 0.5),
        'rwkv_w_up': nrm((L, RWKV_W_LORA, G), 0.5 * RWKV_W_LORA ** -0.5),
        'rwkv_a0': nrm((L, G), 0.5),
        'rwkv_a_up': nrm((L, RWKV_A_LORA, G), 0.5 * RWKV_A_LORA ** -0.5),
        'rwkv_g_up': nrm((L, RWKV_G_LORA, G), RWKV_G_LORA ** -0.5),
        'rwkv_k_k': gain((L, G)),
        'rwkv_k_a': gain((L, G)),
        'rwkv_r_k': nrm((L, G), 0.1),
        'rwkv_ln_w': gain((L, G)),
        'rwkv_ln_b': nrm((L, G), 0.02),
        'rwkv_v0': nrm((L - 1, G), 0.5),
        'rwkv_v_down': nrm((L - 1, G, RWKV_V_LORA), G ** -0.5),
        'rwkv_v_up': nrm((L - 1, RWKV_V_LORA, G), 0.5 * RWKV_V_LORA ** -0.5),
    }


def reference(x, w_in, w_out, norm_pre_mix, norm_post_mix, norm_pre_ffn, norm_post_ffn,
              w_ffn_gate, w_ffn_up, w_ffn_down, hgrn_lb_logits, hgrn_norm_w,
              mlstm_conv_w, mlstm_conv_b, mlstm_i_bias, mlstm_f_bias, mlstm_norm_w,
              rwkv_mu, rwkv_w0, rwkv_w_up, rwkv_a0, rwkv_a_up, rwkv_g_up,
              rwkv_k_k, rwkv_k_a, rwkv_r_k, rwkv_ln_w, rwkv_ln_b,
              rwkv_v0, rwkv_v_down, rwkv_v_up):
    lb_cum = jnp.cumsum(jax.nn.softmax(hgrn_lb_logits.astype(jnp.float32), axis=0), axis=0)
    lower_bounds = lb_cum - lb_cum[0]
    v_first = None
    h = x
    for l in range(DEPTH):
        u = rms_norm(h, norm_pre_mix[l])
        p = (u @ w_in[l]).astype(jnp.float32)
        p_ret, p_hgrn, p_ml, p_rw = split_cols(p, GROUP_COLS)
        o_ret = retention_group(p_ret)
        o_hgrn = hgrn2_group(p_hgrn, lower_bounds[l], hgrn_norm_w[l])
        o_ml = mlstm_group(p_ml, mlstm_conv_w[l], mlstm_conv_b[l], mlstm_i_bias[l],
                           mlstm_f_bias[l], mlstm_norm_w[l])
        vmix = None if l == 0 else (rwkv_v0[l - 1], rwkv_v_down[l - 1], rwkv_v_up[l - 1])
        o_rw, v_first = rwkv7_group(p_rw, v_first, rwkv_mu[l], rwkv_w0[l], rwkv_w_up[l],
                                    rwkv_a0[l], rwkv_a_up[l], rwkv_g_up[l], rwkv_k_k[l],
                                    rwkv_k_a[l], rwkv_r_k[l], rwkv_ln_w[l], rwkv_ln_b[l], vmix)
        mixed = jnp.concatenate([o_ret, o_hgrn, o_ml, o_rw], axis=-1).astype(h.dtype) @ w_out[l]
        h = h + rms_norm(mixed, norm_post_mix[l])
        u = rms_norm(h, norm_pre_ffn[l])
        ffn = (jax.nn.silu(u @ w_ffn_gate[l]) * (u @ w_ffn_up[l])) @ w_ffn_down[l]
        h = h + rms_norm(ffn, norm_post_ffn[l])
    return h
```

```python
from contextlib import ExitStack
import numpy as np
import ml_dtypes
import concourse.bass as bass
import concourse.mybir as mybir
from concourse.bass_utils import run_bass_kernel_spmd

F32 = mybir.dt.float32
BF16 = mybir.dt.bfloat16
ALU = mybir.AluOpType
AF = mybir.ActivationFunctionType
AX = mybir.AxisListType

D = 2048
NIN = 7952
DFF = 5632
G = 512
NH = 8
HD = 64
NORM_EPS = 1e-6


class Prog:
    ENG = ("pe", "dve", "act", "pool", "sp")
    NDMASEM = 6

    def __init__(self, nc, stack):
        self.nc = nc
        self.stack = stack
        self.ops = {e: [] for e in self.ENG}
        self.cnt = {e: 0 for e in self.ENG}
        self.known = {e: {} for e in self.ENG}
        self.sem = {e: stack.enter_context(nc.semaphore("s_" + e)) for e in self.ENG if e != "sp"}
        self.semname = {}
        for e, s in self.sem.items():
            self.semname[id(s)] = e
        self.dq = {}
        for q in ("sp", "pool", "act"):
            self.dq[q] = {"sems": [stack.enter_context(nc.semaphore(f"d_{q}{i}")) for i in range(self.NDMASEM)],
                          "n": 0}
        self.bufs = {}
        self.nops = 0

    def _buf(self, k):
        b = self.bufs.get(k)
        if b is None:
            b = [None, {}]
            self.bufs[k] = b
        return b

    def _deps(self, e, reads, writes, extra=()):
        toks = list(extra)
        for k in reads:
            b = self._buf(k)
            if b[0] is not None:
                toks.append((b[0], False))
        for k in writes:
            b = self._buf(k)
            if b[0] is not None:
                toks.append((b[0], False))
            for s, v in b[1].values():
                toks.append(((s, v), True))
        own = self.sem.get(e)
        waits = []
        kn = self.known[e]
        for (s, v), is_reader in toks:
            if s is own and (e == "pe" or is_reader):
                continue
            if kn.get(id(s), 0) >= v:
                continue
            kn[id(s)] = v
            waits.append((s, v))
        return waits

    def _commit(self, tok, reads, writes):
        s, v = tok
        for k in reads:
            b = self._buf(k)
            b[1][id(s)] = (s, v)
        for k in writes:
            b = self._buf(k)
            b[0] = tok
            b[1] = {}

    def op(self, e, fn, reads=(), writes=()):
        reads = tuple(reads)
        writes = tuple(writes)
        waits = self._deps(e, reads, writes)
        self.cnt[e] += 1
        tok = (self.sem[e], self.cnt[e])
        self.ops[e].append((waits, fn, (self.sem[e], 1)))
        self._commit(tok, reads, writes)
        self.nops += 1
        return tok

    def dma(self, q, out, in_, reads=(), writes=(), **kw):
        reads = tuple(reads)
        writes = tuple(writes)
        dq = self.dq[q]
        n = dq["n"]
        dq["n"] += 1
        s = dq["sems"][n % self.NDMASEM]
        prev = 16 * (n // self.NDMASEM)
        extra = [((s, prev), False)] if prev > 0 else []
        waits = self._deps(q, reads, writes, extra)
        tok = (s, prev + 16)
        self.ops[q].append((waits, lambda eng: eng.dma_start(out=out, in_=in_, **kw), (s, 16)))
        self._commit(tok, reads, writes)
        self.nops += 1
        return tok

    def barrier(self):
        toks = [(self.sem[e], self.cnt[e]) for e in self.sem if self.cnt[e] > 0] + self.all_dma_tokens()
        for e in self.ENG:
            waits = []
            kn = self.known[e]
            for s, v in toks:
                if e == "pe" and s is self.sem.get("pe"):
                    continue
                if kn.get(id(s), 0) >= v:
                    continue
                kn[id(s)] = v
                waits.append((s, v))
            if waits:
                self.ops[e].append((waits, None, None))
        self.bufs = {}

    def finish(self, e, toks):
        waits = []
        for s, v in toks:
            waits.append((s, v))
        self.ops[e].append((waits, None, None))

    def all_dma_tokens(self):
        toks = []
        for q, dq in self.dq.items():
            n = dq["n"]
            for i in range(self.NDMASEM):
                c = (n - i + self.NDMASEM - 1) // self.NDMASEM if n > i else 0
                if c > 0:
                    toks.append((dq["sems"][i], 16 * c))
        return toks

    def emit(self):
        nc = self.nc
        ops = self.ops

        def run(eng, lst):
            for waits, fn, inc in lst:
                for s, v in waits:
                    eng.wait_ge(s, v)
                if fn is not None:
                    ins = fn(eng)
                    ins.then_inc(inc[0], inc[1])

        with nc.Block() as block:
            @block.tensor
            def _(eng):
                run(eng, ops["pe"])

            @block.vector
            def _(eng):
                run(eng, ops["dve"])

            @block.scalar
            def _(eng):
                run(eng, ops["act"])

            @block.gpsimd
            def _(eng):
                run(eng, ops["pool"])

            @block.sync
            def _(eng):
                run(eng, ops["sp"])


RWC = 1792
OFFS = {"A": 0, "B": 2048, "C": 4096, "D": 6160}
VECS = {"A": ["qA", "kA"], "B": ["qB", "kB", "wB"], "C": ["qC", "kC", "wC"], "D": ["rD", "kD", "wD", "kkD", "bD"]}
ALLV = [v for m in "ABCD" for v in VECS[m]]
TB = 16
SMALL = ["hgrn_norm_w", "mlstm_norm_w", "rwkv_w0", "rwkv_a0", "rwkv_k_k", "rwkv_k_a", "rwkv_r_k", "rwkv_ln_w",
         "rwkv_ln_b"]


class Cfg:
    def __init__(self, T=2048, NSEQ=2, L=4):
        self.T = T
        self.NSEQ = NSEQ
        self.L = L
        self.NT = T * NSEQ
        self.TPS = T // 128
        self.NTILE = self.NT // 128


def wb_shape(K, N, CB=512):
    ncb = (N + CB - 1) // CB
    return [ncb, 128, K // 128, CB]


class Builder:
    def __init__(self, cfg):
        self.cfg = cfg
        self.nc = bass.Bass("TRN2", target_bir_lowering=False)
        self.stack = ExitStack()
        self.P = None

    def dram_in(self, name, shape, dt=F32):
        return self.nc.dram_tensor(name, list(shape), dt, kind="ExternalInput").ap()

    def dram_out(self, name, shape, dt=F32):
        return self.nc.dram_tensor(name, list(shape), dt, kind="ExternalOutput").ap()

    def dram_tmp(self, name, shape, dt=F32):
        return self.nc.dram_tensor(name, list(shape), dt, kind="Internal").ap()

    def sb(self, name, shape, dt=F32):
        h = self.stack.enter_context(self.nc.sbuf_tensor(name, list(shape), dt))
        return h[tuple(slice(None) for _ in shape)]

    def ps(self, name, shape, dt=F32):
        return self.stack.enter_context(self.nc.psum_tensor(name, list(shape), dt))

    def carve_reset(self):
        self.cptr = 0

    def carve(self, ncols, dt=F32):
        a = self.arena[:, self.cptr:self.cptr + ncols]
        self.cptr += ncols
        assert self.cptr <= self.NARENA, self.cptr
        if dt == BF16:
            a = a.bitcast(BF16)
        return a

    def tt(self, out, a, b, op, r, w, eng="dve"):
        self.P.op(eng, lambda e: e.tensor_tensor(out=out, in0=a, in1=b, op=op), reads=r, writes=w)

    def ts(self, out, a, s1, s2, op0, op1, r, w, eng="dve"):
        if op1 is None:
            self.P.op(eng, lambda e: e.tensor_scalar(out=out, in0=a, scalar1=s1, scalar2=None, op0=op0), reads=r, writes=w)
        else:
            self.P.op(eng, lambda e: e.tensor_scalar(out=out, in0=a, scalar1=s1, scalar2=s2, op0=op0, op1=op1),
                      reads=r, writes=w)

    def stt(self, out, a, scalar, b, op0, op1, r, w):
        self.P.op("dve", lambda e: e.scalar_tensor_tensor(out=out, in0=a, scalar=scalar, in1=b, op0=op0, op1=op1),
                  reads=r, writes=w)

    def actf(self, out, a, func, r, w, scale=1.0, bias=None):
        if bias is None:
            self.P.op("act", lambda e: e.activation(out=out, in_=a, func=func, scale=scale), reads=r, writes=w)
        else:
            self.P.op("act", lambda e: e.activation(out=out, in_=a, func=func, scale=scale, bias=bias), reads=r, writes=w)

    def red(self, out, a, r, w):
        self.P.op("dve", lambda e: e.tensor_reduce(out=out, in_=a, axis=AX.X, op=ALU.add), reads=r, writes=w)

    def cp(self, out, a, r, w, eng="act"):
        if eng == "act":
            self.P.op("act", lambda e: e.copy(out=out, in_=a), reads=r, writes=w)
        else:
            self.P.op(eng, lambda e: e.tensor_copy(out=out, in_=a), reads=r, writes=w)

    def psbank(self):
        b = self.psel % self.NPS
        self.psel += 1
        return b

    def cast_weight(self, W, Wb, K, N, CB=512):
        P = self.P
        KC = K // 128
        ncb = (N + CB - 1) // CB
        Wv = W.rearrange("(kc p) n -> p kc n", p=128)
        for cb in range(ncb):
            c0 = cb * CB
            nc_ = min(CB, N - c0)
            for k0 in range(0, KC, 16):
                k1 = min(KC, k0 + 16)
                P.dma("pool", Wb[cb, :, k0:k1, 0:nc_], Wv[:, k0:k1, c0:c0 + nc_], reads=(), writes=(("wb", id(Wb), cb),))

    def linear_tm(self, xT, xkeys, ng, KC, Wb, N, consume, CB=512):
        P = self.P
        ncb = (N + CB - 1) // CB
        for cb in range(ncb):
            nc_ = min(CB, N - cb * CB)
            b = self.wsel % 2
            self.wsel += 1
            wbuf = self.wbuf[b][:, 0:KC * CB].rearrange("p (k n) -> p k n", n=CB)
            P.dma("sp", wbuf[:, :, 0:nc_], Wb[cb, :, 0:KC, 0:nc_], reads=(("wb", id(Wb), cb),), writes=(("wbuf", b),))
            for g in range(ng):
                bank = self.psbank()
                pst = self.pslin[:, bank, 0:nc_]
                for kc in range(KC):
                    P.op("pe", (lambda e, pst=pst, g=g, kc=kc, wbuf=wbuf, nc_=nc_:
                                e.matmul(pst, lhsT=xT[:, kc, g, :], rhs=wbuf[:, kc, 0:nc_],
                                         start=(kc == 0), stop=(kc == KC - 1))),
                         reads=tuple(xkeys) + (("wbuf", b),), writes=(("pslin", bank),))
                consume(g, cb, nc_, pst, ("pslin", bank))

    def rstd_of(self, src, src_key, n, eps_ap):
        P = self.P
        ss = self.ss_t
        junk = self.junk
        P.op("act", lambda e: e.activation(out=junk[:, 0:n], in_=src, func=AF.Square, accum_out=ss[:, 0:1]),
             reads=(src_key,), writes=("junk", "ss"))
        P.op("act", lambda e: e.activation(out=ss[:, 1:2], in_=ss[:, 0:1], func=AF.Sqrt, scale=1.0 / n, bias=eps_ap),
             reads=("ss", "eps"), writes=("ss1",))
        P.op("dve", lambda e: e.reciprocal(out=ss[:, 2:3], in_=ss[:, 1:2]), reads=("ss1",), writes=("ss2",))

    def norm_transpose(self, src_tile, src_key, wn_b, xT, g, xkey):
        self.rstd_of(src_tile, src_key, 2048, self.eps_t[:, 0:1])
        ub = self.ub
        self.stt(ub[:, 0:2048], src_tile, self.ss_t[:, 2:3], wn_b, ALU.mult, ALU.mult, (src_key, "ss2", "wnb"), ("ub",))
        self.transpose_to(ub, "ub", xT, g, xkey, 16)

    def transpose_to(self, ub, ubkey, xT, g, xkey, KC):
        P = self.P
        for k0 in range(0, KC, 4):
            k1 = min(KC, k0 + 4)
            bank = self.tsel % 2
            self.tsel += 1
            pt = self.pstr[:, bank, :]
            for kc in range(k0, k1):
                P.op("pe", (lambda e, kc=kc, pt=pt, k0=k0:
                            e.transpose(pt[:, (kc - k0) * 128:(kc - k0 + 1) * 128], ub[:, kc * 128:(kc + 1) * 128],
                                        self.ident_b[:, :])),
                     reads=(ubkey, "ident"), writes=(("pstr", bank),))
            n = k1 - k0
            P.op("act", (lambda e, pt=pt, k0=k0, n=n:
                         e.copy(out=xT[:, k0:k0 + n, g, :], in_=pt[:, 0:n * 128].rearrange("p (k t) -> p k t", t=128))),
                 reads=(("pstr", bank),), writes=(xkey,))

    def store_T(self, src, src_key, dstT, s, g):
        P = self.P
        vt = self.vt
        for hp in range(4):
            bank = self.psbank()
            pt = self.pslin[:, bank, 0:128]
            P.op("pe", lambda e, pt=pt, hp=hp: e.transpose(pt, src[:, hp * 128:(hp + 1) * 128], self.ident_f[:, :]),
                 reads=(src_key, "identf"), writes=(("pslin", bank),))
            self.cp(vt[:, hp, :], pt, (("pslin", bank),), ("vt",))
        P.dma("sp", dstT[:, :, s, g * 128:(g + 1) * 128].rearrange("hp p t -> p hp t"), vt[:, :, :], reads=("vt",),
              writes=(("T", id(dstT), s, g),))

    def load_T(self, dst, dst_key, srcT, s, g):
        P = self.P
        vt = self.vt
        P.dma("sp", vt[:, :, :], srcT[:, :, s, g * 128:(g + 1) * 128].rearrange("hp p t -> p hp t"),
              reads=(("T", id(srcT), s, g),), writes=("vt",))
        for hp in range(4):
            bank = self.psbank()
            pt = self.pslin[:, bank, 0:128]
            P.op("pe", lambda e, pt=pt, hp=hp: e.transpose(pt, vt[:, hp, :], self.ident_f[:, :]),
                 reads=("vt", "identf"), writes=(("pslin", bank),))
            self.cp(dst[:, hp * 128:(hp + 1) * 128], pt, (("pslin", bank),), (dst_key,))

    def load_shift(self, dst, key, c0, n, tile, k, extra_keys=()):
        P = self.P
        keys = (key,) + tuple(extra_keys)
        cfg = self.cfg
        g = tile % cfg.TPS
        r0 = tile * 128
        if g == 0:
            P.op("dve", lambda e: e.memset(dst[:, 0:n], 0.0), writes=keys)
            P.dma("sp", dst[k:128, 0:n], self.pbuf[r0:r0 + 128 - k, c0:c0 + n],
                  reads=tuple(("p", tile, cb) for cb in range(16)), writes=keys)
        else:
            P.dma("sp", dst[:, 0:n], self.pbuf[r0 - k:r0 - k + 128, c0:c0 + n],
                  reads=tuple(("p", tile, cb) for cb in range(16)) + tuple(("p", tile - 1, cb) for cb in range(16)),
                  writes=keys)

    def bcast_load(self, dst, src_row, key):
        self.P.dma("sp", dst, src_row.partition_broadcast(128), writes=(key,))

    def build(self, dbg=None, layers=None):
        cfg = self.cfg
        nc = self.nc
        L = cfg.L
        NT = cfg.NT
        T = cfg.T
        NSEQ = cfg.NSEQ
        self.P = P = Prog(nc, self.stack)
        layers = list(range(L)) if layers is None else layers
        I = {}
        x = self.dram_in("x", [NT, D])
        out = self.dram_out("out", [NT, D])
        w_in = self.dram_in("w_in", [L, D, NIN])
        w_out = self.dram_in("w_out", [L, D, D])
        w_g = self.dram_in("w_ffn_gate", [L, D, DFF])
        w_u = self.dram_in("w_ffn_up", [L, D, DFF])
        w_d = self.dram_in("w_ffn_down", [L, DFF, D])
        norms = self.dram_in("norms", [L, 4, D])
        for nm in SMALL:
            I[nm] = self.dram_in(nm, [L, G])
        I["hgrn_lb_logits"] = self.dram_in("hgrn_lb_logits", [L, G])
        I["mlstm_conv_w"] = self.dram_in("mlstm_conv_w", [L, 4, 2 * G])
        I["mlstm_conv_b"] = self.dram_in("mlstm_conv_b", [L, 2 * G])
        I["mlstm_if_bias"] = self.dram_in("mlstm_if_bias", [L, 16])
        I["rwkv_mu"] = self.dram_in("rwkv_mu", [L, RWC])
        I["rwkv_up_pad"] = self.dram_in("rwkv_up_pad", [L, 128, 1024])
        I["rwkv_g_up"] = self.dram_in("rwkv_g_up", [L, 128, G])
        I["rwkv_v0"] = self.dram_in("rwkv_v0", [max(L - 1, 1), G])
        I["rwkv_v_down"] = self.dram_in("rwkv_v_down", [max(L - 1, 1), G, 32])
        I["rwkv_v_up"] = self.dram_in("rwkv_v_up", [max(L - 1, 1), 32, G])
        ident_b_d = self.dram_in("ident_b", [128, 128], BF16)
        ident_f_d = self.dram_in("ident_f", [128, 128])
        cs_d = self.dram_in("cossin", [T, 64])
        sel_d = self.dram_in("sel", [32, TB, 128])
        gam_d = self.dram_in("gamA", [128, 512])

        hbuf = self.dram_tmp("hbuf", [NT, D])
        self.pbuf = pbuf = self.dram_tmp("pbuf", [NT, NIN])
        obuf = self.dram_tmp("obuf", [NT, D])
        rows = {v: self.dram_tmp("row_" + v, [NT, G]) for v in ALLV}
        vT = {m: self.dram_tmp("vT_" + m, [4, 128, NSEQ, T]) for m in "ABCD"}
        yT = {m: self.dram_tmp("yT_" + m, [4, 128, NSEQ, T]) for m in "ABCD"}
        denT = self.dram_tmp("denT", [4, 128, NSEQ, T])
        gate = {m: self.dram_tmp("gate_" + m, [NT, G]) for m in "ABCD"}
        bonus = self.dram_tmp("bonus", [NT, G])
        vfirst = self.dram_tmp("vfirst", [NT, G])
        wb_in = [self.dram_tmp(f"wb_in{l}", wb_shape(D, NIN), BF16) for l in range(L)]
        wb_out = [self.dram_tmp(f"wb_out{l}", wb_shape(D, D), BF16) for l in range(L)]
        wb_g = [self.dram_tmp(f"wb_g{l}", wb_shape(D, DFF), BF16) for l in range(L)]
        wb_u = [self.dram_tmp(f"wb_u{l}", wb_shape(D, DFF), BF16) for l in range(L)]
        wb_d = [self.dram_tmp(f"wb_d{l}", wb_shape(DFF, D, 128), BF16) for l in range(L)]
        dbg_t = None
        if dbg is not None:
            dbg_t = self.dram_out("dbg", dbg[1])

        self.ident_b = self.sb("ident_b_s", [128, 128], BF16)
        self.ident_f = self.sb("ident_f_s", [128, 128])
        self.ss_t = self.sb("ss", [128, 8])
        self.eps_t = self.sb("eps_t", [128, 4])
        gamA = self.sb("gamA_s", [128, 512])
        lbd = self.dram_tmp("lbd", [2, 128, L, G])
        wnb = self.sb("wnb", [128, 4, D])
        self.NARENA = 41000
        self.arena = self.sb("arena", [128, self.NARENA])
        self.NPS = 6
        self.pslin = self.ps("pslin", [128, self.NPS, 512])
        self.psel = 0
        self.pstr = self.ps("pstr", [128, 2, 1024], BF16)
        self.tsel = 0
        self.wsel = 0

        P.dma("sp", self.ident_b[:, :], ident_b_d[:, :], writes=("ident",))
        P.dma("sp", self.ident_f[:, :], ident_f_d[:, :], writes=("identf",))
        P.dma("sp", gamA[:, :], gam_d[:, :], writes=("gamA",))
        P.op("dve", lambda e: e.memset(self.eps_t[:, 0:1], NORM_EPS), writes=("eps",))
        P.op("dve", lambda e: e.memset(self.eps_t[:, 1:2], 64e-5), writes=("eps",))
        P.op("dve", lambda e: e.memset(self.eps_t[:, 2:3], 0.0), writes=("eps",))
        for i in range(0, NT, 512):
            P.dma("sp", hbuf[i:i + 512, :], x[i:i + 512, :], writes=tuple(("h", j) for j in range(i // 128, i // 128 + 4)))
        for l in layers:
            self.cast_weight(w_in[l], wb_in[l], D, NIN)
            self.cast_weight(w_out[l], wb_out[l], D, D)
            self.cast_weight(w_g[l], wb_g[l], D, DFF)
            self.cast_weight(w_u[l], wb_u[l], D, DFF)
            self.cast_weight(w_d[l], wb_d[l], DFF, D, 128)

        self.carve_reset()
        ex = self.carve(L * G).rearrange("p (l g) -> p l g", g=G)
        sm = self.carve(G)
        lbt = self.carve(L * G).rearrange("p (l g) -> p l g", g=G)
        oml = self.carve(L * G).rearrange("p (l g) -> p l g", g=G)
        P.dma("sp", ex, I["hgrn_lb_logits"].partition_broadcast(128), writes=("ex",))
        self.actf(ex, ex, AF.Exp, ("ex",), ("ex",))
        self.cp(sm, ex[:, 0, :], ("ex",), ("sm",), eng="dve")
        for j in range(1, L):
            self.tt(sm, sm, ex[:, j, :], ALU.add, ("sm", "ex"), ("sm",))
        P.op("dve", lambda e: e.reciprocal(out=sm, in_=sm), reads=("sm",), writes=("sm",))
        P.op("dve", lambda e: e.memset(lbt[:, 0, :], 0.0), writes=("lbt",))
        for j in range(1, L):
            self.tt(ex[:, j, :], ex[:, j, :], sm, ALU.mult, ("ex", "sm"), ("ex",))
            self.tt(lbt[:, j, :], lbt[:, j - 1, :], ex[:, j, :], ALU.add, ("lbt", "ex"), ("lbt",))
        self.ts(oml[:, :, :], lbt[:, :, :], -1.0, 1.0, ALU.mult, ALU.add, ("lbt",), ("oml",))
        P.dma("sp", lbd[0], lbt, reads=("lbt",), writes=("lbd",))
        P.dma("sp", lbd[1], oml, reads=("oml",), writes=("lbd",))
        P.barrier()

        for l in layers:
            P.dma("sp", wnb[:, :, :], norms[l].partition_broadcast(128), writes=("wnb",))
            self.phase_p1(l, hbuf, wb_in[l], wnb)
            P.barrier()
            if dbg is not None and dbg[0] == "p" and l == layers[-1]:
                break
            self.phase_prep(l, I, rows, vT, gate, bonus, vfirst, cs_d, lbd)
            P.barrier()
            if dbg is not None and dbg[0] == "prep" and l == layers[-1]:
                break
            self.phase_scan(rows, vT, yT, denT, sel_d, gamA)
            P.barrier()
            if dbg is not None and dbg[0] == "scan" and l == layers[-1]:
                break
            self.phase_post(l, I, yT, denT, gate, bonus, obuf)
            P.barrier()
            if dbg is not None and dbg[0] == "post" and l == layers[-1]:
                break
            self.phase_ffn(l, hbuf, obuf, wb_out[l], wb_g[l], wb_u[l], wb_d[l], wnb)
            P.barrier()

        if dbg is not None:
            src = {"p": pbuf, "post": obuf, "h": hbuf}.get(dbg[0])
            if src is None:
                src = dbg[2](locals())
            P.dma("sp", dbg_t, src)
        for i in range(0, NT, 512):
            P.dma("sp", out[i:i + 512, :], hbuf[i:i + 512, :])
        P.finish("sp", P.all_dma_tokens())
        P.emit()
        return nc

    def carve_linear(self, ng):
        self.carve_reset()
        self.xT = self.carve(16 * ng * 64, BF16).rearrange("p (k g t) -> p k g t", g=ng, t=128)
        self.wbuf = [self.carve(4096, BF16) for _ in range(2)]
        self.htile = [self.carve(2048) for _ in range(2)]
        self.junk = self.carve(2048)
        self.ub = self.carve(1024, BF16)
        self.stage = [self.carve(512) for _ in range(4)]
        self.ssel = 0

    def phase_p1(self, l, hbuf, wb, wnb):
        P = self.P
        cfg = self.cfg
        NG = 4
        self.carve_linear(NG)
        for grp in range(cfg.NTILE // NG):
            for g in range(NG):
                tile = grp * NG + g
                ht = self.htile[tile % 2]
                P.dma("sp", ht, hbuf[tile * 128:(tile + 1) * 128, :], reads=(("h", tile),), writes=(("htile", tile % 2),))
                self.norm_transpose(ht, ("htile", tile % 2), wnb[:, 0, :], self.xT, g, "xT")

            def consume(g, cb, nc_, pst, pskey, grp=grp):
                st = self.stage[self.ssel % 4]
                skey = ("stage", self.ssel % 4)
                self.ssel += 1
                self.cp(st[:, 0:nc_], pst, (pskey,), (skey,))
                tile = grp * NG + g
                P.dma("sp", self.pbuf[tile * 128:(tile + 1) * 128, cb * 512:cb * 512 + nc_], st[:, 0:nc_],
                      reads=(skey,), writes=(("p", tile, cb),))

            self.linear_tm(self.xT, ("xT",), NG, 16, wb, NIN, consume)

    def phase_prep(self, l, I, rows, vT, gate, bonus, vfirst, cs_d, lbd):
        P = self.P
        cfg = self.cfg
        self.carve_reset()
        seg = self.carve(2064)
        sh = self.carve(3072)
        Wt = [self.carve(512) for _ in range(10)]
        R = self.carve(1024)
        cst = self.carve(64)
        self.vt = self.carve(512).rearrange("p (a b) -> p a b", b=128)
        cw = self.carve(4096).rearrange("p (j c) -> p j c", c=1024)
        cb_ = self.carve(1024)
        mu = self.carve(RWC)
        sv = {nm: self.carve(512) for nm in SMALL}
        v0b = self.carve(512)
        ifb = self.carve(16)
        upw = self.carve(1024)
        gup = self.carve(512)
        vdn = self.carve(128).rearrange("p (c n) -> p c n", n=32)
        vup = self.carve(512)
        small = self.carve(64)
        lbt = self.carve(512)
        oml = self.carve(512)
        P.dma("sp", lbt, lbd[0, :, l, :], writes=("lbt",))
        P.dma("sp", oml, lbd[1, :, l, :], writes=("oml",))
        for nm in SMALL:
            self.bcast_load(sv[nm], I[nm][l], "c_" + nm)
        self.bcast_load(cw, I["mlstm_conv_w"][l], "cw")
        self.bcast_load(cb_, I["mlstm_conv_b"][l], "cb")
        self.bcast_load(mu, I["rwkv_mu"][l], "mu")
        self.bcast_load(ifb, I["mlstm_if_bias"][l], "ifb")
        P.dma("sp", upw, I["rwkv_up_pad"][l], writes=("upw",))
        P.dma("sp", gup, I["rwkv_g_up"][l], writes=("gup",))
        if l > 0:
            self.bcast_load(v0b, I["rwkv_v0"][l - 1], "v0b")
            P.dma("sp", vdn, I["rwkv_v_down"][l - 1].rearrange("(c p) n -> p c n", p=128), writes=("vdn",))
            P.dma("sp", vup[0:32, :], I["rwkv_v_up"][l - 1], writes=("vup",))
        pall = lambda tile: tuple(("p", tile, cb) for cb in range(16))
        W = Wt
        wk = lambda i: ("W", i)

        def h8(ap):
            return ap.rearrange("p (h d) -> p h d", d=64)

        def b8(ap8):
            return ap8.unsqueeze(2).to_broadcast([128, 8, 64])

        for tile in range(cfg.NTILE):
            s, g = tile // cfg.TPS, tile % cfg.TPS
            r0 = tile * 128
            rs = slice(r0, r0 + 128)
            P.dma("sp", seg[:, 0:2048], self.pbuf[rs, 0:2048], reads=pall(tile), writes=("seg",))
            P.dma("sp", cst, cs_d[g * 128:(g + 1) * 128, :], writes=("cst",))
            qk = seg[:, 0:1024].rearrange("p (h two d) -> p h two d", two=2, d=32)
            Rv = R.rearrange("p (h two d) -> p h two d", two=2, d=32)
            t1, t2 = qk[:, :, 0, :], qk[:, :, 1, :]
            cosb = cst[:, 0:32].unsqueeze(1).to_broadcast([128, 16, 32])
            sinb = cst[:, 32:64].unsqueeze(1).to_broadcast([128, 16, 32])
            w0v = W[0].rearrange("p (h d) -> p h d", d=32)
            w1v = W[1].rearrange("p (h d) -> p h d", d=32)
            self.tt(w0v, t1, cosb, ALU.mult, ("seg", "cst"), (wk(0),))
            self.tt(w1v, t2, sinb, ALU.mult, ("seg", "cst"), (wk(1),))
            self.tt(Rv[:, :, 0, :], w0v, w1v, ALU.subtract, (wk(0), wk(1)), ("R",))
            self.tt(w0v, t1, sinb, ALU.mult, ("seg", "cst"), (wk(0),))
            self.tt(w1v, t2, cosb, ALU.mult, ("seg", "cst"), (wk(1),))
            self.tt(Rv[:, :, 1, :], w0v, w1v, ALU.add, (wk(0), wk(1)), ("R",))
            self.ts(R[:, 512:1024], R[:, 512:1024], 0.125, None, ALU.mult, None, ("R",), ("R",))
            P.dma("sp", rows["qA"][rs, :], R[:, 0:512], reads=("R",), writes=(("row", "qA", tile),))
            P.dma("sp", rows["kA"][rs, :], R[:, 512:1024], reads=("R",), writes=(("row", "kA", tile),))
            self.store_T(seg[:, 1024:1536], "seg", vT["A"], s, g)
            self.actf(W[2], seg[:, 1536:2048], AF.Silu, ("seg",), (wk(2),))
            P.dma("sp", gate["A"][rs, :], W[2], reads=(wk(2),), writes=(("gate", "A", tile),))
            P.dma("sp", seg[:, 0:2048], self.pbuf[rs, 2048:4096], reads=pall(tile), writes=("seg",))
            self.actf(W[0], seg[:, 0:512], AF.Silu, ("seg",), (wk(0),))
            self.ts(W[0], W[0], 0.125, None, ALU.mult, None, (wk(0),), (wk(0),))
            P.dma("sp", rows["qB"][rs, :], W[0], reads=(wk(0),), writes=(("row", "qB", tile),))
            self.actf(W[1], seg[:, 512:1024], AF.Sigmoid, ("seg",), (wk(1),))
            self.tt(W[1], W[1], oml, ALU.mult, (wk(1), "oml"), (wk(1),))
            self.tt(W[1], W[1], lbt, ALU.add, (wk(1), "lbt"), (wk(1),))
            P.dma("sp", rows["wB"][rs, :], W[1], reads=(wk(1),), writes=(("row", "wB", tile),))
            self.ts(W[3], W[1], -1.0, 1.0, ALU.mult, ALU.add, (wk(1),), (wk(3),))
            P.dma("sp", rows["kB"][rs, :], W[3], reads=(wk(3),), writes=(("row", "kB", tile),))
            self.store_T(seg[:, 1024:1536], "seg", vT["B"], s, g)
            self.actf(W[2], seg[:, 1536:2048], AF.Sigmoid, ("seg",), (wk(2),))
            P.dma("sp", gate["B"][rs, :], W[2], reads=(wk(2),), writes=(("gate", "B", tile),))
            P.dma("sp", seg[:, 0:2064], self.pbuf[rs, 4096:6160], reads=pall(tile), writes=("seg",))
            shv = sh.rearrange("p (j c) -> p j c", c=1024)
            for j in range(3):
                self.load_shift(shv[:, j, :], ("sh", j), 4096, 1024, tile, 3 - j)
            acc = R
            self.tt(acc, seg[:, 0:1024], cw[:, 3, :], ALU.mult, ("seg", "cw"), ("R",))
            self.tt(acc, acc, cb_, ALU.add, ("R", "cb"), ("R",))
            for j in range(3):
                self.tt(shv[:, j, :], shv[:, j, :], cw[:, j, :], ALU.mult, (("sh", j), "cw"), (("sh", j),))
                self.tt(acc, acc, shv[:, j, :], ALU.add, ("R", ("sh", j)), ("R",))
            self.actf(acc, acc, AF.Silu, ("R",), ("R",))
            P.dma("sp", rows["qC"][rs, :], acc[:, 0:512], reads=("R",), writes=(("row", "qC", tile),))
            self.tt(small[:, 0:16], seg[:, 2048:2064], ifb, ALU.add, ("seg", "ifb"), ("small",))
            self.actf(small[:, 16:24], small[:, 0:8], AF.Exp, ("small",), ("small2",))
            self.actf(small[:, 24:32], small[:, 8:16], AF.Sigmoid, ("small",), ("small3",))
            self.stt(h8(W[0]), h8(acc[:, 512:1024]), 0.125, b8(small[:, 16:24]), ALU.mult, ALU.mult, ("R", "small2"), (wk(0),))
            P.dma("sp", rows["kC"][rs, :], W[0], reads=(wk(0),), writes=(("row", "kC", tile),))
            self.cp(h8(W[1]), b8(small[:, 24:32]), ("small3",), (wk(1),), eng="dve")
            P.dma("sp", rows["wC"][rs, :], W[1], reads=(wk(1),), writes=(("row", "wC", tile),))
            self.store_T(seg[:, 1024:1536], "seg", vT["C"], s, g)
            self.actf(W[2], seg[:, 1536:2048], AF.Sigmoid, ("seg",), (wk(2),))
            P.dma("sp", gate["C"][rs, :], W[2], reads=(wk(2),), writes=(("gate", "C", tile),))
            P.dma("sp", seg[:, 0:RWC], self.pbuf[rs, 6160:6160 + RWC], reads=pall(tile), writes=("seg",))
            prev = sh[:, 0:RWC]
            self.load_shift(prev, ("sh", 0), 6160, RWC, tile, 1, extra_keys=(("sh", 1),))
            self.tt(prev, prev, seg[:, 0:RWC], ALU.subtract, (("sh", 0), ("sh", 1), "seg"), (("sh", 0), ("sh", 1)))
            self.tt(prev, prev, mu, ALU.mult, (("sh", 0), ("sh", 1), "mu"), (("sh", 0), ("sh", 1)))
            self.tt(seg[:, 0:RWC], seg[:, 0:RWC], prev, ALU.add, ("seg", ("sh", 0), ("sh", 1)), ("seg",))
            r_, k_, v_ = seg[:, 0:512], seg[:, 512:1024], seg[:, 1024:1536]
            Lt = W[0][:, 0:256]
            self.actf(Lt[:, 0:64], seg[:, 1536:1600], AF.Tanh, ("seg",), (wk(0),))
            self.cp(Lt[:, 64:128], seg[:, 1600:1664], ("seg",), (wk(0),))
            self.actf(Lt[:, 128:256], seg[:, 1664:1792], AF.Sigmoid, ("seg",), (wk(0),))
            LT = W[1][:, 0:256]
            for c in range(2):
                bank = self.psbank()
                pt = self.pslin[:, bank, 0:128]
                P.op("pe", lambda e, pt=pt, c=c: e.transpose(pt, Lt[:, c * 128:(c + 1) * 128], self.ident_f[:, :]),
                     reads=(wk(0), "identf"), writes=(("pslin", bank),))
                self.cp(LT[:, c * 128:(c + 1) * 128], pt, (("pslin", bank),), (wk(1),))
            bank = self.psbank()
            pw = self.pslin[:, bank, :]
            P.op("pe", lambda e, pw=pw: e.matmul(pw, lhsT=LT[:, 0:128], rhs=upw[:, 0:512], start=True, stop=True),
                 reads=(wk(1), "upw"), writes=(("pslin", bank),))
            self.tt(W[2], pw, sv["rwkv_w0"], ALU.add, (("pslin", bank), "c_rwkv_w0"), (wk(2),))
            self.actf(W[2], W[2], AF.Sigmoid, (wk(2),), (wk(2),))
            self.actf(W[2], W[2], AF.Exp, (wk(2),), (wk(2),), scale=-0.6065306597126334)
            P.dma("sp", rows["wD"][rs, :], W[2], reads=(wk(2),), writes=(("row", "wD", tile),))
            bank = self.psbank()
            pa = self.pslin[:, bank, :]
            P.op("pe", lambda e, pa=pa: e.matmul(pa, lhsT=LT[:, 0:128], rhs=upw[:, 512:1024], start=True, stop=True),
                 reads=(wk(1), "upw"), writes=(("pslin", bank),))
            self.tt(W[3], pa, sv["rwkv_a0"], ALU.add, (("pslin", bank), "c_rwkv_a0"), (wk(3),))
            self.actf(W[3], W[3], AF.Sigmoid, (wk(3),), (wk(3),))
            bank = self.psbank()
            pg = self.pslin[:, bank, :]
            P.op("pe", lambda e, pg=pg: e.matmul(pg, lhsT=LT[:, 128:256], rhs=gup, start=True, stop=True),
                 reads=(wk(1), "gup"), writes=(("pslin", bank),))
            self.cp(W[4], pg, (("pslin", bank),), (wk(4),))
            P.dma("sp", gate["D"][rs, :], W[4], reads=(wk(4),), writes=(("gate", "D", tile),))
            if l == 0:
                P.dma("sp", vfirst[rs, :], v_, reads=("seg",), writes=(("vfirst", tile),))
            else:
                vtt = self.vt
                for c in range(4):
                    bank = self.psbank()
                    pt = self.pslin[:, bank, 0:128]
                    P.op("pe", lambda e, pt=pt, c=c: e.transpose(pt, v_[:, c * 128:(c + 1) * 128], self.ident_f[:, :]),
                         reads=("seg", "identf"), writes=(("pslin", bank),))
                    self.cp(vtt[:, c, :], pt, (("pslin", bank),), ("vt",))
                bank = self.psbank()
                pv = self.pslin[:, bank, 0:32]
                for c in range(4):
                    P.op("pe", lambda e, pv=pv, c=c: e.matmul(pv, lhsT=vtt[:, c, :], rhs=vdn[:, c, :], start=(c == 0),
                                                              stop=(c == 3)),
                         reads=("vt", "vdn"), writes=(("pslin", bank),))
                self.cp(W[5][:, 0:32], pv, (("pslin", bank),), (wk(5),))
                bank = self.psbank()
                pt = self.pslin[0:32, bank, 0:128]
                P.op("pe", lambda e, pt=pt: e.transpose(pt, W[5][:, 0:32], self.ident_f[:, :]),
                     reads=(wk(5), "identf"), writes=(("pslin", bank),))
                self.cp(W[6][0:32, 0:128], pt, (("pslin", bank),), (wk(6),))
                bank = self.psbank()
                pv2 = self.pslin[:, bank, :]
                P.op("pe", lambda e, pv2=pv2: e.matmul(pv2, lhsT=W[6][0:32, 0:128], rhs=vup[0:32, :], start=True, stop=True),
                     reads=(wk(6), "vup"), writes=(("pslin", bank),))
                self.tt(W[5], pv2, v0b, ALU.add, (("pslin", bank), "v0b"), (wk(5),))
                self.actf(W[5], W[5], AF.Sigmoid, (wk(5),), (wk(5),))
                P.dma("sp", W[6], vfirst[rs, :], reads=(("vfirst", tile),), writes=(wk(6),))
                self.tt(W[6], W[6], v_, ALU.subtract, (wk(6), "seg"), (wk(6),))
                self.tt(W[6], W[6], W[5], ALU.mult, (wk(6), wk(5)), (wk(6),))
                self.tt(v_, v_, W[6], ALU.add, ("seg", wk(6)), ("seg",))
            self.tt(W[5], k_, sv["rwkv_k_k"], ALU.mult, ("seg", "c_rwkv_k_k"), (wk(5),))
            self.tt(W[6], W[5], W[5], ALU.mult, (wk(5),), (wk(6),))
            self.red(small[:, 32:40], h8(W[6]), (wk(6),), ("small4",))
            self.actf(small[:, 40:48], small[:, 32:40], AF.Sqrt, ("small4",), ("small5",))
            self.ts(small[:, 40:48], small[:, 40:48], 1e-12, None, ALU.max, None, ("small5",), ("small5",))
            P.op("dve", lambda e: e.reciprocal(out=small[:, 48:56], in_=small[:, 40:48]), reads=("small5",), writes=("small6",))
            self.tt(h8(W[5]), h8(W[5]), b8(small[:, 48:56]), ALU.mult, (wk(5), "small6"), (wk(5),))
            P.dma("sp", rows["kkD"][rs, :], W[5], reads=(wk(5),), writes=(("row", "kkD", tile),))
            self.stt(W[7], W[3], -1.0, sv["rwkv_k_a"], ALU.add, ALU.mult, (wk(3), "c_rwkv_k_a"), (wk(7),))
            self.stt(W[7], W[7], 1.0, k_, ALU.add, ALU.mult, (wk(7), "seg"), (wk(7),))
            P.dma("sp", rows["kD"][rs, :], W[7], reads=(wk(7),), writes=(("row", "kD", tile),))
            self.tt(W[8], W[3], W[5], ALU.mult, (wk(3), wk(5)), (wk(8),))
            P.dma("sp", rows["bD"][rs, :], W[8], reads=(wk(8),), writes=(("row", "bD", tile),))
            P.dma("sp", rows["rD"][rs, :], r_, reads=("seg",), writes=(("row", "rD", tile),))
            self.tt(W[9], r_, W[7], ALU.mult, ("seg", wk(7)), (wk(9),))
            self.tt(W[9], W[9], sv["rwkv_r_k"], ALU.mult, (wk(9), "c_rwkv_r_k"), (wk(9),))
            self.red(small[:, 56:64], h8(W[9]), (wk(9),), ("small7",))
            self.tt(h8(W[9]), h8(v_), b8(small[:, 56:64]), ALU.mult, ("seg", "small7"), (wk(9),))
            P.dma("sp", bonus[rs, :], W[9], reads=(wk(9),), writes=(("bonus", tile),))
            self.store_T(v_, "seg", vT["D"], s, g)

    def phase_scan(self, rows, vT, yT, denT, sel_d, gamA):
        P = self.P
        cfg = self.cfg
        T = cfg.T
        self.carve_reset()
        sel = self.carve(TB * 128).rearrange("p (t m) -> p t m", m=128)
        P.dma("sp", sel[0:32, :, :], sel_d[:, :, :], writes=("sel",))
        Rb = [self.carve(5 * 512).rearrange("p (v n) -> p v n", n=512) for _ in range(2)]
        Vb = [self.carve(8 * TB).rearrange("p (g t) -> p g t", t=TB) for _ in range(2)]
        Yb = [self.carve(8 * TB).rearrange("p (g t) -> p g t", t=TB) for _ in range(2)]
        Db = [self.carve(8 * TB).rearrange("p (g t) -> p g t", t=TB) for _ in range(2)]
        S = self.carve(512)
        S2 = self.carve(512)
        T1 = self.carve(512)
        T2 = self.carve(512)
        sk = self.carve(8)
        h8 = lambda ap: ap.rearrange("p (h d) -> p h d", d=64)
        b8 = lambda ap8: ap8.unsqueeze(2).to_broadcast([128, 8, 64])
        for m in "ABCD":
            vecs = VECS[m]
            P.op("dve", lambda e: e.memset(S, 0.0), writes=("S",))
            if m == "C":
                P.op("dve", lambda e: e.memset(S2, 0.0), writes=("S2",))
            for bi in range(T // TB):
                t0 = bi * TB
                pb = bi % 2
                for j, vname in enumerate(vecs):
                    src = rows[vname].rearrange("(s t) (hp hh d) -> t s hp hh d", s=cfg.NSEQ, hh=2, d=64)
                    for hh in range(2):
                        for s in range(cfg.NSEQ):
                            P.dma("sp", Rb[pb][hh * TB:(hh + 1) * TB, j, s * 256:(s + 1) * 256].rearrange(
                                "t (hp d) -> t hp d", d=64),
                                src[t0:t0 + TB, s, :, hh, :],
                                reads=tuple(("row", vname, (s * T + t0) // 128) for _ in (0,)), writes=(("Rb", pb, j),))
                P.dma("sp", Vb[pb].rearrange("p (s hp) t -> p s hp t", hp=4),
                      vT[m].rearrange("hp p s t -> p s hp t")[:, :, :, t0:t0 + TB],
                      reads=tuple(("T", id(vT[m]), s, t0 // 128) for s in range(cfg.NSEQ)), writes=(("Vb", pb),))
                for tl in range(TB):
                    ps = {}
                    for j, vname in enumerate(vecs):
                        bank = self.psbank()
                        pt = self.pslin[:, bank, :]
                        P.op("pe", lambda e, pt=pt, j=j, tl=tl, pb=pb: e.matmul(pt, lhsT=sel[0:32, tl, :], rhs=Rb[pb][0:32, j, :],
                                                                               start=True, stop=True),
                             reads=("sel", ("Rb", pb, j)), writes=(("pslin", bank),))
                        ps[j] = (pt, ("pslin", bank))
                    vb = b8(Vb[pb][:, :, tl])
                    yo = Yb[pb][:, :, tl]
                    if m in "ABC":
                        (qp, qk_), (kp, kk_) = ps[0], ps[1]
                        if m == "A":
                            self.tt(S, S, gamA, ALU.mult, ("S", "gamA"), ("S",))
                        else:
                            self.tt(S, S, ps[2][0], ALU.mult, ("S", ps[2][1]), ("S",))
                        self.tt(h8(T1), h8(kp), vb, ALU.mult, (kk_, ("Vb", pb)), ("T1",))
                        self.tt(S, S, T1, ALU.add, ("S", "T1"), ("S",))
                        self.tt(T2, S, qp, ALU.mult, ("S", qk_), ("T2",))
                        self.red(yo, h8(T2), ("T2",), (("Yb", pb),))
                        if m == "C":
                            self.tt(S2, S2, ps[2][0], ALU.mult, ("S2", ps[2][1]), ("S2",))
                            self.tt(S2, S2, kp, ALU.add, ("S2", kk_), ("S2",))
                            self.tt(T2, S2, qp, ALU.mult, ("S2", qk_), ("T2",))
                            self.red(Db[pb][:, :, tl], h8(T2), ("T2",), (("Db", pb),))
                    else:
                        (rp, rk_), (kp, kk_), (wp, wk_), (cp_, ck_), (bp, bk_) = ps[0], ps[1], ps[2], ps[3], ps[4]
                        self.tt(T1, S, cp_, ALU.mult, ("S", ck_), ("T1",))
                        self.red(sk, h8(T1), ("T1",), ("sk",))
                        self.tt(h8(T1), h8(bp), b8(sk), ALU.mult, (bk_, "sk"), ("T1",))
                        self.tt(S, S, wp, ALU.mult, ("S", wk_), ("S",))
                        self.tt(S, S, T1, ALU.subtract, ("S", "T1"), ("S",))
                        self.tt(h8(T2), h8(kp), vb, ALU.mult, (kk_, ("Vb", pb)), ("T2",))
                        self.tt(S, S, T2, ALU.add, ("S", "T2"), ("S",))
                        self.tt(T2, S, rp, ALU.mult, ("S", rk_), ("T2",))
                        self.red(yo, h8(T2), ("T2",), (("Yb", pb),))
                P.dma("sp", yT[m].rearrange("hp p s t -> p s hp t")[:, :, :, t0:t0 + TB],
                      Yb[pb].rearrange("p (s hp) t -> p s hp t", hp=4), reads=(("Yb", pb),),
                      writes=tuple(("T", id(yT[m]), s, t0 // 128) for s in range(cfg.NSEQ)))
                if m == "C":
                    P.dma("sp", denT.rearrange("hp p s t -> p s hp t")[:, :, :, t0:t0 + TB],
                          Db[pb].rearrange("p (s hp) t -> p s hp t", hp=4), reads=(("Db", pb),),
                          writes=tuple(("T", id(denT), s, t0 // 128) for s in range(cfg.NSEQ)))

    def phase_post(self, l, I, yT, denT, gate, bonus, obuf):
        P = self.P
        cfg = self.cfg
        self.carve_reset()
        o = self.carve(2048)
        y = self.carve(512)
        dn = self.carve(512)
        gt = self.carve(512)
        W = [self.carve(512) for _ in range(3)]
        self.vt = self.carve(512).rearrange("p (a b) -> p a b", b=128)
        sv = {nm: self.carve(512) for nm in ("hgrn_norm_w", "mlstm_norm_w", "rwkv_ln_w", "rwkv_ln_b")}
        small = self.carve(64)
        for nm in sv:
            self.bcast_load(sv[nm], I[nm][l], "c_" + nm)
        h8 = lambda ap: ap.rearrange("p (h d) -> p h d", d=64)
        b8 = lambda ap8: ap8.unsqueeze(2).to_broadcast([128, 8, 64])
        wk = lambda i: ("W", i)
        for tile in range(cfg.NTILE):
            s, g = tile // cfg.TPS, tile % cfg.TPS
            rs = slice(tile * 128, (tile + 1) * 128)
            for mi, m in enumerate("ABCD"):
                self.load_T(y, "y", yT[m], s, g)
                P.dma("sp", gt, gate[m][rs, :], reads=(("gate", m, tile),), writes=("gt",))
                osl = o[:, mi * 512:(mi + 1) * 512]
                if m == "C":
                    self.load_T(dn, "dn", denT, s, g)
                    d8 = dn.rearrange("p (h d) -> p h d", d=64)[:, :, 0]
                    self.ts(small[:, 0:8], d8, -1.0, None, ALU.mult, None, ("dn",), ("sm0",))
                    self.tt(small[:, 0:8], small[:, 0:8], d8, ALU.max, ("sm0", "dn"), ("sm0",))
                    self.ts(small[:, 0:8], small[:, 0:8], 1.0, None, ALU.max, None, ("sm0",), ("sm0",))
                    P.op("dve", lambda e: e.reciprocal(out=small[:, 8:16], in_=small[:, 0:8]), reads=("sm0",), writes=("sm1",))
                    self.tt(h8(y), h8(y), b8(small[:, 8:16]), ALU.mult, ("y", "sm1"), ("y",))
                if m in "ABC":
                    self.tt(W[0], y, y, ALU.mult, ("y",), (wk(0),))
                    self.red(small[:, 16:24], h8(W[0]), (wk(0),), ("sm2",))
                    self.actf(small[:, 24:32], small[:, 16:24], AF.Sqrt, ("sm2", "eps"), ("sm3",), scale=1.0 / 64,
                              bias=self.eps_t[:, 0:1])
                    P.op("dve", lambda e: e.reciprocal(out=small[:, 32:40], in_=small[:, 24:32]), reads=("sm3",), writes=("sm4",))
                    self.tt(h8(y), h8(y), b8(small[:, 32:40]), ALU.mult, ("y", "sm4"), ("y",))
                    if m == "B":
                        self.tt(y, y, sv["hgrn_norm_w"], ALU.mult, ("y", "c_hgrn_norm_w"), ("y",))
                    if m == "C":
                        self.tt(y, y, sv["mlstm_norm_w"], ALU.mult, ("y", "c_mlstm_norm_w"), ("y",))
                    self.tt(osl, y, gt, ALU.mult, ("y", "gt"), ("o",))
                else:
                    self.red(small[:, 16:24], h8(y), ("y",), ("sm2",))
                    self.ts(small[:, 16:24], small[:, 16:24], -1.0 / 64, None, ALU.mult, None, ("sm2",), ("sm2",))
                    self.tt(h8(y), h8(y), b8(small[:, 16:24]), ALU.add, ("y", "sm2"), ("y",))
                    self.tt(W[0], y, y, ALU.mult, ("y",), (wk(0),))
                    self.red(small[:, 24:32], h8(W[0]), (wk(0),), ("sm3",))
                    self.actf(small[:, 32:40], small[:, 24:32], AF.Sqrt, ("sm3", "eps"), ("sm4",), scale=1.0 / 64,
                              bias=self.eps_t[:, 1:2])
                    P.op("dve", lambda e: e.reciprocal(out=small[:, 40:48], in_=small[:, 32:40]), reads=("sm4",), writes=("sm5",))
                    self.tt(h8(y), h8(y), b8(small[:, 40:48]), ALU.mult, ("y", "sm5"), ("y",))
                    self.tt(y, y, sv["rwkv_ln_w"], ALU.mult, ("y", "c_rwkv_ln_w"), ("y",))
                    self.tt(y, y, sv["rwkv_ln_b"], ALU.add, ("y", "c_rwkv_ln_b"), ("y",))
                    P.dma("sp", W[1], bonus[rs, :], reads=(("bonus", tile),), writes=(wk(1),))
                    self.tt(y, y, W[1], ALU.add, ("y", wk(1)), ("y",))
                    self.tt(osl, y, gt, ALU.mult, ("y", "gt"), ("o",))
            P.dma("sp", obuf[rs, :], o, reads=("o",), writes=(("o", tile),))

    def phase_ffn(self, l, hbuf, obuf, wbo, wbg, wbu, wbd, wnb):
        P = self.P
        cfg = self.cfg
        NG = 4
        self.carve_linear(NG)
        actT = self.carve(44 * 256, BF16).rearrange("p (k g t) -> p k g t", g=NG, t=128)
        acc = [self.carve(2048) for _ in range(NG)]
        xT = self.xT
        for grp in range(cfg.NTILE // NG):
            for g in range(NG):
                tile = grp * NG + g
                ht = self.htile[tile % 2]
                P.dma("sp", ht, obuf[tile * 128:(tile + 1) * 128, :], reads=(("o", tile),), writes=(("htile", tile % 2),))
                self.cp(self.ub[:, 0:2048], ht, (("htile", tile % 2),), ("ub",), eng="dve")
                self.transpose_to(self.ub, "ub", xT, g, "xT", 16)

            def consume(g, cb, nc_, pst, pskey):
                self.cp(acc[g][:, cb * 512:cb * 512 + nc_], pst, (pskey,), (("acc", g),))

            self.linear_tm(xT, ("xT",), NG, 16, wbo, D, consume)
            self.resid_update(grp, NG, acc, hbuf, wnb[:, 1, :])
            for g in range(NG):
                tile = grp * NG + g
                ht = self.htile[tile % 2]
                P.dma("sp", ht, hbuf[tile * 128:(tile + 1) * 128, :], reads=(("h", tile),), writes=(("htile", tile % 2),))
                self.norm_transpose(ht, ("htile", tile % 2), wnb[:, 2, :], xT, g, "xT")
            for cb in range(DFF // 512):
                wg = self.wbuf[0].rearrange("p (k n) -> p k n", n=512)
                wu = self.wbuf[1].rearrange("p (k n) -> p k n", n=512)
                P.dma("sp", wg, wbg[cb, :, :, :], reads=(("wb", id(wbg), cb),), writes=(("wbuf", 0),))
                P.dma("sp", wu, wbu[cb, :, :, :], reads=(("wb", id(wbu), cb),), writes=(("wbuf", 1),))
                for j in range(4):
                    nchunk = cb * 4 + j
                    bg = self.psbank()
                    bu = self.psbank()
                    pg = self.pslin[:, bg, :]
                    pu = self.pslin[:, bu, :]
                    for kc in range(16):
                        P.op("pe", lambda e, pg=pg, kc=kc, j=j: e.matmul(pg, lhsT=wg[:, kc, j * 128:(j + 1) * 128],
                                                                         rhs=xT[:, kc, :, :].rearrange("p g t -> p (g t)"),
                                                                         start=(kc == 0), stop=(kc == 15)),
                             reads=("xT", ("wbuf", 0)), writes=(("pslin", bg),))
                    for kc in range(16):
                        P.op("pe", lambda e, pu=pu, kc=kc, j=j: e.matmul(pu, lhsT=wu[:, kc, j * 128:(j + 1) * 128],
                                                                         rhs=xT[:, kc, :, :].rearrange("p g t -> p (g t)"),
                                                                         start=(kc == 0), stop=(kc == 15)),
                             reads=("xT", ("wbuf", 1)), writes=(("pslin", bu),))
                    st = self.stage[self.ssel % 4]
                    skey = ("stage", self.ssel % 4)
                    self.ssel += 1
                    self.actf(st, pg, AF.Silu, (("pslin", bg),), (skey,))
                    self.tt(actT[:, nchunk, :, :].rearrange("p g t -> p (g t)"), st, pu, ALU.mult, (skey, ("pslin", bu)),
                            ("actT",))

            def consume2(g, cb, nc_, pst, pskey):
                self.cp(acc[g][:, cb * 128:cb * 128 + nc_], pst, (pskey,), (("acc", g),))

            self.linear_tm(actT, ("actT",), NG, 44, wbd, D, consume2, CB=128)
            self.resid_update(grp, NG, acc, hbuf, wnb[:, 3, :])

    def resid_update(self, grp, NG, acc, hbuf, wn):
        P = self.P
        for g in range(NG):
            tile = grp * NG + g
            ht = self.htile[tile % 2]
            hk = ("htile", tile % 2)
            P.dma("sp", ht, hbuf[tile * 128:(tile + 1) * 128, :], reads=(("h", tile),), writes=(hk,))
            self.rstd_of(acc[g], ("acc", g), 2048, self.eps_t[:, 0:1])
            self.stt(acc[g], acc[g], self.ss_t[:, 2:3], wn, ALU.mult, ALU.mult, (("acc", g), "ss2", "wnb"), (("acc", g),))
            self.tt(ht, ht, acc[g], ALU.add, (hk, ("acc", g)), (hk,))
            P.dma("sp", hbuf[tile * 128:(tile + 1) * 128, :], ht, reads=(hk,), writes=(("h", tile),))


def make_consts(T):
    half = 32
    inv_freq = 10000.0 ** (-np.arange(half, dtype=np.float32) / half)
    ang = np.arange(T, dtype=np.float32)[:, None] * inv_freq[None, :]
    cossin = np.concatenate([np.cos(ang), np.sin(ang)], axis=1).astype(np.float32)
    sel = np.zeros((32, TB, 128), np.float32)
    for tl in range(TB):
        for m in range(128):
            sel[(m // 64) * TB + tl, tl, m] = 1.0
    gam = np.zeros((128, 2, 4, 64), np.float32)
    for p in range(128):
        for hp in range(4):
            h = 2 * hp + p // 64
            gam[p, :, hp, :] = 1.0 - 2.0 ** (-5.0 - h)
    return dict(cossin=cossin, sel=sel, gamA=gam.reshape(128, 512),
                ident_b=np.eye(128).astype(ml_dtypes.bfloat16), ident_f=np.eye(128, dtype=np.float32))


def make_shared(inp, L, T):
    f = lambda a: np.ascontiguousarray(np.asarray(a, dtype=np.float32))
    sh = dict(w_in=f(inp["w_in"]), w_out=f(inp["w_out"]), w_ffn_gate=f(inp["w_ffn_gate"]), w_ffn_up=f(inp["w_ffn_up"]),
              w_ffn_down=f(inp["w_ffn_down"]))
    sh["norms"] = np.ascontiguousarray(np.stack([f(inp["norm_pre_mix"]), f(inp["norm_post_mix"]), f(inp["norm_pre_ffn"]),
                                                 f(inp["norm_post_ffn"])], axis=1))
    for nm in SMALL + ["hgrn_lb_logits", "mlstm_conv_w", "mlstm_conv_b", "rwkv_mu"]:
        sh[nm] = f(inp[nm])
    sh["mlstm_if_bias"] = np.ascontiguousarray(np.concatenate([f(inp["mlstm_i_bias"]), f(inp["mlstm_f_bias"])], axis=1))
    up = np.zeros((L, 128, 1024), np.float32)
    up[:, 0:64, 0:512] = f(inp["rwkv_w_up"])
    up[:, 64:128, 512:1024] = f(inp["rwkv_a_up"])
    sh["rwkv_up_pad"] = up
    sh["rwkv_g_up"] = f(inp["rwkv_g_up"])
    if L > 1:
        sh["rwkv_v0"] = f(inp["rwkv_v0"])
        sh["rwkv_v_down"] = f(inp["rwkv_v_down"])
        sh["rwkv_v_up"] = f(inp["rwkv_v_up"])
    else:
        sh["rwkv_v0"] = np.zeros((1, G), np.float32)
        sh["rwkv_v_down"] = np.zeros((1, G, 32), np.float32)
        sh["rwkv_v_up"] = np.zeros((1, 32, G), np.float32)
    sh.update(make_consts(T))
    return sh


_CACHE = {}


def kernel(**inp):
    x = np.asarray(inp["x"], dtype=np.float32)
    B, T, _ = x.shape
    L = inp["w_in"].shape[0]
    ncores = 8
    nseq = B // ncores
    cfg = Cfg(T=T, NSEQ=nseq, L=L)
    nc = Builder(cfg).build()
    sh = make_shared(inp, L, T)
    in_maps = []
    for c in range(ncores):
        m = dict(sh)
        m["x"] = np.ascontiguousarray(x[c * nseq:(c + 1) * nseq].reshape(nseq * T, D))
        in_maps.append(m)
    res = run_bass_kernel_spmd(nc, in_maps, core_ids=list(range(ncores)))
    outs = [np.asarray(r["out"]).reshape(nseq, T, D) for r in res.results]
    return np.concatenate(outs, axis=0).astype(np.float32)
```

```python
from contextlib import ExitStack
import numpy as np
import ml_dtypes
import concourse.bass as bass
import concourse.mybir as mybir
from concourse.bass_utils import run_bass_kernel_spmd

F32 = mybir.dt.float32
BF16 = mybir.dt.bfloat16
ALU = mybir.AluOpType
AF = mybir.ActivationFunctionType
AX = mybir.AxisListType

D = 2048
NIN = 7952
DFF = 5632
G = 512
NH = 8
HD = 64
NORM_EPS = 1e-6


class Prog:
    ENG = ("pe", "dve", "act", "pool", "sp")
    NDMASEM = 6

    def __init__(self, nc, stack):
        self.nc = nc
        self.stack = stack
        self.ops = {e: [] for e in self.ENG}
        self.cnt = {e: 0 for e in self.ENG}
        self.known = {e: {} for e in self.ENG}
        self.sem = {e: stack.enter_context(nc.semaphore("s_" + e)) for e in self.ENG if e != "sp"}
        self.semname = {}
        for e, s in self.sem.items():
            self.semname[id(s)] = e
        self.dq = {}
        for q in ("sp", "pool", "act"):
            self.dq[q] = {"sems": [stack.enter_context(nc.semaphore(f"d_{q}{i}")) for i in range(self.NDMASEM)],
                          "n": 0}
        self.bufs = {}
        self.nops = 0

    def _buf(self, k):
        b = self.bufs.get(k)
        if b is None:
            b = [None, {}]
            self.bufs[k] = b
        return b

    def _deps(self, e, reads, writes, extra=()):
        toks = list(extra)
        for k in reads:
            b = self._buf(k)
            if b[0] is not None:
                toks.append((b[0], False))
        for k in writes:
            b = self._buf(k)
            if b[0] is not None:
                toks.append((b[0], False))
            for s, v in b[1].values():
                toks.append(((s, v), True))
        own = self.sem.get(e)
        waits = []
        kn = self.known[e]
        for (s, v), is_reader in toks:
            if s is own and (e == "pe" or is_reader):
                continue
            if kn.get(id(s), 0) >= v:
                continue
            kn[id(s)] = v
            waits.append((s, v))
        return waits

    def _commit(self, tok, reads, writes):
        s, v = tok
        for k in reads:
            b = self._buf(k)
            b[1][id(s)] = (s, v)
        for k in writes:
            b = self._buf(k)
            b[0] = tok
            b[1] = {}

    def op(self, e, fn, reads=(), writes=()):
        reads = tuple(reads)
        writes = tuple(writes)
        waits = self._deps(e, reads, writes)
        self.cnt[e] += 1
        tok = (self.sem[e], self.cnt[e])
        self.ops[e].append((waits, fn, (self.sem[e], 1)))
        self._commit(tok, reads, writes)
        self.nops += 1
        return tok

    def dma(self, q, out, in_, reads=(), writes=(), **kw):
        reads = tuple(reads)
        writes = tuple(writes)
        dq = self.dq[q]
        n = dq["n"]
        dq["n"] += 1
        s = dq["sems"][n % self.NDMASEM]
        prev = 16 * (n // self.NDMASEM)
        extra = [((s, prev), False)] if prev > 0 else []
        waits = self._deps(q, reads, writes, extra)
        tok = (s, prev + 16)
        self.ops[q].append((waits, lambda eng: eng.dma_start(out=out, in_=in_, **kw), (s, 16)))
        self._commit(tok, reads, writes)
        self.nops += 1
        return tok

    def barrier(self):
        toks = [(self.sem[e], self.cnt[e]) for e in self.sem if self.cnt[e] > 0] + self.all_dma_tokens()
        for e in self.ENG:
            waits = []
            kn = self.known[e]
            for s, v in toks:
                if e == "pe" and s is self.sem.get("pe"):
                    continue
                if kn.get(id(s), 0) >= v:
                    continue
                kn[id(s)] = v
                waits.append((s, v))
            if waits:
                self.ops[e].append((waits, None, None))
        self.bufs = {}

    def finish(self, e, toks):
        waits = []
        for s, v in toks:
            waits.append((s, v))
        self.ops[e].append((waits, None, None))

    def all_dma_tokens(self):
        toks = []
        for q, dq in self.dq.items():
            n = dq["n"]
            for i in range(self.NDMASEM):
                c = (n - i + self.NDMASEM - 1) // self.NDMASEM if n > i else 0
                if c > 0:
                    toks.append((dq["sems"][i], 16 * c))
        return toks

    def emit(self):
        nc = self.nc
        ops = self.ops

        def run(eng, lst):
            for waits, fn, inc in lst:
                for s, v in waits:
                    eng.wait_ge(s, v)
                if fn is not None:
                    ins = fn(eng)
                    ins.then_inc(inc[0], inc[1])

        with nc.Block() as block:
            @block.tensor
            def _(eng):
                run(eng, ops["pe"])

            @block.vector
            def _(eng):
                run(eng, ops["dve"])

            @block.scalar
            def _(eng):
                run(eng, ops["act"])

            @block.gpsimd
            def _(eng):
                run(eng, ops["pool"])

            @block.sync
            def _(eng):
                run(eng, ops["sp"])


RWC = 1792
OFFS = {"A": 0, "B": 2048, "C": 4096, "D": 6160}
VECS = {"D": ["rD", "kD", "wD", "kkD", "bD"]}
ALLV = VECS["D"]
CHK = {"A": 64, "B": 32, "C": 64}
TB = 16
SMALL = ["hgrn_norm_w", "mlstm_norm_w", "rwkv_w0", "rwkv_a0", "rwkv_k_k", "rwkv_k_a", "rwkv_r_k", "rwkv_ln_w",
         "rwkv_ln_b"]


class Cfg:
    def __init__(self, T=2048, NSEQ=2, L=4):
        self.T = T
        self.NSEQ = NSEQ
        self.L = L
        self.NT = T * NSEQ
        self.TPS = T // 128
        self.NTILE = self.NT // 128


def wb_shape(K, N, CB=512):
    ncb = (N + CB - 1) // CB
    return [ncb, 128, K // 128, CB]


class Builder:
    def __init__(self, cfg):
        self.cfg = cfg
        self.nc = bass.Bass("TRN2", target_bir_lowering=False)
        self.stack = ExitStack()
        self.P = None

    def dram_in(self, name, shape, dt=F32):
        return self.nc.dram_tensor(name, list(shape), dt, kind="ExternalInput").ap()

    def dram_out(self, name, shape, dt=F32):
        return self.nc.dram_tensor(name, list(shape), dt, kind="ExternalOutput").ap()

    def dram_tmp(self, name, shape, dt=F32):
        return self.nc.dram_tensor(name, list(shape), dt, kind="Internal").ap()

    def sb(self, name, shape, dt=F32):
        h = self.stack.enter_context(self.nc.sbuf_tensor(name, list(shape), dt))
        return h[tuple(slice(None) for _ in shape)]

    def ps(self, name, shape, dt=F32):
        return self.stack.enter_context(self.nc.psum_tensor(name, list(shape), dt))

    def carve_reset(self):
        self.cptr = 0

    def carve(self, ncols, dt=F32):
        a = self.arena[:, self.cptr:self.cptr + ncols]
        self.cptr += ncols
        assert self.cptr <= self.NARENA, self.cptr
        if dt == BF16:
            a = a.bitcast(BF16)
        return a

    def tt(self, out, a, b, op, r, w, eng="dve"):
        self.P.op(eng, lambda e: e.tensor_tensor(out=out, in0=a, in1=b, op=op), reads=r, writes=w)

    def ts(self, out, a, s1, s2, op0, op1, r, w, eng="dve"):
        if op1 is None:
            self.P.op(eng, lambda e: e.tensor_scalar(out=out, in0=a, scalar1=s1, scalar2=None, op0=op0), reads=r, writes=w)
        else:
            self.P.op(eng, lambda e: e.tensor_scalar(out=out, in0=a, scalar1=s1, scalar2=s2, op0=op0, op1=op1),
                      reads=r, writes=w)

    def stt(self, out, a, scalar, b, op0, op1, r, w):
        self.P.op("dve", lambda e: e.scalar_tensor_tensor(out=out, in0=a, scalar=scalar, in1=b, op0=op0, op1=op1),
                  reads=r, writes=w)

    def actf(self, out, a, func, r, w, scale=1.0, bias=None):
        if bias is None:
            self.P.op("act", lambda e: e.activation(out=out, in_=a, func=func, scale=scale), reads=r, writes=w)
        else:
            self.P.op("act", lambda e: e.activation(out=out, in_=a, func=func, scale=scale, bias=bias), reads=r, writes=w)

    def red(self, out, a, r, w):
        self.P.op("dve", lambda e: e.tensor_reduce(out=out, in_=a, axis=AX.X, op=ALU.add), reads=r, writes=w)

    def cp(self, out, a, r, w, eng="act"):
        if eng == "act":
            self.P.op("act", lambda e: e.copy(out=out, in_=a), reads=r, writes=w)
        else:
            self.P.op(eng, lambda e: e.tensor_copy(out=out, in_=a), reads=r, writes=w)

    def psbank(self):
        b = self.psel % self.NPS
        self.psel += 1
        return b

    def cast_weight(self, W, Wb, K, N, CB=512):
        P = self.P
        KC = K // 128
        ncb = (N + CB - 1) // CB
        Wv = W.rearrange("(kc p) n -> p kc n", p=128)
        for cb in range(ncb):
            c0 = cb * CB
            nc_ = min(CB, N - c0)
            for k0 in range(0, KC, 16):
                k1 = min(KC, k0 + 16)
                P.dma("pool", Wb[cb, :, k0:k1, 0:nc_], Wv[:, k0:k1, c0:c0 + nc_], reads=(), writes=(("wb", id(Wb), cb),))

    def linear_tm(self, xT, xkeys, ng, KC, Wb, N, consume, CB=512):
        P = self.P
        ncb = (N + CB - 1) // CB
        for cb in range(ncb):
            nc_ = min(CB, N - cb * CB)
            b = self.wsel % 2
            self.wsel += 1
            wbuf = self.wbuf[b][:, 0:KC * CB].rearrange("p (k n) -> p k n", n=CB)
            P.dma("sp", wbuf[:, :, 0:nc_], Wb[cb, :, 0:KC, 0:nc_], reads=(("wb", id(Wb), cb),), writes=(("wbuf", b),))
            for g in range(ng):
                bank = self.psbank()
                pst = self.pslin[:, bank, 0:nc_]
                for kc in range(KC):
                    P.op("pe", (lambda e, pst=pst, g=g, kc=kc, wbuf=wbuf, nc_=nc_:
                                e.matmul(pst, lhsT=xT[:, kc, g, :], rhs=wbuf[:, kc, 0:nc_],
                                         start=(kc == 0), stop=(kc == KC - 1))),
                         reads=tuple(xkeys) + (("wbuf", b),), writes=(("pslin", bank),))
                consume(g, cb, nc_, pst, ("pslin", bank))

    def rstd_of(self, src, src_key, n, eps_ap):
        P = self.P
        ss = self.ss_t
        junk = self.junk
        P.op("act", lambda e: e.activation(out=junk[:, 0:n], in_=src, func=AF.Square, accum_out=ss[:, 0:1]),
             reads=(src_key,), writes=("junk", "ss"))
        P.op("act", lambda e: e.activation(out=ss[:, 1:2], in_=ss[:, 0:1], func=AF.Sqrt, scale=1.0 / n, bias=eps_ap),
             reads=("ss", "eps"), writes=("ss1",))
        P.op("dve", lambda e: e.reciprocal(out=ss[:, 2:3], in_=ss[:, 1:2]), reads=("ss1",), writes=("ss2",))

    def norm_transpose(self, src_tile, src_key, wn_b, xT, g, xkey):
        self.rstd_of(src_tile, src_key, 2048, self.eps_t[:, 0:1])
        ub = self.ub
        self.stt(ub[:, 0:2048], src_tile, self.ss_t[:, 2:3], wn_b, ALU.mult, ALU.mult, (src_key, "ss2", "wnb"), ("ub",))
        self.transpose_to(ub, "ub", xT, g, xkey, 16)

    def transpose_to(self, ub, ubkey, xT, g, xkey, KC):
        P = self.P
        for k0 in range(0, KC, 4):
            k1 = min(KC, k0 + 4)
            bank = self.tsel % 2
            self.tsel += 1
            pt = self.pstr[:, bank, :]
            for kc in range(k0, k1):
                P.op("pe", (lambda e, kc=kc, pt=pt, k0=k0:
                            e.transpose(pt[:, (kc - k0) * 128:(kc - k0 + 1) * 128], ub[:, kc * 128:(kc + 1) * 128],
                                        self.ident_b[:, :])),
                     reads=(ubkey, "ident"), writes=(("pstr", bank),))
            n = k1 - k0
            P.op("act", (lambda e, pt=pt, k0=k0, n=n:
                         e.copy(out=xT[:, k0:k0 + n, g, :], in_=pt[:, 0:n * 128].rearrange("p (k t) -> p k t", t=128))),
                 reads=(("pstr", bank),), writes=(xkey,))

    def store_T(self, src, src_key, dstT, s, g):
        P = self.P
        vt = self.vt
        for hp in range(4):
            bank = self.psbank()
            pt = self.pslin[:, bank, 0:128]
            P.op("pe", lambda e, pt=pt, hp=hp: e.transpose(pt, src[:, hp * 128:(hp + 1) * 128], self.ident_f[:, :]),
                 reads=(src_key, "identf"), writes=(("pslin", bank),))
            self.cp(vt[:, hp, :], pt, (("pslin", bank),), ("vt",))
        P.dma("sp", dstT[:, :, s, g * 128:(g + 1) * 128].rearrange("hp p t -> p hp t"), vt[:, :, :], reads=("vt",),
              writes=(("T", id(dstT), s, g),))

    def bt_store(self, src_bf, src_key, dstT, s, g):
        P = self.P
        bank = self.tsel % 2
        self.tsel += 1
        pt = self.pstr[:, bank, :]
        for hp in range(4):
            P.op("pe", lambda e, hp=hp, pt=pt: e.transpose(pt[:, hp * 128:(hp + 1) * 128], src_bf[:, hp * 128:(hp + 1) * 128],
                                                          self.ident_b[:, :]),
                 reads=(src_key, "ident"), writes=(("pstr", bank),))
        tb = self.tbt
        P.op("act", lambda e, pt=pt: e.copy(out=tb[:, :, :], in_=pt[:, 0:512].rearrange("p (k t) -> p k t", t=128)),
             reads=(("pstr", bank),), writes=("tbt",))
        P.dma("sp", dstT[:, :, s, g * 128:(g + 1) * 128].rearrange("hp p t -> p hp t"), tb[:, :, :], reads=("tbt",),
              writes=(("T", id(dstT), s, g),))

    def store_v(self, v_src, v_key, v_d, rs):
        self.cp(self.vb[:, :, 0:64], v_src.rearrange("p (h e) -> p h e", e=64), (v_key,), ("vb",), eng="dve")
        self.P.dma("sp", v_d[rs, :], self.vb.rearrange("p h e -> p (h e)"), reads=("vb",), writes=(("vd", id(v_d)),))

    def load_T(self, dst, dst_key, srcT, s, g):
        P = self.P
        vt = self.vt
        P.dma("sp", vt[:, :, :], srcT[:, :, s, g * 128:(g + 1) * 128].rearrange("hp p t -> p hp t"),
              reads=(("T", id(srcT), s, g),), writes=("vt",))
        for hp in range(4):
            bank = self.psbank()
            pt = self.pslin[:, bank, 0:128]
            P.op("pe", lambda e, pt=pt, hp=hp: e.transpose(pt, vt[:, hp, :], self.ident_f[:, :]),
                 reads=("vt", "identf"), writes=(("pslin", bank),))
            self.cp(dst[:, hp * 128:(hp + 1) * 128], pt, (("pslin", bank),), (dst_key,))

    def load_shift(self, dst, key, c0, n, tile, k, extra_keys=()):
        P = self.P
        keys = (key,) + tuple(extra_keys)
        cfg = self.cfg
        g = tile % cfg.TPS
        r0 = tile * 128
        if g == 0:
            P.op("dve", lambda e: e.memset(dst[:, 0:n], 0.0), writes=keys)
            P.dma("sp", dst[k:128, 0:n], self.pbuf[r0:r0 + 128 - k, c0:c0 + n],
                  reads=tuple(("p", tile, cb) for cb in range(16)), writes=keys)
        else:
            P.dma("sp", dst[:, 0:n], self.pbuf[r0 - k:r0 - k + 128, c0:c0 + n],
                  reads=tuple(("p", tile, cb) for cb in range(16)) + tuple(("p", tile - 1, cb) for cb in range(16)),
                  writes=keys)

    def bcast_load(self, dst, src_row, key):
        self.P.dma("sp", dst, src_row.partition_broadcast(128), writes=(key,))

    def build(self, dbg=None, layers=None):
        cfg = self.cfg
        nc = self.nc
        L = cfg.L
        NT = cfg.NT
        T = cfg.T
        NSEQ = cfg.NSEQ
        self.P = P = Prog(nc, self.stack)
        layers = list(range(L)) if layers is None else layers
        I = {}
        x = self.dram_in("x", [NT, D])
        out = self.dram_out("out", [NT, D])
        w_in = self.dram_in("w_in", [L, D, NIN])
        w_out = self.dram_in("w_out", [L, D, D])
        w_g = self.dram_in("w_ffn_gate", [L, D, DFF])
        w_u = self.dram_in("w_ffn_up", [L, D, DFF])
        w_d = self.dram_in("w_ffn_down", [L, DFF, D])
        norms = self.dram_in("norms", [L, 4, D])
        for nm in SMALL:
            I[nm] = self.dram_in(nm, [L, G])
        I["hgrn_lb_logits"] = self.dram_in("hgrn_lb_logits", [L, G])
        I["mlstm_conv_w"] = self.dram_in("mlstm_conv_w", [L, 4, 2 * G])
        I["mlstm_conv_b"] = self.dram_in("mlstm_conv_b", [L, 2 * G])
        I["mlstm_if_bias"] = self.dram_in("mlstm_if_bias", [L, 16])
        I["rwkv_mu"] = self.dram_in("rwkv_mu", [L, RWC])
        I["rwkv_up_pad"] = self.dram_in("rwkv_up_pad", [L, 128, 1024])
        I["rwkv_g_up"] = self.dram_in("rwkv_g_up", [L, 128, G])
        I["rwkv_v0"] = self.dram_in("rwkv_v0", [max(L - 1, 1), G])
        I["rwkv_v_down"] = self.dram_in("rwkv_v_down", [max(L - 1, 1), G, 32])
        I["rwkv_v_up"] = self.dram_in("rwkv_v_up", [max(L - 1, 1), 32, G])
        ident_b_d = self.dram_in("ident_b", [128, 128], BF16)
        ident_f_d = self.dram_in("ident_f", [128, 128])
        cs_d = self.dram_in("cossin", [T, 64])
        sel_d = self.dram_in("sel", [32, TB, 128])
        gam_d = self.dram_in("gamA", [128, 512])
        atab_d = self.dram_in("atab", [4, 128, 512])
        tri_d = self.dram_in("trimats", [4, 128, 128])
        mask_d = self.dram_in("cmask", [64, 64])

        hbuf = self.dram_tmp("hbuf", [NT, D])
        self.pbuf = pbuf = self.dram_tmp("pbuf", [NT, NIN])
        obuf = self.dram_tmp("obuf", [NT, D])
        rows = {v: self.dram_tmp("row_" + v, [NT, G]) for v in ALLV}
        vT = {m: self.dram_tmp("vT_" + m, [4, 128, NSEQ, T]) for m in "D"}
        yT = {m: self.dram_tmp("yT_" + m, [4, 128, NSEQ, T]) for m in "D"}
        ck = dict(
            qT={m: self.dram_tmp("cqT_" + m, [4, 128, NSEQ, T], BF16) for m in "ABC"},
            kT={m: self.dram_tmp("ckT_" + m, [4, 128, NSEQ, T], BF16) for m in "ABC"},
            kh={m: self.dram_tmp("ckh_" + m, [NT, G], BF16) for m in "ABC"},
            v={m: self.dram_tmp("cv_" + m, [NT, 520], BF16) for m in "ABC"},
            dec={m: self.dram_tmp("cdec_" + m, [NT, G]) for m in "ABC"},
            y={m: self.dram_tmp("cy_" + m, [NT, 520]) for m in "ABC"},
            atab=atab_d, tri=tri_d, mask=mask_d)
        gate = {m: self.dram_tmp("gate_" + m, [NT, G]) for m in "ABCD"}
        bonus = self.dram_tmp("bonus", [NT, G])
        vfirst = self.dram_tmp("vfirst", [NT, G])
        wb_in = [self.dram_tmp(f"wb_in{l}", wb_shape(D, NIN), BF16) for l in range(L)]
        wb_out = [self.dram_tmp(f"wb_out{l}", wb_shape(D, D), BF16) for l in range(L)]
        wb_g = [self.dram_tmp(f"wb_g{l}", wb_shape(D, DFF), BF16) for l in range(L)]
        wb_u = [self.dram_tmp(f"wb_u{l}", wb_shape(D, DFF), BF16) for l in range(L)]
        wb_d = [self.dram_tmp(f"wb_d{l}", wb_shape(DFF, D, 128), BF16) for l in range(L)]
        dbg_t = None
        if dbg is not None and dbg[1] is not None:
            dbg_t = self.dram_out("dbg", dbg[1])

        self.ident_b = self.sb("ident_b_s", [128, 128], BF16)
        self.ident_f = self.sb("ident_f_s", [128, 128])
        self.ss_t = self.sb("ss", [128, 8])
        self.eps_t = self.sb("eps_t", [128, 4])
        gamA = self.sb("gamA_s", [128, 512])
        lbd = self.dram_tmp("lbd", [2, 128, L, G])
        wnb = self.sb("wnb", [128, 4, D])
        self.NARENA = 41000
        self.arena = self.sb("arena", [128, self.NARENA])
        self.NPS = 6
        self.pslin = self.ps("pslin", [128, self.NPS, 512])
        self.psel = 0
        self.pstr = self.ps("pstr", [128, 2, 1024], BF16)
        self.tsel = 0
        self.wsel = 0

        P.dma("sp", self.ident_b[:, :], ident_b_d[:, :], writes=("ident",))
        P.dma("sp", self.ident_f[:, :], ident_f_d[:, :], writes=("identf",))
        P.dma("sp", gamA[:, :], gam_d[:, :], writes=("gamA",))
        P.op("dve", lambda e: e.memset(self.eps_t[:, 0:1], NORM_EPS), writes=("eps",))
        P.op("dve", lambda e: e.memset(self.eps_t[:, 1:2], 64e-5), writes=("eps",))
        P.op("dve", lambda e: e.memset(self.eps_t[:, 2:3], 0.0), writes=("eps",))
        for i in range(0, NT, 512):
            P.dma("sp", hbuf[i:i + 512, :], x[i:i + 512, :], writes=tuple(("h", j) for j in range(i // 128, i // 128 + 4)))
        for l in layers:
            self.cast_weight(w_in[l], wb_in[l], D, NIN)
            self.cast_weight(w_out[l], wb_out[l], D, D)
            self.cast_weight(w_g[l], wb_g[l], D, DFF)
            self.cast_weight(w_u[l], wb_u[l], D, DFF)
            self.cast_weight(w_d[l], wb_d[l], DFF, D, 128)

        self.carve_reset()
        ex = self.carve(L * G).rearrange("p (l g) -> p l g", g=G)
        sm = self.carve(G)
        lbt = self.carve(L * G).rearrange("p (l g) -> p l g", g=G)
        oml = self.carve(L * G).rearrange("p (l g) -> p l g", g=G)
        P.dma("sp", ex, I["hgrn_lb_logits"].partition_broadcast(128), writes=("ex",))
        self.actf(ex, ex, AF.Exp, ("ex",), ("ex",))
        self.cp(sm, ex[:, 0, :], ("ex",), ("sm",), eng="dve")
        for j in range(1, L):
            self.tt(sm, sm, ex[:, j, :], ALU.add, ("sm", "ex"), ("sm",))
        P.op("dve", lambda e: e.reciprocal(out=sm, in_=sm), reads=("sm",), writes=("sm",))
        P.op("dve", lambda e: e.memset(lbt[:, 0, :], 0.0), writes=("lbt",))
        for j in range(1, L):
            self.tt(ex[:, j, :], ex[:, j, :], sm, ALU.mult, ("ex", "sm"), ("ex",))
            self.tt(lbt[:, j, :], lbt[:, j - 1, :], ex[:, j, :], ALU.add, ("lbt", "ex"), ("lbt",))
        self.ts(oml[:, :, :], lbt[:, :, :], -1.0, 1.0, ALU.mult, ALU.add, ("lbt",), ("oml",))
        P.dma("sp", lbd[0], lbt, reads=("lbt",), writes=("lbd",))
        P.dma("sp", lbd[1], oml, reads=("oml",), writes=("lbd",))
        P.barrier()

        for l in layers:
            P.dma("sp", wnb[:, :, :], norms[l].partition_broadcast(128), writes=("wnb",))
            self.phase_p1(l, hbuf, wb_in[l], wnb)
            P.barrier()
            if dbg is not None and dbg[0] == "p" and l == layers[-1]:
                break
            self.phase_prep(l, I, rows, vT, gate, bonus, vfirst, cs_d, lbd, ck)
            P.barrier()
            if dbg is not None and dbg[0] == "prep" and l == layers[-1]:
                break
            self.phase_scan(rows, vT, yT, sel_d, ck)
            P.barrier()
            if dbg is not None and dbg[0] == "scan" and l == layers[-1]:
                break
            self.phase_post(l, I, yT, ck, gate, bonus, obuf)
            P.barrier()
            if dbg is not None and dbg[0] == "post" and l == layers[-1]:
                break
            self.phase_ffn(l, hbuf, obuf, wb_out[l], wb_g[l], wb_u[l], wb_d[l], wnb)
            P.barrier()

        if dbg_t is not None:
            src = {"p": pbuf, "post": obuf, "h": hbuf}.get(dbg[0])
            P.dma("sp", dbg_t, src)
        for i in range(0, NT, 512):
            P.dma("sp", out[i:i + 512, :], hbuf[i:i + 512, :])
        P.finish("sp", P.all_dma_tokens())
        P.emit()
        return nc

    def carve_linear(self, ng):
        self.carve_reset()
        self.xT = self.carve(16 * ng * 64, BF16).rearrange("p (k g t) -> p k g t", g=ng, t=128)
        self.wbuf = [self.carve(4096, BF16) for _ in range(2)]
        self.htile = [self.carve(2048) for _ in range(2)]
        self.junk = self.carve(2048)
        self.ub = self.carve(1024, BF16)
        self.stage = [self.carve(512) for _ in range(4)]
        self.ssel = 0

    def phase_p1(self, l, hbuf, wb, wnb):
        P = self.P
        cfg = self.cfg
        NG = 4
        self.carve_linear(NG)
        for grp in range(cfg.NTILE // NG):
            for g in range(NG):
                tile = grp * NG + g
                ht = self.htile[tile % 2]
                P.dma("sp", ht, hbuf[tile * 128:(tile + 1) * 128, :], reads=(("h", tile),), writes=(("htile", tile % 2),))
                self.norm_transpose(ht, ("htile", tile % 2), wnb[:, 0, :], self.xT, g, "xT")

            def consume(g, cb, nc_, pst, pskey, grp=grp):
                st = self.stage[self.ssel % 4]
                skey = ("stage", self.ssel % 4)
                self.ssel += 1
                self.cp(st[:, 0:nc_], pst, (pskey,), (skey,))
                tile = grp * NG + g
                P.dma("sp", self.pbuf[tile * 128:(tile + 1) * 128, cb * 512:cb * 512 + nc_], st[:, 0:nc_],
                      reads=(skey,), writes=(("p", tile, cb),))

            self.linear_tm(self.xT, ("xT",), NG, 16, wb, NIN, consume)

    def phase_prep(self, l, I, rows, vT, gate, bonus, vfirst, cs_d, lbd, ck):
        P = self.P
        cfg = self.cfg
        self.carve_reset()
        seg = self.carve(2064)
        sh = self.carve(3072)
        Wt = [self.carve(512) for _ in range(10)]
        R = self.carve(1024)
        cst = self.carve(64)
        self.vt = self.carve(512).rearrange("p (a b) -> p a b", b=128)
        cw = self.carve(4096).rearrange("p (j c) -> p j c", c=1024)
        cb_ = self.carve(1024)
        mu = self.carve(RWC)
        sv = {nm: self.carve(512) for nm in SMALL}
        v0b = self.carve(512)
        ifb = self.carve(16)
        upw = self.carve(1024)
        gup = self.carve(512)
        vdn = self.carve(128).rearrange("p (c n) -> p c n", n=32)
        vup = self.carve(512)
        small = self.carve(64)
        small2 = self.carve(64)
        QB = self.carve(256, BF16)
        KB = self.carve(256, BF16)
        KH = self.carve(256, BF16)
        self.tbt = self.carve(256, BF16).rearrange("p (a b) -> p a b", b=128)
        self.vb = self.carve(260, BF16).rearrange("p (h e) -> p h e", e=65)
        AT = self.carve(2048).rearrange("p (a c) -> p a c", c=512)
        tri = self.carve(512).rearrange("p (a c) -> p a c", c=128)
        P.dma("sp", AT, ck["atab"].rearrange("a p c -> p a c"), writes=("AT",))
        P.dma("sp", tri, ck["tri"].rearrange("a p c -> p a c"), writes=("tri",))
        P.op("dve", lambda e: e.memset(self.vb[:, :, 64:65], 1.0), writes=("vb",))
        lbt = self.carve(512)
        oml = self.carve(512)
        P.dma("sp", lbt, lbd[0, :, l, :], writes=("lbt",))
        P.dma("sp", oml, lbd[1, :, l, :], writes=("oml",))
        for nm in SMALL:
            self.bcast_load(sv[nm], I[nm][l], "c_" + nm)
        self.bcast_load(cw, I["mlstm_conv_w"][l], "cw")
        self.bcast_load(cb_, I["mlstm_conv_b"][l], "cb")
        self.bcast_load(mu, I["rwkv_mu"][l], "mu")
        self.bcast_load(ifb, I["mlstm_if_bias"][l], "ifb")
        P.dma("sp", upw, I["rwkv_up_pad"][l], writes=("upw",))
        P.dma("sp", gup, I["rwkv_g_up"][l], writes=("gup",))
        if l > 0:
            self.bcast_load(v0b, I["rwkv_v0"][l - 1], "v0b")
            P.dma("sp", vdn, I["rwkv_v_down"][l - 1].rearrange("(c p) n -> p c n", p=128), writes=("vdn",))
            P.dma("sp", vup[0:32, :], I["rwkv_v_up"][l - 1], writes=("vup",))
        pall = lambda tile: tuple(("p", tile, cb) for cb in range(16))
        W = Wt
        wk = lambda i: ("W", i)

        def h8(ap):
            return ap.rearrange("p (h d) -> p h d", d=64)

        def b8(ap8):
            return ap8.unsqueeze(2).to_broadcast([128, 8, 64])

        for tile in range(cfg.NTILE):
            s, g = tile // cfg.TPS, tile % cfg.TPS
            r0 = tile * 128
            rs = slice(r0, r0 + 128)
            P.dma("sp", seg[:, 0:2048], self.pbuf[rs, 0:2048], reads=pall(tile), writes=("seg",))
            P.dma("sp", cst, cs_d[g * 128:(g + 1) * 128, :], writes=("cst",))
            qk = seg[:, 0:1024].rearrange("p (h two d) -> p h two d", two=2, d=32)
            Rv = R.rearrange("p (h two d) -> p h two d", two=2, d=32)
            t1, t2 = qk[:, :, 0, :], qk[:, :, 1, :]
            cosb = cst[:, 0:32].unsqueeze(1).to_broadcast([128, 16, 32])
            sinb = cst[:, 32:64].unsqueeze(1).to_broadcast([128, 16, 32])
            w0v = W[0].rearrange("p (h d) -> p h d", d=32)
            w1v = W[1].rearrange("p (h d) -> p h d", d=32)
            self.tt(w0v, t1, cosb, ALU.mult, ("seg", "cst"), (wk(0),))
            self.tt(w1v, t2, sinb, ALU.mult, ("seg", "cst"), (wk(1),))
            self.tt(Rv[:, :, 0, :], w0v, w1v, ALU.subtract, (wk(0), wk(1)), ("R",))
            self.tt(w0v, t1, sinb, ALU.mult, ("seg", "cst"), (wk(0),))
            self.tt(w1v, t2, cosb, ALU.mult, ("seg", "cst"), (wk(1),))
            self.tt(Rv[:, :, 1, :], w0v, w1v, ALU.add, (wk(0), wk(1)), ("R",))
            self.tt(QB, R[:, 0:512], AT[:, 0, :], ALU.mult, ("R", "AT"), ("QB",))
            self.bt_store(QB, "QB", ck["qT"]["A"], s, g)
            self.tt(KB, R[:, 512:1024], AT[:, 1, :], ALU.mult, ("R", "AT"), ("KB",))
            self.bt_store(KB, "KB", ck["kT"]["A"], s, g)
            self.tt(KH, R[:, 512:1024], AT[:, 2, :], ALU.mult, ("R", "AT"), ("KH",))
            P.dma("sp", ck["kh"]["A"][rs, :], KH, reads=("KH",), writes=(("kh", "A"),))
            self.store_v(seg[:, 1024:1536], "seg", ck["v"]["A"], rs)
            P.dma("sp", ck["dec"]["A"][rs, :], AT[:, 3, :], reads=("AT",), writes=(("dec", "A"),))
            self.actf(W[2], seg[:, 1536:2048], AF.Silu, ("seg",), (wk(2),))
            P.dma("sp", gate["A"][rs, :], W[2], reads=(wk(2),), writes=(("gate", "A", tile),))
            P.dma("sp", seg[:, 0:2048], self.pbuf[rs, 2048:4096], reads=pall(tile), writes=("seg",))
            self.actf(W[0], seg[:, 0:512], AF.Silu, ("seg",), (wk(0),))
            self.ts(W[0], W[0], 0.125, None, ALU.mult, None, (wk(0),), (wk(0),))
            self.actf(W[1], seg[:, 512:1024], AF.Sigmoid, ("seg",), (wk(1),))
            self.tt(W[1], W[1], oml, ALU.mult, (wk(1), "oml"), (wk(1),))
            self.tt(W[1], W[1], lbt, ALU.add, (wk(1), "lbt"), (wk(1),))
            self.ts(W[3], W[1], -1.0, 1.0, ALU.mult, ALU.add, (wk(1),), (wk(3),))
            self.actf(W[4], W[1], AF.Ln, (wk(1),), (wk(4),))
            ba = self.psbank()
            pc = self.pslin[:, ba, :]
            P.op("pe", lambda e, pc=pc: e.matmul(pc, lhsT=tri[:, 2, :], rhs=W[4], start=True, stop=True),
                 reads=(wk(4), "tri"), writes=(("pslin", ba),))
            bb = self.psbank()
            ptot = self.pslin[:, bb, :]
            P.op("pe", lambda e, ptot=ptot: e.matmul(ptot, lhsT=tri[:, 3, :], rhs=W[4], start=True, stop=True),
                 reads=(wk(4), "tri"), writes=(("pslin", bb),))
            self.actf(W[5], pc, AF.Exp, (("pslin", ba),), (wk(5),))
            self.tt(QB, W[0], W[5], ALU.mult, (wk(0), wk(5)), ("QB",))
            self.bt_store(QB, "QB", ck["qT"]["B"], s, g)
            self.actf(W[5], pc, AF.Exp, (("pslin", ba),), (wk(5),), scale=-1.0)
            self.tt(KB, W[3], W[5], ALU.mult, (wk(3), wk(5)), ("KB",))
            self.bt_store(KB, "KB", ck["kT"]["B"], s, g)
            self.cp(W[6], ptot, (("pslin", bb),), (wk(6),))
            self.actf(W[7], W[6], AF.Exp, (wk(6),), (wk(7),))
            P.dma("sp", ck["dec"]["B"][rs, :], W[7], reads=(wk(7),), writes=(("dec", "B"),))
            self.tt(W[6], W[6], pc, ALU.subtract, (wk(6), ("pslin", ba)), (wk(6),))
            self.actf(W[6], W[6], AF.Exp, (wk(6),), (wk(6),))
            self.tt(KH, W[3], W[6], ALU.mult, (wk(3), wk(6)), ("KH",))
            P.dma("sp", ck["kh"]["B"][rs, :], KH, reads=("KH",), writes=(("kh", "B"),))
            self.store_v(seg[:, 1024:1536], "seg", ck["v"]["B"], rs)
            self.actf(W[2], seg[:, 1536:2048], AF.Sigmoid, ("seg",), (wk(2),))
            P.dma("sp", gate["B"][rs, :], W[2], reads=(wk(2),), writes=(("gate", "B", tile),))
            P.dma("sp", seg[:, 0:2064], self.pbuf[rs, 4096:6160], reads=pall(tile), writes=("seg",))
            shv = sh.rearrange("p (j c) -> p j c", c=1024)
            for j in range(3):
                self.load_shift(shv[:, j, :], ("sh", j), 4096, 1024, tile, 3 - j)
            acc = R
            self.tt(acc, seg[:, 0:1024], cw[:, 3, :], ALU.mult, ("seg", "cw"), ("R",))
            self.tt(acc, acc, cb_, ALU.add, ("R", "cb"), ("R",))
            for j in range(3):
                self.tt(shv[:, j, :], shv[:, j, :], cw[:, j, :], ALU.mult, (("sh", j), "cw"), (("sh", j),))
                self.tt(acc, acc, shv[:, j, :], ALU.add, ("R", ("sh", j)), ("R",))
            self.actf(acc, acc, AF.Silu, ("R",), ("R",))
            self.tt(small[:, 0:16], seg[:, 2048:2064], ifb, ALU.add, ("seg", "ifb"), ("small",))
            self.actf(small[:, 24:32], small[:, 8:16], AF.Sigmoid, ("small",), ("small3",))
            self.actf(small[:, 16:24], small[:, 24:32], AF.Ln, ("small3",), ("small2",))
            ba = self.psbank()
            pc8 = self.pslin[:, ba, 0:8]
            P.op("pe", lambda e, pc8=pc8: e.matmul(pc8, lhsT=tri[:, 0, :], rhs=small[:, 16:24], start=True, stop=True),
                 reads=("small2", "tri"), writes=(("pslin", ba),))
            bb = self.psbank()
            pt8 = self.pslin[:, bb, 0:8]
            P.op("pe", lambda e, pt8=pt8: e.matmul(pt8, lhsT=tri[:, 1, :], rhs=small[:, 16:24], start=True, stop=True),
                 reads=("small2", "tri"), writes=(("pslin", bb),))
            self.actf(small2[:, 0:8], pc8, AF.Exp, (("pslin", ba),), ("s2a",))
            self.tt(h8(QB), h8(acc[:, 0:512]), b8(small2[:, 0:8]), ALU.mult, ("R", "s2a"), ("QB",))
            self.bt_store(QB, "QB", ck["qT"]["C"], s, g)
            self.tt(small2[:, 8:16], small[:, 0:8], pc8, ALU.subtract, ("small", ("pslin", ba)), ("s2b",))
            self.actf(small2[:, 16:24], small2[:, 8:16], AF.Exp, ("s2b",), ("s2c",))
            self.stt(h8(KB), h8(acc[:, 512:1024]), 0.125, b8(small2[:, 16:24]), ALU.mult, ALU.mult, ("R", "s2c"), ("KB",))
            self.bt_store(KB, "KB", ck["kT"]["C"], s, g)
            self.tt(small2[:, 24:32], small2[:, 8:16], pt8, ALU.add, ("s2b", ("pslin", bb)), ("s2d",))
            self.actf(small2[:, 32:40], small2[:, 24:32], AF.Exp, ("s2d",), ("s2e",))
            self.stt(h8(KH), h8(acc[:, 512:1024]), 0.125, b8(small2[:, 32:40]), ALU.mult, ALU.mult, ("R", "s2e"), ("KH",))
            P.dma("sp", ck["kh"]["C"][rs, :], KH, reads=("KH",), writes=(("kh", "C"),))
            self.actf(small2[:, 40:48], pt8, AF.Exp, (("pslin", bb),), ("s2f",))
            self.cp(h8(W[1]), b8(small2[:, 40:48]), ("s2f",), (wk(1),), eng="dve")
            P.dma("sp", ck["dec"]["C"][rs, :], W[1], reads=(wk(1),), writes=(("dec", "C"),))
            self.store_v(seg[:, 1024:1536], "seg", ck["v"]["C"], rs)
            self.actf(W[2], seg[:, 1536:2048], AF.Sigmoid, ("seg",), (wk(2),))
            P.dma("sp", gate["C"][rs, :], W[2], reads=(wk(2),), writes=(("gate", "C", tile),))
            P.dma("sp", seg[:, 0:RWC], self.pbuf[rs, 6160:6160 + RWC], reads=pall(tile), writes=("seg",))
            prev = sh[:, 0:RWC]
            self.load_shift(prev, ("sh", 0), 6160, RWC, tile, 1, extra_keys=(("sh", 1),))
            self.tt(prev, prev, seg[:, 0:RWC], ALU.subtract, (("sh", 0), ("sh", 1), "seg"), (("sh", 0), ("sh", 1)))
            self.tt(prev, prev, mu, ALU.mult, (("sh", 0), ("sh", 1), "mu"), (("sh", 0), ("sh", 1)))
            self.tt(seg[:, 0:RWC], seg[:, 0:RWC], prev, ALU.add, ("seg", ("sh", 0), ("sh", 1)), ("seg",))
            r_, k_, v_ = seg[:, 0:512], seg[:, 512:1024], seg[:, 1024:1536]
            Lt = W[0][:, 0:256]
            self.actf(Lt[:, 0:64], seg[:, 1536:1600], AF.Tanh, ("seg",), (wk(0),))
            self.cp(Lt[:, 64:128], seg[:, 1600:1664], ("seg",), (wk(0),))
            self.actf(Lt[:, 128:256], seg[:, 1664:1792], AF.Sigmoid, ("seg",), (wk(0),))
            LT = W[1][:, 0:256]
            for c in range(2):
                bank = self.psbank()
                pt = self.pslin[:, bank, 0:128]
                P.op("pe", lambda e, pt=pt, c=c: e.transpose(pt, Lt[:, c * 128:(c + 1) * 128], self.ident_f[:, :]),
                     reads=(wk(0), "identf"), writes=(("pslin", bank),))
                self.cp(LT[:, c * 128:(c + 1) * 128], pt, (("pslin", bank),), (wk(1),))
            bank = self.psbank()
            pw = self.pslin[:, bank, :]
            P.op("pe", lambda e, pw=pw: e.matmul(pw, lhsT=LT[:, 0:128], rhs=upw[:, 0:512], start=True, stop=True),
                 reads=(wk(1), "upw"), writes=(("pslin", bank),))
            self.tt(W[2], pw, sv["rwkv_w0"], ALU.add, (("pslin", bank), "c_rwkv_w0"), (wk(2),))
            self.actf(W[2], W[2], AF.Sigmoid, (wk(2),), (wk(2),))
            self.actf(W[2], W[2], AF.Exp, (wk(2),), (wk(2),), scale=-0.6065306597126334)
            P.dma("sp", rows["wD"][rs, :], W[2], reads=(wk(2),), writes=(("row", "wD", tile),))
            bank = self.psbank()
            pa = self.pslin[:, bank, :]
            P.op("pe", lambda e, pa=pa: e.matmul(pa, lhsT=LT[:, 0:128], rhs=upw[:, 512:1024], start=True, stop=True),
                 reads=(wk(1), "upw"), writes=(("pslin", bank),))
            self.tt(W[3], pa, sv["rwkv_a0"], ALU.add, (("pslin", bank), "c_rwkv_a0"), (wk(3),))
            self.actf(W[3], W[3], AF.Sigmoid, (wk(3),), (wk(3),))
            bank = self.psbank()
            pg = self.pslin[:, bank, :]
            P.op("pe", lambda e, pg=pg: e.matmul(pg, lhsT=LT[:, 128:256], rhs=gup, start=True, stop=True),
                 reads=(wk(1), "gup"), writes=(("pslin", bank),))
            self.cp(W[4], pg, (("pslin", bank),), (wk(4),))
            P.dma("sp", gate["D"][rs, :], W[4], reads=(wk(4),), writes=(("gate", "D", tile),))
            if l == 0:
                P.dma("sp", vfirst[rs, :], v_, reads=("seg",), writes=(("vfirst", tile),))
            else:
                vtt = self.vt
                for c in range(4):
                    bank = self.psbank()
                    pt = self.pslin[:, bank, 0:128]
                    P.op("pe", lambda e, pt=pt, c=c: e.transpose(pt, v_[:, c * 128:(c + 1) * 128], self.ident_f[:, :]),
                         reads=("seg", "identf"), writes=(("pslin", bank),))
                    self.cp(vtt[:, c, :], pt, (("pslin", bank),), ("vt",))
                bank = self.psbank()
                pv = self.pslin[:, bank, 0:32]
                for c in range(4):
                    P.op("pe", lambda e, pv=pv, c=c: e.matmul(pv, lhsT=vtt[:, c, :], rhs=vdn[:, c, :], start=(c == 0),
                                                              stop=(c == 3)),
                         reads=("vt", "vdn"), writes=(("pslin", bank),))
                self.cp(W[5][:, 0:32], pv, (("pslin", bank),), (wk(5),))
                bank = self.psbank()
                pt = self.pslin[0:32, bank, 0:128]
                P.op("pe", lambda e, pt=pt: e.transpose(pt, W[5][:, 0:32], self.ident_f[:, :]),
                     reads=(wk(5), "identf"), writes=(("pslin", bank),))
                self.cp(W[6][0:32, 0:128], pt, (("pslin", bank),), (wk(6),))
                bank = self.psbank()
                pv2 = self.pslin[:, bank, :]
                P.op("pe", lambda e, pv2=pv2: e.matmul(pv2, lhsT=W[6][0:32, 0:128], rhs=vup[0:32, :], start=True, stop=True),
                     reads=(wk(6), "vup"), writes=(("pslin", bank),))
                self.tt(W[5], pv2, v0b, ALU.add, (("pslin", bank), "v0b"), (wk(5),))
                self.actf(W[5], W[5], AF.Sigmoid, (wk(5),), (wk(5),))
                P.dma("sp", W[6], vfirst[rs, :], reads=(("vfirst", tile),), writes=(wk(6),))
                self.tt(W[6], W[6], v_, ALU.subtract, (wk(6), "seg"), (wk(6),))
                self.tt(W[6], W[6], W[5], ALU.mult, (wk(6), wk(5)), (wk(6),))
                self.tt(v_, v_, W[6], ALU.add, ("seg", wk(6)), ("seg",))
            self.tt(W[5], k_, sv["rwkv_k_k"], ALU.mult, ("seg", "c_rwkv_k_k"), (wk(5),))
            self.tt(W[6], W[5], W[5], ALU.mult, (wk(5),), (wk(6),))
            self.red(small[:, 32:40], h8(W[6]), (wk(6),), ("small4",))
            self.actf(small[:, 40:48], small[:, 32:40], AF.Sqrt, ("small4",), ("small5",))
            self.ts(small[:, 40:48], small[:, 40:48], 1e-12, None, ALU.max, None, ("small5",), ("small5",))
            P.op("dve", lambda e: e.reciprocal(out=small[:, 48:56], in_=small[:, 40:48]), reads=("small5",), writes=("small6",))
            self.tt(h8(W[5]), h8(W[5]), b8(small[:, 48:56]), ALU.mult, (wk(5), "small6"), (wk(5),))
            P.dma("sp", rows["kkD"][rs, :], W[5], reads=(wk(5),), writes=(("row", "kkD", tile),))
            self.stt(W[7], W[3], -1.0, sv["rwkv_k_a"], ALU.add, ALU.mult, (wk(3), "c_rwkv_k_a"), (wk(7),))
            self.stt(W[7], W[7], 1.0, k_, ALU.add, ALU.mult, (wk(7), "seg"), (wk(7),))
            P.dma("sp", rows["kD"][rs, :], W[7], reads=(wk(7),), writes=(("row", "kD", tile),))
            self.tt(W[8], W[3], W[5], ALU.mult, (wk(3), wk(5)), (wk(8),))
            P.dma("sp", rows["bD"][rs, :], W[8], reads=(wk(8),), writes=(("row", "bD", tile),))
            P.dma("sp", rows["rD"][rs, :], r_, reads=("seg",), writes=(("row", "rD", tile),))
            self.tt(W[9], r_, W[7], ALU.mult, ("seg", wk(7)), (wk(9),))
            self.tt(W[9], W[9], sv["rwkv_r_k"], ALU.mult, (wk(9), "c_rwkv_r_k"), (wk(9),))
            self.red(small[:, 56:64], h8(W[9]), (wk(9),), ("small7",))
            self.tt(h8(W[9]), h8(v_), b8(small[:, 56:64]), ALU.mult, ("seg", "small7"), (wk(9),))
            P.dma("sp", bonus[rs, :], W[9], reads=(wk(9),), writes=(("bonus", tile),))
            self.store_T(v_, "seg", vT["D"], s, g)

    def phase_scan(self, rows, vT, yT, sel_d, ck):
        P = self.P
        cfg = self.cfg
        T = cfg.T
        self.carve_reset()
        self._phase_id = getattr(self, "_phase_id", 0) + 1
        gd = self.gen_scan_D(rows, vT, yT, sel_d)
        gens = [self.gen_chunk(m, CHK[m], ck) for m in "ABC"]
        def chain():
            for gch in gens:
                for _ in gch:
                    yield
        gc = chain()
        nchunks = sum((T // CHK[m]) * cfg.NSEQ * 4 for m in "ABC")
        ratio = max(1, T // max(1, nchunks // 1))
        d_done = c_done = False
        per = max(1, int(round(nchunks / float(T))))
        while not (d_done and c_done):
            if not d_done:
                try:
                    next(gd)
                except StopIteration:
                    d_done = True
            for _ in range(per if not d_done else 64):
                if c_done:
                    break
                try:
                    next(gc)
                except StopIteration:
                    c_done = True

    def gen_scan_D(self, rows, vT, yT, sel_d):
        P = self.P
        cfg = self.cfg
        T = cfg.T
        sel = self.carve(TB * 128).rearrange("p (t m) -> p t m", m=128)
        P.dma("sp", sel[0:32, :, :], sel_d[:, :, :], writes=("sel",))
        Rb = [self.carve(5 * 512).rearrange("p (v n) -> p v n", n=512) for _ in range(2)]
        Vb = [self.carve(8 * TB).rearrange("p (g t) -> p g t", t=TB) for _ in range(2)]
        Yb = [self.carve(8 * TB).rearrange("p (g t) -> p g t", t=TB) for _ in range(2)]
        NR = 3
        BR = [self.carve(5 * 512).rearrange("p (v n) -> p v n", n=512) for _ in range(NR)]
        S = self.carve(512)
        T1 = self.carve(512)
        T2 = self.carve(512)
        sk = self.carve(8)
        h8 = lambda ap: ap.rearrange("p (h d) -> p h d", d=64)
        b8 = lambda ap8: ap8.unsqueeze(2).to_broadcast([128, 8, 64])
        m = "D"
        vecs = VECS[m]
        P.op("dve", lambda e: e.memset(S, 0.0), writes=("S",))
        step = 0
        dps = 0
        for bi in range(T // TB):
            t0 = bi * TB
            pb = bi % 2
            for j, vname in enumerate(vecs):
                src = rows[vname].rearrange("(s t) (hp hh d) -> t s hp hh d", s=cfg.NSEQ, hh=2, d=64)
                for hh in range(2):
                    for s in range(cfg.NSEQ):
                        P.dma("sp", Rb[pb][hh * TB:(hh + 1) * TB, j, s * 256:(s + 1) * 256].rearrange(
                            "t (hp d) -> t hp d", d=64),
                            src[t0:t0 + TB, s, :, hh, :],
                            reads=(("row", vname, (s * T + t0) // 128),), writes=(("Rb", pb, j),))
            P.dma("sp", Vb[pb].rearrange("p (s hp) t -> p s hp t", hp=4),
                  vT[m].rearrange("hp p s t -> p s hp t")[:, :, :, t0:t0 + TB],
                  reads=tuple(("T", id(vT[m]), s, t0 // 128) for s in range(cfg.NSEQ)), writes=(("Vb", pb),))
            for tl in range(TB):
                rb = step % NR
                step += 1
                ps = {}
                for j, vname in enumerate(vecs):
                    bank = dps % 3
                    dps += 1
                    pt = self.pslin[:, bank, :]
                    P.op("pe", lambda e, pt=pt, j=j, tl=tl, pb=pb: e.matmul(pt, lhsT=sel[0:32, tl, :], rhs=Rb[pb][0:32, j, :],
                                                                           start=True, stop=True),
                         reads=("sel", ("Rb", pb, j)), writes=(("pslin", bank),))
                    self.cp(BR[rb][:, j, :], pt, (("pslin", bank),), (("BR", rb, j),))
                    ps[j] = (BR[rb][:, j, :], ("BR", rb, j))
                vb = b8(Vb[pb][:, :, tl])
                yo = Yb[pb][:, :, tl]
                (rp, rk_), (kp, kk_), (wp, wk_), (cp_, ck_), (bp, bk_) = ps[0], ps[1], ps[2], ps[3], ps[4]
                self.tt(T1, S, cp_, ALU.mult, ("S", ck_), ("T1",))
                self.red(sk, h8(T1), ("T1",), ("sk",))
                self.tt(h8(T1), h8(bp), b8(sk), ALU.mult, (bk_, "sk"), ("T1",))
                self.tt(S, S, wp, ALU.mult, ("S", wk_), ("S",))
                self.tt(S, S, T1, ALU.subtract, ("S", "T1"), ("S",))
                self.tt(h8(T2), h8(kp), vb, ALU.mult, (kk_, ("Vb", pb)), ("T2",))
                self.tt(S, S, T2, ALU.add, ("S", "T2"), ("S",))
                self.tt(T2, S, rp, ALU.mult, ("S", rk_), ("T2",))
                self.red(yo, h8(T2), ("T2",), (("Yb", pb),))
                yield
            P.dma("sp", yT[m].rearrange("hp p s t -> p s hp t")[:, :, :, t0:t0 + TB],
                  Yb[pb].rearrange("p (s hp) t -> p s hp t", hp=4), reads=(("Yb", pb),),
                  writes=tuple(("T", id(yT[m]), s, t0 // 128) for s in range(cfg.NSEQ)))

    def gen_chunk(self, m, C, ck):
        P = self.P
        cfg = self.cfg
        T = cfg.T
        nch = T // C
        cb = self.chunk_bufs(T)
        qT, kT, q0, q1, kh, vv, yo, dch, decT, st_f, st_b, scm, mask = cb
        pstr_f = self.pstr[:, :, :].bitcast(F32)
        qT_d, kT_d, kh_d, v_d, dec_d, y_d = ck["qT"][m], ck["kT"][m], ck["kh"][m], ck["v"][m], ck["dec"][m], ck["y"][m]
        if not self._mask_loaded:
            P.dma("sp", mask[0:64, 0:64], ck["mask"], writes=("mask",))
            self._mask_loaded = True
        cnt = 0
        qs = [q0, q1]
        for s in range(cfg.NSEQ):
            for hp in range(4):
                allT = tuple(("T", id(qT_d), s, g) for g in range(cfg.TPS))
                P.dma("sp", qT[:, 0:T], qT_d[hp, :, s, :], reads=allT, writes=("c_qT",))
                P.dma("sp", kT[:, 0:T], kT_d[hp, :, s, :], reads=tuple(("T", id(kT_d), s, g) for g in range(cfg.TPS)),
                      writes=("c_kT",))
                P.dma("sp", kh[0:C, 0:nch, :], kh_d[s * T:(s + 1) * T, hp * 128:(hp + 1) * 128].rearrange(
                    "(n c) k -> c n k", c=C), reads=(("kh", m),), writes=("c_kh",))
                P.dma("sp", vv[0:C, 0:nch, :], v_d[s * T:(s + 1) * T, hp * 130:(hp + 1) * 130].rearrange(
                    "(n c) k -> c n k", c=C), reads=(("vd", id(v_d)),), writes=("c_vv",))
                P.dma("sp", dch[0:nch, :], dec_d[s * T:(s + 1) * T, hp * 128:(hp + 1) * 128].rearrange(
                    "(n c) k -> c n k", c=C)[C - 1, :, :], reads=(("dec", m),), writes=("c_dch",))
                pt = self.pslin[:, 5, 0:nch]
                P.op("pe", lambda e, pt=pt: e.transpose(pt, dch[0:nch, :], self.ident_f[0:nch, 0:nch]),
                     reads=("c_dch", "identf"), writes=(("pslin", 5),))
                self.cp(decT[:, 0:nch], pt, (("pslin", 5),), ("c_decT",))
                P.op("dve", lambda e: e.memset(q0[64:128, 0:T], 0.0), writes=("c_q0",))
                P.op("dve", lambda e: e.memset(q1[0:64, 0:T], 0.0), writes=("c_q1",))
                self.cp(q0[0:64, 0:T], qT[0:64, 0:T], ("c_qT",), ("c_q0",), eng="dve")
                self.cp(q1[64:128, 0:T], qT[64:128, 0:T], ("c_qT",), ("c_q1",), eng="dve")
                P.op("dve", lambda e: e.memset(st_f[:, 0:66], 0.0), writes=("st_f",))
                P.op("dve", lambda e: e.memset(st_b[:, 0:66], 0.0), writes=("st_b",))
                for c in range(nch):
                    cs = slice(c * C, (c + 1) * C)
                    for hh in range(2):
                        qh = qs[hh]
                        qk = "c_q%d" % hh
                        bsc = 3 + (cnt % 2)
                        psc = self.pslin[0:C, bsc, 0:C]
                        P.op("pe", lambda e, psc=psc, cs=cs, qh=qh: e.matmul(psc, lhsT=kT[:, cs], rhs=qh[:, cs], start=True,
                                                                             stop=True),
                             reads=("c_kT", qk), writes=(("pslin", bsc),))
                        sm = scm[cnt % 2][0:C, 0:C]
                        self.tt(sm, psc, mask[0:C, 0:C], ALU.mult, (("pslin", bsc), "mask"), (("scm", cnt % 2),))
                        bo = cnt % 2
                        pso = pstr_f[0:C, bo, 0:65]
                        vsl = vv[0:C, c, hh * 65:(hh + 1) * 65]
                        P.op("pe", lambda e, pso=pso, sm=sm, vsl=vsl: e.matmul(pso, lhsT=sm, rhs=vsl, start=True, stop=False),
                             reads=(("scm", cnt % 2), "c_vv"), writes=(("pstr", bo),))
                        P.op("pe", lambda e, pso=pso, cs=cs, qh=qh: e.matmul(pso, lhsT=qh[:, cs], rhs=st_b[:, 0:65],
                                                                             start=False, stop=True),
                             reads=(qk, "st_b"), writes=(("pstr", bo),))
                        self.cp(yo[0:C, c, hh * 65:(hh + 1) * 65], pso, (("pstr", bo),), ("c_yo",))
                        pkv = self.pslin[hh * 64:(hh + 1) * 64, 5, 0:65]
                        khs = kh[0:C, c, hh * 64:(hh + 1) * 64]
                        P.op("pe", lambda e, pkv=pkv, khs=khs, vsl=vsl: e.matmul(pkv, lhsT=khs, rhs=vsl, start=True, stop=True),
                             reads=("c_kh", "c_vv"), writes=(("pslin", 5),))
                        cnt += 1
                    self.stt(st_f[:, 0:65], st_f[:, 0:65], decT[:, c:c + 1], self.pslin[:, 5, 0:65], ALU.mult, ALU.add,
                             ("st_f", "c_decT", ("pslin", 5)), ("st_f",))
                    self.cp(st_b[:, 0:65], st_f[:, 0:65], ("st_f",), ("st_b",))
                    yield
                P.dma("sp", y_d[s * T:(s + 1) * T, hp * 130:(hp + 1) * 130].rearrange("(n c) k -> c n k", c=C),
                      yo[0:C, 0:nch, :], reads=("c_yo",), writes=(("cy", m),))

    def chunk_bufs(self, T):
        if getattr(self, "_cbufs_at", None) == id(self.P.ops["pe"]) and getattr(self, "_cbufs_phase", -1) == self._phase_id:
            return self._cbufs
        qT = self.carve(T // 2, BF16)
        kT = self.carve(T // 2, BF16)
        q0 = self.carve(T // 2, BF16)
        q1 = self.carve(T // 2, BF16)
        nmax = T // 32
        kh = self.carve(nmax * 64, BF16).rearrange("p (n k) -> p n k", k=128)
        vv = self.carve(nmax * 65, BF16).rearrange("p (n k) -> p n k", k=130)
        yo = self.carve(nmax * 130).rearrange("p (n k) -> p n k", k=130)
        dch = self.carve(128)
        decT = self.carve(64)
        st_f = self.carve(66)
        st_b = self.carve(33, BF16)
        scm = [self.carve(32, BF16) for _ in range(2)]
        mask = self.carve(64)
        self._cbufs = (qT, kT, q0, q1, kh, vv, yo, dch, decT, st_f, st_b, scm, mask)
        self._cbufs_at = id(self.P.ops["pe"])
        self._cbufs_phase = self._phase_id
        self._mask_loaded = False
        return self._cbufs

    def phase_post(self, l, I, yT, ck, gate, bonus, obuf):
        P = self.P
        cfg = self.cfg
        self.carve_reset()
        o = self.carve(2048)
        y = self.carve(512)
        y65 = self.carve(520).rearrange("p (h e) -> p h e", e=65)
        gt = self.carve(512)
        W = [self.carve(512) for _ in range(3)]
        self.vt = self.carve(512).rearrange("p (a b) -> p a b", b=128)
        sv = {nm: self.carve(512) for nm in ("hgrn_norm_w", "mlstm_norm_w", "rwkv_ln_w", "rwkv_ln_b")}
        small = self.carve(64)
        for nm in sv:
            self.bcast_load(sv[nm], I[nm][l], "c_" + nm)
        h8 = lambda ap: ap.rearrange("p (h d) -> p h d", d=64)
        b8 = lambda ap8: ap8.unsqueeze(2).to_broadcast([128, 8, 64])
        wk = lambda i: ("W", i)
        for tile in range(cfg.NTILE):
            s, g = tile // cfg.TPS, tile % cfg.TPS
            rs = slice(tile * 128, (tile + 1) * 128)
            for mi, m in enumerate("ABCD"):
                if m == "D":
                    self.load_T(y, "y", yT[m], s, g)
                else:
                    P.dma("sp", y65.rearrange("p h e -> p (h e)"), ck["y"][m][rs, :], reads=(("cy", m),), writes=("y65",))
                    self.cp(h8(y), y65[:, :, 0:64], ("y65",), ("y",), eng="dve")
                P.dma("sp", gt, gate[m][rs, :], reads=(("gate", m, tile),), writes=("gt",))
                osl = o[:, mi * 512:(mi + 1) * 512]
                if m == "C":
                    d8 = y65[:, :, 64]
                    self.ts(small[:, 0:8], d8, -1.0, None, ALU.mult, None, ("y65",), ("sm0",))
                    self.tt(small[:, 0:8], small[:, 0:8], d8, ALU.max, ("sm0", "y65"), ("sm0",))
                    self.ts(small[:, 0:8], small[:, 0:8], 1.0, None, ALU.max, None, ("sm0",), ("sm0",))
                    P.op("dve", lambda e: e.reciprocal(out=small[:, 8:16], in_=small[:, 0:8]), reads=("sm0",), writes=("sm1",))
                    self.tt(h8(y), h8(y), b8(small[:, 8:16]), ALU.mult, ("y", "sm1"), ("y",))
                if m in "ABC":
                    self.tt(W[0], y, y, ALU.mult, ("y",), (wk(0),))
                    self.red(small[:, 16:24], h8(W[0]), (wk(0),), ("sm2",))
                    self.actf(small[:, 24:32], small[:, 16:24], AF.Sqrt, ("sm2", "eps"), ("sm3",), scale=1.0 / 64,
                              bias=self.eps_t[:, 0:1])
                    P.op("dve", lambda e: e.reciprocal(out=small[:, 32:40], in_=small[:, 24:32]), reads=("sm3",), writes=("sm4",))
                    self.tt(h8(y), h8(y), b8(small[:, 32:40]), ALU.mult, ("y", "sm4"), ("y",))
                    if m == "B":
                        self.tt(y, y, sv["hgrn_norm_w"], ALU.mult, ("y", "c_hgrn_norm_w"), ("y",))
                    if m == "C":
                        self.tt(y, y, sv["mlstm_norm_w"], ALU.mult, ("y", "c_mlstm_norm_w"), ("y",))
                    self.tt(osl, y, gt, ALU.mult, ("y", "gt"), ("o",))
                else:
                    self.red(small[:, 16:24], h8(y), ("y",), ("sm2",))
                    self.ts(small[:, 16:24], small[:, 16:24], -1.0 / 64, None, ALU.mult, None, ("sm2",), ("sm2",))
                    self.tt(h8(y), h8(y), b8(small[:, 16:24]), ALU.add, ("y", "sm2"), ("y",))
                    self.tt(W[0], y, y, ALU.mult, ("y",), (wk(0),))
                    self.red(small[:, 24:32], h8(W[0]), (wk(0),), ("sm3",))
                    self.actf(small[:, 32:40], small[:, 24:32], AF.Sqrt, ("sm3", "eps"), ("sm4",), scale=1.0 / 64,
                              bias=self.eps_t[:, 1:2])
                    P.op("dve", lambda e: e.reciprocal(out=small[:, 40:48], in_=small[:, 32:40]), reads=("sm4",), writes=("sm5",))
                    self.tt(h8(y), h8(y), b8(small[:, 40:48]), ALU.mult, ("y", "sm5"), ("y",))
                    self.tt(y, y, sv["rwkv_ln_w"], ALU.mult, ("y", "c_rwkv_ln_w"), ("y",))
                    self.tt(y, y, sv["rwkv_ln_b"], ALU.add, ("y", "c_rwkv_ln_b"), ("y",))
                    P.dma("sp", W[1], bonus[rs, :], reads=(("bonus", tile),), writes=(wk(1),))
                    self.tt(y, y, W[1], ALU.add, ("y", wk(1)), ("y",))
                    self.tt(osl, y, gt, ALU.mult, ("y", "gt"), ("o",))
            P.dma("sp", obuf[rs, :], o, reads=("o",), writes=(("o", tile),))

    def phase_ffn(self, l, hbuf, obuf, wbo, wbg, wbu, wbd, wnb):
        P = self.P
        cfg = self.cfg
        NG = 4
        self.carve_linear(NG)
        actT = self.carve(44 * 256, BF16).rearrange("p (k g t) -> p k g t", g=NG, t=128)
        acc = [self.carve(2048) for _ in range(NG)]
        xT = self.xT
        for grp in range(cfg.NTILE // NG):
            for g in range(NG):
                tile = grp * NG + g
                ht = self.htile[tile % 2]
                P.dma("sp", ht, obuf[tile * 128:(tile + 1) * 128, :], reads=(("o", tile),), writes=(("htile", tile % 2),))
                self.cp(self.ub[:, 0:2048], ht, (("htile", tile % 2),), ("ub",), eng="dve")
                self.transpose_to(self.ub, "ub", xT, g, "xT", 16)

            def consume(g, cb, nc_, pst, pskey):
                self.cp(acc[g][:, cb * 512:cb * 512 + nc_], pst, (pskey,), (("acc", g),))

            self.linear_tm(xT, ("xT",), NG, 16, wbo, D, consume)
            self.resid_update(grp, NG, acc, hbuf, wnb[:, 1, :])
            for g in range(NG):
                tile = grp * NG + g
                ht = self.htile[tile % 2]
                P.dma("sp", ht, hbuf[tile * 128:(tile + 1) * 128, :], reads=(("h", tile),), writes=(("htile", tile % 2),))
                self.norm_transpose(ht, ("htile", tile % 2), wnb[:, 2, :], xT, g, "xT")
            for cb in range(DFF // 512):
                wg = self.wbuf[0].rearrange("p (k n) -> p k n", n=512)
                wu = self.wbuf[1].rearrange("p (k n) -> p k n", n=512)
                P.dma("sp", wg, wbg[cb, :, :, :], reads=(("wb", id(wbg), cb),), writes=(("wbuf", 0),))
                P.dma("sp", wu, wbu[cb, :, :, :], reads=(("wb", id(wbu), cb),), writes=(("wbuf", 1),))
                for j in range(4):
                    nchunk = cb * 4 + j
                    bg = self.psbank()
                    bu = self.psbank()
                    pg = self.pslin[:, bg, :]
                    pu = self.pslin[:, bu, :]
                    for kc in range(16):
                        P.op("pe", lambda e, pg=pg, kc=kc, j=j: e.matmul(pg, lhsT=wg[:, kc, j * 128:(j + 1) * 128],
                                                                         rhs=xT[:, kc, :, :].rearrange("p g t -> p (g t)"),
                                                                         start=(kc == 0), stop=(kc == 15)),
                             reads=("xT", ("wbuf", 0)), writes=(("pslin", bg),))
                    for kc in range(16):
                        P.op("pe", lambda e, pu=pu, kc=kc, j=j: e.matmul(pu, lhsT=wu[:, kc, j * 128:(j + 1) * 128],
                                                                         rhs=xT[:, kc, :, :].rearrange("p g t -> p (g t)"),
                                                                         start=(kc == 0), stop=(kc == 15)),
                             reads=("xT", ("wbuf", 1)), writes=(("pslin", bu),))
                    st = self.stage[self.ssel % 4]
                    skey = ("stage", self.ssel % 4)
                    self.ssel += 1
                    self.actf(st, pg, AF.Silu, (("pslin", bg),), (skey,))
                    self.tt(actT[:, nchunk, :, :].rearrange("p g t -> p (g t)"), st, pu, ALU.mult, (skey, ("pslin", bu)),
                            ("actT",))

            def consume2(g, cb, nc_, pst, pskey):
                self.cp(acc[g][:, cb * 128:cb * 128 + nc_], pst, (pskey,), (("acc", g),))

            self.linear_tm(actT, ("actT",), NG, 44, wbd, D, consume2, CB=128)
            self.resid_update(grp, NG, acc, hbuf, wnb[:, 3, :])

    def resid_update(self, grp, NG, acc, hbuf, wn):
        P = self.P
        for g in range(NG):
            tile = grp * NG + g
            ht = self.htile[tile % 2]
            hk = ("htile", tile % 2)
            P.dma("sp", ht, hbuf[tile * 128:(tile + 1) * 128, :], reads=(("h", tile),), writes=(hk,))
            self.rstd_of(acc[g], ("acc", g), 2048, self.eps_t[:, 0:1])
            self.stt(acc[g], acc[g], self.ss_t[:, 2:3], wn, ALU.mult, ALU.mult, (("acc", g), "ss2", "wnb"), (("acc", g),))
            self.tt(ht, ht, acc[g], ALU.add, (hk, ("acc", g)), (hk,))
            P.dma("sp", hbuf[tile * 128:(tile + 1) * 128, :], ht, reads=(hk,), writes=(("h", tile),))


def make_consts(T):
    half = 32
    inv_freq = 10000.0 ** (-np.arange(half, dtype=np.float32) / half)
    ang = np.arange(T, dtype=np.float32)[:, None] * inv_freq[None, :]
    cossin = np.concatenate([np.cos(ang), np.sin(ang)], axis=1).astype(np.float32)
    sel = np.zeros((32, TB, 128), np.float32)
    for tl in range(TB):
        for m in range(128):
            sel[(m // 64) * TB + tl, tl, m] = 1.0
    gam = np.zeros((128, 2, 4, 64), np.float32)
    for p in range(128):
        for hp in range(4):
            h = 2 * hp + p // 64
            gam[p, :, hp, :] = 1.0 - 2.0 ** (-5.0 - h)
    C = CHK["A"]
    j = (np.arange(128) % C).astype(np.float64)
    gh = 1.0 - 2.0 ** (-5.0 - np.arange(8, dtype=np.float64))
    atab = np.zeros((4, 128, 8, 64), np.float64)
    atab[0] = (gh[None, :] ** (j[:, None] + 1.0))[:, :, None]
    atab[1] = (0.125 * gh[None, :] ** (-(j[:, None] + 1.0)))[:, :, None]
    atab[2] = (0.125 * gh[None, :] ** (C - 1.0 - j[:, None]))[:, :, None]
    atab[3] = (gh ** float(C))[None, :, None]
    idx = np.arange(128)
    def trib(c):
        same = (idx[:, None] // c) == (idx[None, :] // c)
        return (same & (idx[:, None] <= idx[None, :])).astype(np.float32), same.astype(np.float32)
    t64, b64 = trib(64)
    t32, b32 = trib(32)
    trimats = np.stack([t64, b64, t32, b32]).astype(np.float32)
    cmask = (np.arange(64)[:, None] <= np.arange(64)[None, :]).astype(np.float32)
    return dict(cossin=cossin, sel=sel, gamA=gam.reshape(128, 512), atab=atab.reshape(4, 128, 512).astype(np.float32),
                trimats=trimats, cmask=cmask,
                ident_b=np.eye(128).astype(ml_dtypes.bfloat16), ident_f=np.eye(128, dtype=np.float32))


def make_shared(inp, L, T):
    f = lambda a: np.ascontiguousarray(np.asarray(a, dtype=np.float32))
    sh = dict(w_in=f(inp["w_in"]), w_out=f(inp["w_out"]), w_ffn_gate=f(inp["w_ffn_gate"]), w_ffn_up=f(inp["w_ffn_up"]),
              w_ffn_down=f(inp["w_ffn_down"]))
    sh["norms"] = np.ascontiguousarray(np.stack([f(inp["norm_pre_mix"]), f(inp["norm_post_mix"]), f(inp["norm_pre_ffn"]),
                                                 f(inp["norm_post_ffn"])], axis=1))
    for nm in SMALL + ["hgrn_lb_logits", "mlstm_conv_w", "mlstm_conv_b", "rwkv_mu"]:
        sh[nm] = f(inp[nm])
    sh["mlstm_if_bias"] = np.ascontiguousarray(np.concatenate([f(inp["mlstm_i_bias"]), f(inp["mlstm_f_bias"])], axis=1))
    up = np.zeros((L, 128, 1024), np.float32)
    up[:, 0:64, 0:512] = f(inp["rwkv_w_up"])
    up[:, 64:128, 512:1024] = f(inp["rwkv_a_up"])
    sh["rwkv_up_pad"] = up
    sh["rwkv_g_up"] = f(inp["rwkv_g_up"])
    if L > 1:
        sh["rwkv_v0"] = f(inp["rwkv_v0"])
        sh["rwkv_v_down"] = f(inp["rwkv_v_down"])
        sh["rwkv_v_up"] = f(inp["rwkv_v_up"])
    else:
        sh["rwkv_v0"] = np.zeros((1, G), np.float32)
        sh["rwkv_v_down"] = np.zeros((1, G, 32), np.float32)
        sh["rwkv_v_up"] = np.zeros((1, 32, G), np.float32)
    sh.update(make_consts(T))
    return sh


_CACHE = {}


def kernel(**inp):
    x = np.asarray(inp["x"], dtype=np.float32)
    B, T, _ = x.shape
    L = inp["w_in"].shape[0]
    ncores = 8
    nseq = B // ncores
    cfg = Cfg(T=T, NSEQ=nseq, L=L)
    nc = Builder(cfg).build()
    sh = make_shared(inp, L, T)
    in_maps = []
    for c in range(ncores):
        m = dict(sh)
        m["x"] = np.ascontiguousarray(x[c * nseq:(c + 1) * nseq].reshape(nseq * T, D))
        in_maps.append(m)
    res = run_bass_kernel_spmd(nc, in_maps, core_ids=list(range(ncores)))
    outs = [np.asarray(r["out"]).reshape(nseq, T, D) for r in res.results]
    return np.concatenate(outs, axis=0).astype(np.float32)
```

```python
from contextlib import ExitStack
import numpy as np
import ml_dtypes
import concourse.bass as bass
import concourse.mybir as mybir
from concourse.bass_utils import run_bass_kernel_spmd

F32 = mybir.dt.float32
BF16 = mybir.dt.bfloat16
ALU = mybir.AluOpType
AF = mybir.ActivationFunctionType
AX = mybir.AxisListType

D = 2048
NIN = 7952
DFF = 5632
G = 512
NH = 8
HD = 64
NORM_EPS = 1e-6


class Prog:
    ENG = ("pe", "dve", "act", "pool", "sp")
    NDMASEM = 6

    def __init__(self, nc, stack):
        self.nc = nc
        self.stack = stack
        self.ops = {e: [] for e in self.ENG}
        self.cnt = {e: 0 for e in self.ENG}
        self.known = {e: {} for e in self.ENG}
        self.sem = {e: stack.enter_context(nc.semaphore("s_" + e)) for e in self.ENG if e != "sp"}
        self.semname = {}
        for e, s in self.sem.items():
            self.semname[id(s)] = e
        self.dq = {}
        for q in ("sp", "pool", "act"):
            self.dq[q] = {"sems": [stack.enter_context(nc.semaphore(f"d_{q}{i}")) for i in range(self.NDMASEM)],
                          "n": 0}
        self.bufs = {}
        self.nops = 0

    def _buf(self, k):
        b = self.bufs.get(k)
        if b is None:
            b = [None, {}]
            self.bufs[k] = b
        return b

    def _deps(self, e, reads, writes, extra=()):
        toks = list(extra)
        for k in reads:
            b = self._buf(k)
            if b[0] is not None:
                toks.append((b[0], False))
        for k in writes:
            b = self._buf(k)
            if b[0] is not None:
                toks.append((b[0], False))
            for s, v in b[1].values():
                toks.append(((s, v), True))
        own = self.sem.get(e)
        waits = []
        kn = self.known[e]
        for (s, v), is_reader in toks:
            if s is own and (e == "pe" or is_reader):
                continue
            if kn.get(id(s), 0) >= v:
                continue
            kn[id(s)] = v
            waits.append((s, v))
        return waits

    def _commit(self, tok, reads, writes):
        s, v = tok
        for k in reads:
            b = self._buf(k)
            b[1][id(s)] = (s, v)
        for k in writes:
            b = self._buf(k)
            b[0] = tok
            b[1] = {}

    def op(self, e, fn, reads=(), writes=()):
        reads = tuple(reads)
        writes = tuple(writes)
        waits = self._deps(e, reads, writes)
        self.cnt[e] += 1
        tok = (self.sem[e], self.cnt[e])
        self.ops[e].append((waits, fn, (self.sem[e], 1)))
        self._commit(tok, reads, writes)
        self.nops += 1
        return tok

    def dma(self, q, out, in_, reads=(), writes=(), **kw):
        reads = tuple(reads)
        writes = tuple(writes)
        dq = self.dq[q]
        n = dq["n"]
        dq["n"] += 1
        s = dq["sems"][n % self.NDMASEM]
        prev = 16 * (n // self.NDMASEM)
        extra = [((s, prev), False)] if prev > 0 else []
        waits = self._deps(q, reads, writes, extra)
        tok = (s, prev + 16)
        self.ops[q].append((waits, lambda eng: eng.dma_start(out=out, in_=in_, **kw), (s, 16)))
        self._commit(tok, reads, writes)
        self.nops += 1
        return tok

    def barrier(self):
        toks = [(self.sem[e], self.cnt[e]) for e in self.sem if self.cnt[e] > 0] + self.all_dma_tokens()
        for e in self.ENG:
            waits = []
            kn = self.known[e]
            for s, v in toks:
                if e == "pe" and s is self.sem.get("pe"):
                    continue
                if kn.get(id(s), 0) >= v:
                    continue
                kn[id(s)] = v
                waits.append((s, v))
            if waits:
                self.ops[e].append((waits, None, None))
        self.bufs = {}

    def finish(self, e, toks):
        waits = []
        for s, v in toks:
            waits.append((s, v))
        self.ops[e].append((waits, None, None))

    def all_dma_tokens(self):
        toks = []
        for q, dq in self.dq.items():
            n = dq["n"]
            for i in range(self.NDMASEM):
                c = (n - i + self.NDMASEM - 1) // self.NDMASEM if n > i else 0
                if c > 0:
                    toks.append((dq["sems"][i], 16 * c))
        return toks

    def emit(self):
        nc = self.nc
        ops = self.ops

        def run(eng, lst):
            for waits, fn, inc in lst:
                for s, v in waits:
                    eng.wait_ge(s, v)
                if fn is not None:
                    ins = fn(eng)
                    ins.then_inc(inc[0], inc[1])

        with nc.Block() as block:
            @block.tensor
            def _(eng):
                run(eng, ops["pe"])

            @block.vector
            def _(eng):
                run(eng, ops["dve"])

            @block.scalar
            def _(eng):
                run(eng, ops["act"])

            @block.gpsimd
            def _(eng):
                run(eng, ops["pool"])

            @block.sync
            def _(eng):
                run(eng, ops["sp"])


RWC = 1792
OFFS = {"A": 0, "B": 2048, "C": 4096, "D": 6160}
VECS = {"D": ["rD", "kD", "wD", "kkD", "bD"]}
ALLV = VECS["D"]
CHK = {"A": 64, "B": 32, "C": 64}
TB = 16
SMALL = ["hgrn_norm_w", "mlstm_norm_w", "rwkv_w0", "rwkv_a0", "rwkv_k_k", "rwkv_k_a", "rwkv_r_k", "rwkv_ln_w",
         "rwkv_ln_b"]


class Cfg:
    def __init__(self, T=2048, NSEQ=2, L=4):
        self.T = T
        self.NSEQ = NSEQ
        self.L = L
        self.NT = T * NSEQ
        self.TPS = T // 128
        self.NTILE = self.NT // 128


def wb_shape(K, N, CB=512):
    ncb = (N + CB - 1) // CB
    return [ncb, 128, K // 128, CB]


class Builder:
    def __init__(self, cfg):
        self.cfg = cfg
        self.nc = bass.Bass("TRN2", target_bir_lowering=False)
        self.stack = ExitStack()
        self.P = None

    def dram_in(self, name, shape, dt=F32):
        return self.nc.dram_tensor(name, list(shape), dt, kind="ExternalInput").ap()

    def dram_out(self, name, shape, dt=F32):
        return self.nc.dram_tensor(name, list(shape), dt, kind="ExternalOutput").ap()

    def dram_tmp(self, name, shape, dt=F32):
        return self.nc.dram_tensor(name, list(shape), dt, kind="Internal").ap()

    def sb(self, name, shape, dt=F32):
        h = self.stack.enter_context(self.nc.sbuf_tensor(name, list(shape), dt))
        return h[tuple(slice(None) for _ in shape)]

    def ps(self, name, shape, dt=F32):
        return self.stack.enter_context(self.nc.psum_tensor(name, list(shape), dt))

    def carve_reset(self):
        self.cptr = 0

    def carve(self, ncols, dt=F32):
        a = self.arena[:, self.cptr:self.cptr + ncols]
        self.cptr += ncols
        assert self.cptr <= self.NARENA, self.cptr
        if dt == BF16:
            a = a.bitcast(BF16)
        return a

    def tt(self, out, a, b, op, r, w, eng="dve"):
        self.P.op(eng, lambda e: e.tensor_tensor(out=out, in0=a, in1=b, op=op), reads=r, writes=w)

    def ts(self, out, a, s1, s2, op0, op1, r, w, eng="dve"):
        if op1 is None:
            self.P.op(eng, lambda e: e.tensor_scalar(out=out, in0=a, scalar1=s1, scalar2=None, op0=op0), reads=r, writes=w)
        else:
            self.P.op(eng, lambda e: e.tensor_scalar(out=out, in0=a, scalar1=s1, scalar2=s2, op0=op0, op1=op1),
                      reads=r, writes=w)

    def stt(self, out, a, scalar, b, op0, op1, r, w):
        self.P.op("dve", lambda e: e.scalar_tensor_tensor(out=out, in0=a, scalar=scalar, in1=b, op0=op0, op1=op1),
                  reads=r, writes=w)

    def actf(self, out, a, func, r, w, scale=1.0, bias=None):
        if bias is None:
            self.P.op("act", lambda e: e.activation(out=out, in_=a, func=func, scale=scale), reads=r, writes=w)
        else:
            self.P.op("act", lambda e: e.activation(out=out, in_=a, func=func, scale=scale, bias=bias), reads=r, writes=w)

    def red(self, out, a, r, w):
        self.P.op("dve", lambda e: e.tensor_reduce(out=out, in_=a, axis=AX.X, op=ALU.add), reads=r, writes=w)

    def cp(self, out, a, r, w, eng="act"):
        if eng == "act":
            self.P.op("act", lambda e: e.copy(out=out, in_=a), reads=r, writes=w)
        else:
            self.P.op(eng, lambda e: e.tensor_copy(out=out, in_=a), reads=r, writes=w)

    def psbank(self):
        b = self.psel % self.NPS
        self.psel += 1
        return b

    def cast_weight(self, W, Wb, K, N, CB=512):
        P = self.P
        KC = K // 128
        ncb = (N + CB - 1) // CB
        Wv = W.rearrange("(kc p) n -> p kc n", p=128)
        for cb in range(ncb):
            c0 = cb * CB
            nc_ = min(CB, N - c0)
            for k0 in range(0, KC, 16):
                k1 = min(KC, k0 + 16)
                P.dma("pool", Wb[cb, :, k0:k1, 0:nc_], Wv[:, k0:k1, c0:c0 + nc_], reads=(), writes=(("wb", id(Wb), cb),))

    def linear_tm(self, xT, xkeys, ng, KC, Wb, N, consume, CB=512):
        P = self.P
        ncb = (N + CB - 1) // CB
        for cb in range(ncb):
            nc_ = min(CB, N - cb * CB)
            b = self.wsel % 2
            self.wsel += 1
            wbuf = self.wbuf[b][:, 0:KC * CB].rearrange("p (k n) -> p k n", n=CB)
            P.dma("sp", wbuf[:, :, 0:nc_], Wb[cb, :, 0:KC, 0:nc_], reads=(("wb", id(Wb), cb),), writes=(("wbuf", b),))
            for g in range(ng):
                bank = self.psbank()
                pst = self.pslin[:, bank, 0:nc_]
                for kc in range(KC):
                    P.op("pe", (lambda e, pst=pst, g=g, kc=kc, wbuf=wbuf, nc_=nc_:
                                e.matmul(pst, lhsT=xT[:, kc, g, :], rhs=wbuf[:, kc, 0:nc_],
                                         start=(kc == 0), stop=(kc == KC - 1))),
                         reads=tuple(xkeys) + (("wbuf", b),), writes=(("pslin", bank),))
                consume(g, cb, nc_, pst, ("pslin", bank))

    def rstd_of(self, src, src_key, n, eps_ap):
        P = self.P
        ss = self.ss_t
        junk = self.junk
        P.op("act", lambda e: e.activation(out=junk[:, 0:n], in_=src, func=AF.Square, accum_out=ss[:, 0:1]),
             reads=(src_key,), writes=("junk", "ss"))
        P.op("act", lambda e: e.activation(out=ss[:, 1:2], in_=ss[:, 0:1], func=AF.Sqrt, scale=1.0 / n, bias=eps_ap),
             reads=("ss", "eps"), writes=("ss1",))
        P.op("dve", lambda e: e.reciprocal(out=ss[:, 2:3], in_=ss[:, 1:2]), reads=("ss1",), writes=("ss2",))

    def norm_transpose(self, src_tile, src_key, wn_b, xT, g, xkey):
        self.rstd_of(src_tile, src_key, 2048, self.eps_t[:, 0:1])
        ub = self.ub
        self.stt(ub[:, 0:2048], src_tile, self.ss_t[:, 2:3], wn_b, ALU.mult, ALU.mult, (src_key, "ss2", "wnb"), ("ub",))
        self.transpose_to(ub, "ub", xT, g, xkey, 16)

    def transpose_to(self, ub, ubkey, xT, g, xkey, KC):
        P = self.P
        for k0 in range(0, KC, 4):
            k1 = min(KC, k0 + 4)
            bank = self.tsel % 2
            self.tsel += 1
            pt = self.pstr[:, bank, :]
            for kc in range(k0, k1):
                P.op("pe", (lambda e, kc=kc, pt=pt, k0=k0:
                            e.transpose(pt[:, (kc - k0) * 128:(kc - k0 + 1) * 128], ub[:, kc * 128:(kc + 1) * 128],
                                        self.ident_b[:, :])),
                     reads=(ubkey, "ident"), writes=(("pstr", bank),))
            n = k1 - k0
            P.op("act", (lambda e, pt=pt, k0=k0, n=n:
                         e.copy(out=xT[:, k0:k0 + n, g, :], in_=pt[:, 0:n * 128].rearrange("p (k t) -> p k t", t=128))),
                 reads=(("pstr", bank),), writes=(xkey,))

    def store_T(self, src, src_key, dstT, s, g):
        P = self.P
        vt = self.vt
        for hp in range(4):
            bank = self.psbank()
            pt = self.pslin[:, bank, 0:128]
            P.op("pe", lambda e, pt=pt, hp=hp: e.transpose(pt, src[:, hp * 128:(hp + 1) * 128], self.ident_f[:, :]),
                 reads=(src_key, "identf"), writes=(("pslin", bank),))
            self.cp(vt[:, hp, :], pt, (("pslin", bank),), ("vt",))
        P.dma("sp", dstT[:, :, s, g * 128:(g + 1) * 128].rearrange("hp p t -> p hp t"), vt[:, :, :], reads=("vt",),
              writes=(("T", id(dstT), s, g),))

    def bt_store(self, src_bf, src_key, dstT, s, g):
        P = self.P
        bank = self.tsel % 2
        self.tsel += 1
        pt = self.pstr[:, bank, :]
        for hp in range(4):
            P.op("pe", lambda e, hp=hp, pt=pt: e.transpose(pt[:, hp * 128:(hp + 1) * 128], src_bf[:, hp * 128:(hp + 1) * 128],
                                                          self.ident_b[:, :]),
                 reads=(src_key, "ident"), writes=(("pstr", bank),))
        tb = self.tbt
        P.op("act", lambda e, pt=pt: e.copy(out=tb[:, :, :], in_=pt[:, 0:512].rearrange("p (k t) -> p k t", t=128)),
             reads=(("pstr", bank),), writes=("tbt",))
        P.dma("sp", dstT[:, :, s, g * 128:(g + 1) * 128].rearrange("hp p t -> p hp t"), tb[:, :, :], reads=("tbt",),
              writes=(("T", id(dstT), s, g),))

    def store_v(self, v_src, v_key, v_d, rs):
        self.cp(self.vb[:, :, 0:64], v_src.rearrange("p (h e) -> p h e", e=64), (v_key,), ("vb",), eng="dve")
        self.P.dma("sp", v_d[rs, :], self.vb.rearrange("p h e -> p (h e)"), reads=("vb",), writes=(("vd", id(v_d)),))

    def load_T(self, dst, dst_key, srcT, s, g):
        P = self.P
        vt = self.vt
        P.dma("sp", vt[:, :, :], srcT[:, :, s, g * 128:(g + 1) * 128].rearrange("hp p t -> p hp t"),
              reads=(("T", id(srcT), s, g),), writes=("vt",))
        for hp in range(4):
            bank = self.psbank()
            pt = self.pslin[:, bank, 0:128]
            P.op("pe", lambda e, pt=pt, hp=hp: e.transpose(pt, vt[:, hp, :], self.ident_f[:, :]),
                 reads=("vt", "identf"), writes=(("pslin", bank),))
            self.cp(dst[:, hp * 128:(hp + 1) * 128], pt, (("pslin", bank),), (dst_key,))

    def load_shift(self, dst, key, c0, n, tile, k, extra_keys=()):
        P = self.P
        keys = (key,) + tuple(extra_keys)
        cfg = self.cfg
        g = tile % cfg.TPS
        r0 = tile * 128
        if g == 0:
            P.op("dve", lambda e: e.memset(dst[:, 0:n], 0.0), writes=keys)
            P.dma("sp", dst[k:128, 0:n], self.pbuf[r0:r0 + 128 - k, c0:c0 + n],
                  reads=tuple(("p", tile, cb) for cb in range(16)), writes=keys)
        else:
            P.dma("sp", dst[:, 0:n], self.pbuf[r0 - k:r0 - k + 128, c0:c0 + n],
                  reads=tuple(("p", tile, cb) for cb in range(16)) + tuple(("p", tile - 1, cb) for cb in range(16)),
                  writes=keys)

    def bcast_load(self, dst, src_row, key):
        self.P.dma("sp", dst, src_row.partition_broadcast(128), writes=(key,))

    def build(self, dbg=None, layers=None):
        cfg = self.cfg
        nc = self.nc
        L = cfg.L
        NT = cfg.NT
        T = cfg.T
        NSEQ = cfg.NSEQ
        self.P = P = Prog(nc, self.stack)
        layers = list(range(L)) if layers is None else layers
        I = {}
        x = self.dram_in("x", [NT, D])
        out = self.dram_out("out", [NT, D])
        w_in = self.dram_in("w_in", [L, D, NIN])
        w_out = self.dram_in("w_out", [L, D, D])
        w_g = self.dram_in("w_ffn_gate", [L, D, DFF])
        w_u = self.dram_in("w_ffn_up", [L, D, DFF])
        w_d = self.dram_in("w_ffn_down", [L, DFF, D])
        norms = self.dram_in("norms", [L, 4, D])
        for nm in SMALL:
            I[nm] = self.dram_in(nm, [L, G])
        I["hgrn_lb_logits"] = self.dram_in("hgrn_lb_logits", [L, G])
        I["mlstm_conv_w"] = self.dram_in("mlstm_conv_w", [L, 4, 2 * G])
        I["mlstm_conv_b"] = self.dram_in("mlstm_conv_b", [L, 2 * G])
        I["mlstm_if_bias"] = self.dram_in("mlstm_if_bias", [L, 16])
        I["rwkv_mu"] = self.dram_in("rwkv_mu", [L, RWC])
        I["rwkv_up_pad"] = self.dram_in("rwkv_up_pad", [L, 128, 1024])
        I["rwkv_g_up"] = self.dram_in("rwkv_g_up", [L, 128, G])
        I["rwkv_v0"] = self.dram_in("rwkv_v0", [max(L - 1, 1), G])
        I["rwkv_v_down"] = self.dram_in("rwkv_v_down", [max(L - 1, 1), G, 32])
        I["rwkv_v_up"] = self.dram_in("rwkv_v_up", [max(L - 1, 1), 32, G])
        ident_b_d = self.dram_in("ident_b", [128, 128], BF16)
        ident_f_d = self.dram_in("ident_f", [128, 128])
        cs_d = self.dram_in("cossin", [T, 64])
        sel_d = self.dram_in("sel", [32, TB, 128])
        gam_d = self.dram_in("gamA", [128, 512])
        atab_d = self.dram_in("atab", [4, 128, 512])
        tri_d = self.dram_in("trimats", [4, 128, 128])
        mask_d = self.dram_in("cmask", [64, 64])

        hbuf = self.dram_tmp("hbuf", [NT, D])
        self.pbuf = pbuf = self.dram_tmp("pbuf", [NT, NIN])
        obuf = self.dram_tmp("obuf", [NT, D])
        rows = {v: self.dram_tmp("row_" + v, [NT, G]) for v in ALLV}
        vT = {m: self.dram_tmp("vT_" + m, [4, 128, NSEQ, T]) for m in "D"}
        yT = {m: self.dram_tmp("yT_" + m, [4, 128, NSEQ, T]) for m in "D"}
        ck = dict(
            qT={m: self.dram_tmp("cqT_" + m, [4, 128, NSEQ, T], BF16) for m in "ABC"},
            kT={m: self.dram_tmp("ckT_" + m, [4, 128, NSEQ, T], BF16) for m in "ABC"},
            kh={m: self.dram_tmp("ckh_" + m, [NT, G], BF16) for m in "ABC"},
            v={m: self.dram_tmp("cv_" + m, [NT, 520], BF16) for m in "ABC"},
            dec={m: self.dram_tmp("cdec_" + m, [NT, G]) for m in "ABC"},
            y={m: self.dram_tmp("cy_" + m, [NT, 520]) for m in "ABC"},
            atab=atab_d, tri=tri_d, mask=mask_d)
        dmask_d = self.dram_in("dmasks", [3, 64, 64])
        dk = dict(rT=self.dram_tmp("d_rT", [4, 128, NSEQ, T], BF16), cT=self.dram_tmp("d_cT", [4, 128, NSEQ, T], BF16),
                  kT=self.dram_tmp("d_kT", [4, 128, NSEQ, T], BF16), bT=self.dram_tmp("d_bT", [4, 128, NSEQ, T], BF16),
                  kh=self.dram_tmp("d_kh", [NT, G], BF16), bh=self.dram_tmp("d_bh", [NT, G], BF16),
                  v=self.dram_tmp("d_v", [NT, G], BF16), dec=self.dram_tmp("d_dec", [NT, G]),
                  y=self.dram_tmp("d_y", [NT, G]), masks=dmask_d)
        ck["D"] = dk
        gate = {m: self.dram_tmp("gate_" + m, [NT, G]) for m in "ABCD"}
        bonus = self.dram_tmp("bonus", [NT, G])
        vfirst = self.dram_tmp("vfirst", [NT, G])
        wb_in = [self.dram_tmp(f"wb_in{l}", wb_shape(D, NIN), BF16) for l in range(L)]
        wb_out = [self.dram_tmp(f"wb_out{l}", wb_shape(D, D), BF16) for l in range(L)]
        wb_g = [self.dram_tmp(f"wb_g{l}", wb_shape(D, DFF), BF16) for l in range(L)]
        wb_u = [self.dram_tmp(f"wb_u{l}", wb_shape(D, DFF), BF16) for l in range(L)]
        wb_d = [self.dram_tmp(f"wb_d{l}", wb_shape(DFF, D, 128), BF16) for l in range(L)]
        dbg_t = None
        if dbg is not None and dbg[1] is not None:
            dbg_t = self.dram_out("dbg", dbg[1])

        self.ident_b = self.sb("ident_b_s", [128, 128], BF16)
        self.ident_f = self.sb("ident_f_s", [128, 128])
        self.ss_t = self.sb("ss", [128, 8])
        self.eps_t = self.sb("eps_t", [128, 4])
        gamA = self.sb("gamA_s", [128, 512])
        lbd = self.dram_tmp("lbd", [2, 128, L, G])
        wnb = self.sb("wnb", [128, 4, D])
        self.NARENA = 41000
        self.arena = self.sb("arena", [128, self.NARENA])
        self.NPS = 6
        self.pslin = self.ps("pslin", [128, self.NPS, 512])
        self.psel = 0
        self.pstr = self.ps("pstr", [128, 2, 1024], BF16)
        self.tsel = 0
        self.wsel = 0

        P.dma("sp", self.ident_b[:, :], ident_b_d[:, :], writes=("ident",))
        P.dma("sp", self.ident_f[:, :], ident_f_d[:, :], writes=("identf",))
        P.dma("sp", gamA[:, :], gam_d[:, :], writes=("gamA",))
        P.op("dve", lambda e: e.memset(self.eps_t[:, 0:1], NORM_EPS), writes=("eps",))
        P.op("dve", lambda e: e.memset(self.eps_t[:, 1:2], 64e-5), writes=("eps",))
        P.op("dve", lambda e: e.memset(self.eps_t[:, 2:3], 0.0), writes=("eps",))
        for i in range(0, NT, 512):
            P.dma("sp", hbuf[i:i + 512, :], x[i:i + 512, :], writes=tuple(("h", j) for j in range(i // 128, i // 128 + 4)))
        for l in layers:
            self.cast_weight(w_in[l], wb_in[l], D, NIN)
            self.cast_weight(w_out[l], wb_out[l], D, D)
            self.cast_weight(w_g[l], wb_g[l], D, DFF)
            self.cast_weight(w_u[l], wb_u[l], D, DFF)
            self.cast_weight(w_d[l], wb_d[l], DFF, D, 128)

        self.carve_reset()
        ex = self.carve(L * G).rearrange("p (l g) -> p l g", g=G)
        sm = self.carve(G)
        lbt = self.carve(L * G).rearrange("p (l g) -> p l g", g=G)
        oml = self.carve(L * G).rearrange("p (l g) -> p l g", g=G)
        P.dma("sp", ex, I["hgrn_lb_logits"].partition_broadcast(128), writes=("ex",))
        self.actf(ex, ex, AF.Exp, ("ex",), ("ex",))
        self.cp(sm, ex[:, 0, :], ("ex",), ("sm",), eng="dve")
        for j in range(1, L):
            self.tt(sm, sm, ex[:, j, :], ALU.add, ("sm", "ex"), ("sm",))
        P.op("dve", lambda e: e.reciprocal(out=sm, in_=sm), reads=("sm",), writes=("sm",))
        P.op("dve", lambda e: e.memset(lbt[:, 0, :], 0.0), writes=("lbt",))
        for j in range(1, L):
            self.tt(ex[:, j, :], ex[:, j, :], sm, ALU.mult, ("ex", "sm"), ("ex",))
            self.tt(lbt[:, j, :], lbt[:, j - 1, :], ex[:, j, :], ALU.add, ("lbt", "ex"), ("lbt",))
        self.ts(oml[:, :, :], lbt[:, :, :], -1.0, 1.0, ALU.mult, ALU.add, ("lbt",), ("oml",))
        P.dma("sp", lbd[0], lbt, reads=("lbt",), writes=("lbd",))
        P.dma("sp", lbd[1], oml, reads=("oml",), writes=("lbd",))
        P.barrier()

        for l in layers:
            P.dma("sp", wnb[:, :, :], norms[l].partition_broadcast(128), writes=("wnb",))
            self.phase_p1(l, hbuf, wb_in[l], wnb)
            P.barrier()
            if dbg is not None and dbg[0] == "p" and l == layers[-1]:
                break
            self.phase_prep(l, I, rows, vT, gate, bonus, vfirst, cs_d, lbd, ck)
            P.barrier()
            if dbg is not None and dbg[0] == "prep" and l == layers[-1]:
                break
            self.phase_scan(rows, vT, yT, sel_d, ck)
            P.barrier()
            if dbg is not None and dbg[0] == "scan" and l == layers[-1]:
                break
            self.phase_post(l, I, yT, ck, gate, bonus, obuf)
            P.barrier()
            if dbg is not None and dbg[0] == "post" and l == layers[-1]:
                break
            self.phase_ffn(l, hbuf, obuf, wb_out[l], wb_g[l], wb_u[l], wb_d[l], wnb)
            P.barrier()

        if dbg_t is not None:
            src = {"p": pbuf, "post": obuf, "h": hbuf}.get(dbg[0])
            P.dma("sp", dbg_t, src)
        for i in range(0, NT, 512):
            P.dma("sp", out[i:i + 512, :], hbuf[i:i + 512, :])
        P.finish("sp", P.all_dma_tokens())
        P.emit()
        return nc

    def carve_linear(self, ng):
        self.carve_reset()
        self.xT = self.carve(16 * ng * 64, BF16).rearrange("p (k g t) -> p k g t", g=ng, t=128)
        self.wbuf = [self.carve(4096, BF16) for _ in range(2)]
        self.htile = [self.carve(2048) for _ in range(2)]
        self.junk = self.carve(2048)
        self.ub = self.carve(1024, BF16)
        self.stage = [self.carve(512) for _ in range(4)]
        self.ssel = 0

    def phase_p1(self, l, hbuf, wb, wnb):
        P = self.P
        cfg = self.cfg
        NG = 4
        self.carve_linear(NG)
        for grp in range(cfg.NTILE // NG):
            for g in range(NG):
                tile = grp * NG + g
                ht = self.htile[tile % 2]
                P.dma("sp", ht, hbuf[tile * 128:(tile + 1) * 128, :], reads=(("h", tile),), writes=(("htile", tile % 2),))
                self.norm_transpose(ht, ("htile", tile % 2), wnb[:, 0, :], self.xT, g, "xT")

            def consume(g, cb, nc_, pst, pskey, grp=grp):
                st = self.stage[self.ssel % 4]
                skey = ("stage", self.ssel % 4)
                self.ssel += 1
                self.cp(st[:, 0:nc_], pst, (pskey,), (skey,))
                tile = grp * NG + g
                P.dma("sp", self.pbuf[tile * 128:(tile + 1) * 128, cb * 512:cb * 512 + nc_], st[:, 0:nc_],
                      reads=(skey,), writes=(("p", tile, cb),))

            self.linear_tm(self.xT, ("xT",), NG, 16, wb, NIN, consume)

    def phase_prep(self, l, I, rows, vT, gate, bonus, vfirst, cs_d, lbd, ck):
        P = self.P
        cfg = self.cfg
        self.carve_reset()
        seg = self.carve(2064)
        sh = self.carve(3072)
        Wt = [self.carve(512) for _ in range(10)]
        R = self.carve(1024)
        cst = self.carve(64)
        self.vt = self.carve(512).rearrange("p (a b) -> p a b", b=128)
        cw = self.carve(4096).rearrange("p (j c) -> p j c", c=1024)
        cb_ = self.carve(1024)
        mu = self.carve(RWC)
        sv = {nm: self.carve(512) for nm in SMALL}
        v0b = self.carve(512)
        ifb = self.carve(16)
        upw = self.carve(1024)
        gup = self.carve(512)
        vdn = self.carve(128).rearrange("p (c n) -> p c n", n=32)
        vup = self.carve(512)
        small = self.carve(64)
        small2 = self.carve(64)
        QB = self.carve(256, BF16)
        KB = self.carve(256, BF16)
        KH = self.carve(256, BF16)
        XB1 = self.carve(256, BF16)
        XB2 = self.carve(256, BF16)
        XB3 = self.carve(256, BF16)
        XB4 = self.carve(256, BF16)
        self.tbt = self.carve(256, BF16).rearrange("p (a b) -> p a b", b=128)
        self.vb = self.carve(260, BF16).rearrange("p (h e) -> p h e", e=65)
        AT = self.carve(2048).rearrange("p (a c) -> p a c", c=512)
        tri = self.carve(512).rearrange("p (a c) -> p a c", c=128)
        P.dma("sp", AT, ck["atab"].rearrange("a p c -> p a c"), writes=("AT",))
        P.dma("sp", tri, ck["tri"].rearrange("a p c -> p a c"), writes=("tri",))
        P.op("dve", lambda e: e.memset(self.vb[:, :, 64:65], 1.0), writes=("vb",))
        lbt = self.carve(512)
        oml = self.carve(512)
        P.dma("sp", lbt, lbd[0, :, l, :], writes=("lbt",))
        P.dma("sp", oml, lbd[1, :, l, :], writes=("oml",))
        for nm in SMALL:
            self.bcast_load(sv[nm], I[nm][l], "c_" + nm)
        self.bcast_load(cw, I["mlstm_conv_w"][l], "cw")
        self.bcast_load(cb_, I["mlstm_conv_b"][l], "cb")
        self.bcast_load(mu, I["rwkv_mu"][l], "mu")
        self.bcast_load(ifb, I["mlstm_if_bias"][l], "ifb")
        P.dma("sp", upw, I["rwkv_up_pad"][l], writes=("upw",))
        P.dma("sp", gup, I["rwkv_g_up"][l], writes=("gup",))
        if l > 0:
            self.bcast_load(v0b, I["rwkv_v0"][l - 1], "v0b")
            P.dma("sp", vdn, I["rwkv_v_down"][l - 1].rearrange("(c p) n -> p c n", p=128), writes=("vdn",))
            P.dma("sp", vup[0:32, :], I["rwkv_v_up"][l - 1], writes=("vup",))
        pall = lambda tile: tuple(("p", tile, cb) for cb in range(16))
        W = Wt
        wk = lambda i: ("W", i)

        def h8(ap):
            return ap.rearrange("p (h d) -> p h d", d=64)

        def b8(ap8):
            return ap8.unsqueeze(2).to_broadcast([128, 8, 64])

        for tile in range(cfg.NTILE):
            s, g = tile // cfg.TPS, tile % cfg.TPS
            r0 = tile * 128
            rs = slice(r0, r0 + 128)
            P.dma("sp", seg[:, 0:2048], self.pbuf[rs, 0:2048], reads=pall(tile), writes=("seg",))
            P.dma("sp", cst, cs_d[g * 128:(g + 1) * 128, :], writes=("cst",))
            qk = seg[:, 0:1024].rearrange("p (h two d) -> p h two d", two=2, d=32)
            Rv = R.rearrange("p (h two d) -> p h two d", two=2, d=32)
            t1, t2 = qk[:, :, 0, :], qk[:, :, 1, :]
            cosb = cst[:, 0:32].unsqueeze(1).to_broadcast([128, 16, 32])
            sinb = cst[:, 32:64].unsqueeze(1).to_broadcast([128, 16, 32])
            w0v = W[0].rearrange("p (h d) -> p h d", d=32)
            w1v = W[1].rearrange("p (h d) -> p h d", d=32)
            self.tt(w0v, t1, cosb, ALU.mult, ("seg", "cst"), (wk(0),))
            self.tt(w1v, t2, sinb, ALU.mult, ("seg", "cst"), (wk(1),))
            self.tt(Rv[:, :, 0, :], w0v, w1v, ALU.subtract, (wk(0), wk(1)), ("R",))
            self.tt(w0v, t1, sinb, ALU.mult, ("seg", "cst"), (wk(0),))
            self.tt(w1v, t2, cosb, ALU.mult, ("seg", "cst"), (wk(1),))
            self.tt(Rv[:, :, 1, :], w0v, w1v, ALU.add, (wk(0), wk(1)), ("R",))
            self.tt(QB, R[:, 0:512], AT[:, 0, :], ALU.mult, ("R", "AT"), ("QB",))
            self.bt_store(QB, "QB", ck["qT"]["A"], s, g)
            self.tt(KB, R[:, 512:1024], AT[:, 1, :], ALU.mult, ("R", "AT"), ("KB",))
            self.bt_store(KB, "KB", ck["kT"]["A"], s, g)
            self.tt(KH, R[:, 512:1024], AT[:, 2, :], ALU.mult, ("R", "AT"), ("KH",))
            P.dma("sp", ck["kh"]["A"][rs, :], KH, reads=("KH",), writes=(("kh", "A"),))
            self.store_v(seg[:, 1024:1536], "seg", ck["v"]["A"], rs)
            P.dma("sp", ck["dec"]["A"][rs, :], AT[:, 3, :], reads=("AT",), writes=(("dec", "A"),))
            self.actf(W[2], seg[:, 1536:2048], AF.Silu, ("seg",), (wk(2),))
            P.dma("sp", gate["A"][rs, :], W[2], reads=(wk(2),), writes=(("gate", "A", tile),))
            P.dma("sp", seg[:, 0:2048], self.pbuf[rs, 2048:4096], reads=pall(tile), writes=("seg",))
            self.actf(W[0], seg[:, 0:512], AF.Silu, ("seg",), (wk(0),))
            self.ts(W[0], W[0], 0.125, None, ALU.mult, None, (wk(0),), (wk(0),))
            self.actf(W[1], seg[:, 512:1024], AF.Sigmoid, ("seg",), (wk(1),))
            self.tt(W[1], W[1], oml, ALU.mult, (wk(1), "oml"), (wk(1),))
            self.tt(W[1], W[1], lbt, ALU.add, (wk(1), "lbt"), (wk(1),))
            self.ts(W[3], W[1], -1.0, 1.0, ALU.mult, ALU.add, (wk(1),), (wk(3),))
            self.actf(W[4], W[1], AF.Ln, (wk(1),), (wk(4),))
            ba = self.psbank()
            pc = self.pslin[:, ba, :]
            P.op("pe", lambda e, pc=pc: e.matmul(pc, lhsT=tri[:, 2, :], rhs=W[4], start=True, stop=True),
                 reads=(wk(4), "tri"), writes=(("pslin", ba),))
            bb = self.psbank()
            ptot = self.pslin[:, bb, :]
            P.op("pe", lambda e, ptot=ptot: e.matmul(ptot, lhsT=tri[:, 3, :], rhs=W[4], start=True, stop=True),
                 reads=(wk(4), "tri"), writes=(("pslin", bb),))
            self.actf(W[5], pc, AF.Exp, (("pslin", ba),), (wk(5),))
            self.tt(QB, W[0], W[5], ALU.mult, (wk(0), wk(5)), ("QB",))
            self.bt_store(QB, "QB", ck["qT"]["B"], s, g)
            self.actf(W[5], pc, AF.Exp, (("pslin", ba),), (wk(5),), scale=-1.0)
            self.tt(KB, W[3], W[5], ALU.mult, (wk(3), wk(5)), ("KB",))
            self.bt_store(KB, "KB", ck["kT"]["B"], s, g)
            self.cp(W[6], ptot, (("pslin", bb),), (wk(6),))
            self.actf(W[7], W[6], AF.Exp, (wk(6),), (wk(7),))
            P.dma("sp", ck["dec"]["B"][rs, :], W[7], reads=(wk(7),), writes=(("dec", "B"),))
            self.tt(W[6], W[6], pc, ALU.subtract, (wk(6), ("pslin", ba)), (wk(6),))
            self.actf(W[6], W[6], AF.Exp, (wk(6),), (wk(6),))
            self.tt(KH, W[3], W[6], ALU.mult, (wk(3), wk(6)), ("KH",))
            P.dma("sp", ck["kh"]["B"][rs, :], KH, reads=("KH",), writes=(("kh", "B"),))
            self.store_v(seg[:, 1024:1536], "seg", ck["v"]["B"], rs)
            self.actf(W[2], seg[:, 1536:2048], AF.Sigmoid, ("seg",), (wk(2),))
            P.dma("sp", gate["B"][rs, :], W[2], reads=(wk(2),), writes=(("gate", "B", tile),))
            P.dma("sp", seg[:, 0:2064], self.pbuf[rs, 4096:6160], reads=pall(tile), writes=("seg",))
            shv = sh.rearrange("p (j c) -> p j c", c=1024)
            for j in range(3):
                self.load_shift(shv[:, j, :], ("sh", j), 4096, 1024, tile, 3 - j)
            acc = R
            self.tt(acc, seg[:, 0:1024], cw[:, 3, :], ALU.mult, ("seg", "cw"), ("R",))
            self.tt(acc, acc, cb_, ALU.add, ("R", "cb"), ("R",))
            for j in range(3):
                self.tt(shv[:, j, :], shv[:, j, :], cw[:, j, :], ALU.mult, (("sh", j), "cw"), (("sh", j),))
                self.tt(acc, acc, shv[:, j, :], ALU.add, ("R", ("sh", j)), ("R",))
            self.actf(acc, acc, AF.Silu, ("R",), ("R",))
            self.tt(small[:, 0:16], seg[:, 2048:2064], ifb, ALU.add, ("seg", "ifb"), ("small",))
            self.actf(small[:, 24:32], small[:, 8:16], AF.Sigmoid, ("small",), ("small3",))
            self.actf(small[:, 16:24], small[:, 24:32], AF.Ln, ("small3",), ("small2",))
            ba = self.psbank()
            pc8 = self.pslin[:, ba, 0:8]
            P.op("pe", lambda e, pc8=pc8: e.matmul(pc8, lhsT=tri[:, 0, :], rhs=small[:, 16:24], start=True, stop=True),
                 reads=("small2", "tri"), writes=(("pslin", ba),))
            bb = self.psbank()
            pt8 = self.pslin[:, bb, 0:8]
            P.op("pe", lambda e, pt8=pt8: e.matmul(pt8, lhsT=tri[:, 1, :], rhs=small[:, 16:24], start=True, stop=True),
                 reads=("small2", "tri"), writes=(("pslin", bb),))
            self.actf(small2[:, 0:8], pc8, AF.Exp, (("pslin", ba),), ("s2a",))
            self.tt(h8(QB), h8(acc[:, 0:512]), b8(small2[:, 0:8]), ALU.mult, ("R", "s2a"), ("QB",))
            self.bt_store(QB, "QB", ck["qT"]["C"], s, g)
            self.tt(small2[:, 8:16], small[:, 0:8], pc8, ALU.subtract, ("small", ("pslin", ba)), ("s2b",))
            self.actf(small2[:, 16:24], small2[:, 8:16], AF.Exp, ("s2b",), ("s2c",))
            self.stt(h8(KB), h8(acc[:, 512:1024]), 0.125, b8(small2[:, 16:24]), ALU.mult, ALU.mult, ("R", "s2c"), ("KB",))
            self.bt_store(KB, "KB", ck["kT"]["C"], s, g)
            self.tt(small2[:, 24:32], small2[:, 8:16], pt8, ALU.add, ("s2b", ("pslin", bb)), ("s2d",))
            self.actf(small2[:, 32:40], small2[:, 24:32], AF.Exp, ("s2d",), ("s2e",))
            self.stt(h8(KH), h8(acc[:, 512:1024]), 0.125, b8(small2[:, 32:40]), ALU.mult, ALU.mult, ("R", "s2e"), ("KH",))
            P.dma("sp", ck["kh"]["C"][rs, :], KH, reads=("KH",), writes=(("kh", "C"),))
            self.actf(small2[:, 40:48], pt8, AF.Exp, (("pslin", bb),), ("s2f",))
            self.cp(h8(W[1]), b8(small2[:, 40:48]), ("s2f",), (wk(1),), eng="dve")
            P.dma("sp", ck["dec"]["C"][rs, :], W[1], reads=(wk(1),), writes=(("dec", "C"),))
            self.store_v(seg[:, 1024:1536], "seg", ck["v"]["C"], rs)
            self.actf(W[2], seg[:, 1536:2048], AF.Sigmoid, ("seg",), (wk(2),))
            P.dma("sp", gate["C"][rs, :], W[2], reads=(wk(2),), writes=(("gate", "C", tile),))
            P.dma("sp", seg[:, 0:RWC], self.pbuf[rs, 6160:6160 + RWC], reads=pall(tile), writes=("seg",))
            prev = sh[:, 0:RWC]
            self.load_shift(prev, ("sh", 0), 6160, RWC, tile, 1, extra_keys=(("sh", 1),))
            self.tt(prev, prev, seg[:, 0:RWC], ALU.subtract, (("sh", 0), ("sh", 1), "seg"), (("sh", 0), ("sh", 1)))
            self.tt(prev, prev, mu, ALU.mult, (("sh", 0), ("sh", 1), "mu"), (("sh", 0), ("sh", 1)))
            self.tt(seg[:, 0:RWC], seg[:, 0:RWC], prev, ALU.add, ("seg", ("sh", 0), ("sh", 1)), ("seg",))
            r_, k_, v_ = seg[:, 0:512], seg[:, 512:1024], seg[:, 1024:1536]
            Lt = W[0][:, 0:256]
            self.actf(Lt[:, 0:64], seg[:, 1536:1600], AF.Tanh, ("seg",), (wk(0),))
            self.cp(Lt[:, 64:128], seg[:, 1600:1664], ("seg",), (wk(0),))
            self.actf(Lt[:, 128:256], seg[:, 1664:1792], AF.Sigmoid, ("seg",), (wk(0),))
            LT = W[1][:, 0:256]
            for c in range(2):
                bank = self.psbank()
                pt = self.pslin[:, bank, 0:128]
                P.op("pe", lambda e, pt=pt, c=c: e.transpose(pt, Lt[:, c * 128:(c + 1) * 128], self.ident_f[:, :]),
                     reads=(wk(0), "identf"), writes=(("pslin", bank),))
                self.cp(LT[:, c * 128:(c + 1) * 128], pt, (("pslin", bank),), (wk(1),))
            bank = self.psbank()
            pw = self.pslin[:, bank, :]
            P.op("pe", lambda e, pw=pw: e.matmul(pw, lhsT=LT[:, 0:128], rhs=upw[:, 0:512], start=True, stop=True),
                 reads=(wk(1), "upw"), writes=(("pslin", bank),))
            self.tt(W[2], pw, sv["rwkv_w0"], ALU.add, (("pslin", bank), "c_rwkv_w0"), (wk(2),))
            self.actf(W[2], W[2], AF.Sigmoid, (wk(2),), (wk(2),))
            self.ts(W[2], W[2], -0.6065306597126334, None, ALU.mult, None, (wk(2),), (wk(2),))
            bank = self.psbank()
            pa = self.pslin[:, bank, :]
            P.op("pe", lambda e, pa=pa: e.matmul(pa, lhsT=LT[:, 0:128], rhs=upw[:, 512:1024], start=True, stop=True),
                 reads=(wk(1), "upw"), writes=(("pslin", bank),))
            self.tt(W[3], pa, sv["rwkv_a0"], ALU.add, (("pslin", bank), "c_rwkv_a0"), (wk(3),))
            self.actf(W[3], W[3], AF.Sigmoid, (wk(3),), (wk(3),))
            bank = self.psbank()
            pg = self.pslin[:, bank, :]
            P.op("pe", lambda e, pg=pg: e.matmul(pg, lhsT=LT[:, 128:256], rhs=gup, start=True, stop=True),
                 reads=(wk(1), "gup"), writes=(("pslin", bank),))
            self.cp(W[4], pg, (("pslin", bank),), (wk(4),))
            P.dma("sp", gate["D"][rs, :], W[4], reads=(wk(4),), writes=(("gate", "D", tile),))
            if l == 0:
                P.dma("sp", vfirst[rs, :], v_, reads=("seg",), writes=(("vfirst", tile),))
            else:
                vtt = self.vt
                for c in range(4):
                    bank = self.psbank()
                    pt = self.pslin[:, bank, 0:128]
                    P.op("pe", lambda e, pt=pt, c=c: e.transpose(pt, v_[:, c * 128:(c + 1) * 128], self.ident_f[:, :]),
                         reads=("seg", "identf"), writes=(("pslin", bank),))
                    self.cp(vtt[:, c, :], pt, (("pslin", bank),), ("vt",))
                bank = self.psbank()
                pv = self.pslin[:, bank, 0:32]
                for c in range(4):
                    P.op("pe", lambda e, pv=pv, c=c: e.matmul(pv, lhsT=vtt[:, c, :], rhs=vdn[:, c, :], start=(c == 0),
                                                              stop=(c == 3)),
                         reads=("vt", "vdn"), writes=(("pslin", bank),))
                self.cp(W[5][:, 0:32], pv, (("pslin", bank),), (wk(5),))
                bank = self.psbank()
                pt = self.pslin[0:32, bank, 0:128]
                P.op("pe", lambda e, pt=pt: e.transpose(pt, W[5][:, 0:32], self.ident_f[:, :]),
                     reads=(wk(5), "identf"), writes=(("pslin", bank),))
                self.cp(W[6][0:32, 0:128], pt, (("pslin", bank),), (wk(6),))
                bank = self.psbank()
                pv2 = self.pslin[:, bank, :]
                P.op("pe", lambda e, pv2=pv2: e.matmul(pv2, lhsT=W[6][0:32, 0:128], rhs=vup[0:32, :], start=True, stop=True),
                     reads=(wk(6), "vup"), writes=(("pslin", bank),))
                self.tt(W[5], pv2, v0b, ALU.add, (("pslin", bank), "v0b"), (wk(5),))
                self.actf(W[5], W[5], AF.Sigmoid, (wk(5),), (wk(5),))
                P.dma("sp", W[6], vfirst[rs, :], reads=(("vfirst", tile),), writes=(wk(6),))
                self.tt(W[6], W[6], v_, ALU.subtract, (wk(6), "seg"), (wk(6),))
                self.tt(W[6], W[6], W[5], ALU.mult, (wk(6), wk(5)), (wk(6),))
                self.tt(v_, v_, W[6], ALU.add, ("seg", wk(6)), ("seg",))
            self.tt(W[5], k_, sv["rwkv_k_k"], ALU.mult, ("seg", "c_rwkv_k_k"), (wk(5),))
            self.tt(W[6], W[5], W[5], ALU.mult, (wk(5),), (wk(6),))
            self.red(small[:, 32:40], h8(W[6]), (wk(6),), ("small4",))
            self.actf(small[:, 40:48], small[:, 32:40], AF.Sqrt, ("small4",), ("small5",))
            self.ts(small[:, 40:48], small[:, 40:48], 1e-12, None, ALU.max, None, ("small5",), ("small5",))
            P.op("dve", lambda e: e.reciprocal(out=small[:, 48:56], in_=small[:, 40:48]), reads=("small5",), writes=("small6",))
            self.tt(h8(W[5]), h8(W[5]), b8(small[:, 48:56]), ALU.mult, (wk(5), "small6"), (wk(5),))
            self.stt(W[7], W[3], -1.0, sv["rwkv_k_a"], ALU.add, ALU.mult, (wk(3), "c_rwkv_k_a"), (wk(7),))
            self.stt(W[7], W[7], 1.0, k_, ALU.add, ALU.mult, (wk(7), "seg"), (wk(7),))
            self.tt(W[8], W[3], W[5], ALU.mult, (wk(3), wk(5)), (wk(8),))
            self.tt(W[9], r_, W[7], ALU.mult, ("seg", wk(7)), (wk(9),))
            self.tt(W[9], W[9], sv["rwkv_r_k"], ALU.mult, (wk(9), "c_rwkv_r_k"), (wk(9),))
            self.red(small[:, 56:64], h8(W[9]), (wk(9),), ("small7",))
            self.tt(h8(W[9]), h8(v_), b8(small[:, 56:64]), ALU.mult, ("seg", "small7"), (wk(9),))
            P.dma("sp", bonus[rs, :], W[9], reads=(wk(9),), writes=(("bonus", tile),))
            dk = ck["D"]
            ba = self.psbank()
            pc = self.pslin[:, ba, :]
            P.op("pe", lambda e, pc=pc: e.matmul(pc, lhsT=tri[:, 2, :], rhs=W[2], start=True, stop=True),
                 reads=(wk(2), "tri"), writes=(("pslin", ba),))
            bb = self.psbank()
            ptot = self.pslin[:, bb, :]
            P.op("pe", lambda e, ptot=ptot: e.matmul(ptot, lhsT=tri[:, 3, :], rhs=W[2], start=True, stop=True),
                 reads=(wk(2), "tri"), writes=(("pslin", bb),))
            self.actf(W[0], pc, AF.Exp, (("pslin", ba),), (wk(0),))
            self.tt(QB, r_, W[0], ALU.mult, ("seg", wk(0)), ("QB",))
            self.bt_store(QB, "QB", dk["rT"], s, g)
            self.tt(W[1], pc, W[2], ALU.subtract, (("pslin", ba), wk(2)), (wk(1),))
            self.actf(W[1], W[1], AF.Exp, (wk(1),), (wk(1),))
            self.tt(KB, W[5], W[1], ALU.mult, (wk(5), wk(1)), ("KB",))
            self.bt_store(KB, "KB", dk["cT"], s, g)
            self.actf(W[0], pc, AF.Exp, (("pslin", ba),), (wk(0),), scale=-1.0)
            self.tt(XB1, W[7], W[0], ALU.mult, (wk(7), wk(0)), ("XB1",))
            self.bt_store(XB1, "XB1", dk["kT"], s, g)
            self.tt(XB2, W[8], W[0], ALU.mult, (wk(8), wk(0)), ("XB2",))
            self.bt_store(XB2, "XB2", dk["bT"], s, g)
            self.cp(W[6], ptot, (("pslin", bb),), (wk(6),))
            self.actf(W[1], W[6], AF.Exp, (wk(6),), (wk(1),))
            P.dma("sp", dk["dec"][rs, :], W[1], reads=(wk(1),), writes=(("dec", "D"),))
            self.tt(W[6], W[6], pc, ALU.subtract, (wk(6), ("pslin", ba)), (wk(6),))
            self.actf(W[6], W[6], AF.Exp, (wk(6),), (wk(6),))
            self.tt(KH, W[7], W[6], ALU.mult, (wk(7), wk(6)), ("KH",))
            P.dma("sp", dk["kh"][rs, :], KH, reads=("KH",), writes=(("kh", "D"),))
            self.stt(XB3, W[8], -1.0, W[6], ALU.mult, ALU.mult, (wk(8), wk(6)), ("XB3",))
            P.dma("sp", dk["bh"][rs, :], XB3, reads=("XB3",), writes=(("bh", "D"),))
            self.cp(XB4, v_, ("seg",), ("XB4",), eng="dve")
            P.dma("sp", dk["v"][rs, :], XB4, reads=("XB4",), writes=(("vd", "D"),))

    def phase_scan(self, rows, vT, yT, sel_d, ck):
        P = self.P
        self.carve_reset()
        self._phase_id = getattr(self, "_phase_id", 0) + 1
        for m in "ABC":
            for _ in self.gen_chunk(m, CHK[m], ck):
                pass
        P.barrier()
        self.carve_reset()
        self.chunk_D(ck["D"])

    def chunk_D(self, dk):
        P = self.P
        cfg = self.cfg
        T = cfg.T
        C = 32
        nch = T // C
        XT = self.carve(T // 2, BF16)
        X2 = {n: self.carve(T, BF16).rearrange("p (n h t) -> p n h t", h=2, t=C) for n in ("r", "c", "k", "b")}
        Vt = self.carve(nch * 32, BF16).rearrange("p (n e) -> p n e", e=64)
        KH2 = self.carve(nch * 64, BF16).rearrange("p (n e) -> p n e", e=128)
        BH2 = self.carve(nch * 64, BF16).rearrange("p (n e) -> p n e", e=128)
        yo = self.carve(nch * 64).rearrange("p (n e) -> p n e", e=64)
        dch = self.carve(128)
        decT = self.carve(64)
        Zf = self.carve(64)
        Zb = self.carve(32, BF16)
        msk = self.carve(192).rearrange("p (a c) -> p a c", c=64)
        ST = [dict(N=[self.carve(64) for _ in range(5)], NT=[self.carve(64) for _ in range(4)], P=self.carve(64),
                   Q=self.carve(64), BTm=self.carve(32, BF16), S1m=self.carve(32, BF16), S2m=self.carve(32, BF16))
              for _ in range(2)]
        RHSs = self.carve(64)
        Ub = self.carve(32, BF16)
        pstr_f = self.pstr[:, :, :].bitcast(F32)
        I64 = self.ident_f[0:64, 0:64]
        P.dma("sp", msk[0:64, :, :], dk["masks"].rearrange("a p c -> p a c"), writes=("msk",))
        for n in X2:
            P.op("dve", lambda e, n=n: e.memset(X2[n], 0.0), writes=(("X2", n),))
        P.op("dve", lambda e: e.memset(KH2, 0.0), writes=("KH2",))
        P.op("dve", lambda e: e.memset(BH2, 0.0), writes=("BH2",))
        Mup, Mlow, Minc = msk[0:64, 0, :], msk[0:64, 1, :], msk[0:64, 2, :]
        srcT = {"r": dk["rT"], "c": dk["cT"], "k": dk["kT"], "b": dk["bT"]}
        pe = lambda fn, r, w: P.op("pe", fn, reads=r, writes=w)
        for s in range(cfg.NSEQ):
            for hp in range(4):
                for n in ("r", "c", "k", "b"):
                    P.dma("sp", XT[:, 0:T], srcT[n][hp, :, s, :], reads=tuple(("T", id(srcT[n]), s, g) for g in range(cfg.TPS)),
                          writes=("XT",))
                    self.cp(X2[n][0:64, :, 0, :], XT[0:64, 0:T].rearrange("p (n t) -> p n t", t=C), ("XT",), (("X2", n),),
                            eng="dve")
                    self.cp(X2[n][64:128, :, 1, :], XT[64:128, 0:T].rearrange("p (n t) -> p n t", t=C), ("XT",), (("X2", n),))
                rsl = slice(s * T, (s + 1) * T)
                for hh in range(2):
                    col = slice((2 * hp + hh) * 64, (2 * hp + hh + 1) * 64)
                    ps_ = slice(hh * 32, (hh + 1) * 32)
                    P.dma("sp", Vt[ps_, 0:nch, :], dk["v"][rsl, col].rearrange("(n c) k -> c n k", c=C), reads=(("vd", "D"),),
                          writes=("Vt",))
                    P.dma("sp", KH2[ps_, 0:nch, hh * 64:(hh + 1) * 64], dk["kh"][rsl, col].rearrange("(n c) k -> c n k", c=C),
                          reads=(("kh", "D"),), writes=("KH2",))
                    P.dma("sp", BH2[ps_, 0:nch, hh * 64:(hh + 1) * 64], dk["bh"][rsl, col].rearrange("(n c) k -> c n k", c=C),
                          reads=(("bh", "D"),), writes=("BH2",))
                P.dma("sp", dch[0:nch, :], dk["dec"][rsl, hp * 128:(hp + 1) * 128].rearrange("(n c) k -> c n k", c=C)[C - 1, :, :],
                      reads=(("dec", "D"),), writes=("dchD",))
                pt = self.pslin[:, 5, 0:nch]
                pe(lambda e, pt=pt: e.transpose(pt, dch[0:nch, :], self.ident_f[0:nch, 0:nch]), ("dchD", "identf"), (("pslin", 5),))
                self.cp(decT[:, 0:nch], pt, (("pslin", 5),), ("decTD",))
                P.op("dve", lambda e: e.memset(Zf, 0.0), writes=("Zf",))
                P.op("dve", lambda e: e.memset(Zb, 0.0), writes=("Zb",))
                r2, c2, k2, b2 = X2["r"], X2["c"], X2["k"], X2["b"]
                xk = lambda n: ("X2", n)

                def front(c):
                    par = c % 2
                    st = ST[par]
                    sk_ = lambda nm: ("ST", par, nm)
                    sc = self.pslin[0:64, par, :]
                    bk = ("pslin", par)
                    fl = lambda ap: ap.rearrange("p h t -> p (h t)")
                    pe(lambda e: e.matmul(sc[:, 0:64], lhsT=fl(b2[:, c]), rhs=fl(c2[:, c]), start=True, stop=True), (xk("b"), xk("c")), (bk,))
                    pe(lambda e: e.matmul(sc[:, 64:128], lhsT=fl(c2[:, c]), rhs=fl(b2[:, c]), start=True, stop=True), (xk("b"), xk("c")), (bk,))
                    pe(lambda e: e.matmul(sc[:, 128:192], lhsT=fl(k2[:, c]), rhs=fl(c2[:, c]), start=True, stop=True), (xk("k"), xk("c")), (bk,))
                    pe(lambda e: e.matmul(sc[:, 192:256], lhsT=fl(k2[:, c]), rhs=fl(r2[:, c]), start=True, stop=True), (xk("k"), xk("r")), (bk,))
                    pe(lambda e: e.matmul(sc[:, 256:320], lhsT=fl(b2[:, c]), rhs=fl(r2[:, c]), start=True, stop=True), (xk("b"), xk("r")), (bk,))
                    N, NT, Pm, Qm = st["N"], st["NT"], st["P"][0:64, :], st["Q"][0:64, :]
                    self.stt(N[0][0:64, :], sc[:, 0:64], -1.0, Mup, ALU.mult, ALU.mult, (bk, "msk"), (sk_("N0"),))
                    self.stt(NT[0][0:64, :], sc[:, 64:128], -1.0, Mlow, ALU.mult, ALU.mult, (bk, "msk"), (sk_("NT0"),))
                    self.tt(st["BTm"][0:64, :], sc[:, 128:192], Mup, ALU.mult, (bk, "msk"), (sk_("BTm"),))
                    self.tt(st["S1m"][0:64, :], sc[:, 192:256], Minc, ALU.mult, (bk, "msk"), (sk_("S1m"),))
                    self.stt(st["S2m"][0:64, :], sc[:, 256:320], -1.0, Minc, ALU.mult, ALU.mult, (bk, "msk"), (sk_("S2m"),))
                    self.tt(Pm, N[0][0:64, :], I64, ALU.add, (sk_("N0"), "identf"), (sk_("P"),), eng="pool")
                    self.tt(Qm, NT[0][0:64, :], I64, ALU.add, (sk_("NT0"), "identf"), (sk_("Q"),), eng="pool")
                    for k in range(1, 5):
                        last = (k == 4)
                        pn = self.pslin[0:64, 2, :]
                        pe(lambda e, k=k: e.matmul(pn[:, 0:64], lhsT=NT[k - 1][0:64, :], rhs=N[k - 1][0:64, :], start=True, stop=True),
                           (sk_("N%d" % (k - 1)), sk_("NT%d" % (k - 1))), (("pslin", 2),))
                        if not last:
                            pe(lambda e, k=k: e.matmul(pn[:, 64:128], lhsT=N[k - 1][0:64, :], rhs=NT[k - 1][0:64, :], start=True,
                                                       stop=True),
                               (sk_("N%d" % (k - 1)), sk_("NT%d" % (k - 1))), (("pslin", 2),))
                        self.cp(N[k][0:64, :], pn[:, 0:64], (("pslin", 2),), (sk_("N%d" % k),))
                        if not last:
                            self.cp(NT[k][0:64, :], pn[:, 64:128], (("pslin", 2),), (sk_("NT%d" % k),))
                        pp = self.pslin[0:64, 3, :]
                        pe(lambda e, k=k: e.matmul(pp[:, 0:64], lhsT=Qm, rhs=N[k][0:64, :], start=True, stop=True),
                           (sk_("Q"), sk_("N%d" % k)), (("pslin", 3),))
                        if not last:
                            pe(lambda e, k=k: e.matmul(pp[:, 64:128], lhsT=N[k][0:64, :], rhs=Qm, start=True, stop=True),
                               (sk_("Q"), sk_("N%d" % k)), (("pslin", 3),))
                        self.tt(Pm, Pm, pp[:, 0:64], ALU.add, (sk_("P"), ("pslin", 3)), (sk_("P"),))
                        if not last:
                            self.tt(Qm, Qm, pp[:, 64:128], ALU.add, (sk_("Q"), ("pslin", 3)), (sk_("Q"),))

                def back(c):
                    par = c % 2
                    st = ST[par]
                    sk_ = lambda nm: ("ST", par, nm)
                    fl = lambda ap: ap.rearrange("p h t -> p (h t)")
                    pr = self.pslin[0:64, 4, 0:64]
                    pe(lambda e: e.matmul(pr, lhsT=fl(c2[:, c]), rhs=Zb[:, 0:64], start=True, stop=False), (xk("c"), "Zb"), (("pslin", 4),))
                    pe(lambda e: e.matmul(pr, lhsT=st["BTm"][0:64, :], rhs=Vt[0:64, c, :], start=False, stop=True),
                       (sk_("BTm"), "Vt"), (("pslin", 4),))
                    self.cp(RHSs[0:64, :], pr, (("pslin", 4),), ("RHSs",))
                    pu = self.pslin[0:64, 4, 64:128]
                    pe(lambda e: e.matmul(pu, lhsT=st["P"][0:64, :], rhs=RHSs[0:64, :], start=True, stop=True), (sk_("P"), "RHSs"),
                       (("pslin", 4),))
                    self.cp(Ub[0:64, :], pu, (("pslin", 4),), ("Ub",))
                    py = pstr_f[0:64, par, 0:64]
                    pe(lambda e: e.matmul(py, lhsT=st["S1m"][0:64, :], rhs=Vt[0:64, c, :], start=True, stop=False), (sk_("S1m"), "Vt"),
                       (("pstr", par),))
                    pe(lambda e: e.matmul(py, lhsT=st["S2m"][0:64, :], rhs=Ub[0:64, :], start=False, stop=False), (sk_("S2m"), "Ub"),
                       (("pstr", par),))
                    pe(lambda e: e.matmul(py, lhsT=fl(r2[:, c]), rhs=Zb[:, 0:64], start=False, stop=True), (xk("r"), "Zb"),
                       (("pstr", par),))
                    self.cp(yo[0:64, c, :], py, (("pstr", par),), ("yoD",))
                    pk = self.pslin[:, 5, 0:64]
                    pe(lambda e: e.matmul(pk, lhsT=KH2[0:64, c, :], rhs=Vt[0:64, c, :], start=True, stop=False), ("KH2", "Vt"),
                       (("pslin", 5),))
                    pe(lambda e: e.matmul(pk, lhsT=BH2[0:64, c, :], rhs=Ub[0:64, :], start=False, stop=True), ("BH2", "Ub"),
                       (("pslin", 5),))
                    self.stt(Zf, Zf, decT[:, c:c + 1], pk, ALU.mult, ALU.add, ("Zf", "decTD", ("pslin", 5)), ("Zf",))
                    self.cp(Zb, Zf, ("Zf",), ("Zb",))

                for c in range(nch + 1):
                    if c < nch:
                        front(c)
                    if c > 0:
                        back(c - 1)
                for hh in range(2):
                    col = slice((2 * hp + hh) * 64, (2 * hp + hh + 1) * 64)
                    P.dma("sp", dk["y"][rsl, col].rearrange("(n c) k -> c n k", c=C), yo[hh * 32:(hh + 1) * 32, 0:nch, :],
                          reads=("yoD",), writes=(("cy", "D"),))

    def gen_scan_D(self, rows, vT, yT, sel_d):
        P = self.P
        cfg = self.cfg
        T = cfg.T
        sel = self.carve(TB * 128).rearrange("p (t m) -> p t m", m=128)
        P.dma("sp", sel[0:32, :, :], sel_d[:, :, :], writes=("sel",))
        Rb = [self.carve(5 * 512).rearrange("p (v n) -> p v n", n=512) for _ in range(2)]
        Vb = [self.carve(8 * TB).rearrange("p (g t) -> p g t", t=TB) for _ in range(2)]
        Yb = [self.carve(8 * TB).rearrange("p (g t) -> p g t", t=TB) for _ in range(2)]
        NR = 3
        BR = [self.carve(5 * 512).rearrange("p (v n) -> p v n", n=512) for _ in range(NR)]
        S = self.carve(512)
        T1 = self.carve(512)
        T2 = self.carve(512)
        sk = self.carve(8)
        h8 = lambda ap: ap.rearrange("p (h d) -> p h d", d=64)
        b8 = lambda ap8: ap8.unsqueeze(2).to_broadcast([128, 8, 64])
        m = "D"
        vecs = VECS[m]
        P.op("dve", lambda e: e.memset(S, 0.0), writes=("S",))
        step = 0
        dps = 0
        for bi in range(T // TB):
            t0 = bi * TB
            pb = bi % 2
            for j, vname in enumerate(vecs):
                src = rows[vname].rearrange("(s t) (hp hh d) -> t s hp hh d", s=cfg.NSEQ, hh=2, d=64)
                for hh in range(2):
                    for s in range(cfg.NSEQ):
                        P.dma("sp", Rb[pb][hh * TB:(hh + 1) * TB, j, s * 256:(s + 1) * 256].rearrange(
                            "t (hp d) -> t hp d", d=64),
                            src[t0:t0 + TB, s, :, hh, :],
                            reads=(("row", vname, (s * T + t0) // 128),), writes=(("Rb", pb, j),))
            P.dma("sp", Vb[pb].rearrange("p (s hp) t -> p s hp t", hp=4),
                  vT[m].rearrange("hp p s t -> p s hp t")[:, :, :, t0:t0 + TB],
                  reads=tuple(("T", id(vT[m]), s, t0 // 128) for s in range(cfg.NSEQ)), writes=(("Vb", pb),))
            for tl in range(TB):
                rb = step % NR
                step += 1
                ps = {}
                for j, vname in enumerate(vecs):
                    bank = dps % 3
                    dps += 1
                    pt = self.pslin[:, bank, :]
                    P.op("pe", lambda e, pt=pt, j=j, tl=tl, pb=pb: e.matmul(pt, lhsT=sel[0:32, tl, :], rhs=Rb[pb][0:32, j, :],
                                                                           start=True, stop=True),
                         reads=("sel", ("Rb", pb, j)), writes=(("pslin", bank),))
                    self.cp(BR[rb][:, j, :], pt, (("pslin", bank),), (("BR", rb, j),))
                    ps[j] = (BR[rb][:, j, :], ("BR", rb, j))
                vb = b8(Vb[pb][:, :, tl])
                yo = Yb[pb][:, :, tl]
                (rp, rk_), (kp, kk_), (wp, wk_), (cp_, ck_), (bp, bk_) = ps[0], ps[1], ps[2], ps[3], ps[4]
                self.tt(T1, S, cp_, ALU.mult, ("S", ck_), ("T1",))
                self.red(sk, h8(T1), ("T1",), ("sk",))
                self.tt(h8(T1), h8(bp), b8(sk), ALU.mult, (bk_, "sk"), ("T1",))
                self.tt(S, S, wp, ALU.mult, ("S", wk_), ("S",))
                self.tt(S, S, T1, ALU.subtract, ("S", "T1"), ("S",))
                self.tt(h8(T2), h8(kp), vb, ALU.mult, (kk_, ("Vb", pb)), ("T2",))
                self.tt(S, S, T2, ALU.add, ("S", "T2"), ("S",))
                self.tt(T2, S, rp, ALU.mult, ("S", rk_), ("T2",))
                self.red(yo, h8(T2), ("T2",), (("Yb", pb),))
                yield
            P.dma("sp", yT[m].rearrange("hp p s t -> p s hp t")[:, :, :, t0:t0 + TB],
                  Yb[pb].rearrange("p (s hp) t -> p s hp t", hp=4), reads=(("Yb", pb),),
                  writes=tuple(("T", id(yT[m]), s, t0 // 128) for s in range(cfg.NSEQ)))

    def gen_chunk(self, m, C, ck):
        P = self.P
        cfg = self.cfg
        T = cfg.T
        nch = T // C
        cb = self.chunk_bufs(T)
        qT, kT, q0, q1, kh, vv, yo, dch, decT, st_f, st_b, scm, mask = cb
        pstr_f = self.pstr[:, :, :].bitcast(F32)
        qT_d, kT_d, kh_d, v_d, dec_d, y_d = ck["qT"][m], ck["kT"][m], ck["kh"][m], ck["v"][m], ck["dec"][m], ck["y"][m]
        if not self._mask_loaded:
            P.dma("sp", mask[0:64, 0:64], ck["mask"], writes=("mask",))
            self._mask_loaded = True
        cnt = 0
        qs = [q0, q1]
        for s in range(cfg.NSEQ):
            for hp in range(4):
                allT = tuple(("T", id(qT_d), s, g) for g in range(cfg.TPS))
                P.dma("sp", qT[:, 0:T], qT_d[hp, :, s, :], reads=allT, writes=("c_qT",))
                P.dma("sp", kT[:, 0:T], kT_d[hp, :, s, :], reads=tuple(("T", id(kT_d), s, g) for g in range(cfg.TPS)),
                      writes=("c_kT",))
                P.dma("sp", kh[0:C, 0:nch, :], kh_d[s * T:(s + 1) * T, hp * 128:(hp + 1) * 128].rearrange(
                    "(n c) k -> c n k", c=C), reads=(("kh", m),), writes=("c_kh",))
                P.dma("sp", vv[0:C, 0:nch, :], v_d[s * T:(s + 1) * T, hp * 130:(hp + 1) * 130].rearrange(
                    "(n c) k -> c n k", c=C), reads=(("vd", id(v_d)),), writes=("c_vv",))
                P.dma("sp", dch[0:nch, :], dec_d[s * T:(s + 1) * T, hp * 128:(hp + 1) * 128].rearrange(
                    "(n c) k -> c n k", c=C)[C - 1, :, :], reads=(("dec", m),), writes=("c_dch",))
                pt = self.pslin[:, 5, 0:nch]
                P.op("pe", lambda e, pt=pt: e.transpose(pt, dch[0:nch, :], self.ident_f[0:nch, 0:nch]),
                     reads=("c_dch", "identf"), writes=(("pslin", 5),))
                self.cp(decT[:, 0:nch], pt, (("pslin", 5),), ("c_decT",))
                P.op("dve", lambda e: e.memset(q0[64:128, 0:T], 0.0), writes=("c_q0",))
                P.op("dve", lambda e: e.memset(q1[0:64, 0:T], 0.0), writes=("c_q1",))
                self.cp(q0[0:64, 0:T], qT[0:64, 0:T], ("c_qT",), ("c_q0",), eng="dve")
                self.cp(q1[64:128, 0:T], qT[64:128, 0:T], ("c_qT",), ("c_q1",), eng="dve")
                P.op("dve", lambda e: e.memset(st_f[:, 0:66], 0.0), writes=("st_f",))
                P.op("dve", lambda e: e.memset(st_b[:, 0:66], 0.0), writes=("st_b",))
                for c in range(nch):
                    cs = slice(c * C, (c + 1) * C)
                    for hh in range(2):
                        qh = qs[hh]
                        qk = "c_q%d" % hh
                        bsc = 3 + (cnt % 2)
                        psc = self.pslin[0:C, bsc, 0:C]
                        P.op("pe", lambda e, psc=psc, cs=cs, qh=qh: e.matmul(psc, lhsT=kT[:, cs], rhs=qh[:, cs], start=True,
                                                                             stop=True),
                             reads=("c_kT", qk), writes=(("pslin", bsc),))
                        sm = scm[cnt % 2][0:C, 0:C]
                        self.tt(sm, psc, mask[0:C, 0:C], ALU.mult, (("pslin", bsc), "mask"), (("scm", cnt % 2),))
                        bo = cnt % 2
                        pso = pstr_f[0:C, bo, 0:65]
                        vsl = vv[0:C, c, hh * 65:(hh + 1) * 65]
                        P.op("pe", lambda e, pso=pso, sm=sm, vsl=vsl: e.matmul(pso, lhsT=sm, rhs=vsl, start=True, stop=False),
                             reads=(("scm", cnt % 2), "c_vv"), writes=(("pstr", bo),))
                        P.op("pe", lambda e, pso=pso, cs=cs, qh=qh: e.matmul(pso, lhsT=qh[:, cs], rhs=st_b[:, 0:65],
                                                                             start=False, stop=True),
                             reads=(qk, "st_b"), writes=(("pstr", bo),))
                        self.cp(yo[0:C, c, hh * 65:(hh + 1) * 65], pso, (("pstr", bo),), ("c_yo",))
                        pkv = self.pslin[hh * 64:(hh + 1) * 64, 5, 0:65]
                        khs = kh[0:C, c, hh * 64:(hh + 1) * 64]
                        P.op("pe", lambda e, pkv=pkv, khs=khs, vsl=vsl: e.matmul(pkv, lhsT=khs, rhs=vsl, start=True, stop=True),
                             reads=("c_kh", "c_vv"), writes=(("pslin", 5),))
                        cnt += 1
                    self.stt(st_f[:, 0:65], st_f[:, 0:65], decT[:, c:c + 1], self.pslin[:, 5, 0:65], ALU.mult, ALU.add,
                             ("st_f", "c_decT", ("pslin", 5)), ("st_f",))
                    self.cp(st_b[:, 0:65], st_f[:, 0:65], ("st_f",), ("st_b",))
                    yield
                P.dma("sp", y_d[s * T:(s + 1) * T, hp * 130:(hp + 1) * 130].rearrange("(n c) k -> c n k", c=C),
                      yo[0:C, 0:nch, :], reads=("c_yo",), writes=(("cy", m),))

    def chunk_bufs(self, T):
        if getattr(self, "_cbufs_at", None) == id(self.P.ops["pe"]) and getattr(self, "_cbufs_phase", -1) == self._phase_id:
            return self._cbufs
        qT = self.carve(T // 2, BF16)
        kT = self.carve(T // 2, BF16)
        q0 = self.carve(T // 2, BF16)
        q1 = self.carve(T // 2, BF16)
        nmax = T // 32
        kh = self.carve(nmax * 64, BF16).rearrange("p (n k) -> p n k", k=128)
        vv = self.carve(nmax * 65, BF16).rearrange("p (n k) -> p n k", k=130)
        yo = self.carve(nmax * 130).rearrange("p (n k) -> p n k", k=130)
        dch = self.carve(128)
        decT = self.carve(64)
        st_f = self.carve(66)
        st_b = self.carve(33, BF16)
        scm = [self.carve(32, BF16) for _ in range(2)]
        mask = self.carve(64)
        self._cbufs = (qT, kT, q0, q1, kh, vv, yo, dch, decT, st_f, st_b, scm, mask)
        self._cbufs_at = id(self.P.ops["pe"])
        self._cbufs_phase = self._phase_id
        self._mask_loaded = False
        return self._cbufs

    def phase_post(self, l, I, yT, ck, gate, bonus, obuf):
        P = self.P
        cfg = self.cfg
        self.carve_reset()
        o = self.carve(2048)
        y = self.carve(512)
        y65 = self.carve(520).rearrange("p (h e) -> p h e", e=65)
        gt = self.carve(512)
        W = [self.carve(512) for _ in range(3)]
        self.vt = self.carve(512).rearrange("p (a b) -> p a b", b=128)
        sv = {nm: self.carve(512) for nm in ("hgrn_norm_w", "mlstm_norm_w", "rwkv_ln_w", "rwkv_ln_b")}
        small = self.carve(64)
        for nm in sv:
            self.bcast_load(sv[nm], I[nm][l], "c_" + nm)
        h8 = lambda ap: ap.rearrange("p (h d) -> p h d", d=64)
        b8 = lambda ap8: ap8.unsqueeze(2).to_broadcast([128, 8, 64])
        wk = lambda i: ("W", i)
        for tile in range(cfg.NTILE):
            s, g = tile // cfg.TPS, tile % cfg.TPS
            rs = slice(tile * 128, (tile + 1) * 128)
            for mi, m in enumerate("ABCD"):
                if m == "D":
                    P.dma("sp", y, ck["D"]["y"][rs, :], reads=(("cy", "D"),), writes=("y",))
                else:
                    P.dma("sp", y65.rearrange("p h e -> p (h e)"), ck["y"][m][rs, :], reads=(("cy", m),), writes=("y65",))
                    self.cp(h8(y), y65[:, :, 0:64], ("y65",), ("y",), eng="dve")
                P.dma("sp", gt, gate[m][rs, :], reads=(("gate", m, tile),), writes=("gt",))
                osl = o[:, mi * 512:(mi + 1) * 512]
                if m == "C":
                    d8 = y65[:, :, 64]
                    self.ts(small[:, 0:8], d8, -1.0, None, ALU.mult, None, ("y65",), ("sm0",))
                    self.tt(small[:, 0:8], small[:, 0:8], d8, ALU.max, ("sm0", "y65"), ("sm0",))
                    self.ts(small[:, 0:8], small[:, 0:8], 1.0, None, ALU.max, None, ("sm0",), ("sm0",))
                    P.op("dve", lambda e: e.reciprocal(out=small[:, 8:16], in_=small[:, 0:8]), reads=("sm0",), writes=("sm1",))
                    self.tt(h8(y), h8(y), b8(small[:, 8:16]), ALU.mult, ("y", "sm1"), ("y",))
                if m in "ABC":
                    self.tt(W[0], y, y, ALU.mult, ("y",), (wk(0),))
                    self.red(small[:, 16:24], h8(W[0]), (wk(0),), ("sm2",))
                    self.actf(small[:, 24:32], small[:, 16:24], AF.Sqrt, ("sm2", "eps"), ("sm3",), scale=1.0 / 64,
                              bias=self.eps_t[:, 0:1])
                    P.op("dve", lambda e: e.reciprocal(out=small[:, 32:40], in_=small[:, 24:32]), reads=("sm3",), writes=("sm4",))
                    self.tt(h8(y), h8(y), b8(small[:, 32:40]), ALU.mult, ("y", "sm4"), ("y",))
                    if m == "B":
                        self.tt(y, y, sv["hgrn_norm_w"], ALU.mult, ("y", "c_hgrn_norm_w"), ("y",))
                    if m == "C":
                        self.tt(y, y, sv["mlstm_norm_w"], ALU.mult, ("y", "c_mlstm_norm_w"), ("y",))
                    self.tt(osl, y, gt, ALU.mult, ("y", "gt"), ("o",))
                else:
                    self.red(small[:, 16:24], h8(y), ("y",), ("sm2",))
                    self.ts(small[:, 16:24], small[:, 16:24], -1.0 / 64, None, ALU.mult, None, ("sm2",), ("sm2",))
                    self.tt(h8(y), h8(y), b8(small[:, 16:24]), ALU.add, ("y", "sm2"), ("y",))
                    self.tt(W[0], y, y, ALU.mult, ("y",), (wk(0),))
                    self.red(small[:, 24:32], h8(W[0]), (wk(0),), ("sm3",))
                    self.actf(small[:, 32:40], small[:, 24:32], AF.Sqrt, ("sm3", "eps"), ("sm4",), scale=1.0 / 64,
                              bias=self.eps_t[:, 1:2])
                    P.op("dve", lambda e: e.reciprocal(out=small[:, 40:48], in_=small[:, 32:40]), reads=("sm4",), writes=("sm5",))
                    self.tt(h8(y), h8(y), b8(small[:, 40:48]), ALU.mult, ("y", "sm5"), ("y",))
                    self.tt(y, y, sv["rwkv_ln_w"], ALU.mult, ("y", "c_rwkv_ln_w"), ("y",))
                    self.tt(y, y, sv["rwkv_ln_b"], ALU.add, ("y", "c_rwkv_ln_b"), ("y",))
                    P.dma("sp", W[1], bonus[rs, :], reads=(("bonus", tile),), writes=(wk(1),))
                    self.tt(y, y, W[1], ALU.add, ("y", wk(1)), ("y",))
                    self.tt(osl, y, gt, ALU.mult, ("y", "gt"), ("o",))
            P.dma("sp", obuf[rs, :], o, reads=("o",), writes=(("o", tile),))

    def phase_ffn(self, l, hbuf, obuf, wbo, wbg, wbu, wbd, wnb):
        P = self.P
        cfg = self.cfg
        NG = 4
        self.carve_linear(NG)
        actT = self.carve(44 * 256, BF16).rearrange("p (k g t) -> p k g t", g=NG, t=128)
        acc = [self.carve(2048) for _ in range(NG)]
        xT = self.xT
        for grp in range(cfg.NTILE // NG):
            for g in range(NG):
                tile = grp * NG + g
                ht = self.htile[tile % 2]
                P.dma("sp", ht, obuf[tile * 128:(tile + 1) * 128, :], reads=(("o", tile),), writes=(("htile", tile % 2),))
                self.cp(self.ub[:, 0:2048], ht, (("htile", tile % 2),), ("ub",), eng="dve")
                self.transpose_to(self.ub, "ub", xT, g, "xT", 16)

            def consume(g, cb, nc_, pst, pskey):
                self.cp(acc[g][:, cb * 512:cb * 512 + nc_], pst, (pskey,), (("acc", g),))

            self.linear_tm(xT, ("xT",), NG, 16, wbo, D, consume)
            self.resid_update(grp, NG, acc, hbuf, wnb[:, 1, :])
            for g in range(NG):
                tile = grp * NG + g
                ht = self.htile[tile % 2]
                P.dma("sp", ht, hbuf[tile * 128:(tile + 1) * 128, :], reads=(("h", tile),), writes=(("htile", tile % 2),))
                self.norm_transpose(ht, ("htile", tile % 2), wnb[:, 2, :], xT, g, "xT")
            for cb in range(DFF // 512):
                wg = self.wbuf[0].rearrange("p (k n) -> p k n", n=512)
                wu = self.wbuf[1].rearrange("p (k n) -> p k n", n=512)
                P.dma("sp", wg, wbg[cb, :, :, :], reads=(("wb", id(wbg), cb),), writes=(("wbuf", 0),))
                P.dma("sp", wu, wbu[cb, :, :, :], reads=(("wb", id(wbu), cb),), writes=(("wbuf", 1),))
                for j in range(4):
                    nchunk = cb * 4 + j
                    bg = self.psbank()
                    bu = self.psbank()
                    pg = self.pslin[:, bg, :]
                    pu = self.pslin[:, bu, :]
                    for kc in range(16):
                        P.op("pe", lambda e, pg=pg, kc=kc, j=j: e.matmul(pg, lhsT=wg[:, kc, j * 128:(j + 1) * 128],
                                                                         rhs=xT[:, kc, :, :].rearrange("p g t -> p (g t)"),
                                                                         start=(kc == 0), stop=(kc == 15)),
                             reads=("xT", ("wbuf", 0)), writes=(("pslin", bg),))
                    for kc in range(16):
                        P.op("pe", lambda e, pu=pu, kc=kc, j=j: e.matmul(pu, lhsT=wu[:, kc, j * 128:(j + 1) * 128],
                                                                         rhs=xT[:, kc, :, :].rearrange("p g t -> p (g t)"),
                                                                         start=(kc == 0), stop=(kc == 15)),
                             reads=("xT", ("wbuf", 1)), writes=(("pslin", bu),))
                    st = self.stage[self.ssel % 4]
                    skey = ("stage", self.ssel % 4)
                    self.ssel += 1
                    self.actf(st, pg, AF.Silu, (("pslin", bg),), (skey,))
                    self.tt(actT[:, nchunk, :, :].rearrange("p g t -> p (g t)"), st, pu, ALU.mult, (skey, ("pslin", bu)),
                            ("actT",))

            def consume2(g, cb, nc_, pst, pskey):
                self.cp(acc[g][:, cb * 128:cb * 128 + nc_], pst, (pskey,), (("acc", g),))

            self.linear_tm(actT, ("actT",), NG, 44, wbd, D, consume2, CB=128)
            self.resid_update(grp, NG, acc, hbuf, wnb[:, 3, :])

    def resid_update(self, grp, NG, acc, hbuf, wn):
        P = self.P
        for g in range(NG):
            tile = grp * NG + g
            ht = self.htile[tile % 2]
            hk = ("htile", tile % 2)
            P.dma("sp", ht, hbuf[tile * 128:(tile + 1) * 128, :], reads=(("h", tile),), writes=(hk,))
            self.rstd_of(acc[g], ("acc", g), 2048, self.eps_t[:, 0:1])
            self.stt(acc[g], acc[g], self.ss_t[:, 2:3], wn, ALU.mult, ALU.mult, (("acc", g), "ss2", "wnb"), (("acc", g),))
            self.tt(ht, ht, acc[g], ALU.add, (hk, ("acc", g)), (hk,))
            P.dma("sp", hbuf[tile * 128:(tile + 1) * 128, :], ht, reads=(hk,), writes=(("h", tile),))


def make_consts(T):
    half = 32
    inv_freq = 10000.0 ** (-np.arange(half, dtype=np.float32) / half)
    ang = np.arange(T, dtype=np.float32)[:, None] * inv_freq[None, :]
    cossin = np.concatenate([np.cos(ang), np.sin(ang)], axis=1).astype(np.float32)
    sel = np.zeros((32, TB, 128), np.float32)
    for tl in range(TB):
        for m in range(128):
            sel[(m // 64) * TB + tl, tl, m] = 1.0
    gam = np.zeros((128, 2, 4, 64), np.float32)
    for p in range(128):
        for hp in range(4):
            h = 2 * hp + p // 64
            gam[p, :, hp, :] = 1.0 - 2.0 ** (-5.0 - h)
    C = CHK["A"]
    j = (np.arange(128) % C).astype(np.float64)
    gh = 1.0 - 2.0 ** (-5.0 - np.arange(8, dtype=np.float64))
    atab = np.zeros((4, 128, 8, 64), np.float64)
    atab[0] = (gh[None, :] ** (j[:, None] + 1.0))[:, :, None]
    atab[1] = (0.125 * gh[None, :] ** (-(j[:, None] + 1.0)))[:, :, None]
    atab[2] = (0.125 * gh[None, :] ** (C - 1.0 - j[:, None]))[:, :, None]
    atab[3] = (gh ** float(C))[None, :, None]
    idx = np.arange(128)
    def trib(c):
        same = (idx[:, None] // c) == (idx[None, :] // c)
        return (same & (idx[:, None] <= idx[None, :])).astype(np.float32), same.astype(np.float32)
    t64, b64 = trib(64)
    t32, b32 = trib(32)
    trimats = np.stack([t64, b64, t32, b32]).astype(np.float32)
    cmask = (np.arange(64)[:, None] <= np.arange(64)[None, :]).astype(np.float32)
    i32 = np.arange(64) % 32
    dmasks = np.stack([(i32[:, None] < i32[None, :]), (i32[:, None] > i32[None, :]), (i32[:, None] <= i32[None, :])]).astype(
        np.float32)
    return dict(cossin=cossin, sel=sel, gamA=gam.reshape(128, 512), atab=atab.reshape(4, 128, 512).astype(np.float32),
                trimats=trimats, cmask=cmask, dmasks=dmasks,
                ident_b=np.eye(128).astype(ml_dtypes.bfloat16), ident_f=np.eye(128, dtype=np.float32))


def make_shared(inp, L, T):
    f = lambda a: np.ascontiguousarray(np.asarray(a, dtype=np.float32))
    sh = dict(w_in=f(inp["w_in"]), w_out=f(inp["w_out"]), w_ffn_gate=f(inp["w_ffn_gate"]), w_ffn_up=f(inp["w_ffn_up"]),
              w_ffn_down=f(inp["w_ffn_down"]))
    sh["norms"] = np.ascontiguousarray(np.stack([f(inp["norm_pre_mix"]), f(inp["norm_post_mix"]), f(inp["norm_pre_ffn"]),
                                                 f(inp["norm_post_ffn"])], axis=1))
    for nm in SMALL + ["hgrn_lb_logits", "mlstm_conv_w", "mlstm_conv_b", "rwkv_mu"]:
        sh[nm] = f(inp[nm])
    sh["mlstm_if_bias"] = np.ascontiguousarray(np.concatenate([f(inp["mlstm_i_bias"]), f(inp["mlstm_f_bias"])], axis=1))
    up = np.zeros((L, 128, 1024), np.float32)
    up[:, 0:64, 0:512] = f(inp["rwkv_w_up"])
    up[:, 64:128, 512:1024] = f(inp["rwkv_a_up"])
    sh["rwkv_up_pad"] = up
    sh["rwkv_g_up"] = f(inp["rwkv_g_up"])
    if L > 1:
        sh["rwkv_v0"] = f(inp["rwkv_v0"])
        sh["rwkv_v_down"] = f(inp["rwkv_v_down"])
        sh["rwkv_v_up"] = f(inp["rwkv_v_up"])
    else:
        sh["rwkv_v0"] = np.zeros((1, G), np.float32)
        sh["rwkv_v_down"] = np.zeros((1, G, 32), np.float32)
        sh["rwkv_v_up"] = np.zeros((1, 32, G), np.float32)
    sh.update(make_consts(T))
    return sh


_CACHE = {}


def kernel(**inp):
    x = np.asarray(inp["x"], dtype=np.float32)
    B, T, _ = x.shape
    L = inp["w_in"].shape[0]
    ncores = 8
    nseq = B // ncores
    cfg = Cfg(T=T, NSEQ=nseq, L=L)
    nc = Builder(cfg).build()
    sh = make_shared(inp, L, T)
    in_maps = []
    for c in range(ncores):
        m = dict(sh)
        m["x"] = np.ascontiguousarray(x[c * nseq:(c + 1) * nseq].reshape(nseq * T, D))
        in_maps.append(m)
    res = run_bass_kernel_spmd(nc, in_maps, core_ids=list(range(ncores)))
    outs = [np.asarray(r["out"]).reshape(nseq, T, D) for r in res.results]
    return np.concatenate(outs, axis=0).astype(np.float32)
```

```python
from contextlib import ExitStack
import numpy as np
import ml_dtypes
import concourse.bass as bass
import concourse.mybir as mybir
from concourse.bass_utils import run_bass_kernel_spmd

F32 = mybir.dt.float32
BF16 = mybir.dt.bfloat16
ALU = mybir.AluOpType
AF = mybir.ActivationFunctionType
AX = mybir.AxisListType

D = 2048
NIN = 7952
DFF = 5632
G = 512
NH = 8
HD = 64
NORM_EPS = 1e-6


class Prog:
    ENG = ("pe", "dve", "act", "pool", "sp")
    NDMASEM = 6

    def __init__(self, nc, stack):
        self.nc = nc
        self.stack = stack
        self.ops = {e: [] for e in self.ENG}
        self.cnt = {e: 0 for e in self.ENG}
        self.known = {e: {} for e in self.ENG}
        self.sem = {e: stack.enter_context(nc.semaphore("s_" + e)) for e in self.ENG if e != "sp"}
        self.semname = {}
        for e, s in self.sem.items():
            self.semname[id(s)] = e
        self.dq = {}
        for q in ("sp", "pool", "act"):
            self.dq[q] = {"sems": [stack.enter_context(nc.semaphore(f"d_{q}{i}")) for i in range(self.NDMASEM)],
                          "n": 0}
        self.bufs = {}
        self.nops = 0

    def _buf(self, k):
        b = self.bufs.get(k)
        if b is None:
            b = [None, {}]
            self.bufs[k] = b
        return b

    def _deps(self, e, reads, writes, extra=()):
        toks = list(extra)
        for k in reads:
            b = self._buf(k)
            if b[0] is not None:
                toks.append((b[0], False))
        for k in writes:
            b = self._buf(k)
            if b[0] is not None:
                toks.append((b[0], False))
            for s, v in b[1].values():
                toks.append(((s, v), True))
        own = self.sem.get(e)
        waits = []
        kn = self.known[e]
        for (s, v), is_reader in toks:
            if s is own and (e == "pe" or is_reader):
                continue
            if kn.get(id(s), 0) >= v:
                continue
            kn[id(s)] = v
            waits.append((s, v))
        return waits

    def _commit(self, tok, reads, writes):
        s, v = tok
        for k in reads:
            b = self._buf(k)
            b[1][id(s)] = (s, v)
        for k in writes:
            b = self._buf(k)
            b[0] = tok
            b[1] = {}

    def op(self, e, fn, reads=(), writes=()):
        reads = tuple(reads)
        writes = tuple(writes)
        waits = self._deps(e, reads, writes)
        self.cnt[e] += 1
        tok = (self.sem[e], self.cnt[e])
        self.ops[e].append((waits, fn, (self.sem[e], 1)))
        self._commit(tok, reads, writes)
        self.nops += 1
        return tok

    def dma(self, q, out, in_, reads=(), writes=(), **kw):
        reads = tuple(reads)
        writes = tuple(writes)
        dq = self.dq[q]
        n = dq["n"]
        dq["n"] += 1
        s = dq["sems"][n % self.NDMASEM]
        prev = 16 * (n // self.NDMASEM)
        extra = [((s, prev), False)] if prev > 0 else []
        waits = self._deps(q, reads, writes, extra)
        tok = (s, prev + 16)
        self.ops[q].append((waits, lambda eng: eng.dma_start(out=out, in_=in_, **kw), (s, 16)))
        self._commit(tok, reads, writes)
        self.nops += 1
        return tok

    def barrier(self):
        toks = [(self.sem[e], self.cnt[e]) for e in self.sem if self.cnt[e] > 0] + self.all_dma_tokens()
        for e in self.ENG:
            waits = []
            kn = self.known[e]
            for s, v in toks:
                if e == "pe" and s is self.sem.get("pe"):
                    continue
                if kn.get(id(s), 0) >= v:
                    continue
                kn[id(s)] = v
                waits.append((s, v))
            if waits:
                self.ops[e].append((waits, None, None))
        self.bufs = {}

    def finish(self, e, toks):
        waits = []
        for s, v in toks:
            waits.append((s, v))
        self.ops[e].append((waits, None, None))

    def all_dma_tokens(self):
        toks = []
        for q, dq in self.dq.items():
            n = dq["n"]
            for i in range(self.NDMASEM):
                c = (n - i + self.NDMASEM - 1) // self.NDMASEM if n > i else 0
                if c > 0:
                    toks.append((dq["sems"][i], 16 * c))
        return toks

    def emit(self):
        nc = self.nc
        ops = self.ops

        def run(eng, lst):
            for waits, fn, inc in lst:
                for s, v in waits:
                    eng.wait_ge(s, v)
                if fn is not None:
                    ins = fn(eng)
                    ins.then_inc(inc[0], inc[1])

        with nc.Block() as block:
            @block.tensor
            def _(eng):
                run(eng, ops["pe"])

            @block.vector
            def _(eng):
                run(eng, ops["dve"])

            @block.scalar
            def _(eng):
                run(eng, ops["act"])

            @block.gpsimd
            def _(eng):
                run(eng, ops["pool"])

            @block.sync
            def _(eng):
                run(eng, ops["sp"])


RWC = 1792
OFFS = {"A": 0, "B": 2048, "C": 4096, "D": 6160}
VECS = {"D": ["rD", "kD", "wD", "kkD", "bD"]}
ALLV = VECS["D"]
CHK = {"A": 64, "B": 32, "C": 64}
TB = 16
SMALL = ["hgrn_norm_w", "mlstm_norm_w", "rwkv_w0", "rwkv_a0", "rwkv_k_k", "rwkv_k_a", "rwkv_r_k", "rwkv_ln_w",
         "rwkv_ln_b"]


class Cfg:
    def __init__(self, T=2048, NSEQ=2, L=4):
        self.T = T
        self.NSEQ = NSEQ
        self.L = L
        self.NT = T * NSEQ
        self.TPS = T // 128
        self.NTILE = self.NT // 128


def wb_shape(K, N, CB=512):
    ncb = (N + CB - 1) // CB
    return [ncb, 128, K // 128, CB]


class Builder:
    def __init__(self, cfg):
        self.cfg = cfg
        self.nc = bass.Bass("TRN2", target_bir_lowering=False)
        self.stack = ExitStack()
        self.P = None

    def dram_in(self, name, shape, dt=F32):
        return self.nc.dram_tensor(name, list(shape), dt, kind="ExternalInput").ap()

    def dram_out(self, name, shape, dt=F32):
        return self.nc.dram_tensor(name, list(shape), dt, kind="ExternalOutput").ap()

    def dram_tmp(self, name, shape, dt=F32):
        return self.nc.dram_tensor(name, list(shape), dt, kind="Internal").ap()

    def sb(self, name, shape, dt=F32):
        h = self.stack.enter_context(self.nc.sbuf_tensor(name, list(shape), dt))
        return h[tuple(slice(None) for _ in shape)]

    def ps(self, name, shape, dt=F32):
        return self.stack.enter_context(self.nc.psum_tensor(name, list(shape), dt))

    def carve_reset(self):
        self.cptr = 0

    def carve(self, ncols, dt=F32):
        a = self.arena[:, self.cptr:self.cptr + ncols]
        self.cptr += ncols
        assert self.cptr <= self.NARENA, self.cptr
        if dt == BF16:
            a = a.bitcast(BF16)
        return a

    def tt(self, out, a, b, op, r, w, eng="dve"):
        self.P.op(eng, lambda e: e.tensor_tensor(out=out, in0=a, in1=b, op=op), reads=r, writes=w)

    def ts(self, out, a, s1, s2, op0, op1, r, w, eng="dve"):
        if op1 is None:
            self.P.op(eng, lambda e: e.tensor_scalar(out=out, in0=a, scalar1=s1, scalar2=None, op0=op0), reads=r, writes=w)
        else:
            self.P.op(eng, lambda e: e.tensor_scalar(out=out, in0=a, scalar1=s1, scalar2=s2, op0=op0, op1=op1),
                      reads=r, writes=w)

    def stt(self, out, a, scalar, b, op0, op1, r, w):
        self.P.op("dve", lambda e: e.scalar_tensor_tensor(out=out, in0=a, scalar=scalar, in1=b, op0=op0, op1=op1),
                  reads=r, writes=w)

    def actf(self, out, a, func, r, w, scale=1.0, bias=None):
        if bias is None:
            self.P.op("act", lambda e: e.activation(out=out, in_=a, func=func, scale=scale), reads=r, writes=w)
        else:
            self.P.op("act", lambda e: e.activation(out=out, in_=a, func=func, scale=scale, bias=bias), reads=r, writes=w)

    def red(self, out, a, r, w):
        self.P.op("dve", lambda e: e.tensor_reduce(out=out, in_=a, axis=AX.X, op=ALU.add), reads=r, writes=w)

    def cp(self, out, a, r, w, eng="act"):
        if eng == "act":
            self.P.op("act", lambda e: e.copy(out=out, in_=a), reads=r, writes=w)
        else:
            self.P.op(eng, lambda e: e.tensor_copy(out=out, in_=a), reads=r, writes=w)

    def psbank(self):
        b = self.psel % self.NPS
        self.psel += 1
        return b

    def cast_weight(self, W, Wb, K, N, CB=512):
        P = self.P
        KC = K // 128
        ncb = (N + CB - 1) // CB
        Wv = W.rearrange("(kc p) n -> p kc n", p=128)
        for cb in range(ncb):
            c0 = cb * CB
            nc_ = min(CB, N - c0)
            for k0 in range(0, KC, 16):
                k1 = min(KC, k0 + 16)
                P.dma("pool", Wb[cb, :, k0:k1, 0:nc_], Wv[:, k0:k1, c0:c0 + nc_], reads=(), writes=(("wb", id(Wb), cb),))

    def linear_tm(self, xT, xkeys, ng, KC, Wb, N, consume, CB=512):
        P = self.P
        ncb = (N + CB - 1) // CB
        for cb in range(ncb):
            nc_ = min(CB, N - cb * CB)
            b = self.wsel % 2
            self.wsel += 1
            wbuf = self.wbuf[b][:, 0:KC * CB].rearrange("p (k n) -> p k n", n=CB)
            P.dma("sp", wbuf[:, :, 0:nc_], Wb[cb, :, 0:KC, 0:nc_], reads=(("wb", id(Wb), cb),), writes=(("wbuf", b),))
            for g in range(ng):
                bank = self.psbank()
                pst = self.pslin[:, bank, 0:nc_]
                for kc in range(KC):
                    P.op("pe", (lambda e, pst=pst, g=g, kc=kc, wbuf=wbuf, nc_=nc_:
                                e.matmul(pst, lhsT=xT[:, kc, g, :], rhs=wbuf[:, kc, 0:nc_],
                                         start=(kc == 0), stop=(kc == KC - 1))),
                         reads=tuple(xkeys) + (("wbuf", b),), writes=(("pslin", bank),))
                consume(g, cb, nc_, pst, ("pslin", bank))

    def rstd_of(self, src, src_key, n, eps_ap):
        P = self.P
        ss = self.ss_t
        junk = self.junk
        P.op("act", lambda e: e.activation(out=junk[:, 0:n], in_=src, func=AF.Square, accum_out=ss[:, 0:1]),
             reads=(src_key,), writes=("junk", "ss"))
        P.op("act", lambda e: e.activation(out=ss[:, 1:2], in_=ss[:, 0:1], func=AF.Sqrt, scale=1.0 / n, bias=eps_ap),
             reads=("ss", "eps"), writes=("ss1",))
        P.op("dve", lambda e: e.reciprocal(out=ss[:, 2:3], in_=ss[:, 1:2]), reads=("ss1",), writes=("ss2",))

    def norm_transpose(self, src_tile, src_key, wn_b, xT, g, xkey):
        self.rstd_of(src_tile, src_key, 2048, self.eps_t[:, 0:1])
        ub = self.ub
        self.stt(ub[:, 0:2048], src_tile, self.ss_t[:, 2:3], wn_b, ALU.mult, ALU.mult, (src_key, "ss2", "wnb"), ("ub",))
        self.transpose_to(ub, "ub", xT, g, xkey, 16)

    def transpose_to(self, ub, ubkey, xT, g, xkey, KC):
        P = self.P
        for k0 in range(0, KC, 4):
            k1 = min(KC, k0 + 4)
            bank = self.tsel % 2
            self.tsel += 1
            pt = self.pstr[:, bank, :]
            for kc in range(k0, k1):
                P.op("pe", (lambda e, kc=kc, pt=pt, k0=k0:
                            e.transpose(pt[:, (kc - k0) * 128:(kc - k0 + 1) * 128], ub[:, kc * 128:(kc + 1) * 128],
                                        self.ident_b[:, :])),
                     reads=(ubkey, "ident"), writes=(("pstr", bank),))
            n = k1 - k0
            P.op("act", (lambda e, pt=pt, k0=k0, n=n:
                         e.copy(out=xT[:, k0:k0 + n, g, :], in_=pt[:, 0:n * 128].rearrange("p (k t) -> p k t", t=128))),
                 reads=(("pstr", bank),), writes=(xkey,))

    def store_T(self, src, src_key, dstT, s, g):
        P = self.P
        vt = self.vt
        for hp in range(4):
            bank = self.psbank()
            pt = self.pslin[:, bank, 0:128]
            P.op("pe", lambda e, pt=pt, hp=hp: e.transpose(pt, src[:, hp * 128:(hp + 1) * 128], self.ident_f[:, :]),
                 reads=(src_key, "identf"), writes=(("pslin", bank),))
            self.cp(vt[:, hp, :], pt, (("pslin", bank),), ("vt",))
        P.dma("sp", dstT[:, :, s, g * 128:(g + 1) * 128].rearrange("hp p t -> p hp t"), vt[:, :, :], reads=("vt",),
              writes=(("T", id(dstT), s, g),))

    def bt_store(self, src_bf, src_key, dstT, s, g):
        P = self.P
        bank = self.tsel % 2
        self.tsel += 1
        pt = self.pstr[:, bank, :]
        for hp in range(4):
            P.op("pe", lambda e, hp=hp, pt=pt: e.transpose(pt[:, hp * 128:(hp + 1) * 128], src_bf[:, hp * 128:(hp + 1) * 128],
                                                          self.ident_b[:, :]),
                 reads=(src_key, "ident"), writes=(("pstr", bank),))
        tb = self.tbt
        P.op("act", lambda e, pt=pt: e.copy(out=tb[:, :, :], in_=pt[:, 0:512].rearrange("p (k t) -> p k t", t=128)),
             reads=(("pstr", bank),), writes=("tbt",))
        P.dma("sp", dstT[:, :, s, g * 128:(g + 1) * 128].rearrange("hp p t -> p hp t"), tb[:, :, :], reads=("tbt",),
              writes=(("T", id(dstT), s, g),))

    def store_v(self, v_src, v_key, v_d, rs):
        self.cp(self.vb[:, :, 0:64], v_src.rearrange("p (h e) -> p h e", e=64), (v_key,), ("vb",), eng="dve")
        self.P.dma("sp", v_d[rs, :], self.vb.rearrange("p h e -> p (h e)"), reads=("vb",), writes=(("vd", id(v_d)),))

    def load_T(self, dst, dst_key, srcT, s, g):
        P = self.P
        vt = self.vt
        P.dma("sp", vt[:, :, :], srcT[:, :, s, g * 128:(g + 1) * 128].rearrange("hp p t -> p hp t"),
              reads=(("T", id(srcT), s, g),), writes=("vt",))
        for hp in range(4):
            bank = self.psbank()
            pt = self.pslin[:, bank, 0:128]
            P.op("pe", lambda e, pt=pt, hp=hp: e.transpose(pt, vt[:, hp, :], self.ident_f[:, :]),
                 reads=("vt", "identf"), writes=(("pslin", bank),))
            self.cp(dst[:, hp * 128:(hp + 1) * 128], pt, (("pslin", bank),), (dst_key,))

    def load_shift(self, dst, key, c0, n, tile, k, extra_keys=()):
        P = self.P
        keys = (key,) + tuple(extra_keys)
        cfg = self.cfg
        g = tile % cfg.TPS
        r0 = tile * 128
        if g == 0:
            P.op("dve", lambda e: e.memset(dst[:, 0:n], 0.0), writes=keys)
            P.dma("sp", dst[k:128, 0:n], self.pbuf[r0:r0 + 128 - k, c0:c0 + n],
                  reads=tuple(("p", tile, cb) for cb in range(16)), writes=keys)
        else:
            P.dma("sp", dst[:, 0:n], self.pbuf[r0 - k:r0 - k + 128, c0:c0 + n],
                  reads=tuple(("p", tile, cb) for cb in range(16)) + tuple(("p", tile - 1, cb) for cb in range(16)),
                  writes=keys)

    def bcast_load(self, dst, src_row, key):
        self.P.dma("sp", dst, src_row.partition_broadcast(128), writes=(key,))

    def build(self, dbg=None, layers=None):
        cfg = self.cfg
        nc = self.nc
        L = cfg.L
        NT = cfg.NT
        T = cfg.T
        NSEQ = cfg.NSEQ
        self.P = P = Prog(nc, self.stack)
        layers = list(range(L)) if layers is None else layers
        I = {}
        x = self.dram_in("x", [NT, D])
        out = self.dram_out("out", [NT, D])
        w_in = self.dram_in("w_in", [L, D, NIN])
        w_out = self.dram_in("w_out", [L, D, D])
        w_g = self.dram_in("w_ffn_gate", [L, D, DFF])
        w_u = self.dram_in("w_ffn_up", [L, D, DFF])
        w_d = self.dram_in("w_ffn_down", [L, DFF, D])
        norms = self.dram_in("norms", [L, 4, D])
        for nm in SMALL:
            I[nm] = self.dram_in(nm, [L, G])
        I["hgrn_lb_logits"] = self.dram_in("hgrn_lb_logits", [L, G])
        I["mlstm_conv_w"] = self.dram_in("mlstm_conv_w", [L, 4, 2 * G])
        I["mlstm_conv_b"] = self.dram_in("mlstm_conv_b", [L, 2 * G])
        I["mlstm_if_bias"] = self.dram_in("mlstm_if_bias", [L, 16])
        I["rwkv_mu"] = self.dram_in("rwkv_mu", [L, RWC])
        I["rwkv_up_pad"] = self.dram_in("rwkv_up_pad", [L, 128, 1024])
        I["rwkv_g_up"] = self.dram_in("rwkv_g_up", [L, 128, G])
        I["rwkv_v0"] = self.dram_in("rwkv_v0", [max(L - 1, 1), G])
        I["rwkv_v_down"] = self.dram_in("rwkv_v_down", [max(L - 1, 1), G, 32])
        I["rwkv_v_up"] = self.dram_in("rwkv_v_up", [max(L - 1, 1), 32, G])
        ident_b_d = self.dram_in("ident_b", [128, 128], BF16)
        ident_f_d = self.dram_in("ident_f", [128, 128])
        cs_d = self.dram_in("cossin", [T, 64])
        sel_d = self.dram_in("sel", [32, TB, 128])
        gam_d = self.dram_in("gamA", [128, 512])
        atab_d = self.dram_in("atab", [4, 128, 512])
        tri_d = self.dram_in("trimats", [4, 128, 128])
        mask_d = self.dram_in("cmask", [64, 64])

        hbuf = self.dram_tmp("hbuf", [NT, D])
        self.pbuf = pbuf = self.dram_tmp("pbuf", [NT, NIN])
        obuf = self.dram_tmp("obuf", [NT, D])
        rows = {v: self.dram_tmp("row_" + v, [NT, G]) for v in ALLV}
        vT = {m: self.dram_tmp("vT_" + m, [4, 128, NSEQ, T]) for m in "D"}
        yT = {m: self.dram_tmp("yT_" + m, [4, 128, NSEQ, T]) for m in "D"}
        ck = dict(
            qT={m: self.dram_tmp("cqT_" + m, [4, 128, NSEQ, T], BF16) for m in "ABC"},
            kT={m: self.dram_tmp("ckT_" + m, [4, 128, NSEQ, T], BF16) for m in "ABC"},
            kh={m: self.dram_tmp("ckh_" + m, [NT, G], BF16) for m in "ABC"},
            v={m: self.dram_tmp("cv_" + m, [NT, 520], BF16) for m in "ABC"},
            dec={m: self.dram_tmp("cdec_" + m, [NT, G]) for m in "ABC"},
            y={m: self.dram_tmp("cy_" + m, [NT, 520]) for m in "ABC"},
            atab=atab_d, tri=tri_d, mask=mask_d)
        dmask_d = self.dram_in("dmasks", [3, 64, 64])
        dk = dict(rT=self.dram_tmp("d_rT", [4, 128, NSEQ, T], BF16), cT=self.dram_tmp("d_cT", [4, 128, NSEQ, T], BF16),
                  kT=self.dram_tmp("d_kT", [4, 128, NSEQ, T], BF16), bT=self.dram_tmp("d_bT", [4, 128, NSEQ, T], BF16),
                  kh=self.dram_tmp("d_kh", [NT, G], BF16), bh=self.dram_tmp("d_bh", [NT, G], BF16),
                  v=self.dram_tmp("d_v", [NT, G], BF16), dec=self.dram_tmp("d_dec", [NT, G]),
                  y=self.dram_tmp("d_y", [NT, G]), masks=dmask_d)
        ck["D"] = dk
        gate = {m: self.dram_tmp("gate_" + m, [NT, G]) for m in "ABCD"}
        bonus = self.dram_tmp("bonus", [NT, G])
        vfirst = self.dram_tmp("vfirst", [NT, G])
        wb_in = [self.dram_tmp(f"wb_in{l}", wb_shape(D, NIN), BF16) for l in range(L)]
        wb_out = [self.dram_tmp(f"wb_out{l}", wb_shape(D, D), BF16) for l in range(L)]
        wb_g = [self.dram_tmp(f"wb_g{l}", wb_shape(D, DFF), BF16) for l in range(L)]
        wb_u = [self.dram_tmp(f"wb_u{l}", wb_shape(D, DFF), BF16) for l in range(L)]
        wb_d = [self.dram_tmp(f"wb_d{l}", wb_shape(DFF, D, 128), BF16) for l in range(L)]
        dbg_t = None
        if dbg is not None and dbg[1] is not None:
            dbg_t = self.dram_out("dbg", dbg[1])

        self.ident_b = self.sb("ident_b_s", [128, 128], BF16)
        self.ident_f = self.sb("ident_f_s", [128, 128])
        self.ss_t = self.sb("ss", [128, 8])
        self.eps_t = self.sb("eps_t", [128, 4])
        gamA = self.sb("gamA_s", [128, 512])
        lbd = self.dram_tmp("lbd", [2, 128, L, G])
        wnb = self.sb("wnb", [128, 4, D])
        self.NARENA = 41000
        self.arena = self.sb("arena", [128, self.NARENA])
        self.NPS = 6
        self.pslin = self.ps("pslin", [128, self.NPS, 512])
        self.psel = 0
        self.pstr = self.ps("pstr", [128, 2, 1024], BF16)
        self.tsel = 0
        self.wsel = 0

        P.dma("sp", self.ident_b[:, :], ident_b_d[:, :], writes=("ident",))
        P.dma("sp", self.ident_f[:, :], ident_f_d[:, :], writes=("identf",))
        P.dma("sp", gamA[:, :], gam_d[:, :], writes=("gamA",))
        P.op("dve", lambda e: e.memset(self.eps_t[:, 0:1], NORM_EPS), writes=("eps",))
        P.op("dve", lambda e: e.memset(self.eps_t[:, 1:2], 64e-5), writes=("eps",))
        P.op("dve", lambda e: e.memset(self.eps_t[:, 2:3], 0.0), writes=("eps",))
        for i in range(0, NT, 512):
            P.dma("sp", hbuf[i:i + 512, :], x[i:i + 512, :], writes=tuple(("h", j) for j in range(i // 128, i // 128 + 4)))
        for l in layers:
            self.cast_weight(w_in[l], wb_in[l], D, NIN)
            self.cast_weight(w_out[l], wb_out[l], D, D)
            self.cast_weight(w_g[l], wb_g[l], D, DFF)
            self.cast_weight(w_u[l], wb_u[l], D, DFF)
            self.cast_weight(w_d[l], wb_d[l], DFF, D, 128)

        self.carve_reset()
        ex = self.carve(L * G).rearrange("p (l g) -> p l g", g=G)
        sm = self.carve(G)
        lbt = self.carve(L * G).rearrange("p (l g) -> p l g", g=G)
        oml = self.carve(L * G).rearrange("p (l g) -> p l g", g=G)
        P.dma("sp", ex, I["hgrn_lb_logits"].partition_broadcast(128), writes=("ex",))
        self.actf(ex, ex, AF.Exp, ("ex",), ("ex",))
        self.cp(sm, ex[:, 0, :], ("ex",), ("sm",), eng="dve")
        for j in range(1, L):
            self.tt(sm, sm, ex[:, j, :], ALU.add, ("sm", "ex"), ("sm",))
        P.op("dve", lambda e: e.reciprocal(out=sm, in_=sm), reads=("sm",), writes=("sm",))
        P.op("dve", lambda e: e.memset(lbt[:, 0, :], 0.0), writes=("lbt",))
        for j in range(1, L):
            self.tt(ex[:, j, :], ex[:, j, :], sm, ALU.mult, ("ex", "sm"), ("ex",))
            self.tt(lbt[:, j, :], lbt[:, j - 1, :], ex[:, j, :], ALU.add, ("lbt", "ex"), ("lbt",))
        self.ts(oml[:, :, :], lbt[:, :, :], -1.0, 1.0, ALU.mult, ALU.add, ("lbt",), ("oml",))
        P.dma("sp", lbd[0], lbt, reads=("lbt",), writes=("lbd",))
        P.dma("sp", lbd[1], oml, reads=("oml",), writes=("lbd",))
        P.barrier()

        for l in layers:
            P.dma("sp", wnb[:, :, :], norms[l].partition_broadcast(128), writes=("wnb",))
            self.phase_p1(l, hbuf, wb_in[l], wnb)
            P.barrier()
            if dbg is not None and dbg[0] == "p" and l == layers[-1]:
                break
            self.phase_prep(l, I, rows, vT, gate, bonus, vfirst, cs_d, lbd, ck)
            P.barrier()
            if dbg is not None and dbg[0] == "prep" and l == layers[-1]:
                break
            self.phase_scan(rows, vT, yT, sel_d, ck, only_abc=(dbg is not None and dbg[0] == "scanabc"))
            P.barrier()
            if dbg is not None and dbg[0] == "scanabc" and l == layers[-1]:
                break
            if dbg is not None and dbg[0] == "scan" and l == layers[-1]:
                break
            self.phase_post(l, I, yT, ck, gate, bonus, obuf)
            P.barrier()
            if dbg is not None and dbg[0] == "post" and l == layers[-1]:
                break
            self.phase_ffn(l, hbuf, obuf, wb_out[l], wb_g[l], wb_u[l], wb_d[l], wnb)
            P.barrier()

        if dbg_t is not None:
            src = {"p": pbuf, "post": obuf, "h": hbuf}.get(dbg[0])
            P.dma("sp", dbg_t, src)
        for i in range(0, NT, 512):
            P.dma("sp", out[i:i + 512, :], hbuf[i:i + 512, :])
        P.finish("sp", P.all_dma_tokens())
        P.emit()
        return nc

    def carve_linear(self, ng):
        self.carve_reset()
        self.xT = self.carve(16 * ng * 64, BF16).rearrange("p (k g t) -> p k g t", g=ng, t=128)
        self.wbuf = [self.carve(4096, BF16) for _ in range(2)]
        self.htile = [self.carve(2048) for _ in range(2)]
        self.junk = self.carve(2048)
        self.ub = self.carve(1024, BF16)
        self.stage = [self.carve(512) for _ in range(4)]
        self.ssel = 0

    def phase_p1(self, l, hbuf, wb, wnb):
        P = self.P
        cfg = self.cfg
        NG = 4
        self.carve_linear(NG)
        for grp in range(cfg.NTILE // NG):
            for g in range(NG):
                tile = grp * NG + g
                ht = self.htile[tile % 2]
                P.dma("sp", ht, hbuf[tile * 128:(tile + 1) * 128, :], reads=(("h", tile),), writes=(("htile", tile % 2),))
                self.norm_transpose(ht, ("htile", tile % 2), wnb[:, 0, :], self.xT, g, "xT")

            def consume(g, cb, nc_, pst, pskey, grp=grp):
                st = self.stage[self.ssel % 4]
                skey = ("stage", self.ssel % 4)
                self.ssel += 1
                self.cp(st[:, 0:nc_], pst, (pskey,), (skey,))
                tile = grp * NG + g
                P.dma("sp", self.pbuf[tile * 128:(tile + 1) * 128, cb * 512:cb * 512 + nc_], st[:, 0:nc_],
                      reads=(skey,), writes=(("p", tile, cb),))

            self.linear_tm(self.xT, ("xT",), NG, 16, wb, NIN, consume)

    def phase_prep(self, l, I, rows, vT, gate, bonus, vfirst, cs_d, lbd, ck):
        P = self.P
        cfg = self.cfg
        self.carve_reset()
        seg = self.carve(2064)
        sh = self.carve(3072)
        Wt = [self.carve(512) for _ in range(10)]
        R = self.carve(1024)
        cst = self.carve(64)
        self.vt = self.carve(512).rearrange("p (a b) -> p a b", b=128)
        cw = self.carve(4096).rearrange("p (j c) -> p j c", c=1024)
        cb_ = self.carve(1024)
        mu = self.carve(RWC)
        sv = {nm: self.carve(512) for nm in SMALL}
        v0b = self.carve(512)
        ifb = self.carve(16)
        upw = self.carve(1024)
        gup = self.carve(512)
        vdn = self.carve(128).rearrange("p (c n) -> p c n", n=32)
        vup = self.carve(512)
        small = self.carve(64)
        small2 = self.carve(64)
        QB = self.carve(256, BF16)
        KB = self.carve(256, BF16)
        KH = self.carve(256, BF16)
        XB1 = self.carve(256, BF16)
        XB2 = self.carve(256, BF16)
        XB3 = self.carve(256, BF16)
        XB4 = self.carve(256, BF16)
        self.tbt = self.carve(256, BF16).rearrange("p (a b) -> p a b", b=128)
        self.vb = self.carve(260, BF16).rearrange("p (h e) -> p h e", e=65)
        AT = self.carve(2048).rearrange("p (a c) -> p a c", c=512)
        tri = self.carve(512).rearrange("p (a c) -> p a c", c=128)
        P.dma("sp", AT, ck["atab"].rearrange("a p c -> p a c"), writes=("AT",))
        P.dma("sp", tri, ck["tri"].rearrange("a p c -> p a c"), writes=("tri",))
        P.op("dve", lambda e: e.memset(self.vb[:, :, 64:65], 1.0), writes=("vb",))
        lbt = self.carve(512)
        oml = self.carve(512)
        P.dma("sp", lbt, lbd[0, :, l, :], writes=("lbt",))
        P.dma("sp", oml, lbd[1, :, l, :], writes=("oml",))
        for nm in SMALL:
            self.bcast_load(sv[nm], I[nm][l], "c_" + nm)
        self.bcast_load(cw, I["mlstm_conv_w"][l], "cw")
        self.bcast_load(cb_, I["mlstm_conv_b"][l], "cb")
        self.bcast_load(mu, I["rwkv_mu"][l], "mu")
        self.bcast_load(ifb, I["mlstm_if_bias"][l], "ifb")
        P.dma("sp", upw, I["rwkv_up_pad"][l], writes=("upw",))
        P.dma("sp", gup, I["rwkv_g_up"][l], writes=("gup",))
        if l > 0:
            self.bcast_load(v0b, I["rwkv_v0"][l - 1], "v0b")
            P.dma("sp", vdn, I["rwkv_v_down"][l - 1].rearrange("(c p) n -> p c n", p=128), writes=("vdn",))
            P.dma("sp", vup[0:32, :], I["rwkv_v_up"][l - 1], writes=("vup",))
        pall = lambda tile: tuple(("p", tile, cb) for cb in range(16))
        W = Wt
        wk = lambda i: ("W", i)

        def h8(ap):
            return ap.rearrange("p (h d) -> p h d", d=64)

        def b8(ap8):
            return ap8.unsqueeze(2).to_broadcast([128, 8, 64])

        for tile in range(cfg.NTILE):
            s, g = tile // cfg.TPS, tile % cfg.TPS
            r0 = tile * 128
            rs = slice(r0, r0 + 128)
            P.dma("sp", seg[:, 0:2048], self.pbuf[rs, 0:2048], reads=pall(tile), writes=("seg",))
            P.dma("sp", cst, cs_d[g * 128:(g + 1) * 128, :], writes=("cst",))
            qk = seg[:, 0:1024].rearrange("p (h two d) -> p h two d", two=2, d=32)
            Rv = R.rearrange("p (h two d) -> p h two d", two=2, d=32)
            t1, t2 = qk[:, :, 0, :], qk[:, :, 1, :]
            cosb = cst[:, 0:32].unsqueeze(1).to_broadcast([128, 16, 32])
            sinb = cst[:, 32:64].unsqueeze(1).to_broadcast([128, 16, 32])
            w0v = W[0].rearrange("p (h d) -> p h d", d=32)
            w1v = W[1].rearrange("p (h d) -> p h d", d=32)
            self.tt(w0v, t1, cosb, ALU.mult, ("seg", "cst"), (wk(0),))
            self.tt(w1v, t2, sinb, ALU.mult, ("seg", "cst"), (wk(1),))
            self.tt(Rv[:, :, 0, :], w0v, w1v, ALU.subtract, (wk(0), wk(1)), ("R",))
            self.tt(w0v, t1, sinb, ALU.mult, ("seg", "cst"), (wk(0),))
            self.tt(w1v, t2, cosb, ALU.mult, ("seg", "cst"), (wk(1),))
            self.tt(Rv[:, :, 1, :], w0v, w1v, ALU.add, (wk(0), wk(1)), ("R",))
            self.tt(QB, R[:, 0:512], AT[:, 0, :], ALU.mult, ("R", "AT"), ("QB",))
            self.bt_store(QB, "QB", ck["qT"]["A"], s, g)
            self.tt(KB, R[:, 512:1024], AT[:, 1, :], ALU.mult, ("R", "AT"), ("KB",))
            self.bt_store(KB, "KB", ck["kT"]["A"], s, g)
            self.tt(KH, R[:, 512:1024], AT[:, 2, :], ALU.mult, ("R", "AT"), ("KH",))
            P.dma("sp", ck["kh"]["A"][rs, :], KH, reads=("KH",), writes=(("kh", "A"),))
            self.store_v(seg[:, 1024:1536], "seg", ck["v"]["A"], rs)
            P.dma("sp", ck["dec"]["A"][rs, :], AT[:, 3, :], reads=("AT",), writes=(("dec", "A"),))
            self.actf(W[2], seg[:, 1536:2048], AF.Silu, ("seg",), (wk(2),))
            P.dma("sp", gate["A"][rs, :], W[2], reads=(wk(2),), writes=(("gate", "A", tile),))
            P.dma("sp", seg[:, 0:2048], self.pbuf[rs, 2048:4096], reads=pall(tile), writes=("seg",))
            self.actf(W[0], seg[:, 0:512], AF.Silu, ("seg",), (wk(0),))
            self.ts(W[0], W[0], 0.125, None, ALU.mult, None, (wk(0),), (wk(0),))
            self.actf(W[1], seg[:, 512:1024], AF.Sigmoid, ("seg",), (wk(1),))
            self.tt(W[1], W[1], oml, ALU.mult, (wk(1), "oml"), (wk(1),))
            self.tt(W[1], W[1], lbt, ALU.add, (wk(1), "lbt"), (wk(1),))
            self.ts(W[3], W[1], -1.0, 1.0, ALU.mult, ALU.add, (wk(1),), (wk(3),))
            self.actf(W[4], W[1], AF.Ln, (wk(1),), (wk(4),))
            ba = self.psbank()
            pc = self.pslin[:, ba, :]
            P.op("pe", lambda e, pc=pc: e.matmul(pc, lhsT=tri[:, 2, :], rhs=W[4], start=True, stop=True),
                 reads=(wk(4), "tri"), writes=(("pslin", ba),))
            bb = self.psbank()
            ptot = self.pslin[:, bb, :]
            P.op("pe", lambda e, ptot=ptot: e.matmul(ptot, lhsT=tri[:, 3, :], rhs=W[4], start=True, stop=True),
                 reads=(wk(4), "tri"), writes=(("pslin", bb),))
            self.actf(W[5], pc, AF.Exp, (("pslin", ba),), (wk(5),))
            self.tt(QB, W[0], W[5], ALU.mult, (wk(0), wk(5)), ("QB",))
            self.bt_store(QB, "QB", ck["qT"]["B"], s, g)
            self.actf(W[5], pc, AF.Exp, (("pslin", ba),), (wk(5),), scale=-1.0)
            self.tt(KB, W[3], W[5], ALU.mult, (wk(3), wk(5)), ("KB",))
            self.bt_store(KB, "KB", ck["kT"]["B"], s, g)
            self.cp(W[6], ptot, (("pslin", bb),), (wk(6),))
            self.actf(W[7], W[6], AF.Exp, (wk(6),), (wk(7),))
            P.dma("sp", ck["dec"]["B"][rs, :], W[7], reads=(wk(7),), writes=(("dec", "B"),))
            self.tt(W[6], W[6], pc, ALU.subtract, (wk(6), ("pslin", ba)), (wk(6),))
            self.actf(W[6], W[6], AF.Exp, (wk(6),), (wk(6),))
            self.tt(KH, W[3], W[6], ALU.mult, (wk(3), wk(6)), ("KH",))
            P.dma("sp", ck["kh"]["B"][rs, :], KH, reads=("KH",), writes=(("kh", "B"),))
            self.store_v(seg[:, 1024:1536], "seg", ck["v"]["B"], rs)
            self.actf(W[2], seg[:, 1536:2048], AF.Sigmoid, ("seg",), (wk(2),))
            P.dma("sp", gate["B"][rs, :], W[2], reads=(wk(2),), writes=(("gate", "B", tile),))
            P.dma("sp", seg[:, 0:2064], self.pbuf[rs, 4096:6160], reads=pall(tile), writes=("seg",))
            shv = sh.rearrange("p (j c) -> p j c", c=1024)
            for j in range(3):
                self.load_shift(shv[:, j, :], ("sh", j), 4096, 1024, tile, 3 - j)
            acc = R
            self.tt(acc, seg[:, 0:1024], cw[:, 3, :], ALU.mult, ("seg", "cw"), ("R",))
            self.tt(acc, acc, cb_, ALU.add, ("R", "cb"), ("R",))
            for j in range(3):
                self.tt(shv[:, j, :], shv[:, j, :], cw[:, j, :], ALU.mult, (("sh", j), "cw"), (("sh", j),))
                self.tt(acc, acc, shv[:, j, :], ALU.add, ("R", ("sh", j)), ("R",))
            self.actf(acc, acc, AF.Silu, ("R",), ("R",))
            self.tt(small[:, 0:16], seg[:, 2048:2064], ifb, ALU.add, ("seg", "ifb"), ("small",))
            self.actf(small[:, 24:32], small[:, 8:16], AF.Sigmoid, ("small",), ("small3",))
            self.actf(small[:, 16:24], small[:, 24:32], AF.Ln, ("small3",), ("small2",))
            ba = self.psbank()
            pc8 = self.pslin[:, ba, 0:8]
            P.op("pe", lambda e, pc8=pc8: e.matmul(pc8, lhsT=tri[:, 0, :], rhs=small[:, 16:24], start=True, stop=True),
                 reads=("small2", "tri"), writes=(("pslin", ba),))
            bb = self.psbank()
            pt8 = self.pslin[:, bb, 0:8]
            P.op("pe", lambda e, pt8=pt8: e.matmul(pt8, lhsT=tri[:, 1, :], rhs=small[:, 16:24], start=True, stop=True),
                 reads=("small2", "tri"), writes=(("pslin", bb),))
            self.actf(small2[:, 0:8], pc8, AF.Exp, (("pslin", ba),), ("s2a",))
            self.tt(h8(QB), h8(acc[:, 0:512]), b8(small2[:, 0:8]), ALU.mult, ("R", "s2a"), ("QB",))
            self.bt_store(QB, "QB", ck["qT"]["C"], s, g)
            self.tt(small2[:, 8:16], small[:, 0:8], pc8, ALU.subtract, ("small", ("pslin", ba)), ("s2b",))
            self.actf(small2[:, 16:24], small2[:, 8:16], AF.Exp, ("s2b",), ("s2c",))
            self.stt(h8(KB), h8(acc[:, 512:1024]), 0.125, b8(small2[:, 16:24]), ALU.mult, ALU.mult, ("R", "s2c"), ("KB",))
            self.bt_store(KB, "KB", ck["kT"]["C"], s, g)
            self.tt(small2[:, 24:32], small2[:, 8:16], pt8, ALU.add, ("s2b", ("pslin", bb)), ("s2d",))
            self.actf(small2[:, 32:40], small2[:, 24:32], AF.Exp, ("s2d",), ("s2e",))
            self.stt(h8(KH), h8(acc[:, 512:1024]), 0.125, b8(small2[:, 32:40]), ALU.mult, ALU.mult, ("R", "s2e"), ("KH",))
            P.dma("sp", ck["kh"]["C"][rs, :], KH, reads=("KH",), writes=(("kh", "C"),))
            self.actf(small2[:, 40:48], pt8, AF.Exp, (("pslin", bb),), ("s2f",))
            self.cp(h8(W[1]), b8(small2[:, 40:48]), ("s2f",), (wk(1),), eng="dve")
            P.dma("sp", ck["dec"]["C"][rs, :], W[1], reads=(wk(1),), writes=(("dec", "C"),))
            self.store_v(seg[:, 1024:1536], "seg", ck["v"]["C"], rs)
            self.actf(W[2], seg[:, 1536:2048], AF.Sigmoid, ("seg",), (wk(2),))
            P.dma("sp", gate["C"][rs, :], W[2], reads=(wk(2),), writes=(("gate", "C", tile),))
            P.dma("sp", seg[:, 0:RWC], self.pbuf[rs, 6160:6160 + RWC], reads=pall(tile), writes=("seg",))
            prev = sh[:, 0:RWC]
            self.load_shift(prev, ("sh", 0), 6160, RWC, tile, 1, extra_keys=(("sh", 1),))
            self.tt(prev, prev, seg[:, 0:RWC], ALU.subtract, (("sh", 0), ("sh", 1), "seg"), (("sh", 0), ("sh", 1)))
            self.tt(prev, prev, mu, ALU.mult, (("sh", 0), ("sh", 1), "mu"), (("sh", 0), ("sh", 1)))
            self.tt(seg[:, 0:RWC], seg[:, 0:RWC], prev, ALU.add, ("seg", ("sh", 0), ("sh", 1)), ("seg",))
            r_, k_, v_ = seg[:, 0:512], seg[:, 512:1024], seg[:, 1024:1536]
            Lt = W[0][:, 0:256]
            self.actf(Lt[:, 0:64], seg[:, 1536:1600], AF.Tanh, ("seg",), (wk(0),))
            self.cp(Lt[:, 64:128], seg[:, 1600:1664], ("seg",), (wk(0),))
            self.actf(Lt[:, 128:256], seg[:, 1664:1792], AF.Sigmoid, ("seg",), (wk(0),))
            LT = W[1][:, 0:256]
            for c in range(2):
                bank = self.psbank()
                pt = self.pslin[:, bank, 0:128]
                P.op("pe", lambda e, pt=pt, c=c: e.transpose(pt, Lt[:, c * 128:(c + 1) * 128], self.ident_f[:, :]),
                     reads=(wk(0), "identf"), writes=(("pslin", bank),))
                self.cp(LT[:, c * 128:(c + 1) * 128], pt, (("pslin", bank),), (wk(1),))
            bank = self.psbank()
            pw = self.pslin[:, bank, :]
            P.op("pe", lambda e, pw=pw: e.matmul(pw, lhsT=LT[:, 0:128], rhs=upw[:, 0:512], start=True, stop=True),
                 reads=(wk(1), "upw"), writes=(("pslin", bank),))
            self.tt(W[2], pw, sv["rwkv_w0"], ALU.add, (("pslin", bank), "c_rwkv_w0"), (wk(2),))
            self.actf(W[2], W[2], AF.Sigmoid, (wk(2),), (wk(2),))
            self.ts(W[2], W[2], -0.6065306597126334, None, ALU.mult, None, (wk(2),), (wk(2),))
            bank = self.psbank()
            pa = self.pslin[:, bank, :]
            P.op("pe", lambda e, pa=pa: e.matmul(pa, lhsT=LT[:, 0:128], rhs=upw[:, 512:1024], start=True, stop=True),
                 reads=(wk(1), "upw"), writes=(("pslin", bank),))
            self.tt(W[3], pa, sv["rwkv_a0"], ALU.add, (("pslin", bank), "c_rwkv_a0"), (wk(3),))
            self.actf(W[3], W[3], AF.Sigmoid, (wk(3),), (wk(3),))
            bank = self.psbank()
            pg = self.pslin[:, bank, :]
            P.op("pe", lambda e, pg=pg: e.matmul(pg, lhsT=LT[:, 128:256], rhs=gup, start=True, stop=True),
                 reads=(wk(1), "gup"), writes=(("pslin", bank),))
            self.cp(W[4], pg, (("pslin", bank),), (wk(4),))
            P.dma("sp", gate["D"][rs, :], W[4], reads=(wk(4),), writes=(("gate", "D", tile),))
            if l == 0:
                P.dma("sp", vfirst[rs, :], v_, reads=("seg",), writes=(("vfirst", tile),))
            else:
                vtt = self.vt
                for c in range(4):
                    bank = self.psbank()
                    pt = self.pslin[:, bank, 0:128]
                    P.op("pe", lambda e, pt=pt, c=c: e.transpose(pt, v_[:, c * 128:(c + 1) * 128], self.ident_f[:, :]),
                         reads=("seg", "identf"), writes=(("pslin", bank),))
                    self.cp(vtt[:, c, :], pt, (("pslin", bank),), ("vt",))
                bank = self.psbank()
                pv = self.pslin[:, bank, 0:32]
                for c in range(4):
                    P.op("pe", lambda e, pv=pv, c=c: e.matmul(pv, lhsT=vtt[:, c, :], rhs=vdn[:, c, :], start=(c == 0),
                                                              stop=(c == 3)),
                         reads=("vt", "vdn"), writes=(("pslin", bank),))
                self.cp(W[5][:, 0:32], pv, (("pslin", bank),), (wk(5),))
                bank = self.psbank()
                pt = self.pslin[0:32, bank, 0:128]
                P.op("pe", lambda e, pt=pt: e.transpose(pt, W[5][:, 0:32], self.ident_f[:, :]),
                     reads=(wk(5), "identf"), writes=(("pslin", bank),))
                self.cp(W[6][0:32, 0:128], pt, (("pslin", bank),), (wk(6),))
                bank = self.psbank()
                pv2 = self.pslin[:, bank, :]
                P.op("pe", lambda e, pv2=pv2: e.matmul(pv2, lhsT=W[6][0:32, 0:128], rhs=vup[0:32, :], start=True, stop=True),
                     reads=(wk(6), "vup"), writes=(("pslin", bank),))
                self.tt(W[5], pv2, v0b, ALU.add, (("pslin", bank), "v0b"), (wk(5),))
                self.actf(W[5], W[5], AF.Sigmoid, (wk(5),), (wk(5),))
                P.dma("sp", W[6], vfirst[rs, :], reads=(("vfirst", tile),), writes=(wk(6),))
                self.tt(W[6], W[6], v_, ALU.subtract, (wk(6), "seg"), (wk(6),))
                self.tt(W[6], W[6], W[5], ALU.mult, (wk(6), wk(5)), (wk(6),))
                self.tt(v_, v_, W[6], ALU.add, ("seg", wk(6)), ("seg",))
            self.tt(W[5], k_, sv["rwkv_k_k"], ALU.mult, ("seg", "c_rwkv_k_k"), (wk(5),))
            self.tt(W[6], W[5], W[5], ALU.mult, (wk(5),), (wk(6),))
            self.red(small[:, 32:40], h8(W[6]), (wk(6),), ("small4",))
            self.actf(small[:, 40:48], small[:, 32:40], AF.Sqrt, ("small4",), ("small5",))
            self.ts(small[:, 40:48], small[:, 40:48], 1e-12, None, ALU.max, None, ("small5",), ("small5",))
            P.op("dve", lambda e: e.reciprocal(out=small[:, 48:56], in_=small[:, 40:48]), reads=("small5",), writes=("small6",))
            self.tt(h8(W[5]), h8(W[5]), b8(small[:, 48:56]), ALU.mult, (wk(5), "small6"), (wk(5),))
            self.stt(W[7], W[3], -1.0, sv["rwkv_k_a"], ALU.add, ALU.mult, (wk(3), "c_rwkv_k_a"), (wk(7),))
            self.stt(W[7], W[7], 1.0, k_, ALU.add, ALU.mult, (wk(7), "seg"), (wk(7),))
            self.tt(W[8], W[3], W[5], ALU.mult, (wk(3), wk(5)), (wk(8),))
            self.tt(W[9], r_, W[7], ALU.mult, ("seg", wk(7)), (wk(9),))
            self.tt(W[9], W[9], sv["rwkv_r_k"], ALU.mult, (wk(9), "c_rwkv_r_k"), (wk(9),))
            self.red(small[:, 56:64], h8(W[9]), (wk(9),), ("small7",))
            self.tt(h8(W[9]), h8(v_), b8(small[:, 56:64]), ALU.mult, ("seg", "small7"), (wk(9),))
            P.dma("sp", bonus[rs, :], W[9], reads=(wk(9),), writes=(("bonus", tile),))
            dk = ck["D"]
            ba = self.psbank()
            pc = self.pslin[:, ba, :]
            P.op("pe", lambda e, pc=pc: e.matmul(pc, lhsT=tri[:, 2, :], rhs=W[2], start=True, stop=True),
                 reads=(wk(2), "tri"), writes=(("pslin", ba),))
            bb = self.psbank()
            ptot = self.pslin[:, bb, :]
            P.op("pe", lambda e, ptot=ptot: e.matmul(ptot, lhsT=tri[:, 3, :], rhs=W[2], start=True, stop=True),
                 reads=(wk(2), "tri"), writes=(("pslin", bb),))
            self.actf(W[0], pc, AF.Exp, (("pslin", ba),), (wk(0),))
            self.tt(QB, r_, W[0], ALU.mult, ("seg", wk(0)), ("QB",))
            self.bt_store(QB, "QB", dk["rT"], s, g)
            self.tt(W[1], pc, W[2], ALU.subtract, (("pslin", ba), wk(2)), (wk(1),))
            self.actf(W[1], W[1], AF.Exp, (wk(1),), (wk(1),))
            self.tt(KB, W[5], W[1], ALU.mult, (wk(5), wk(1)), ("KB",))
            self.bt_store(KB, "KB", dk["cT"], s, g)
            self.actf(W[0], pc, AF.Exp, (("pslin", ba),), (wk(0),), scale=-1.0)
            self.tt(XB1, W[7], W[0], ALU.mult, (wk(7), wk(0)), ("XB1",))
            self.bt_store(XB1, "XB1", dk["kT"], s, g)
            self.tt(XB2, W[8], W[0], ALU.mult, (wk(8), wk(0)), ("XB2",))
            self.bt_store(XB2, "XB2", dk["bT"], s, g)
            self.cp(W[6], ptot, (("pslin", bb),), (wk(6),))
            self.actf(W[1], W[6], AF.Exp, (wk(6),), (wk(1),))
            P.dma("sp", dk["dec"][rs, :], W[1], reads=(wk(1),), writes=(("dec", "D"),))
            self.tt(W[6], W[6], pc, ALU.subtract, (wk(6), ("pslin", ba)), (wk(6),))
            self.actf(W[6], W[6], AF.Exp, (wk(6),), (wk(6),))
            self.tt(KH, W[7], W[6], ALU.mult, (wk(7), wk(6)), ("KH",))
            P.dma("sp", dk["kh"][rs, :], KH, reads=("KH",), writes=(("kh", "D"),))
            self.stt(XB3, W[8], -1.0, W[6], ALU.mult, ALU.mult, (wk(8), wk(6)), ("XB3",))
            P.dma("sp", dk["bh"][rs, :], XB3, reads=("XB3",), writes=(("bh", "D"),))
            self.cp(XB4, v_, ("seg",), ("XB4",), eng="dve")
            P.dma("sp", dk["v"][rs, :], XB4, reads=("XB4",), writes=(("vd", "D"),))

    def phase_scan(self, rows, vT, yT, sel_d, ck, only_abc=False):
        P = self.P
        cfg = self.cfg
        self.carve_reset()
        self._phase_id = getattr(self, "_phase_id", 0) + 1

        def chain():
            for m in "ABC":
                for _ in self.gen_chunk(m, CHK[m], ck):
                    yield
        ga = chain()
        gd = self.chunk_D(ck["D"])
        na = 5 * sum((cfg.T // CHK[m]) * cfg.NSEQ * 4 for m in "ABC")
        nd = 19 * (cfg.T // 32 + 1) * cfg.NSEQ * 4
        a_done = d_done = False
        ia = idd = 0
        while not (a_done and d_done):
            if not d_done and (a_done or idd * na <= ia * nd):
                try:
                    next(gd)
                    idd += 1
                except StopIteration:
                    d_done = True
            elif not a_done:
                try:
                    next(ga)
                    ia += 1
                except StopIteration:
                    a_done = True

    def chunk_D(self, dk):
        P = self.P
        cfg = self.cfg
        T = cfg.T
        C = 32
        nch = T // C
        XT = self.carve(T // 2, BF16)
        X2 = {n: self.carve(T, BF16).rearrange("p (n h t) -> p n h t", h=2, t=C) for n in ("r", "c", "k", "b")}
        Vt = self.carve(nch * 32, BF16).rearrange("p (n e) -> p n e", e=64)
        KH2 = self.carve(nch * 64, BF16).rearrange("p (n e) -> p n e", e=128)
        BH2 = self.carve(nch * 64, BF16).rearrange("p (n e) -> p n e", e=128)
        YH = 32
        yo = self.carve(YH * 64).rearrange("p (n e) -> p n e", e=64)
        dch = self.carve(128)
        decT = self.carve(64)
        Zf = self.carve(64)
        Zb = self.carve(32, BF16)
        msk = self.carve(192).rearrange("p (a c) -> p a c", c=64)
        ST = [dict(N=[self.carve(64) for _ in range(5)], NT=[self.carve(64) for _ in range(4)], P=self.carve(64),
                   Q=self.carve(64), BTm=self.carve(32, BF16), S1m=self.carve(32, BF16), S2m=self.carve(32, BF16))
              for _ in range(2)]
        RHSs = self.carve(64)
        Ub = self.carve(32, BF16)
        pstr_f = self.pstr[:, :, :].bitcast(F32)
        I64 = self.ident_f[0:64, 0:64]
        P.dma("sp", msk[0:64, :, :], dk["masks"].rearrange("a p c -> p a c"), writes=("msk",))
        for n in X2:
            P.op("dve", lambda e, n=n: e.memset(X2[n], 0.0), writes=(("X2", n),))
        P.op("dve", lambda e: e.memset(KH2, 0.0), writes=("KH2",))
        P.op("dve", lambda e: e.memset(BH2, 0.0), writes=("BH2",))
        Mup, Mlow, Minc = msk[0:64, 0, :], msk[0:64, 1, :], msk[0:64, 2, :]
        srcT = {"r": dk["rT"], "c": dk["cT"], "k": dk["kT"], "b": dk["bT"]}
        pe = lambda fn, r, w: P.op("pe", fn, reads=r, writes=w)
        for s in range(cfg.NSEQ):
            for hp in range(4):
                for n in ("r", "c", "k", "b"):
                    P.dma("sp", XT[:, 0:T], srcT[n][hp, :, s, :], reads=tuple(("T", id(srcT[n]), s, g) for g in range(cfg.TPS)),
                          writes=("XT",))
                    self.cp(X2[n][0:64, :, 0, :], XT[0:64, 0:T].rearrange("p (n t) -> p n t", t=C), ("XT",), (("X2", n),),
                            eng="dve")
                    self.cp(X2[n][64:128, :, 1, :], XT[64:128, 0:T].rearrange("p (n t) -> p n t", t=C), ("XT",), (("X2", n),))
                rsl = slice(s * T, (s + 1) * T)
                for hh in range(2):
                    col = slice((2 * hp + hh) * 64, (2 * hp + hh + 1) * 64)
                    ps_ = slice(hh * 32, (hh + 1) * 32)
                    P.dma("sp", Vt[ps_, 0:nch, :], dk["v"][rsl, col].rearrange("(n c) k -> c n k", c=C), reads=(("vd", "D"),),
                          writes=("Vt",))
                    P.dma("sp", KH2[ps_, 0:nch, hh * 64:(hh + 1) * 64], dk["kh"][rsl, col].rearrange("(n c) k -> c n k", c=C),
                          reads=(("kh", "D"),), writes=("KH2",))
                    P.dma("sp", BH2[ps_, 0:nch, hh * 64:(hh + 1) * 64], dk["bh"][rsl, col].rearrange("(n c) k -> c n k", c=C),
                          reads=(("bh", "D"),), writes=("BH2",))
                P.dma("sp", dch[0:nch, :], dk["dec"][rsl, hp * 128:(hp + 1) * 128].rearrange("(n c) k -> c n k", c=C)[C - 1, :, :],
                      reads=(("dec", "D"),), writes=("dchD",))
                pt = self.pslin[:, 5, 0:nch]
                pe(lambda e, pt=pt: e.transpose(pt, dch[0:nch, :], self.ident_f[0:nch, 0:nch]), ("dchD", "identf"), (("pslin", 5),))
                self.cp(decT[:, 0:nch], pt, (("pslin", 5),), ("decTD",))
                P.op("dve", lambda e: e.memset(Zf, 0.0), writes=("Zf",))
                P.op("dve", lambda e: e.memset(Zb, 0.0), writes=("Zb",))
                r2, c2, k2, b2 = X2["r"], X2["c"], X2["k"], X2["b"]
                xk = lambda n: ("X2", n)

                def front(c):
                    par = c % 2
                    st = ST[par]
                    sk_ = lambda nm: ("ST", par, nm)
                    sc = self.pslin[0:64, par, :]
                    bk = ("pslin", par)
                    fl = lambda ap: ap.rearrange("p h t -> p (h t)")
                    pe(lambda e: e.matmul(sc[:, 0:64], lhsT=fl(b2[:, c]), rhs=fl(c2[:, c]), start=True, stop=True), (xk("b"), xk("c")), (bk,))
                    pe(lambda e: e.matmul(sc[:, 64:128], lhsT=fl(c2[:, c]), rhs=fl(b2[:, c]), start=True, stop=True), (xk("b"), xk("c")), (bk,))
                    pe(lambda e: e.matmul(sc[:, 128:192], lhsT=fl(k2[:, c]), rhs=fl(c2[:, c]), start=True, stop=True), (xk("k"), xk("c")), (bk,))
                    pe(lambda e: e.matmul(sc[:, 192:256], lhsT=fl(k2[:, c]), rhs=fl(r2[:, c]), start=True, stop=True), (xk("k"), xk("r")), (bk,))
                    pe(lambda e: e.matmul(sc[:, 256:320], lhsT=fl(b2[:, c]), rhs=fl(r2[:, c]), start=True, stop=True), (xk("b"), xk("r")), (bk,))
                    yield
                    N, NT, Pm, Qm = st["N"], st["NT"], st["P"][0:64, :], st["Q"][0:64, :]
                    self.stt(N[0][0:64, :], sc[:, 0:64], -1.0, Mup, ALU.mult, ALU.mult, (bk, "msk"), (sk_("N0"),))
                    self.stt(NT[0][0:64, :], sc[:, 64:128], -1.0, Mlow, ALU.mult, ALU.mult, (bk, "msk"), (sk_("NT0"),))
                    self.tt(st["BTm"][0:64, :], sc[:, 128:192], Mup, ALU.mult, (bk, "msk"), (sk_("BTm"),))
                    self.tt(st["S1m"][0:64, :], sc[:, 192:256], Minc, ALU.mult, (bk, "msk"), (sk_("S1m"),))
                    self.stt(st["S2m"][0:64, :], sc[:, 256:320], -1.0, Minc, ALU.mult, ALU.mult, (bk, "msk"), (sk_("S2m"),))
                    self.tt(Pm, N[0][0:64, :], I64, ALU.add, (sk_("N0"), "identf"), (sk_("P"),), eng="pool")
                    self.tt(Qm, NT[0][0:64, :], I64, ALU.add, (sk_("NT0"), "identf"), (sk_("Q"),), eng="pool")
                    yield
                    for k in range(1, 5):
                        last = (k == 4)
                        pn = self.pslin[0:64, 2, 0:128]
                        pe(lambda e, k=k: e.matmul(pn[:, 0:64], lhsT=NT[k - 1][0:64, :], rhs=N[k - 1][0:64, :], start=True, stop=True),
                           (sk_("N%d" % (k - 1)), sk_("NT%d" % (k - 1))), (("pslin", 2),))
                        if not last:
                            pe(lambda e, k=k: e.matmul(pn[:, 64:128], lhsT=N[k - 1][0:64, :], rhs=NT[k - 1][0:64, :], start=True,
                                                       stop=True),
                               (sk_("N%d" % (k - 1)), sk_("NT%d" % (k - 1))), (("pslin", 2),))
                        yield
                        self.cp(N[k][0:64, :], pn[:, 0:64], (("pslin", 2),), (sk_("N%d" % k),))
                        if not last:
                            self.cp(NT[k][0:64, :], pn[:, 64:128], (("pslin", 2),), (sk_("NT%d" % k),))
                        yield
                        pp = self.pslin[0:64, 2, 128:256]
                        pe(lambda e, k=k: e.matmul(pp[:, 0:64], lhsT=Qm, rhs=N[k][0:64, :], start=True, stop=True),
                           (sk_("Q"), sk_("N%d" % k)), (("pslin", 2),))
                        if not last:
                            pe(lambda e, k=k: e.matmul(pp[:, 64:128], lhsT=N[k][0:64, :], rhs=Qm, start=True, stop=True),
                               (sk_("Q"), sk_("N%d" % k)), (("pslin", 2),))
                        yield
                        self.tt(Pm, Pm, pp[:, 0:64], ALU.add, (sk_("P"), ("pslin", 2)), (sk_("P"),))
                        if not last:
                            self.tt(Qm, Qm, pp[:, 64:128], ALU.add, (sk_("Q"), ("pslin", 2)), (sk_("Q"),))
                        yield

                def back(c):
                    par = c % 2
                    st = ST[par]
                    sk_ = lambda nm: ("ST", par, nm)
                    fl = lambda ap: ap.rearrange("p h t -> p (h t)")
                    pr = self.pslin[0:64, 3, 0:64]
                    pe(lambda e: e.matmul(pr, lhsT=fl(c2[:, c]), rhs=Zb[:, 0:64], start=True, stop=False), (xk("c"), "Zb"), (("pslin", 3),))
                    pe(lambda e: e.matmul(pr, lhsT=st["BTm"][0:64, :], rhs=Vt[0:64, c, :], start=False, stop=True),
                       (sk_("BTm"), "Vt"), (("pslin", 3),))
                    yield
                    self.cp(RHSs[0:64, :], pr, (("pslin", 3),), ("RHSs",))
                    yield
                    pu = self.pslin[0:64, 3, 64:128]
                    pe(lambda e: e.matmul(pu, lhsT=st["P"][0:64, :], rhs=RHSs[0:64, :], start=True, stop=True), (sk_("P"), "RHSs"),
                       (("pslin", 3),))
                    yield
                    self.cp(Ub[0:64, :], pu, (("pslin", 3),), ("Ub",))
                    yield
                    pk = self.pslin[:, 5, 0:64]
                    pe(lambda e: e.matmul(pk, lhsT=KH2[0:64, c, :], rhs=Vt[0:64, c, :], start=True, stop=False), ("KH2", "Vt"),
                       (("pslin", 5),))
                    pe(lambda e: e.matmul(pk, lhsT=BH2[0:64, c, :], rhs=Ub[0:64, :], start=False, stop=True), ("BH2", "Ub"),
                       (("pslin", 5),))
                    py = self.pslin[0:64, 3, 128:192]
                    pe(lambda e: e.matmul(py, lhsT=st["S1m"][0:64, :], rhs=Vt[0:64, c, :], start=True, stop=False), (sk_("S1m"), "Vt"),
                       (("pslin", 3),))
                    pe(lambda e: e.matmul(py, lhsT=st["S2m"][0:64, :], rhs=Ub[0:64, :], start=False, stop=False), (sk_("S2m"), "Ub"),
                       (("pslin", 3),))
                    pe(lambda e: e.matmul(py, lhsT=fl(r2[:, c]), rhs=Zb[:, 0:64], start=False, stop=True), (xk("r"), "Zb"),
                       (("pslin", 3),))
                    yield
                    self.stt(Zf, Zf, decT[:, c:c + 1], pk, ALU.mult, ALU.add, ("Zf", "decTD", ("pslin", 5)), ("Zf",))
                    self.cp(yo[0:64, c % YH, :], py, (("pslin", 3),), ("yoD",))
                    yield
                    self.cp(Zb, Zf, ("Zf",), ("Zb",))
                    if c % YH == YH - 1 or c == nch - 1:
                        c0 = c - (c % YH)
                        for hh in range(2):
                            col = slice((2 * hp + hh) * 64, (2 * hp + hh + 1) * 64)
                            P.dma("sp", dk["y"][s * T + c0 * C:s * T + (c + 1) * C, col].rearrange("(n c) k -> c n k", c=C),
                                  yo[hh * 32:(hh + 1) * 32, 0:c - c0 + 1, :], reads=("yoD",), writes=(("cy", "D"),))
                    yield

                import itertools
                for c in range(nch + 1):
                    gf = front(c) if c < nch else iter(())
                    gb = back(c - 1) if c > 0 else iter(())
                    for _ in itertools.zip_longest(gf, gb):
                        yield

    def gen_scan_D(self, rows, vT, yT, sel_d):
        P = self.P
        cfg = self.cfg
        T = cfg.T
        sel = self.carve(TB * 128).rearrange("p (t m) -> p t m", m=128)
        P.dma("sp", sel[0:32, :, :], sel_d[:, :, :], writes=("sel",))
        Rb = [self.carve(5 * 512).rearrange("p (v n) -> p v n", n=512) for _ in range(2)]
        Vb = [self.carve(8 * TB).rearrange("p (g t) -> p g t", t=TB) for _ in range(2)]
        Yb = [self.carve(8 * TB).rearrange("p (g t) -> p g t", t=TB) for _ in range(2)]
        NR = 3
        BR = [self.carve(5 * 512).rearrange("p (v n) -> p v n", n=512) for _ in range(NR)]
        S = self.carve(512)
        T1 = self.carve(512)
        T2 = self.carve(512)
        sk = self.carve(8)
        h8 = lambda ap: ap.rearrange("p (h d) -> p h d", d=64)
        b8 = lambda ap8: ap8.unsqueeze(2).to_broadcast([128, 8, 64])
        m = "D"
        vecs = VECS[m]
        P.op("dve", lambda e: e.memset(S, 0.0), writes=("S",))
        step = 0
        dps = 0
        for bi in range(T // TB):
            t0 = bi * TB
            pb = bi % 2
            for j, vname in enumerate(vecs):
                src = rows[vname].rearrange("(s t) (hp hh d) -> t s hp hh d", s=cfg.NSEQ, hh=2, d=64)
                for hh in range(2):
                    for s in range(cfg.NSEQ):
                        P.dma("sp", Rb[pb][hh * TB:(hh + 1) * TB, j, s * 256:(s + 1) * 256].rearrange(
                            "t (hp d) -> t hp d", d=64),
                            src[t0:t0 + TB, s, :, hh, :],
                            reads=(("row", vname, (s * T + t0) // 128),), writes=(("Rb", pb, j),))
            P.dma("sp", Vb[pb].rearrange("p (s hp) t -> p s hp t", hp=4),
                  vT[m].rearrange("hp p s t -> p s hp t")[:, :, :, t0:t0 + TB],
                  reads=tuple(("T", id(vT[m]), s, t0 // 128) for s in range(cfg.NSEQ)), writes=(("Vb", pb),))
            for tl in range(TB):
                rb = step % NR
                step += 1
                ps = {}
                for j, vname in enumerate(vecs):
                    bank = dps % 3
                    dps += 1
                    pt = self.pslin[:, bank, :]
                    P.op("pe", lambda e, pt=pt, j=j, tl=tl, pb=pb: e.matmul(pt, lhsT=sel[0:32, tl, :], rhs=Rb[pb][0:32, j, :],
                                                                           start=True, stop=True),
                         reads=("sel", ("Rb", pb, j)), writes=(("pslin", bank),))
                    self.cp(BR[rb][:, j, :], pt, (("pslin", bank),), (("BR", rb, j),))
                    ps[j] = (BR[rb][:, j, :], ("BR", rb, j))
                vb = b8(Vb[pb][:, :, tl])
                yo = Yb[pb][:, :, tl]
                (rp, rk_), (kp, kk_), (wp, wk_), (cp_, ck_), (bp, bk_) = ps[0], ps[1], ps[2], ps[3], ps[4]
                self.tt(T1, S, cp_, ALU.mult, ("S", ck_), ("T1",))
                self.red(sk, h8(T1), ("T1",), ("sk",))
                self.tt(h8(T1), h8(bp), b8(sk), ALU.mult, (bk_, "sk"), ("T1",))
                self.tt(S, S, wp, ALU.mult, ("S", wk_), ("S",))
                self.tt(S, S, T1, ALU.subtract, ("S", "T1"), ("S",))
                self.tt(h8(T2), h8(kp), vb, ALU.mult, (kk_, ("Vb", pb)), ("T2",))
                self.tt(S, S, T2, ALU.add, ("S", "T2"), ("S",))
                self.tt(T2, S, rp, ALU.mult, ("S", rk_), ("T2",))
                self.red(yo, h8(T2), ("T2",), (("Yb", pb),))
                yield
            P.dma("sp", yT[m].rearrange("hp p s t -> p s hp t")[:, :, :, t0:t0 + TB],
                  Yb[pb].rearrange("p (s hp) t -> p s hp t", hp=4), reads=(("Yb", pb),),
                  writes=tuple(("T", id(yT[m]), s, t0 // 128) for s in range(cfg.NSEQ)))

    def gen_chunk(self, m, C, ck):
        P = self.P
        cfg = self.cfg
        T = cfg.T
        nch = T // C
        cb = self.chunk_bufs(T)
        qT, kT, q0, q1, kh, vv, yo, dch, decT, st_f, st_b, scm, mask = cb
        YH = 32
        pstr_f = self.pstr[:, :, :].bitcast(F32)
        qT_d, kT_d, kh_d, v_d, dec_d, y_d = ck["qT"][m], ck["kT"][m], ck["kh"][m], ck["v"][m], ck["dec"][m], ck["y"][m]
        P.dma("sp", mask[0:C, 0:C], ck["mask"][0:C, 0:C], writes=("mask",))
        P.dma("sp", mask[0:C, C:2 * C], ck["mask"][0:C, 0:C], writes=("mask",))
        cnt = 0
        qs = [q0, q1]
        for s in range(cfg.NSEQ):
            for hp in range(4):
                allT = tuple(("T", id(qT_d), s, g) for g in range(cfg.TPS))
                P.dma("sp", qT[:, 0:T], qT_d[hp, :, s, :], reads=allT, writes=("c_qT",))
                P.dma("sp", kT[:, 0:T], kT_d[hp, :, s, :], reads=tuple(("T", id(kT_d), s, g) for g in range(cfg.TPS)),
                      writes=("c_kT",))
                P.dma("sp", kh[0:C, 0:nch, :], kh_d[s * T:(s + 1) * T, hp * 128:(hp + 1) * 128].rearrange(
                    "(n c) k -> c n k", c=C), reads=(("kh", m),), writes=("c_kh",))
                P.dma("sp", vv[0:C, 0:nch, :], v_d[s * T:(s + 1) * T, hp * 130:(hp + 1) * 130].rearrange(
                    "(n c) k -> c n k", c=C), reads=(("vd", id(v_d)),), writes=("c_vv",))
                P.dma("sp", dch[0:nch, :], dec_d[s * T:(s + 1) * T, hp * 128:(hp + 1) * 128].rearrange(
                    "(n c) k -> c n k", c=C)[C - 1, :, :], reads=(("dec", m),), writes=("c_dch",))
                pt = pstr_f[:, 1, 0:nch]
                P.op("pe", lambda e, pt=pt: e.transpose(pt, dch[0:nch, :], self.ident_f[0:nch, 0:nch]),
                     reads=("c_dch", "identf"), writes=(("pstr", 1),))
                self.cp(decT[:, 0:nch], pt, (("pstr", 1),), ("c_decT",))
                P.op("dve", lambda e: e.memset(q0[64:128, 0:T], 0.0), writes=("c_q0",))
                P.op("dve", lambda e: e.memset(q1[0:64, 0:T], 0.0), writes=("c_q1",))
                self.cp(q0[0:64, 0:T], qT[0:64, 0:T], ("c_qT",), ("c_q0",), eng="dve")
                self.cp(q1[64:128, 0:T], qT[64:128, 0:T], ("c_qT",), ("c_q1",), eng="dve")
                P.op("dve", lambda e: e.memset(st_f[:, 0:66], 0.0), writes=("st_f",))
                P.op("dve", lambda e: e.memset(st_b[:, 0:66], 0.0), writes=("st_b",))
                for c in range(nch):
                    cs = slice(c * C, (c + 1) * C)
                    par = c % 2
                    psc = self.pslin[0:C, 4, 0:2 * C]
                    for hh in range(2):
                        qh = qs[hh]
                        qk = "c_q%d" % hh
                        P.op("pe", lambda e, cs=cs, qh=qh, hh=hh: e.matmul(psc[:, hh * C:(hh + 1) * C], lhsT=kT[:, cs], rhs=qh[:, cs],
                                                                           start=True, stop=True),
                             reads=("c_kT", qk), writes=(("pslin", 4),))
                    yield
                    sm = scm[par][0:C, 0:2 * C]
                    self.tt(sm, psc, mask[0:C, 0:2 * C], ALU.mult, (("pslin", 4), "mask"), (("scm", par),))
                    yield
                    pso = pstr_f[0:C, 0, 0:130]
                    for hh in range(2):
                        qh = qs[hh]
                        qk = "c_q%d" % hh
                        vsl = vv[0:C, c, hh * 65:(hh + 1) * 65]
                        po = pso[:, hh * 65:(hh + 1) * 65]
                        P.op("pe", lambda e, po=po, vsl=vsl, hh=hh, sm=sm: e.matmul(po, lhsT=sm[:, hh * C:(hh + 1) * C], rhs=vsl,
                                                                                   start=True, stop=False),
                             reads=(("scm", par), "c_vv"), writes=(("pstr", 0),))
                        P.op("pe", lambda e, po=po, cs=cs, qh=qh: e.matmul(po, lhsT=qh[:, cs], rhs=st_b[:, 0:65], start=False, stop=True),
                             reads=(qk, "st_b"), writes=(("pstr", 0),))
                        pkv = pstr_f[hh * 64:(hh + 1) * 64, 1, 0:65]
                        khs = kh[0:C, c, hh * 64:(hh + 1) * 64]
                        P.op("pe", lambda e, pkv=pkv, khs=khs, vsl=vsl: e.matmul(pkv, lhsT=khs, rhs=vsl, start=True, stop=True),
                             reads=("c_kh", "c_vv"), writes=(("pstr", 1),))
                    yield
                    self.stt(st_f[:, 0:65], st_f[:, 0:65], decT[:, c:c + 1], pstr_f[:, 1, 0:65], ALU.mult, ALU.add,
                             ("st_f", "c_decT", ("pstr", 1)), ("st_f",))
                    self.cp(yo[0:C, c % YH, :], pso, (("pstr", 0),), ("c_yo",))
                    yield
                    self.cp(st_b[:, 0:65], st_f[:, 0:65], ("st_f",), ("st_b",))
                    if c % YH == YH - 1 or c == nch - 1:
                        c0 = c - (c % YH)
                        P.dma("sp", y_d[s * T + c0 * C:s * T + (c + 1) * C, hp * 130:(hp + 1) * 130].rearrange(
                            "(n c) k -> c n k", c=C), yo[0:C, 0:c - c0 + 1, :], reads=("c_yo",), writes=(("cy", m),))
                    yield

    def chunk_bufs(self, T):
        if getattr(self, "_cbufs_at", None) == id(self.P.ops["pe"]) and getattr(self, "_cbufs_phase", -1) == self._phase_id:
            return self._cbufs
        qT = self.carve(T // 2, BF16)
        kT = self.carve(T // 2, BF16)
        q0 = self.carve(T // 2, BF16)
        q1 = self.carve(T // 2, BF16)
        nmax = T // 32
        kh = self.carve(nmax * 64, BF16).rearrange("p (n k) -> p n k", k=128)
        vv = self.carve(nmax * 65, BF16).rearrange("p (n k) -> p n k", k=130)
        yo = self.carve(32 * 130).rearrange("p (n k) -> p n k", k=130)
        dch = self.carve(128)
        decT = self.carve(64)
        st_f = self.carve(66)
        st_b = self.carve(33, BF16)
        scm = [self.carve(64, BF16) for _ in range(2)]
        mask = self.carve(128)
        self._cbufs = (qT, kT, q0, q1, kh, vv, yo, dch, decT, st_f, st_b, scm, mask)
        self._cbufs_at = id(self.P.ops["pe"])
        self._cbufs_phase = self._phase_id
        self._mask_loaded = False
        return self._cbufs

    def phase_post(self, l, I, yT, ck, gate, bonus, obuf):
        P = self.P
        cfg = self.cfg
        self.carve_reset()
        o = self.carve(2048)
        y = self.carve(512)
        y65 = self.carve(520).rearrange("p (h e) -> p h e", e=65)
        gt = self.carve(512)
        W = [self.carve(512) for _ in range(3)]
        self.vt = self.carve(512).rearrange("p (a b) -> p a b", b=128)
        sv = {nm: self.carve(512) for nm in ("hgrn_norm_w", "mlstm_norm_w", "rwkv_ln_w", "rwkv_ln_b")}
        small = self.carve(64)
        for nm in sv:
            self.bcast_load(sv[nm], I[nm][l], "c_" + nm)
        h8 = lambda ap: ap.rearrange("p (h d) -> p h d", d=64)
        b8 = lambda ap8: ap8.unsqueeze(2).to_broadcast([128, 8, 64])
        wk = lambda i: ("W", i)
        for tile in range(cfg.NTILE):
            s, g = tile // cfg.TPS, tile % cfg.TPS
            rs = slice(tile * 128, (tile + 1) * 128)
            for mi, m in enumerate("ABCD"):
                if m == "D":
                    P.dma("sp", y, ck["D"]["y"][rs, :], reads=(("cy", "D"),), writes=("y",))
                else:
                    P.dma("sp", y65.rearrange("p h e -> p (h e)"), ck["y"][m][rs, :], reads=(("cy", m),), writes=("y65",))
                    self.cp(h8(y), y65[:, :, 0:64], ("y65",), ("y",), eng="dve")
                P.dma("sp", gt, gate[m][rs, :], reads=(("gate", m, tile),), writes=("gt",))
                osl = o[:, mi * 512:(mi + 1) * 512]
                if m == "C":
                    d8 = y65[:, :, 64]
                    self.ts(small[:, 0:8], d8, -1.0, None, ALU.mult, None, ("y65",), ("sm0",))
                    self.tt(small[:, 0:8], small[:, 0:8], d8, ALU.max, ("sm0", "y65"), ("sm0",))
                    self.ts(small[:, 0:8], small[:, 0:8], 1.0, None, ALU.max, None, ("sm0",), ("sm0",))
                    P.op("dve", lambda e: e.reciprocal(out=small[:, 8:16], in_=small[:, 0:8]), reads=("sm0",), writes=("sm1",))
                    self.tt(h8(y), h8(y), b8(small[:, 8:16]), ALU.mult, ("y", "sm1"), ("y",))
                if m in "ABC":
                    self.tt(W[0], y, y, ALU.mult, ("y",), (wk(0),))
                    self.red(small[:, 16:24], h8(W[0]), (wk(0),), ("sm2",))
                    self.actf(small[:, 24:32], small[:, 16:24], AF.Sqrt, ("sm2", "eps"), ("sm3",), scale=1.0 / 64,
                              bias=self.eps_t[:, 0:1])
                    P.op("dve", lambda e: e.reciprocal(out=small[:, 32:40], in_=small[:, 24:32]), reads=("sm3",), writes=("sm4",))
                    self.tt(h8(y), h8(y), b8(small[:, 32:40]), ALU.mult, ("y", "sm4"), ("y",))
                    if m == "B":
                        self.tt(y, y, sv["hgrn_norm_w"], ALU.mult, ("y", "c_hgrn_norm_w"), ("y",))
                    if m == "C":
                        self.tt(y, y, sv["mlstm_norm_w"], ALU.mult, ("y", "c_mlstm_norm_w"), ("y",))
                    self.tt(osl, y, gt, ALU.mult, ("y", "gt"), ("o",))
                else:
                    self.red(small[:, 16:24], h8(y), ("y",), ("sm2",))
                    self.ts(small[:, 16:24], small[:, 16:24], -1.0 / 64, None, ALU.mult, None, ("sm2",), ("sm2",))
                    self.tt(h8(y), h8(y), b8(small[:, 16:24]), ALU.add, ("y", "sm2"), ("y",))
                    self.tt(W[0], y, y, ALU.mult, ("y",), (wk(0),))
                    self.red(small[:, 24:32], h8(W[0]), (wk(0),), ("sm3",))
                    self.actf(small[:, 32:40], small[:, 24:32], AF.Sqrt, ("sm3", "eps"), ("sm4",), scale=1.0 / 64,
                              bias=self.eps_t[:, 1:2])
                    P.op("dve", lambda e: e.reciprocal(out=small[:, 40:48], in_=small[:, 32:40]), reads=("sm4",), writes=("sm5",))
                    self.tt(h8(y), h8(y), b8(small[:, 40:48]), ALU.mult, ("y", "sm5"), ("y",))
                    self.tt(y, y, sv["rwkv_ln_w"], ALU.mult, ("y", "c_rwkv_ln_w"), ("y",))
                    self.tt(y, y, sv["rwkv_ln_b"], ALU.add, ("y", "c_rwkv_ln_b"), ("y",))
                    P.dma("sp", W[1], bonus[rs, :], reads=(("bonus", tile),), writes=(wk(1),))
                    self.tt(y, y, W[1], ALU.add, ("y", wk(1)), ("y",))
                    self.tt(osl, y, gt, ALU.mult, ("y", "gt"), ("o",))
            P.dma("sp", obuf[rs, :], o, reads=("o",), writes=(("o", tile),))

    def phase_ffn(self, l, hbuf, obuf, wbo, wbg, wbu, wbd, wnb):
        P = self.P
        cfg = self.cfg
        NG = 4
        self.carve_linear(NG)
        actT = self.carve(44 * 256, BF16).rearrange("p (k g t) -> p k g t", g=NG, t=128)
        acc = [self.carve(2048) for _ in range(NG)]
        xT = self.xT
        for grp in range(cfg.NTILE // NG):
            for g in range(NG):
                tile = grp * NG + g
                ht = self.htile[tile % 2]
                P.dma("sp", ht, obuf[tile * 128:(tile + 1) * 128, :], reads=(("o", tile),), writes=(("htile", tile % 2),))
                self.cp(self.ub[:, 0:2048], ht, (("htile", tile % 2),), ("ub",), eng="dve")
                self.transpose_to(self.ub, "ub", xT, g, "xT", 16)

            def consume(g, cb, nc_, pst, pskey):
                self.cp(acc[g][:, cb * 512:cb * 512 + nc_], pst, (pskey,), (("acc", g),))

            self.linear_tm(xT, ("xT",), NG, 16, wbo, D, consume)
            self.resid_update(grp, NG, acc, hbuf, wnb[:, 1, :])
            for g in range(NG):
                tile = grp * NG + g
                ht = self.htile[tile % 2]
                P.dma("sp", ht, hbuf[tile * 128:(tile + 1) * 128, :], reads=(("h", tile),), writes=(("htile", tile % 2),))
                self.norm_transpose(ht, ("htile", tile % 2), wnb[:, 2, :], xT, g, "xT")
            for cb in range(DFF // 512):
                wg = self.wbuf[0].rearrange("p (k n) -> p k n", n=512)
                wu = self.wbuf[1].rearrange("p (k n) -> p k n", n=512)
                P.dma("sp", wg, wbg[cb, :, :, :], reads=(("wb", id(wbg), cb),), writes=(("wbuf", 0),))
                P.dma("sp", wu, wbu[cb, :, :, :], reads=(("wb", id(wbu), cb),), writes=(("wbuf", 1),))
                for j in range(4):
                    nchunk = cb * 4 + j
                    bg = self.psbank()
                    bu = self.psbank()
                    pg = self.pslin[:, bg, :]
                    pu = self.pslin[:, bu, :]
                    for kc in range(16):
                        P.op("pe", lambda e, pg=pg, kc=kc, j=j: e.matmul(pg, lhsT=wg[:, kc, j * 128:(j + 1) * 128],
                                                                         rhs=xT[:, kc, :, :].rearrange("p g t -> p (g t)"),
                                                                         start=(kc == 0), stop=(kc == 15)),
                             reads=("xT", ("wbuf", 0)), writes=(("pslin", bg),))
                    for kc in range(16):
                        P.op("pe", lambda e, pu=pu, kc=kc, j=j: e.matmul(pu, lhsT=wu[:, kc, j * 128:(j + 1) * 128],
                                                                         rhs=xT[:, kc, :, :].rearrange("p g t -> p (g t)"),
                                                                         start=(kc == 0), stop=(kc == 15)),
                             reads=("xT", ("wbuf", 1)), writes=(("pslin", bu),))
                    st = self.stage[self.ssel % 4]
                    skey = ("stage", self.ssel % 4)
                    self.ssel += 1
                    self.actf(st, pg, AF.Silu, (("pslin", bg),), (skey,))
                    self.tt(actT[:, nchunk, :, :].rearrange("p g t -> p (g t)"), st, pu, ALU.mult, (skey, ("pslin", bu)),
                            ("actT",))

            def consume2(g, cb, nc_, pst, pskey):
                self.cp(acc[g][:, cb * 128:cb * 128 + nc_], pst, (pskey,), (("acc", g),))

            self.linear_tm(actT, ("actT",), NG, 44, wbd, D, consume2, CB=128)
            self.resid_update(grp, NG, acc, hbuf, wnb[:, 3, :])

    def resid_update(self, grp, NG, acc, hbuf, wn):
        P = self.P
        for g in range(NG):
            tile = grp * NG + g
            ht = self.htile[tile % 2]
            hk = ("htile", tile % 2)
            P.dma("sp", ht, hbuf[tile * 128:(tile + 1) * 128, :], reads=(("h", tile),), writes=(hk,))
            self.rstd_of(acc[g], ("acc", g), 2048, self.eps_t[:, 0:1])
            self.stt(acc[g], acc[g], self.ss_t[:, 2:3], wn, ALU.mult, ALU.mult, (("acc", g), "ss2", "wnb"), (("acc", g),))
            self.tt(ht, ht, acc[g], ALU.add, (hk, ("acc", g)), (hk,))
            P.dma("sp", hbuf[tile * 128:(tile + 1) * 128, :], ht, reads=(hk,), writes=(("h", tile),))


def make_consts(T):
    half = 32
    inv_freq = 10000.0 ** (-np.arange(half, dtype=np.float32) / half)
    ang = np.arange(T, dtype=np.float32)[:, None] * inv_freq[None, :]
    cossin = np.concatenate([np.cos(ang), np.sin(ang)], axis=1).astype(np.float32)
    sel = np.zeros((32, TB, 128), np.float32)
    for tl in range(TB):
        for m in range(128):
            sel[(m // 64) * TB + tl, tl, m] = 1.0
    gam = np.zeros((128, 2, 4, 64), np.float32)
    for p in range(128):
        for hp in range(4):
            h = 2 * hp + p // 64
            gam[p, :, hp, :] = 1.0 - 2.0 ** (-5.0 - h)
    C = CHK["A"]
    j = (np.arange(128) % C).astype(np.float64)
    gh = 1.0 - 2.0 ** (-5.0 - np.arange(8, dtype=np.float64))
    atab = np.zeros((4, 128, 8, 64), np.float64)
    atab[0] = (gh[None, :] ** (j[:, None] + 1.0))[:, :, None]
    atab[1] = (0.125 * gh[None, :] ** (-(j[:, None] + 1.0)))[:, :, None]
    atab[2] = (0.125 * gh[None, :] ** (C - 1.0 - j[:, None]))[:, :, None]
    atab[3] = (gh ** float(C))[None, :, None]
    idx = np.arange(128)
    def trib(c):
        same = (idx[:, None] // c) == (idx[None, :] // c)
        return (same & (idx[:, None] <= idx[None, :])).astype(np.float32), same.astype(np.float32)
    t64, b64 = trib(64)
    t32, b32 = trib(32)
    trimats = np.stack([t64, b64, t32, b32]).astype(np.float32)
    cmask = (np.arange(64)[:, None] <= np.arange(64)[None, :]).astype(np.float32)
    i32 = np.arange(64) % 32
    dmasks = np.stack([(i32[:, None] < i32[None, :]), (i32[:, None] > i32[None, :]), (i32[:, None] <= i32[None, :])]).astype(
        np.float32)
    return dict(cossin=cossin, sel=sel, gamA=gam.reshape(128, 512), atab=atab.reshape(4, 128, 512).astype(np.float32),
                trimats=trimats, cmask=cmask, dmasks=dmasks,
                ident_b=np.eye(128).astype(ml_dtypes.bfloat16), ident_f=np.eye(128, dtype=np.float32))


def make_shared(inp, L, T):
    f = lambda a: np.ascontiguousarray(np.asarray(a, dtype=np.float32))
    sh = dict(w_in=f(inp["w_in"]), w_out=f(inp["w_out"]), w_ffn_gate=f(inp["w_ffn_gate"]), w_ffn_up=f(inp["w_ffn_up"]),
              w_ffn_down=f(inp["w_ffn_down"]))
    sh["norms"] = np.ascontiguousarray(np.stack([f(inp["norm_pre_mix"]), f(inp["norm_post_mix"]), f(inp["norm_pre_ffn"]),
                                                 f(inp["norm_post_ffn"])], axis=1))
    for nm in SMALL + ["hgrn_lb_logits", "mlstm_conv_w", "mlstm_conv_b", "rwkv_mu"]:
        sh[nm] = f(inp[nm])
    sh["mlstm_if_bias"] = np.ascontiguousarray(np.concatenate([f(inp["mlstm_i_bias"]), f(inp["mlstm_f_bias"])], axis=1))
    up = np.zeros((L, 128, 1024), np.float32)
    up[:, 0:64, 0:512] = f(inp["rwkv_w_up"])
    up[:, 64:128, 512:1024] = f(inp["rwkv_a_up"])
    sh["rwkv_up_pad"] = up
    sh["rwkv_g_up"] = f(inp["rwkv_g_up"])
    if L > 1:
        sh["rwkv_v0"] = f(inp["rwkv_v0"])
        sh["rwkv_v_down"] = f(inp["rwkv_v_down"])
        sh["rwkv_v_up"] = f(inp["rwkv_v_up"])
    else:
        sh["rwkv_v0"] = np.zeros((1, G), np.float32)
        sh["rwkv_v_down"] = np.zeros((1, G, 32), np.float32)
        sh["rwkv_v_up"] = np.zeros((1, 32, G), np.float32)
    sh.update(make_consts(T))
    return sh


_CACHE = {}


def kernel(**inp):
    x = np.asarray(inp["x"], dtype=np.float32)
    B, T, _ = x.shape
    L = inp["w_in"].shape[0]
    ncores = 8
    nseq = B // ncores
    cfg = Cfg(T=T, NSEQ=nseq, L=L)
    nc = Builder(cfg).build()
    sh = make_shared(inp, L, T)
    in_maps = []
    for c in range(ncores):
        m = dict(sh)
        m["x"] = np.ascontiguousarray(x[c * nseq:(c + 1) * nseq].reshape(nseq * T, D))
        in_maps.append(m)
    res = run_bass_kernel_spmd(nc, in_maps, core_ids=list(range(ncores)))
    outs = [np.asarray(r["out"]).reshape(nseq, T, D) for r in res.results]
    return np.concatenate(outs, axis=0).astype(np.float32)
```

```python
from contextlib import ExitStack
import numpy as np
import ml_dtypes
import concourse.bass as bass
import concourse.mybir as mybir
from concourse.bass_utils import run_bass_kernel_spmd

F32 = mybir.dt.float32
BF16 = mybir.dt.bfloat16
ALU = mybir.AluOpType
AF = mybir.ActivationFunctionType
AX = mybir.AxisListType

D = 2048
NIN = 7952
DFF = 5632
G = 512
NH = 8
HD = 64
NORM_EPS = 1e-6


class Prog:
    ENG = ("pe", "dve", "act", "pool", "sp")
    NDMASEM = 6

    def __init__(self, nc, stack):
        self.nc = nc
        self.stack = stack
        self.ops = {e: [] for e in self.ENG}
        self.cnt = {e: 0 for e in self.ENG}
        self.known = {e: {} for e in self.ENG}
        self.sem = {e: stack.enter_context(nc.semaphore("s_" + e)) for e in self.ENG if e != "sp"}
        self.semname = {}
        for e, s in self.sem.items():
            self.semname[id(s)] = e
        self.dq = {}
        for q in ("sp", "pool", "act"):
            self.dq[q] = {"sems": [stack.enter_context(nc.semaphore(f"d_{q}{i}")) for i in range(self.NDMASEM)],
                          "n": 0}
        self.bufs = {}
        self.nops = 0

    def _buf(self, k):
        b = self.bufs.get(k)
        if b is None:
            b = [None, {}]
            self.bufs[k] = b
        return b

    def _deps(self, e, reads, writes, extra=()):
        toks = list(extra)
        for k in reads:
            b = self._buf(k)
            if b[0] is not None:
                toks.append((b[0], False))
        for k in writes:
            b = self._buf(k)
            if b[0] is not None:
                toks.append((b[0], False))
            for s, v in b[1].values():
                toks.append(((s, v), True))
        own = self.sem.get(e)
        waits = []
        kn = self.known[e]
        for (s, v), is_reader in toks:
            if s is own and (e == "pe" or is_reader):
                continue
            if kn.get(id(s), 0) >= v:
                continue
            kn[id(s)] = v
            waits.append((s, v))
        return waits

    def _commit(self, tok, reads, writes):
        s, v = tok
        for k in reads:
            b = self._buf(k)
            b[1][id(s)] = (s, v)
        for k in writes:
            b = self._buf(k)
            b[0] = tok
            b[1] = {}

    def op(self, e, fn, reads=(), writes=()):
        reads = tuple(reads)
        writes = tuple(writes)
        waits = self._deps(e, reads, writes)
        self.cnt[e] += 1
        tok = (self.sem[e], self.cnt[e])
        self.ops[e].append((waits, fn, (self.sem[e], 1)))
        self._commit(tok, reads, writes)
        self.nops += 1
        return tok

    def dma(self, q, out, in_, reads=(), writes=(), **kw):
        reads = tuple(reads)
        writes = tuple(writes)
        dq = self.dq[q]
        n = dq["n"]
        dq["n"] += 1
        s = dq["sems"][n % self.NDMASEM]
        prev = 16 * (n // self.NDMASEM)
        extra = [((s, prev), False)] if prev > 0 else []
        waits = self._deps(q, reads, writes, extra)
        tok = (s, prev + 16)
        self.ops[q].append((waits, lambda eng: eng.dma_start(out=out, in_=in_, **kw), (s, 16)))
        self._commit(tok, reads, writes)
        self.nops += 1
        return tok

    def barrier(self):
        toks = [(self.sem[e], self.cnt[e]) for e in self.sem if self.cnt[e] > 0] + self.all_dma_tokens(skip=("pool",))
        for e in self.ENG:
            waits = []
            kn = self.known[e]
            for s, v in toks:
                if e == "pe" and s is self.sem.get("pe"):
                    continue
                if kn.get(id(s), 0) >= v:
                    continue
                kn[id(s)] = v
                waits.append((s, v))
            if waits:
                self.ops[e].append((waits, None, None))
        self.bufs = {k: v for k, v in self.bufs.items() if isinstance(k, tuple) and k and k[0] == "wb"}

    def finish(self, e, toks):
        waits = []
        for s, v in toks:
            waits.append((s, v))
        self.ops[e].append((waits, None, None))

    def all_dma_tokens(self, skip=()):
        toks = []
        for q, dq in self.dq.items():
            if q in skip:
                continue
            n = dq["n"]
            for i in range(self.NDMASEM):
                c = (n - i + self.NDMASEM - 1) // self.NDMASEM if n > i else 0
                if c > 0:
                    toks.append((dq["sems"][i], 16 * c))
        return toks

    def emit(self):
        nc = self.nc
        ops = self.ops

        def run(eng, lst):
            for waits, fn, inc in lst:
                for s, v in waits:
                    eng.wait_ge(s, v)
                if fn is not None:
                    ins = fn(eng)
                    ins.then_inc(inc[0], inc[1])

        with nc.Block() as block:
            @block.tensor
            def _(eng):
                run(eng, ops["pe"])

            @block.vector
            def _(eng):
                run(eng, ops["dve"])

            @block.scalar
            def _(eng):
                run(eng, ops["act"])

            @block.gpsimd
            def _(eng):
                run(eng, ops["pool"])

            @block.sync
            def _(eng):
                run(eng, ops["sp"])


RWC = 1792
OFFS = {"A": 0, "B": 2048, "C": 4096, "D": 6160}
VECS = {"D": ["rD", "kD", "wD", "kkD", "bD"]}
ALLV = VECS["D"]
CHK = {"A": 64, "B": 32, "C": 64}
TB = 16
SMALL = ["hgrn_norm_w", "mlstm_norm_w", "rwkv_w0", "rwkv_a0", "rwkv_k_k", "rwkv_k_a", "rwkv_r_k", "rwkv_ln_w",
         "rwkv_ln_b"]


class Cfg:
    def __init__(self, T=2048, NSEQ=2, L=4):
        self.T = T
        self.NSEQ = NSEQ
        self.L = L
        self.NT = T * NSEQ
        self.TPS = T // 128
        self.NTILE = self.NT // 128


def wb_shape(K, N, CB=512):
    ncb = (N + CB - 1) // CB
    return [ncb, 128, K // 128, CB]


class Builder:
    def __init__(self, cfg):
        self.cfg = cfg
        self.nc = bass.Bass("TRN2", target_bir_lowering=False)
        self.stack = ExitStack()
        self.P = None

    def dram_in(self, name, shape, dt=F32):
        return self.nc.dram_tensor(name, list(shape), dt, kind="ExternalInput").ap()

    def dram_out(self, name, shape, dt=F32):
        return self.nc.dram_tensor(name, list(shape), dt, kind="ExternalOutput").ap()

    def dram_tmp(self, name, shape, dt=F32):
        return self.nc.dram_tensor(name, list(shape), dt, kind="Internal").ap()

    def sb(self, name, shape, dt=F32):
        h = self.stack.enter_context(self.nc.sbuf_tensor(name, list(shape), dt))
        return h[tuple(slice(None) for _ in shape)]

    def ps(self, name, shape, dt=F32):
        return self.stack.enter_context(self.nc.psum_tensor(name, list(shape), dt))

    def carve_reset(self):
        self.cptr = 0

    def carve(self, ncols, dt=F32):
        a = self.arena[:, self.cptr:self.cptr + ncols]
        self.cptr += ncols
        assert self.cptr <= self.NARENA, self.cptr
        if dt == BF16:
            a = a.bitcast(BF16)
        return a

    def tt(self, out, a, b, op, r, w, eng="dve"):
        self.P.op(eng, lambda e: e.tensor_tensor(out=out, in0=a, in1=b, op=op), reads=r, writes=w)

    def ts(self, out, a, s1, s2, op0, op1, r, w, eng="dve"):
        if op1 is None:
            self.P.op(eng, lambda e: e.tensor_scalar(out=out, in0=a, scalar1=s1, scalar2=None, op0=op0), reads=r, writes=w)
        else:
            self.P.op(eng, lambda e: e.tensor_scalar(out=out, in0=a, scalar1=s1, scalar2=s2, op0=op0, op1=op1),
                      reads=r, writes=w)

    def stt(self, out, a, scalar, b, op0, op1, r, w):
        self.P.op("dve", lambda e: e.scalar_tensor_tensor(out=out, in0=a, scalar=scalar, in1=b, op0=op0, op1=op1),
                  reads=r, writes=w)

    def actf(self, out, a, func, r, w, scale=1.0, bias=None):
        if bias is None:
            self.P.op("act", lambda e: e.activation(out=out, in_=a, func=func, scale=scale), reads=r, writes=w)
        else:
            self.P.op("act", lambda e: e.activation(out=out, in_=a, func=func, scale=scale, bias=bias), reads=r, writes=w)

    def red(self, out, a, r, w):
        self.P.op("dve", lambda e: e.tensor_reduce(out=out, in_=a, axis=AX.X, op=ALU.add), reads=r, writes=w)

    def cp(self, out, a, r, w, eng="act"):
        if eng == "act":
            self.P.op("act", lambda e: e.copy(out=out, in_=a), reads=r, writes=w)
        else:
            self.P.op(eng, lambda e: e.tensor_copy(out=out, in_=a), reads=r, writes=w)

    def psbank(self):
        b = self.psel % self.NPS
        self.psel += 1
        return b

    def cast_weight(self, W, Wb, K, N, CB=512):
        P = self.P
        KC = K // 128
        ncb = (N + CB - 1) // CB
        Wv = W.rearrange("(kc p) n -> p kc n", p=128)
        for cb in range(ncb):
            c0 = cb * CB
            nc_ = min(CB, N - c0)
            for k0 in range(0, KC, 16):
                k1 = min(KC, k0 + 16)
                P.dma("pool", Wb[cb, :, k0:k1, 0:nc_], Wv[:, k0:k1, c0:c0 + nc_], reads=(), writes=(("wb", id(Wb), cb),))

    def linear_tm(self, xT, xkeys, ng, KC, Wb, N, consume, CB=512):
        P = self.P
        ncb = (N + CB - 1) // CB
        for cb in range(ncb):
            nc_ = min(CB, N - cb * CB)
            b = self.wsel % 2
            self.wsel += 1
            wbuf = self.wbuf[b][:, 0:KC * CB].rearrange("p (k n) -> p k n", n=CB)
            P.dma("sp", wbuf[:, :, 0:nc_], Wb[cb, :, 0:KC, 0:nc_], reads=(("wb", id(Wb), cb),), writes=(("wbuf", b),))
            for g in range(ng):
                bank = self.psbank()
                pst = self.pslin[:, bank, 0:nc_]
                for kc in range(KC):
                    P.op("pe", (lambda e, pst=pst, g=g, kc=kc, wbuf=wbuf, nc_=nc_:
                                e.matmul(pst, lhsT=xT[:, kc, g, :], rhs=wbuf[:, kc, 0:nc_],
                                         start=(kc == 0), stop=(kc == KC - 1))),
                         reads=tuple(xkeys) + (("wbuf", b),), writes=(("pslin", bank),))
                consume(g, cb, nc_, pst, ("pslin", bank))

    def rstd_of(self, src, src_key, n, eps_ap):
        P = self.P
        ss = self.ss_t
        junk = self.junk
        P.op("act", lambda e: e.activation(out=junk[:, 0:n], in_=src, func=AF.Square, accum_out=ss[:, 0:1]),
             reads=(src_key,), writes=("junk", "ss"))
        P.op("act", lambda e: e.activation(out=ss[:, 1:2], in_=ss[:, 0:1], func=AF.Sqrt, scale=1.0 / n, bias=eps_ap),
             reads=("ss", "eps"), writes=("ss1",))
        P.op("dve", lambda e: e.reciprocal(out=ss[:, 2:3], in_=ss[:, 1:2]), reads=("ss1",), writes=("ss2",))

    def norm_transpose(self, src_tile, src_key, wn_b, xT, g, xkey):
        self.rstd_of(src_tile, src_key, 2048, self.eps_t[:, 0:1])
        ub = self.ub
        self.stt(ub[:, 0:2048], src_tile, self.ss_t[:, 2:3], wn_b, ALU.mult, ALU.mult, (src_key, "ss2", "wnb"), ("ub",))
        self.transpose_to(ub, "ub", xT, g, xkey, 16)

    def transpose_to(self, ub, ubkey, xT, g, xkey, KC):
        P = self.P
        for k0 in range(0, KC, 4):
            k1 = min(KC, k0 + 4)
            bank = self.tsel % 2
            self.tsel += 1
            pt = self.pstr[:, bank, :]
            for kc in range(k0, k1):
                P.op("pe", (lambda e, kc=kc, pt=pt, k0=k0:
                            e.transpose(pt[:, (kc - k0) * 128:(kc - k0 + 1) * 128], ub[:, kc * 128:(kc + 1) * 128],
                                        self.ident_b[:, :])),
                     reads=(ubkey, "ident"), writes=(("pstr", bank),))
            n = k1 - k0
            P.op("act", (lambda e, pt=pt, k0=k0, n=n:
                         e.copy(out=xT[:, k0:k0 + n, g, :], in_=pt[:, 0:n * 128].rearrange("p (k t) -> p k t", t=128))),
                 reads=(("pstr", bank),), writes=(xkey,))

    def store_T(self, src, src_key, dstT, s, g):
        P = self.P
        vt = self.vt
        for hp in range(4):
            bank = self.psbank()
            pt = self.pslin[:, bank, 0:128]
            P.op("pe", lambda e, pt=pt, hp=hp: e.transpose(pt, src[:, hp * 128:(hp + 1) * 128], self.ident_f[:, :]),
                 reads=(src_key, "identf"), writes=(("pslin", bank),))
            self.cp(vt[:, hp, :], pt, (("pslin", bank),), ("vt",))
        P.dma("sp", dstT[:, :, s, g * 128:(g + 1) * 128].rearrange("hp p t -> p hp t"), vt[:, :, :], reads=("vt",),
              writes=(("T", id(dstT), s, g),))

    def bt_store(self, src_bf, src_key, dstT, s, g):
        P = self.P
        bank = self.tsel % 2
        self.tsel += 1
        pt = self.pstr[:, bank, :]
        for hp in range(4):
            P.op("pe", lambda e, hp=hp, pt=pt: e.transpose(pt[:, hp * 128:(hp + 1) * 128], src_bf[:, hp * 128:(hp + 1) * 128],
                                                          self.ident_b[:, :]),
                 reads=(src_key, "ident"), writes=(("pstr", bank),))
        tb = self.tbt
        P.op("act", lambda e, pt=pt: e.copy(out=tb[:, :, :], in_=pt[:, 0:512].rearrange("p (k t) -> p k t", t=128)),
             reads=(("pstr", bank),), writes=("tbt",))
        P.dma("sp", dstT[:, :, s, g * 128:(g + 1) * 128].rearrange("hp p t -> p hp t"), tb[:, :, :], reads=("tbt",),
              writes=(("T", id(dstT), s, g),))

    def store_v(self, v_src, v_key, v_d, rs):
        self.cp(self.vb[:, :, 0:64], v_src.rearrange("p (h e) -> p h e", e=64), (v_key,), ("vb",), eng="dve")
        self.P.dma("sp", v_d[rs, :], self.vb.rearrange("p h e -> p (h e)"), reads=("vb",), writes=(("vd", id(v_d)),))

    def load_T(self, dst, dst_key, srcT, s, g):
        P = self.P
        vt = self.vt
        P.dma("sp", vt[:, :, :], srcT[:, :, s, g * 128:(g + 1) * 128].rearrange("hp p t -> p hp t"),
              reads=(("T", id(srcT), s, g),), writes=("vt",))
        for hp in range(4):
            bank = self.psbank()
            pt = self.pslin[:, bank, 0:128]
            P.op("pe", lambda e, pt=pt, hp=hp: e.transpose(pt, vt[:, hp, :], self.ident_f[:, :]),
                 reads=("vt", "identf"), writes=(("pslin", bank),))
            self.cp(dst[:, hp * 128:(hp + 1) * 128], pt, (("pslin", bank),), (dst_key,))

    def load_shift(self, dst, key, c0, n, tile, k, extra_keys=()):
        P = self.P
        keys = (key,) + tuple(extra_keys)
        cfg = self.cfg
        g = tile % cfg.TPS
        r0 = tile * 128
        if g == 0:
            P.op("dve", lambda e: e.memset(dst[:, 0:n], 0.0), writes=keys)
            P.dma("sp", dst[k:128, 0:n], self.pbuf[r0:r0 + 128 - k, c0:c0 + n],
                  reads=tuple(("p", tile, cb) for cb in range(16)), writes=keys)
        else:
            P.dma("sp", dst[:, 0:n], self.pbuf[r0 - k:r0 - k + 128, c0:c0 + n],
                  reads=tuple(("p", tile, cb) for cb in range(16)) + tuple(("p", tile - 1, cb) for cb in range(16)),
                  writes=keys)

    def bcast_load(self, dst, src_row, key):
        self.P.dma("sp", dst, src_row.partition_broadcast(128), writes=(key,))

    def build(self, dbg=None, layers=None):
        cfg = self.cfg
        nc = self.nc
        L = cfg.L
        NT = cfg.NT
        T = cfg.T
        NSEQ = cfg.NSEQ
        self.P = P = Prog(nc, self.stack)
        layers = list(range(L)) if layers is None else layers
        I = {}
        x = self.dram_in("x", [NT, D])
        out = self.dram_out("out", [NT, D])
        w_in = self.dram_in("w_in", [L, D, NIN])
        w_out = self.dram_in("w_out", [L, D, D])
        w_g = self.dram_in("w_ffn_gate", [L, D, DFF])
        w_u = self.dram_in("w_ffn_up", [L, D, DFF])
        w_d = self.dram_in("w_ffn_down", [L, DFF, D])
        norms = self.dram_in("norms", [L, 4, D])
        for nm in SMALL:
            I[nm] = self.dram_in(nm, [L, G])
        I["hgrn_lb_logits"] = self.dram_in("hgrn_lb_logits", [L, G])
        I["mlstm_conv_w"] = self.dram_in("mlstm_conv_w", [L, 4, 2 * G])
        I["mlstm_conv_b"] = self.dram_in("mlstm_conv_b", [L, 2 * G])
        I["mlstm_if_bias"] = self.dram_in("mlstm_if_bias", [L, 16])
        I["rwkv_mu"] = self.dram_in("rwkv_mu", [L, RWC])
        I["rwkv_up_pad"] = self.dram_in("rwkv_up_pad", [L, 128, 1024])
        I["rwkv_g_up"] = self.dram_in("rwkv_g_up", [L, 128, G])
        I["rwkv_v0"] = self.dram_in("rwkv_v0", [max(L - 1, 1), G])
        I["rwkv_v_down"] = self.dram_in("rwkv_v_down", [max(L - 1, 1), G, 32])
        I["rwkv_v_up"] = self.dram_in("rwkv_v_up", [max(L - 1, 1), 32, G])
        ident_b_d = self.dram_in("ident_b", [128, 128], BF16)
        ident_f_d = self.dram_in("ident_f", [128, 128])
        cs_d = self.dram_in("cossin", [T, 64])
        sel_d = self.dram_in("sel", [32, TB, 128])
        gam_d = self.dram_in("gamA", [128, 512])
        atab_d = self.dram_in("atab", [4, 128, 512])
        tri_d = self.dram_in("trimats", [4, 128, 128])
        mask_d = self.dram_in("cmask", [64, 64])

        hbuf = self.dram_tmp("hbuf", [NT, D])
        self.pbuf = pbuf = self.dram_tmp("pbuf", [NT, NIN])
        obuf = self.dram_tmp("obuf", [NT, D])
        rows = {v: self.dram_tmp("row_" + v, [NT, G]) for v in ALLV}
        vT = {m: self.dram_tmp("vT_" + m, [4, 128, NSEQ, T]) for m in "D"}
        yT = {m: self.dram_tmp("yT_" + m, [4, 128, NSEQ, T]) for m in "D"}
        ck = dict(
            qT={m: self.dram_tmp("cqT_" + m, [4, 128, NSEQ, T], BF16) for m in "ABC"},
            kT={m: self.dram_tmp("ckT_" + m, [4, 128, NSEQ, T], BF16) for m in "ABC"},
            kh={m: self.dram_tmp("ckh_" + m, [NT, G], BF16) for m in "ABC"},
            v={m: self.dram_tmp("cv_" + m, [NT, 520], BF16) for m in "ABC"},
            dec={m: self.dram_tmp("cdec_" + m, [NT, G]) for m in "ABC"},
            y={m: self.dram_tmp("cy_" + m, [NT, 520]) for m in "ABC"},
            atab=atab_d, tri=tri_d, mask=mask_d)
        dmask_d = self.dram_in("dmasks", [3, 64, 64])
        dk = dict(rT=self.dram_tmp("d_rT", [4, 128, NSEQ, T], BF16), cT=self.dram_tmp("d_cT", [4, 128, NSEQ, T], BF16),
                  kT=self.dram_tmp("d_kT", [4, 128, NSEQ, T], BF16), bT=self.dram_tmp("d_bT", [4, 128, NSEQ, T], BF16),
                  kh=self.dram_tmp("d_kh", [NT, G], BF16), bh=self.dram_tmp("d_bh", [NT, G], BF16),
                  v=self.dram_tmp("d_v", [NT, G], BF16), dec=self.dram_tmp("d_dec", [NT, G]),
                  y=self.dram_tmp("d_y", [NT, G]), masks=dmask_d)
        ck["D"] = dk
        gate = {m: self.dram_tmp("gate_" + m, [NT, G]) for m in "ABCD"}
        bonus = self.dram_tmp("bonus", [NT, G])
        vfirst = self.dram_tmp("vfirst", [NT, G])
        wb_in = [self.dram_tmp(f"wb_in{l}", wb_shape(D, NIN), BF16) for l in range(L)]
        wb_out = [self.dram_tmp(f"wb_out{l}", wb_shape(D, D), BF16) for l in range(L)]
        wb_g = [self.dram_tmp(f"wb_g{l}", wb_shape(D, DFF), BF16) for l in range(L)]
        wb_u = [self.dram_tmp(f"wb_u{l}", wb_shape(D, DFF), BF16) for l in range(L)]
        wb_d = [self.dram_tmp(f"wb_d{l}", wb_shape(DFF, D, 128), BF16) for l in range(L)]
        dbg_t = None
        if dbg is not None and dbg[1] is not None:
            dbg_t = self.dram_out("dbg", dbg[1])

        self.ident_b = self.sb("ident_b_s", [128, 128], BF16)
        self.ident_f = self.sb("ident_f_s", [128, 128])
        self.ss_t = self.sb("ss", [128, 8])
        self.eps_t = self.sb("eps_t", [128, 4])
        gamA = self.sb("gamA_s", [128, 512])
        lbd = self.dram_tmp("lbd", [2, 128, L, G])
        wnb = self.sb("wnb", [128, 4, D])
        self.NARENA = 41000
        self.arena = self.sb("arena", [128, self.NARENA])
        self.NPS = 6
        self.pslin = self.ps("pslin", [128, self.NPS, 512])
        self.psel = 0
        self.pstr = self.ps("pstr", [128, 2, 1024], BF16)
        self.tsel = 0
        self.wsel = 0

        P.dma("sp", self.ident_b[:, :], ident_b_d[:, :], writes=("ident",))
        P.dma("sp", self.ident_f[:, :], ident_f_d[:, :], writes=("identf",))
        P.dma("sp", gamA[:, :], gam_d[:, :], writes=("gamA",))
        P.op("dve", lambda e: e.memset(self.eps_t[:, 0:1], NORM_EPS), writes=("eps",))
        P.op("dve", lambda e: e.memset(self.eps_t[:, 1:2], 64e-5), writes=("eps",))
        P.op("dve", lambda e: e.memset(self.eps_t[:, 2:3], 0.0), writes=("eps",))
        for i in range(0, NT, 512):
            P.dma("sp", hbuf[i:i + 512, :], x[i:i + 512, :], writes=tuple(("h", j) for j in range(i // 128, i // 128 + 4)))
        for l in layers:
            self.cast_weight(w_in[l], wb_in[l], D, NIN)
            self.cast_weight(w_out[l], wb_out[l], D, D)
            self.cast_weight(w_g[l], wb_g[l], D, DFF)
            self.cast_weight(w_u[l], wb_u[l], D, DFF)
            self.cast_weight(w_d[l], wb_d[l], DFF, D, 128)

        self.carve_reset()
        ex = self.carve(L * G).rearrange("p (l g) -> p l g", g=G)
        sm = self.carve(G)
        lbt = self.carve(L * G).rearrange("p (l g) -> p l g", g=G)
        oml = self.carve(L * G).rearrange("p (l g) -> p l g", g=G)
        P.dma("sp", ex, I["hgrn_lb_logits"].partition_broadcast(128), writes=("ex",))
        self.actf(ex, ex, AF.Exp, ("ex",), ("ex",))
        self.cp(sm, ex[:, 0, :], ("ex",), ("sm",), eng="dve")
        for j in range(1, L):
            self.tt(sm, sm, ex[:, j, :], ALU.add, ("sm", "ex"), ("sm",))
        P.op("dve", lambda e: e.reciprocal(out=sm, in_=sm), reads=("sm",), writes=("sm",))
        P.op("dve", lambda e: e.memset(lbt[:, 0, :], 0.0), writes=("lbt",))
        for j in range(1, L):
            self.tt(ex[:, j, :], ex[:, j, :], sm, ALU.mult, ("ex", "sm"), ("ex",))
            self.tt(lbt[:, j, :], lbt[:, j - 1, :], ex[:, j, :], ALU.add, ("lbt", "ex"), ("lbt",))
        self.ts(oml[:, :, :], lbt[:, :, :], -1.0, 1.0, ALU.mult, ALU.add, ("lbt",), ("oml",))
        P.dma("sp", lbd[0], lbt, reads=("lbt",), writes=("lbd",))
        P.dma("sp", lbd[1], oml, reads=("oml",), writes=("lbd",))
        P.barrier()

        for l in layers:
            P.dma("sp", wnb[:, :, :], norms[l].partition_broadcast(128), writes=("wnb",))
            self.phase_p1(l, hbuf, wb_in[l], wnb)
            P.barrier()
            if dbg is not None and dbg[0] == "p" and l == layers[-1]:
                break
            self.phase_prep(l, I, rows, vT, gate, bonus, vfirst, cs_d, lbd, ck)
            P.barrier()
            if dbg is not None and dbg[0] == "prep" and l == layers[-1]:
                break
            self.phase_scan(rows, vT, yT, sel_d, ck, only_abc=(dbg is not None and dbg[0] == "scanabc"))
            P.barrier()
            if dbg is not None and dbg[0] == "scanabc" and l == layers[-1]:
                break
            if dbg is not None and dbg[0] == "scan" and l == layers[-1]:
                break
            self.phase_post(l, I, yT, ck, gate, bonus, obuf)
            P.barrier()
            if dbg is not None and dbg[0] == "post" and l == layers[-1]:
                break
            self.phase_ffn(l, hbuf, obuf, wb_out[l], wb_g[l], wb_u[l], wb_d[l], wnb)
            P.barrier()

        if dbg_t is not None:
            src = {"p": pbuf, "post": obuf, "h": hbuf}.get(dbg[0])
            P.dma("sp", dbg_t, src)
        for i in range(0, NT, 512):
            P.dma("sp", out[i:i + 512, :], hbuf[i:i + 512, :])
        P.finish("sp", P.all_dma_tokens())
        P.emit()
        return nc

    def carve_linear(self, ng):
        self.carve_reset()
        self.xT = self.carve(16 * ng * 64, BF16).rearrange("p (k g t) -> p k g t", g=ng, t=128)
        self.wbuf = [self.carve(4096, BF16) for _ in range(2)]
        self.htile = [self.carve(2048) for _ in range(2)]
        self.junk = self.carve(2048)
        self.ub = self.carve(1024, BF16)
        self.stage = [self.carve(512) for _ in range(4)]
        self.ssel = 0

    def phase_p1(self, l, hbuf, wb, wnb):
        P = self.P
        cfg = self.cfg
        NG = 4
        self.carve_linear(NG)
        for grp in range(cfg.NTILE // NG):
            for g in range(NG):
                tile = grp * NG + g
                ht = self.htile[tile % 2]
                P.dma("sp", ht, hbuf[tile * 128:(tile + 1) * 128, :], reads=(("h", tile),), writes=(("htile", tile % 2),))
                self.norm_transpose(ht, ("htile", tile % 2), wnb[:, 0, :], self.xT, g, "xT")

            def consume(g, cb, nc_, pst, pskey, grp=grp):
                st = self.stage[self.ssel % 4]
                skey = ("stage", self.ssel % 4)
                self.ssel += 1
                self.cp(st[:, 0:nc_], pst, (pskey,), (skey,))
                tile = grp * NG + g
                P.dma("sp", self.pbuf[tile * 128:(tile + 1) * 128, cb * 512:cb * 512 + nc_], st[:, 0:nc_],
                      reads=(skey,), writes=(("p", tile, cb),))

            self.linear_tm(self.xT, ("xT",), NG, 16, wb, NIN, consume)

    def phase_prep(self, l, I, rows, vT, gate, bonus, vfirst, cs_d, lbd, ck):
        P = self.P
        cfg = self.cfg
        self.carve_reset()
        seg = self.carve(2064)
        sh = self.carve(3072)
        Wt = [self.carve(512) for _ in range(10)]
        R = self.carve(1024)
        cst = self.carve(64)
        self.vt = self.carve(512).rearrange("p (a b) -> p a b", b=128)
        cw = self.carve(4096).rearrange("p (j c) -> p j c", c=1024)
        cb_ = self.carve(1024)
        mu = self.carve(RWC)
        sv = {nm: self.carve(512) for nm in SMALL}
        v0b = self.carve(512)
        ifb = self.carve(16)
        upw = self.carve(1024)
        gup = self.carve(512)
        vdn = self.carve(128).rearrange("p (c n) -> p c n", n=32)
        vup = self.carve(512)
        small = self.carve(64)
        small2 = self.carve(64)
        QB = self.carve(256, BF16)
        KB = self.carve(256, BF16)
        KH = self.carve(256, BF16)
        XB1 = self.carve(256, BF16)
        XB2 = self.carve(256, BF16)
        XB3 = self.carve(256, BF16)
        XB4 = self.carve(256, BF16)
        self.tbt = self.carve(256, BF16).rearrange("p (a b) -> p a b", b=128)
        self.vb = self.carve(260, BF16).rearrange("p (h e) -> p h e", e=65)
        AT = self.carve(2048).rearrange("p (a c) -> p a c", c=512)
        tri = self.carve(512).rearrange("p (a c) -> p a c", c=128)
        P.dma("sp", AT, ck["atab"].rearrange("a p c -> p a c"), writes=("AT",))
        P.dma("sp", tri, ck["tri"].rearrange("a p c -> p a c"), writes=("tri",))
        P.op("dve", lambda e: e.memset(self.vb[:, :, 64:65], 1.0), writes=("vb",))
        lbt = self.carve(512)
        oml = self.carve(512)
        P.dma("sp", lbt, lbd[0, :, l, :], writes=("lbt",))
        P.dma("sp", oml, lbd[1, :, l, :], writes=("oml",))
        for nm in SMALL:
            self.bcast_load(sv[nm], I[nm][l], "c_" + nm)
        self.bcast_load(cw, I["mlstm_conv_w"][l], "cw")
        self.bcast_load(cb_, I["mlstm_conv_b"][l], "cb")
        self.bcast_load(mu, I["rwkv_mu"][l], "mu")
        self.bcast_load(ifb, I["mlstm_if_bias"][l], "ifb")
        P.dma("sp", upw, I["rwkv_up_pad"][l], writes=("upw",))
        P.dma("sp", gup, I["rwkv_g_up"][l], writes=("gup",))
        if l > 0:
            self.bcast_load(v0b, I["rwkv_v0"][l - 1], "v0b")
            P.dma("sp", vdn, I["rwkv_v_down"][l - 1].rearrange("(c p) n -> p c n", p=128), writes=("vdn",))
            P.dma("sp", vup[0:32, :], I["rwkv_v_up"][l - 1], writes=("vup",))
        pall = lambda tile: tuple(("p", tile, cb) for cb in range(16))
        W = Wt
        wk = lambda i: ("W", i)

        def h8(ap):
            return ap.rearrange("p (h d) -> p h d", d=64)

        def b8(ap8):
            return ap8.unsqueeze(2).to_broadcast([128, 8, 64])

        for tile in range(cfg.NTILE):
            s, g = tile // cfg.TPS, tile % cfg.TPS
            r0 = tile * 128
            rs = slice(r0, r0 + 128)
            P.dma("sp", seg[:, 0:2048], self.pbuf[rs, 0:2048], reads=pall(tile), writes=("seg",))
            P.dma("sp", cst, cs_d[g * 128:(g + 1) * 128, :], writes=("cst",))
            qk = seg[:, 0:1024].rearrange("p (h two d) -> p h two d", two=2, d=32)
            Rv = R.rearrange("p (h two d) -> p h two d", two=2, d=32)
            t1, t2 = qk[:, :, 0, :], qk[:, :, 1, :]
            cosb = cst[:, 0:32].unsqueeze(1).to_broadcast([128, 16, 32])
            sinb = cst[:, 32:64].unsqueeze(1).to_broadcast([128, 16, 32])
            w0v = W[0].rearrange("p (h d) -> p h d", d=32)
            w1v = W[1].rearrange("p (h d) -> p h d", d=32)
            self.tt(w0v, t1, cosb, ALU.mult, ("seg", "cst"), (wk(0),))
            self.tt(w1v, t2, sinb, ALU.mult, ("seg", "cst"), (wk(1),))
            self.tt(Rv[:, :, 0, :], w0v, w1v, ALU.subtract, (wk(0), wk(1)), ("R",))
            self.tt(w0v, t1, sinb, ALU.mult, ("seg", "cst"), (wk(0),))
            self.tt(w1v, t2, cosb, ALU.mult, ("seg", "cst"), (wk(1),))
            self.tt(Rv[:, :, 1, :], w0v, w1v, ALU.add, (wk(0), wk(1)), ("R",))
            self.tt(QB, R[:, 0:512], AT[:, 0, :], ALU.mult, ("R", "AT"), ("QB",))
            self.bt_store(QB, "QB", ck["qT"]["A"], s, g)
            self.tt(KB, R[:, 512:1024], AT[:, 1, :], ALU.mult, ("R", "AT"), ("KB",))
            self.bt_store(KB, "KB", ck["kT"]["A"], s, g)
            self.tt(KH, R[:, 512:1024], AT[:, 2, :], ALU.mult, ("R", "AT"), ("KH",))
            P.dma("sp", ck["kh"]["A"][rs, :], KH, reads=("KH",), writes=(("kh", "A"),))
            self.store_v(seg[:, 1024:1536], "seg", ck["v"]["A"], rs)
            P.dma("sp", ck["dec"]["A"][rs, :], AT[:, 3, :], reads=("AT",), writes=(("dec", "A"),))
            self.actf(W[2], seg[:, 1536:2048], AF.Silu, ("seg",), (wk(2),))
            P.dma("sp", gate["A"][rs, :], W[2], reads=(wk(2),), writes=(("gate", "A", tile),))
            P.dma("sp", seg[:, 0:2048], self.pbuf[rs, 2048:4096], reads=pall(tile), writes=("seg",))
            self.actf(W[0], seg[:, 0:512], AF.Silu, ("seg",), (wk(0),))
            self.ts(W[0], W[0], 0.125, None, ALU.mult, None, (wk(0),), (wk(0),))
            self.actf(W[1], seg[:, 512:1024], AF.Sigmoid, ("seg",), (wk(1),))
            self.tt(W[1], W[1], oml, ALU.mult, (wk(1), "oml"), (wk(1),))
            self.tt(W[1], W[1], lbt, ALU.add, (wk(1), "lbt"), (wk(1),))
            self.ts(W[3], W[1], -1.0, 1.0, ALU.mult, ALU.add, (wk(1),), (wk(3),))
            self.actf(W[4], W[1], AF.Ln, (wk(1),), (wk(4),))
            ba = self.psbank()
            pc = self.pslin[:, ba, :]
            P.op("pe", lambda e, pc=pc: e.matmul(pc, lhsT=tri[:, 2, :], rhs=W[4], start=True, stop=True),
                 reads=(wk(4), "tri"), writes=(("pslin", ba),))
            bb = self.psbank()
            ptot = self.pslin[:, bb, :]
            P.op("pe", lambda e, ptot=ptot: e.matmul(ptot, lhsT=tri[:, 3, :], rhs=W[4], start=True, stop=True),
                 reads=(wk(4), "tri"), writes=(("pslin", bb),))
            self.actf(W[5], pc, AF.Exp, (("pslin", ba),), (wk(5),))
            self.tt(QB, W[0], W[5], ALU.mult, (wk(0), wk(5)), ("QB",))
            self.bt_store(QB, "QB", ck["qT"]["B"], s, g)
            self.actf(W[5], pc, AF.Exp, (("pslin", ba),), (wk(5),), scale=-1.0)
            self.tt(KB, W[3], W[5], ALU.mult, (wk(3), wk(5)), ("KB",))
            self.bt_store(KB, "KB", ck["kT"]["B"], s, g)
            self.cp(W[6], ptot, (("pslin", bb),), (wk(6),))
            self.actf(W[7], W[6], AF.Exp, (wk(6),), (wk(7),))
            P.dma("sp", ck["dec"]["B"][rs, :], W[7], reads=(wk(7),), writes=(("dec", "B"),))
            self.tt(W[6], W[6], pc, ALU.subtract, (wk(6), ("pslin", ba)), (wk(6),))
            self.actf(W[6], W[6], AF.Exp, (wk(6),), (wk(6),))
            self.tt(KH, W[3], W[6], ALU.mult, (wk(3), wk(6)), ("KH",))
            P.dma("sp", ck["kh"]["B"][rs, :], KH, reads=("KH",), writes=(("kh", "B"),))
            self.store_v(seg[:, 1024:1536], "seg", ck["v"]["B"], rs)
            self.actf(W[2], seg[:, 1536:2048], AF.Sigmoid, ("seg",), (wk(2),))
            P.dma("sp", gate["B"][rs, :], W[2], reads=(wk(2),), writes=(("gate", "B", tile),))
            P.dma("sp", seg[:, 0:2064], self.pbuf[rs, 4096:6160], reads=pall(tile), writes=("seg",))
            shv = sh.rearrange("p (j c) -> p j c", c=1024)
            for j in range(3):
                self.load_shift(shv[:, j, :], ("sh", j), 4096, 1024, tile, 3 - j)
            acc = R
            self.tt(acc, seg[:, 0:1024], cw[:, 3, :], ALU.mult, ("seg", "cw"), ("R",))
            self.tt(acc, acc, cb_, ALU.add, ("R", "cb"), ("R",))
            for j in range(3):
                self.tt(shv[:, j, :], shv[:, j, :], cw[:, j, :], ALU.mult, (("sh", j), "cw"), (("sh", j),))
                self.tt(acc, acc, shv[:, j, :], ALU.add, ("R", ("sh", j)), ("R",))
            self.actf(acc, acc, AF.Silu, ("R",), ("R",))
            self.tt(small[:, 0:16], seg[:, 2048:2064], ifb, ALU.add, ("seg", "ifb"), ("small",))
            self.actf(small[:, 24:32], small[:, 8:16], AF.Sigmoid, ("small",), ("small3",))
            self.actf(small[:, 16:24], small[:, 24:32], AF.Ln, ("small3",), ("small2",))
            ba = self.psbank()
            pc8 = self.pslin[:, ba, 0:8]
            P.op("pe", lambda e, pc8=pc8: e.matmul(pc8, lhsT=tri[:, 0, :], rhs=small[:, 16:24], start=True, stop=True),
                 reads=("small2", "tri"), writes=(("pslin", ba),))
            bb = self.psbank()
            pt8 = self.pslin[:, bb, 0:8]
            P.op("pe", lambda e, pt8=pt8: e.matmul(pt8, lhsT=tri[:, 1, :], rhs=small[:, 16:24], start=True, stop=True),
                 reads=("small2", "tri"), writes=(("pslin", bb),))
            self.actf(small2[:, 0:8], pc8, AF.Exp, (("pslin", ba),), ("s2a",))
            self.tt(h8(QB), h8(acc[:, 0:512]), b8(small2[:, 0:8]), ALU.mult, ("R", "s2a"), ("QB",))
            self.bt_store(QB, "QB", ck["qT"]["C"], s, g)
            self.tt(small2[:, 8:16], small[:, 0:8], pc8, ALU.subtract, ("small", ("pslin", ba)), ("s2b",))
            self.actf(small2[:, 16:24], small2[:, 8:16], AF.Exp, ("s2b",), ("s2c",))
            self.stt(h8(KB), h8(acc[:, 512:1024]), 0.125, b8(small2[:, 16:24]), ALU.mult, ALU.mult, ("R", "s2c"), ("KB",))
            self.bt_store(KB, "KB", ck["kT"]["C"], s, g)
            self.tt(small2[:, 24:32], small2[:, 8:16], pt8, ALU.add, ("s2b", ("pslin", bb)), ("s2d",))
            self.actf(small2[:, 32:40], small2[:, 24:32], AF.Exp, ("s2d",), ("s2e",))
            self.stt(h8(KH), h8(acc[:, 512:1024]), 0.125, b8(small2[:, 32:40]), ALU.mult, ALU.mult, ("R", "s2e"), ("KH",))
            P.dma("sp", ck["kh"]["C"][rs, :], KH, reads=("KH",), writes=(("kh", "C"),))
            self.actf(small2[:, 40:48], pt8, AF.Exp, (("pslin", bb),), ("s2f",))
            self.cp(h8(W[1]), b8(small2[:, 40:48]), ("s2f",), (wk(1),), eng="dve")
            P.dma("sp", ck["dec"]["C"][rs, :], W[1], reads=(wk(1),), writes=(("dec", "C"),))
            self.store_v(seg[:, 1024:1536], "seg", ck["v"]["C"], rs)
            self.actf(W[2], seg[:, 1536:2048], AF.Sigmoid, ("seg",), (wk(2),))
            P.dma("sp", gate["C"][rs, :], W[2], reads=(wk(2),), writes=(("gate", "C", tile),))
            P.dma("sp", seg[:, 0:RWC], self.pbuf[rs, 6160:6160 + RWC], reads=pall(tile), writes=("seg",))
            prev = sh[:, 0:RWC]
            self.load_shift(prev, ("sh", 0), 6160, RWC, tile, 1, extra_keys=(("sh", 1),))
            self.tt(prev, prev, seg[:, 0:RWC], ALU.subtract, (("sh", 0), ("sh", 1), "seg"), (("sh", 0), ("sh", 1)))
            self.tt(prev, prev, mu, ALU.mult, (("sh", 0), ("sh", 1), "mu"), (("sh", 0), ("sh", 1)))
            self.tt(seg[:, 0:RWC], seg[:, 0:RWC], prev, ALU.add, ("seg", ("sh", 0), ("sh", 1)), ("seg",))
            r_, k_, v_ = seg[:, 0:512], seg[:, 512:1024], seg[:, 1024:1536]
            Lt = W[0][:, 0:256]
            self.actf(Lt[:, 0:64], seg[:, 1536:1600], AF.Tanh, ("seg",), (wk(0),))
            self.cp(Lt[:, 64:128], seg[:, 1600:1664], ("seg",), (wk(0),))
            self.actf(Lt[:, 128:256], seg[:, 1664:1792], AF.Sigmoid, ("seg",), (wk(0),))
            LT = W[1][:, 0:256]
            for c in range(2):
                bank = self.psbank()
                pt = self.pslin[:, bank, 0:128]
                P.op("pe", lambda e, pt=pt, c=c: e.transpose(pt, Lt[:, c * 128:(c + 1) * 128], self.ident_f[:, :]),
                     reads=(wk(0), "identf"), writes=(("pslin", bank),))
                self.cp(LT[:, c * 128:(c + 1) * 128], pt, (("pslin", bank),), (wk(1),))
            bank = self.psbank()
            pw = self.pslin[:, bank, :]
            P.op("pe", lambda e, pw=pw: e.matmul(pw, lhsT=LT[:, 0:128], rhs=upw[:, 0:512], start=True, stop=True),
                 reads=(wk(1), "upw"), writes=(("pslin", bank),))
            self.tt(W[2], pw, sv["rwkv_w0"], ALU.add, (("pslin", bank), "c_rwkv_w0"), (wk(2),))
            self.actf(W[2], W[2], AF.Sigmoid, (wk(2),), (wk(2),))
            self.ts(W[2], W[2], -0.6065306597126334, None, ALU.mult, None, (wk(2),), (wk(2),))
            bank = self.psbank()
            pa = self.pslin[:, bank, :]
            P.op("pe", lambda e, pa=pa: e.matmul(pa, lhsT=LT[:, 0:128], rhs=upw[:, 512:1024], start=True, stop=True),
                 reads=(wk(1), "upw"), writes=(("pslin", bank),))
            self.tt(W[3], pa, sv["rwkv_a0"], ALU.add, (("pslin", bank), "c_rwkv_a0"), (wk(3),))
            self.actf(W[3], W[3], AF.Sigmoid, (wk(3),), (wk(3),))
            bank = self.psbank()
            pg = self.pslin[:, bank, :]
            P.op("pe", lambda e, pg=pg: e.matmul(pg, lhsT=LT[:, 128:256], rhs=gup, start=True, stop=True),
                 reads=(wk(1), "gup"), writes=(("pslin", bank),))
            self.cp(W[4], pg, (("pslin", bank),), (wk(4),))
            P.dma("sp", gate["D"][rs, :], W[4], reads=(wk(4),), writes=(("gate", "D", tile),))
            if l == 0:
                P.dma("sp", vfirst[rs, :], v_, reads=("seg",), writes=(("vfirst", tile),))
            else:
                vtt = self.vt
                for c in range(4):
                    bank = self.psbank()
                    pt = self.pslin[:, bank, 0:128]
                    P.op("pe", lambda e, pt=pt, c=c: e.transpose(pt, v_[:, c * 128:(c + 1) * 128], self.ident_f[:, :]),
                         reads=("seg", "identf"), writes=(("pslin", bank),))
                    self.cp(vtt[:, c, :], pt, (("pslin", bank),), ("vt",))
                bank = self.psbank()
                pv = self.pslin[:, bank, 0:32]
                for c in range(4):
                    P.op("pe", lambda e, pv=pv, c=c: e.matmul(pv, lhsT=vtt[:, c, :], rhs=vdn[:, c, :], start=(c == 0),
                                                              stop=(c == 3)),
                         reads=("vt", "vdn"), writes=(("pslin", bank),))
                self.cp(W[5][:, 0:32], pv, (("pslin", bank),), (wk(5),))
                bank = self.psbank()
                pt = self.pslin[0:32, bank, 0:128]
                P.op("pe", lambda e, pt=pt: e.transpose(pt, W[5][:, 0:32], self.ident_f[:, :]),
                     reads=(wk(5), "identf"), writes=(("pslin", bank),))
                self.cp(W[6][0:32, 0:128], pt, (("pslin", bank),), (wk(6),))
                bank = self.psbank()
                pv2 = self.pslin[:, bank, :]
                P.op("pe", lambda e, pv2=pv2: e.matmul(pv2, lhsT=W[6][0:32, 0:128], rhs=vup[0:32, :], start=True, stop=True),
                     reads=(wk(6), "vup"), writes=(("pslin", bank),))
                self.tt(W[5], pv2, v0b, ALU.add, (("pslin", bank), "v0b"), (wk(5),))
                self.actf(W[5], W[5], AF.Sigmoid, (wk(5),), (wk(5),))
                P.dma("sp", W[6], vfirst[rs, :], reads=(("vfirst", tile),), writes=(wk(6),))
                self.tt(W[6], W[6], v_, ALU.subtract, (wk(6), "seg"), (wk(6),))
                self.tt(W[6], W[6], W[5], ALU.mult, (wk(6), wk(5)), (wk(6),))
                self.tt(v_, v_, W[6], ALU.add, ("seg", wk(6)), ("seg",))
            self.tt(W[5], k_, sv["rwkv_k_k"], ALU.mult, ("seg", "c_rwkv_k_k"), (wk(5),))
            self.tt(W[6], W[5], W[5], ALU.mult, (wk(5),), (wk(6),))
            self.red(small[:, 32:40], h8(W[6]), (wk(6),), ("small4",))
            self.actf(small[:, 40:48], small[:, 32:40], AF.Sqrt, ("small4",), ("small5",))
            self.ts(small[:, 40:48], small[:, 40:48], 1e-12, None, ALU.max, None, ("small5",), ("small5",))
            P.op("dve", lambda e: e.reciprocal(out=small[:, 48:56], in_=small[:, 40:48]), reads=("small5",), writes=("small6",))
            self.tt(h8(W[5]), h8(W[5]), b8(small[:, 48:56]), ALU.mult, (wk(5), "small6"), (wk(5),))
            self.stt(W[7], W[3], -1.0, sv["rwkv_k_a"], ALU.add, ALU.mult, (wk(3), "c_rwkv_k_a"), (wk(7),))
            self.stt(W[7], W[7], 1.0, k_, ALU.add, ALU.mult, (wk(7), "seg"), (wk(7),))
            self.tt(W[8], W[3], W[5], ALU.mult, (wk(3), wk(5)), (wk(8),))
            self.tt(W[9], r_, W[7], ALU.mult, ("seg", wk(7)), (wk(9),))
            self.tt(W[9], W[9], sv["rwkv_r_k"], ALU.mult, (wk(9), "c_rwkv_r_k"), (wk(9),))
            self.red(small[:, 56:64], h8(W[9]), (wk(9),), ("small7",))
            self.tt(h8(W[9]), h8(v_), b8(small[:, 56:64]), ALU.mult, ("seg", "small7"), (wk(9),))
            P.dma("sp", bonus[rs, :], W[9], reads=(wk(9),), writes=(("bonus", tile),))
            dk = ck["D"]
            ba = self.psbank()
            pc = self.pslin[:, ba, :]
            P.op("pe", lambda e, pc=pc: e.matmul(pc, lhsT=tri[:, 2, :], rhs=W[2], start=True, stop=True),
                 reads=(wk(2), "tri"), writes=(("pslin", ba),))
            bb = self.psbank()
            ptot = self.pslin[:, bb, :]
            P.op("pe", lambda e, ptot=ptot: e.matmul(ptot, lhsT=tri[:, 3, :], rhs=W[2], start=True, stop=True),
                 reads=(wk(2), "tri"), writes=(("pslin", bb),))
            self.actf(W[0], pc, AF.Exp, (("pslin", ba),), (wk(0),))
            self.tt(QB, r_, W[0], ALU.mult, ("seg", wk(0)), ("QB",))
            self.bt_store(QB, "QB", dk["rT"], s, g)
            self.tt(W[1], pc, W[2], ALU.subtract, (("pslin", ba), wk(2)), (wk(1),))
            self.actf(W[1], W[1], AF.Exp, (wk(1),), (wk(1),))
            self.tt(KB, W[5], W[1], ALU.mult, (wk(5), wk(1)), ("KB",))
            self.bt_store(KB, "KB", dk["cT"], s, g)
            self.actf(W[0], pc, AF.Exp, (("pslin", ba),), (wk(0),), scale=-1.0)
            self.tt(XB1, W[7], W[0], ALU.mult, (wk(7), wk(0)), ("XB1",))
            self.bt_store(XB1, "XB1", dk["kT"], s, g)
            self.tt(XB2, W[8], W[0], ALU.mult, (wk(8), wk(0)), ("XB2",))
            self.bt_store(XB2, "XB2", dk["bT"], s, g)
            self.cp(W[6], ptot, (("pslin", bb),), (wk(6),))
            self.actf(W[1], W[6], AF.Exp, (wk(6),), (wk(1),))
            P.dma("sp", dk["dec"][rs, :], W[1], reads=(wk(1),), writes=(("dec", "D"),))
            self.tt(W[6], W[6], pc, ALU.subtract, (wk(6), ("pslin", ba)), (wk(6),))
            self.actf(W[6], W[6], AF.Exp, (wk(6),), (wk(6),))
            self.tt(KH, W[7], W[6], ALU.mult, (wk(7), wk(6)), ("KH",))
            P.dma("sp", dk["kh"][rs, :], KH, reads=("KH",), writes=(("kh", "D"),))
            self.stt(XB3, W[8], -1.0, W[6], ALU.mult, ALU.mult, (wk(8), wk(6)), ("XB3",))
            P.dma("sp", dk["bh"][rs, :], XB3, reads=("XB3",), writes=(("bh", "D"),))
            self.cp(XB4, v_, ("seg",), ("XB4",), eng="dve")
            P.dma("sp", dk["v"][rs, :], XB4, reads=("XB4",), writes=(("vd", "D"),))

    def phase_scan(self, rows, vT, yT, sel_d, ck, only_abc=False):
        P = self.P
        cfg = self.cfg
        self.carve_reset()
        self._phase_id = getattr(self, "_phase_id", 0) + 1

        def chain():
            for m in "ABC":
                for _ in self.gen_chunk(m, CHK[m], ck):
                    yield
        ga = chain()
        gd = self.chunk_D(ck["D"])
        na = 5 * sum((cfg.T // CHK[m]) * cfg.NSEQ * 4 for m in "ABC")
        nd = 19 * (cfg.T // 32 + 1) * cfg.NSEQ * 4
        a_done = d_done = False
        ia = idd = 0
        while not (a_done and d_done):
            if not d_done and (a_done or idd * na <= ia * nd):
                try:
                    next(gd)
                    idd += 1
                except StopIteration:
                    d_done = True
            elif not a_done:
                try:
                    next(ga)
                    ia += 1
                except StopIteration:
                    a_done = True

    def chunk_D(self, dk):
        P = self.P
        cfg = self.cfg
        T = cfg.T
        C = 32
        nch = T // C
        XT = self.carve(T // 2, BF16)
        X2 = {n: self.carve(T, BF16).rearrange("p (n h t) -> p n h t", h=2, t=C) for n in ("r", "c", "k", "b")}
        Vt = self.carve(nch * 32, BF16).rearrange("p (n e) -> p n e", e=64)
        KH2 = self.carve(nch * 64, BF16).rearrange("p (n e) -> p n e", e=128)
        BH2 = self.carve(nch * 64, BF16).rearrange("p (n e) -> p n e", e=128)
        YH = 32
        yo = self.carve(YH * 64).rearrange("p (n e) -> p n e", e=64)
        dch = self.carve(128)
        decT = self.carve(64)
        Zf = self.carve(64)
        Zb = self.carve(32, BF16)
        msk = self.carve(192).rearrange("p (a c) -> p a c", c=64)
        ST = [dict(N=[self.carve(64) for _ in range(5)], NT=[self.carve(64) for _ in range(4)], P=self.carve(64),
                   Q=self.carve(64), BTm=self.carve(32, BF16), S1m=self.carve(32, BF16), S2m=self.carve(32, BF16))
              for _ in range(2)]
        RHSs = self.carve(64)
        Ub = self.carve(32, BF16)
        pstr_f = self.pstr[:, :, :].bitcast(F32)
        I64 = self.ident_f[0:64, 0:64]
        P.dma("sp", msk[0:64, :, :], dk["masks"].rearrange("a p c -> p a c"), writes=("msk",))
        for n in X2:
            P.op("dve", lambda e, n=n: e.memset(X2[n], 0.0), writes=(("X2", n),))
        P.op("dve", lambda e: e.memset(KH2, 0.0), writes=("KH2",))
        P.op("dve", lambda e: e.memset(BH2, 0.0), writes=("BH2",))
        Mup, Mlow, Minc = msk[0:64, 0, :], msk[0:64, 1, :], msk[0:64, 2, :]
        srcT = {"r": dk["rT"], "c": dk["cT"], "k": dk["kT"], "b": dk["bT"]}
        pe = lambda fn, r, w: P.op("pe", fn, reads=r, writes=w)
        for s in range(cfg.NSEQ):
            for hp in range(4):
                for n in ("r", "c", "k", "b"):
                    P.dma("sp", XT[:, 0:T], srcT[n][hp, :, s, :], reads=tuple(("T", id(srcT[n]), s, g) for g in range(cfg.TPS)),
                          writes=("XT",))
                    self.cp(X2[n][0:64, :, 0, :], XT[0:64, 0:T].rearrange("p (n t) -> p n t", t=C), ("XT",), (("X2", n),),
                            eng="dve")
                    self.cp(X2[n][64:128, :, 1, :], XT[64:128, 0:T].rearrange("p (n t) -> p n t", t=C), ("XT",), (("X2", n),))
                rsl = slice(s * T, (s + 1) * T)
                for hh in range(2):
                    col = slice((2 * hp + hh) * 64, (2 * hp + hh + 1) * 64)
                    ps_ = slice(hh * 32, (hh + 1) * 32)
                    P.dma("sp", Vt[ps_, 0:nch, :], dk["v"][rsl, col].rearrange("(n c) k -> c n k", c=C), reads=(("vd", "D"),),
                          writes=("Vt",))
                    P.dma("sp", KH2[ps_, 0:nch, hh * 64:(hh + 1) * 64], dk["kh"][rsl, col].rearrange("(n c) k -> c n k", c=C),
                          reads=(("kh", "D"),), writes=("KH2",))
                    P.dma("sp", BH2[ps_, 0:nch, hh * 64:(hh + 1) * 64], dk["bh"][rsl, col].rearrange("(n c) k -> c n k", c=C),
                          reads=(("bh", "D"),), writes=("BH2",))
                P.dma("sp", dch[0:nch, :], dk["dec"][rsl, hp * 128:(hp + 1) * 128].rearrange("(n c) k -> c n k", c=C)[C - 1, :, :],
                      reads=(("dec", "D"),), writes=("dchD",))
                pt = self.pslin[:, 5, 0:nch]
                pe(lambda e, pt=pt: e.transpose(pt, dch[0:nch, :], self.ident_f[0:nch, 0:nch]), ("dchD", "identf"), (("pslin", 5),))
                self.cp(decT[:, 0:nch], pt, (("pslin", 5),), ("decTD",))
                P.op("dve", lambda e: e.memset(Zf, 0.0), writes=("Zf",))
                P.op("dve", lambda e: e.memset(Zb, 0.0), writes=("Zb",))
                r2, c2, k2, b2 = X2["r"], X2["c"], X2["k"], X2["b"]
                xk = lambda n: ("X2", n)

                def front(c):
                    par = c % 2
                    st = ST[par]
                    sk_ = lambda nm: ("ST", par, nm)
                    sc = self.pslin[0:64, par, :]
                    bk = ("pslin", par)
                    fl = lambda ap: ap.rearrange("p h t -> p (h t)")
                    pe(lambda e: e.matmul(sc[:, 0:64], lhsT=fl(b2[:, c]), rhs=fl(c2[:, c]), start=True, stop=True), (xk("b"), xk("c")), (bk,))
                    pe(lambda e: e.matmul(sc[:, 64:128], lhsT=fl(c2[:, c]), rhs=fl(b2[:, c]), start=True, stop=True), (xk("b"), xk("c")), (bk,))
                    pe(lambda e: e.matmul(sc[:, 128:192], lhsT=fl(k2[:, c]), rhs=fl(c2[:, c]), start=True, stop=True), (xk("k"), xk("c")), (bk,))
                    pe(lambda e: e.matmul(sc[:, 192:256], lhsT=fl(k2[:, c]), rhs=fl(r2[:, c]), start=True, stop=True), (xk("k"), xk("r")), (bk,))
                    pe(lambda e: e.matmul(sc[:, 256:320], lhsT=fl(b2[:, c]), rhs=fl(r2[:, c]), start=True, stop=True), (xk("b"), xk("r")), (bk,))
                    yield
                    N, NT, Pm, Qm = st["N"], st["NT"], st["P"][0:64, :], st["Q"][0:64, :]
                    self.stt(N[0][0:64, :], sc[:, 0:64], -1.0, Mup, ALU.mult, ALU.mult, (bk, "msk"), (sk_("N0"),))
                    self.stt(NT[0][0:64, :], sc[:, 64:128], -1.0, Mlow, ALU.mult, ALU.mult, (bk, "msk"), (sk_("NT0"),))
                    self.tt(st["BTm"][0:64, :], sc[:, 128:192], Mup, ALU.mult, (bk, "msk"), (sk_("BTm"),))
                    self.tt(st["S1m"][0:64, :], sc[:, 192:256], Minc, ALU.mult, (bk, "msk"), (sk_("S1m"),))
                    self.stt(st["S2m"][0:64, :], sc[:, 256:320], -1.0, Minc, ALU.mult, ALU.mult, (bk, "msk"), (sk_("S2m"),))
                    self.tt(Pm, N[0][0:64, :], I64, ALU.add, (sk_("N0"), "identf"), (sk_("P"),))
                    self.tt(Qm, NT[0][0:64, :], I64, ALU.add, (sk_("NT0"), "identf"), (sk_("Q"),))
                    yield
                    for k in range(1, 5):
                        last = (k == 4)
                        pn = self.pslin[0:64, 2, 0:128]
                        pe(lambda e, k=k: e.matmul(pn[:, 0:64], lhsT=NT[k - 1][0:64, :], rhs=N[k - 1][0:64, :], start=True, stop=True),
                           (sk_("N%d" % (k - 1)), sk_("NT%d" % (k - 1))), (("pslin", 2),))
                        if not last:
                            pe(lambda e, k=k: e.matmul(pn[:, 64:128], lhsT=N[k - 1][0:64, :], rhs=NT[k - 1][0:64, :], start=True,
                                                       stop=True),
                               (sk_("N%d" % (k - 1)), sk_("NT%d" % (k - 1))), (("pslin", 2),))
                        yield
                        self.cp(N[k][0:64, :], pn[:, 0:64], (("pslin", 2),), (sk_("N%d" % k),))
                        if not last:
                            self.cp(NT[k][0:64, :], pn[:, 64:128], (("pslin", 2),), (sk_("NT%d" % k),))
                        yield
                        pp = self.pslin[0:64, 2, 128:256]
                        pe(lambda e, k=k: e.matmul(pp[:, 0:64], lhsT=Qm, rhs=N[k][0:64, :], start=True, stop=True),
                           (sk_("Q"), sk_("N%d" % k)), (("pslin", 2),))
                        if not last:
                            pe(lambda e, k=k: e.matmul(pp[:, 64:128], lhsT=N[k][0:64, :], rhs=Qm, start=True, stop=True),
                               (sk_("Q"), sk_("N%d" % k)), (("pslin", 2),))
                        yield
                        self.tt(Pm, Pm, pp[:, 0:64], ALU.add, (sk_("P"), ("pslin", 2)), (sk_("P"),))
                        if not last:
                            self.tt(Qm, Qm, pp[:, 64:128], ALU.add, (sk_("Q"), ("pslin", 2)), (sk_("Q"),))
                        yield

                def back(c):
                    par = c % 2
                    st = ST[par]
                    sk_ = lambda nm: ("ST", par, nm)
                    fl = lambda ap: ap.rearrange("p h t -> p (h t)")
                    pr = self.pslin[0:64, 3, 0:64]
                    pe(lambda e: e.matmul(pr, lhsT=fl(c2[:, c]), rhs=Zb[:, 0:64], start=True, stop=False), (xk("c"), "Zb"), (("pslin", 3),))
                    pe(lambda e: e.matmul(pr, lhsT=st["BTm"][0:64, :], rhs=Vt[0:64, c, :], start=False, stop=True),
                       (sk_("BTm"), "Vt"), (("pslin", 3),))
                    yield
                    self.cp(RHSs[0:64, :], pr, (("pslin", 3),), ("RHSs",))
                    yield
                    pu = self.pslin[0:64, 3, 64:128]
                    pe(lambda e: e.matmul(pu, lhsT=st["P"][0:64, :], rhs=RHSs[0:64, :], start=True, stop=True), (sk_("P"), "RHSs"),
                       (("pslin", 3),))
                    yield
                    self.cp(Ub[0:64, :], pu, (("pslin", 3),), ("Ub",))
                    yield
                    pk = self.pslin[:, 5, 0:64]
                    pe(lambda e: e.matmul(pk, lhsT=KH2[0:64, c, :], rhs=Vt[0:64, c, :], start=True, stop=False), ("KH2", "Vt"),
                       (("pslin", 5),))
                    pe(lambda e: e.matmul(pk, lhsT=BH2[0:64, c, :], rhs=Ub[0:64, :], start=False, stop=True), ("BH2", "Ub"),
                       (("pslin", 5),))
                    py = self.pslin[0:64, 3, 128:192]
                    pe(lambda e: e.matmul(py, lhsT=st["S1m"][0:64, :], rhs=Vt[0:64, c, :], start=True, stop=False), (sk_("S1m"), "Vt"),
                       (("pslin", 3),))
                    pe(lambda e: e.matmul(py, lhsT=st["S2m"][0:64, :], rhs=Ub[0:64, :], start=False, stop=False), (sk_("S2m"), "Ub"),
                       (("pslin", 3),))
                    pe(lambda e: e.matmul(py, lhsT=fl(r2[:, c]), rhs=Zb[:, 0:64], start=False, stop=True), (xk("r"), "Zb"),
                       (("pslin", 3),))
                    yield
                    self.stt(Zf, Zf, decT[:, c:c + 1], pk, ALU.mult, ALU.add, ("Zf", "decTD", ("pslin", 5)), ("Zf",))
                    self.cp(yo[0:64, c % YH, :], py, (("pslin", 3),), ("yoD",))
                    yield
                    self.cp(Zb, Zf, ("Zf",), ("Zb",))
                    if c % YH == YH - 1 or c == nch - 1:
                        c0 = c - (c % YH)
                        for hh in range(2):
                            col = slice((2 * hp + hh) * 64, (2 * hp + hh + 1) * 64)
                            P.dma("sp", dk["y"][s * T + c0 * C:s * T + (c + 1) * C, col].rearrange("(n c) k -> c n k", c=C),
                                  yo[hh * 32:(hh + 1) * 32, 0:c - c0 + 1, :], reads=("yoD",), writes=(("cy", "D"),))
                    yield

                import itertools
                for c in range(nch + 1):
                    gf = front(c) if c < nch else iter(())
                    gb = back(c - 1) if c > 0 else iter(())
                    for _ in itertools.zip_longest(gf, gb):
                        yield

    def gen_scan_D(self, rows, vT, yT, sel_d):
        P = self.P
        cfg = self.cfg
        T = cfg.T
        sel = self.carve(TB * 128).rearrange("p (t m) -> p t m", m=128)
        P.dma("sp", sel[0:32, :, :], sel_d[:, :, :], writes=("sel",))
        Rb = [self.carve(5 * 512).rearrange("p (v n) -> p v n", n=512) for _ in range(2)]
        Vb = [self.carve(8 * TB).rearrange("p (g t) -> p g t", t=TB) for _ in range(2)]
        Yb = [self.carve(8 * TB).rearrange("p (g t) -> p g t", t=TB) for _ in range(2)]
        NR = 3
        BR = [self.carve(5 * 512).rearrange("p (v n) -> p v n", n=512) for _ in range(NR)]
        S = self.carve(512)
        T1 = self.carve(512)
        T2 = self.carve(512)
        sk = self.carve(8)
        h8 = lambda ap: ap.rearrange("p (h d) -> p h d", d=64)
        b8 = lambda ap8: ap8.unsqueeze(2).to_broadcast([128, 8, 64])
        m = "D"
        vecs = VECS[m]
        P.op("dve", lambda e: e.memset(S, 0.0), writes=("S",))
        step = 0
        dps = 0
        for bi in range(T // TB):
            t0 = bi * TB
            pb = bi % 2
            for j, vname in enumerate(vecs):
                src = rows[vname].rearrange("(s t) (hp hh d) -> t s hp hh d", s=cfg.NSEQ, hh=2, d=64)
                for hh in range(2):
                    for s in range(cfg.NSEQ):
                        P.dma("sp", Rb[pb][hh * TB:(hh + 1) * TB, j, s * 256:(s + 1) * 256].rearrange(
                            "t (hp d) -> t hp d", d=64),
                            src[t0:t0 + TB, s, :, hh, :],
                            reads=(("row", vname, (s * T + t0) // 128),), writes=(("Rb", pb, j),))
            P.dma("sp", Vb[pb].rearrange("p (s hp) t -> p s hp t", hp=4),
                  vT[m].rearrange("hp p s t -> p s hp t")[:, :, :, t0:t0 + TB],
                  reads=tuple(("T", id(vT[m]), s, t0 // 128) for s in range(cfg.NSEQ)), writes=(("Vb", pb),))
            for tl in range(TB):
                rb = step % NR
                step += 1
                ps = {}
                for j, vname in enumerate(vecs):
                    bank = dps % 3
                    dps += 1
                    pt = self.pslin[:, bank, :]
                    P.op("pe", lambda e, pt=pt, j=j, tl=tl, pb=pb: e.matmul(pt, lhsT=sel[0:32, tl, :], rhs=Rb[pb][0:32, j, :],
                                                                           start=True, stop=True),
                         reads=("sel", ("Rb", pb, j)), writes=(("pslin", bank),))
                    self.cp(BR[rb][:, j, :], pt, (("pslin", bank),), (("BR", rb, j),))
                    ps[j] = (BR[rb][:, j, :], ("BR", rb, j))
                vb = b8(Vb[pb][:, :, tl])
                yo = Yb[pb][:, :, tl]
                (rp, rk_), (kp, kk_), (wp, wk_), (cp_, ck_), (bp, bk_) = ps[0], ps[1], ps[2], ps[3], ps[4]
                self.tt(T1, S, cp_, ALU.mult, ("S", ck_), ("T1",))
                self.red(sk, h8(T1), ("T1",), ("sk",))
                self.tt(h8(T1), h8(bp), b8(sk), ALU.mult, (bk_, "sk"), ("T1",))
                self.tt(S, S, wp, ALU.mult, ("S", wk_), ("S",))
                self.tt(S, S, T1, ALU.subtract, ("S", "T1"), ("S",))
                self.tt(h8(T2), h8(kp), vb, ALU.mult, (kk_, ("Vb", pb)), ("T2",))
                self.tt(S, S, T2, ALU.add, ("S", "T2"), ("S",))
                self.tt(T2, S, rp, ALU.mult, ("S", rk_), ("T2",))
                self.red(yo, h8(T2), ("T2",), (("Yb", pb),))
                yield
            P.dma("sp", yT[m].rearrange("hp p s t -> p s hp t")[:, :, :, t0:t0 + TB],
                  Yb[pb].rearrange("p (s hp) t -> p s hp t", hp=4), reads=(("Yb", pb),),
                  writes=tuple(("T", id(yT[m]), s, t0 // 128) for s in range(cfg.NSEQ)))

    def gen_chunk(self, m, C, ck):
        P = self.P
        cfg = self.cfg
        T = cfg.T
        nch = T // C
        cb = self.chunk_bufs(T)
        qT, kT, q0, q1, kh, vv, yo, dch, decT, st_f, st_b, scm, mask = cb
        YH = 32
        pstr_f = self.pstr[:, :, :].bitcast(F32)
        qT_d, kT_d, kh_d, v_d, dec_d, y_d = ck["qT"][m], ck["kT"][m], ck["kh"][m], ck["v"][m], ck["dec"][m], ck["y"][m]
        P.dma("sp", mask[0:C, 0:C], ck["mask"][0:C, 0:C], writes=("mask",))
        P.dma("sp", mask[0:C, C:2 * C], ck["mask"][0:C, 0:C], writes=("mask",))
        cnt = 0
        qs = [q0, q1]
        for s in range(cfg.NSEQ):
            for hp in range(4):
                allT = tuple(("T", id(qT_d), s, g) for g in range(cfg.TPS))
                P.dma("sp", qT[:, 0:T], qT_d[hp, :, s, :], reads=allT, writes=("c_qT",))
                P.dma("sp", kT[:, 0:T], kT_d[hp, :, s, :], reads=tuple(("T", id(kT_d), s, g) for g in range(cfg.TPS)),
                      writes=("c_kT",))
                P.dma("sp", kh[0:C, 0:nch, :], kh_d[s * T:(s + 1) * T, hp * 128:(hp + 1) * 128].rearrange(
                    "(n c) k -> c n k", c=C), reads=(("kh", m),), writes=("c_kh",))
                P.dma("sp", vv[0:C, 0:nch, :], v_d[s * T:(s + 1) * T, hp * 130:(hp + 1) * 130].rearrange(
                    "(n c) k -> c n k", c=C), reads=(("vd", id(v_d)),), writes=("c_vv",))
                P.dma("sp", dch[0:nch, :], dec_d[s * T:(s + 1) * T, hp * 128:(hp + 1) * 128].rearrange(
                    "(n c) k -> c n k", c=C)[C - 1, :, :], reads=(("dec", m),), writes=("c_dch",))
                pt = pstr_f[:, 1, 0:nch]
                P.op("pe", lambda e, pt=pt: e.transpose(pt, dch[0:nch, :], self.ident_f[0:nch, 0:nch]),
                     reads=("c_dch", "identf"), writes=(("pstr", 1),))
                self.cp(decT[:, 0:nch], pt, (("pstr", 1),), ("c_decT",))
                P.op("dve", lambda e: e.memset(q0[64:128, 0:T], 0.0), writes=("c_q0",))
                P.op("dve", lambda e: e.memset(q1[0:64, 0:T], 0.0), writes=("c_q1",))
                self.cp(q0[0:64, 0:T], qT[0:64, 0:T], ("c_qT",), ("c_q0",), eng="dve")
                self.cp(q1[64:128, 0:T], qT[64:128, 0:T], ("c_qT",), ("c_q1",), eng="dve")
                P.op("dve", lambda e: e.memset(st_f[:, 0:66], 0.0), writes=("st_f",))
                P.op("dve", lambda e: e.memset(st_b[:, 0:66], 0.0), writes=("st_b",))
                for c in range(nch):
                    cs = slice(c * C, (c + 1) * C)
                    par = c % 2
                    psc = self.pslin[0:C, 4, 0:2 * C]
                    for hh in range(2):
                        qh = qs[hh]
                        qk = "c_q%d" % hh
                        P.op("pe", lambda e, cs=cs, qh=qh, hh=hh: e.matmul(psc[:, hh * C:(hh + 1) * C], lhsT=kT[:, cs], rhs=qh[:, cs],
                                                                           start=True, stop=True),
                             reads=("c_kT", qk), writes=(("pslin", 4),))
                    yield
                    sm = scm[par][0:C, 0:2 * C]
                    self.tt(sm, psc, mask[0:C, 0:2 * C], ALU.mult, (("pslin", 4), "mask"), (("scm", par),))
                    yield
                    pso = pstr_f[0:C, 0, 0:130]
                    for hh in range(2):
                        qh = qs[hh]
                        qk = "c_q%d" % hh
                        vsl = vv[0:C, c, hh * 65:(hh + 1) * 65]
                        po = pso[:, hh * 65:(hh + 1) * 65]
                        P.op("pe", lambda e, po=po, vsl=vsl, hh=hh, sm=sm: e.matmul(po, lhsT=sm[:, hh * C:(hh + 1) * C], rhs=vsl,
                                                                                   start=True, stop=False),
                             reads=(("scm", par), "c_vv"), writes=(("pstr", 0),))
                        P.op("pe", lambda e, po=po, cs=cs, qh=qh: e.matmul(po, lhsT=qh[:, cs], rhs=st_b[:, 0:65], start=False, stop=True),
                             reads=(qk, "st_b"), writes=(("pstr", 0),))
                        pkv = pstr_f[hh * 64:(hh + 1) * 64, 1, 0:65]
                        khs = kh[0:C, c, hh * 64:(hh + 1) * 64]
                        P.op("pe", lambda e, pkv=pkv, khs=khs, vsl=vsl: e.matmul(pkv, lhsT=khs, rhs=vsl, start=True, stop=True),
                             reads=("c_kh", "c_vv"), writes=(("pstr", 1),))
                    yield
                    self.stt(st_f[:, 0:65], st_f[:, 0:65], decT[:, c:c + 1], pstr_f[:, 1, 0:65], ALU.mult, ALU.add,
                             ("st_f", "c_decT", ("pstr", 1)), ("st_f",))
                    self.cp(yo[0:C, c % YH, :], pso, (("pstr", 0),), ("c_yo",))
                    yield
                    self.cp(st_b[:, 0:65], st_f[:, 0:65], ("st_f",), ("st_b",))
                    if c % YH == YH - 1 or c == nch - 1:
                        c0 = c - (c % YH)
                        P.dma("sp", y_d[s * T + c0 * C:s * T + (c + 1) * C, hp * 130:(hp + 1) * 130].rearrange(
                            "(n c) k -> c n k", c=C), yo[0:C, 0:c - c0 + 1, :], reads=("c_yo",), writes=(("cy", m),))
                    yield

    def chunk_bufs(self, T):
        if getattr(self, "_cbufs_at", None) == id(self.P.ops["pe"]) and getattr(self, "_cbufs_phase", -1) == self._phase_id:
            return self._cbufs
        qT = self.carve(T // 2, BF16)
        kT = self.carve(T // 2, BF16)
        q0 = self.carve(T // 2, BF16)
        q1 = self.carve(T // 2, BF16)
        nmax = T // 32
        kh = self.carve(nmax * 64, BF16).rearrange("p (n k) -> p n k", k=128)
        vv = self.carve(nmax * 65, BF16).rearrange("p (n k) -> p n k", k=130)
        yo = self.carve(32 * 130).rearrange("p (n k) -> p n k", k=130)
        dch = self.carve(128)
        decT = self.carve(64)
        st_f = self.carve(66)
        st_b = self.carve(33, BF16)
        scm = [self.carve(64, BF16) for _ in range(2)]
        mask = self.carve(128)
        self._cbufs = (qT, kT, q0, q1, kh, vv, yo, dch, decT, st_f, st_b, scm, mask)
        self._cbufs_at = id(self.P.ops["pe"])
        self._cbufs_phase = self._phase_id
        self._mask_loaded = False
        return self._cbufs

    def phase_post(self, l, I, yT, ck, gate, bonus, obuf):
        P = self.P
        cfg = self.cfg
        self.carve_reset()
        o = self.carve(2048)
        y = self.carve(512)
        y65 = self.carve(520).rearrange("p (h e) -> p h e", e=65)
        gt = self.carve(512)
        W = [self.carve(512) for _ in range(3)]
        self.vt = self.carve(512).rearrange("p (a b) -> p a b", b=128)
        sv = {nm: self.carve(512) for nm in ("hgrn_norm_w", "mlstm_norm_w", "rwkv_ln_w", "rwkv_ln_b")}
        small = self.carve(64)
        for nm in sv:
            self.bcast_load(sv[nm], I[nm][l], "c_" + nm)
        h8 = lambda ap: ap.rearrange("p (h d) -> p h d", d=64)
        b8 = lambda ap8: ap8.unsqueeze(2).to_broadcast([128, 8, 64])
        wk = lambda i: ("W", i)
        for tile in range(cfg.NTILE):
            s, g = tile // cfg.TPS, tile % cfg.TPS
            rs = slice(tile * 128, (tile + 1) * 128)
            for mi, m in enumerate("ABCD"):
                if m == "D":
                    P.dma("sp", y, ck["D"]["y"][rs, :], reads=(("cy", "D"),), writes=("y",))
                else:
                    P.dma("sp", y65.rearrange("p h e -> p (h e)"), ck["y"][m][rs, :], reads=(("cy", m),), writes=("y65",))
                    self.cp(h8(y), y65[:, :, 0:64], ("y65",), ("y",), eng="dve")
                P.dma("sp", gt, gate[m][rs, :], reads=(("gate", m, tile),), writes=("gt",))
                osl = o[:, mi * 512:(mi + 1) * 512]
                if m == "C":
                    d8 = y65[:, :, 64]
                    self.ts(small[:, 0:8], d8, -1.0, None, ALU.mult, None, ("y65",), ("sm0",))
                    self.tt(small[:, 0:8], small[:, 0:8], d8, ALU.max, ("sm0", "y65"), ("sm0",))
                    self.ts(small[:, 0:8], small[:, 0:8], 1.0, None, ALU.max, None, ("sm0",), ("sm0",))
                    P.op("dve", lambda e: e.reciprocal(out=small[:, 8:16], in_=small[:, 0:8]), reads=("sm0",), writes=("sm1",))
                    self.tt(h8(y), h8(y), b8(small[:, 8:16]), ALU.mult, ("y", "sm1"), ("y",))
                if m in "ABC":
                    self.tt(W[0], y, y, ALU.mult, ("y",), (wk(0),))
                    self.red(small[:, 16:24], h8(W[0]), (wk(0),), ("sm2",))
                    self.actf(small[:, 24:32], small[:, 16:24], AF.Sqrt, ("sm2", "eps"), ("sm3",), scale=1.0 / 64,
                              bias=self.eps_t[:, 0:1])
                    P.op("dve", lambda e: e.reciprocal(out=small[:, 32:40], in_=small[:, 24:32]), reads=("sm3",), writes=("sm4",))
                    self.tt(h8(y), h8(y), b8(small[:, 32:40]), ALU.mult, ("y", "sm4"), ("y",))
                    if m == "B":
                        self.tt(y, y, sv["hgrn_norm_w"], ALU.mult, ("y", "c_hgrn_norm_w"), ("y",))
                    if m == "C":
                        self.tt(y, y, sv["mlstm_norm_w"], ALU.mult, ("y", "c_mlstm_norm_w"), ("y",))
                    self.tt(osl, y, gt, ALU.mult, ("y", "gt"), ("o",))
                else:
                    self.red(small[:, 16:24], h8(y), ("y",), ("sm2",))
                    self.ts(small[:, 16:24], small[:, 16:24], -1.0 / 64, None, ALU.mult, None, ("sm2",), ("sm2",))
                    self.tt(h8(y), h8(y), b8(small[:, 16:24]), ALU.add, ("y", "sm2"), ("y",))
                    self.tt(W[0], y, y, ALU.mult, ("y",), (wk(0),))
                    self.red(small[:, 24:32], h8(W[0]), (wk(0),), ("sm3",))
                    self.actf(small[:, 32:40], small[:, 24:32], AF.Sqrt, ("sm3", "eps"), ("sm4",), scale=1.0 / 64,
                              bias=self.eps_t[:, 1:2])
                    P.op("dve", lambda e: e.reciprocal(out=small[:, 40:48], in_=small[:, 32:40]), reads=("sm4",), writes=("sm5",))
                    self.tt(h8(y), h8(y), b8(small[:, 40:48]), ALU.mult, ("y", "sm5"), ("y",))
                    self.tt(y, y, sv["rwkv_ln_w"], ALU.mult, ("y", "c_rwkv_ln_w"), ("y",))
                    self.tt(y, y, sv["rwkv_ln_b"], ALU.add, ("y", "c_rwkv_ln_b"), ("y",))
                    P.dma("sp", W[1], bonus[rs, :], reads=(("bonus", tile),), writes=(wk(1),))
                    self.tt(y, y, W[1], ALU.add, ("y", wk(1)), ("y",))
                    self.tt(osl, y, gt, ALU.mult, ("y", "gt"), ("o",))
            P.dma("sp", obuf[rs, :], o, reads=("o",), writes=(("o", tile),))

    def phase_ffn(self, l, hbuf, obuf, wbo, wbg, wbu, wbd, wnb):
        P = self.P
        cfg = self.cfg
        NG = 4
        self.carve_linear(NG)
        actT = self.carve(44 * 256, BF16).rearrange("p (k g t) -> p k g t", g=NG, t=128)
        acc = [self.carve(2048) for _ in range(NG)]
        xT = self.xT
        for grp in range(cfg.NTILE // NG):
            for g in range(NG):
                tile = grp * NG + g
                ht = self.htile[tile % 2]
                P.dma("sp", ht, obuf[tile * 128:(tile + 1) * 128, :], reads=(("o", tile),), writes=(("htile", tile % 2),))
                self.cp(self.ub[:, 0:2048], ht, (("htile", tile % 2),), ("ub",), eng="dve")
                self.transpose_to(self.ub, "ub", xT, g, "xT", 16)

            def consume(g, cb, nc_, pst, pskey):
                self.cp(acc[g][:, cb * 512:cb * 512 + nc_], pst, (pskey,), (("acc", g),))

            self.linear_tm(xT, ("xT",), NG, 16, wbo, D, consume)
            self.resid_update(grp, NG, acc, hbuf, wnb[:, 1, :])
            for g in range(NG):
                tile = grp * NG + g
                ht = self.htile[tile % 2]
                P.dma("sp", ht, hbuf[tile * 128:(tile + 1) * 128, :], reads=(("h", tile),), writes=(("htile", tile % 2),))
                self.norm_transpose(ht, ("htile", tile % 2), wnb[:, 2, :], xT, g, "xT")
            for cb in range(DFF // 512):
                wg = self.wbuf[0].rearrange("p (k n) -> p k n", n=512)
                wu = self.wbuf[1].rearrange("p (k n) -> p k n", n=512)
                P.dma("sp", wg, wbg[cb, :, :, :], reads=(("wb", id(wbg), cb),), writes=(("wbuf", 0),))
                P.dma("sp", wu, wbu[cb, :, :, :], reads=(("wb", id(wbu), cb),), writes=(("wbuf", 1),))
                for j in range(4):
                    nchunk = cb * 4 + j
                    bg = self.psbank()
                    bu = self.psbank()
                    pg = self.pslin[:, bg, :]
                    pu = self.pslin[:, bu, :]
                    for kc in range(16):
                        P.op("pe", lambda e, pg=pg, kc=kc, j=j: e.matmul(pg, lhsT=wg[:, kc, j * 128:(j + 1) * 128],
                                                                         rhs=xT[:, kc, :, :].rearrange("p g t -> p (g t)"),
                                                                         start=(kc == 0), stop=(kc == 15)),
                             reads=("xT", ("wbuf", 0)), writes=(("pslin", bg),))
                    for kc in range(16):
                        P.op("pe", lambda e, pu=pu, kc=kc, j=j: e.matmul(pu, lhsT=wu[:, kc, j * 128:(j + 1) * 128],
                                                                         rhs=xT[:, kc, :, :].rearrange("p g t -> p (g t)"),
                                                                         start=(kc == 0), stop=(kc == 15)),
                             reads=("xT", ("wbuf", 1)), writes=(("pslin", bu),))
                    st = self.stage[self.ssel % 4]
                    skey = ("stage", self.ssel % 4)
                    self.ssel += 1
                    self.actf(st, pg, AF.Silu, (("pslin", bg),), (skey,))
                    self.tt(actT[:, nchunk, :, :].rearrange("p g t -> p (g t)"), st, pu, ALU.mult, (skey, ("pslin", bu)),
                            ("actT",))

            def consume2(g, cb, nc_, pst, pskey):
                self.cp(acc[g][:, cb * 128:cb * 128 + nc_], pst, (pskey,), (("acc", g),))

            self.linear_tm(actT, ("actT",), NG, 44, wbd, D, consume2, CB=128)
            self.resid_update(grp, NG, acc, hbuf, wnb[:, 3, :])

    def resid_update(self, grp, NG, acc, hbuf, wn):
        P = self.P
        for g in range(NG):
            tile = grp * NG + g
            ht = self.htile[tile % 2]
            hk = ("htile", tile % 2)
            P.dma("sp", ht, hbuf[tile * 128:(tile + 1) * 128, :], reads=(("h", tile),), writes=(hk,))
            self.rstd_of(acc[g], ("acc", g), 2048, self.eps_t[:, 0:1])
            self.stt(acc[g], acc[g], self.ss_t[:, 2:3], wn, ALU.mult, ALU.mult, (("acc", g), "ss2", "wnb"), (("acc", g),))
            self.tt(ht, ht, acc[g], ALU.add, (hk, ("acc", g)), (hk,))
            P.dma("sp", hbuf[tile * 128:(tile + 1) * 128, :], ht, reads=(hk,), writes=(("h", tile),))


def make_consts(T):
    half = 32
    inv_freq = 10000.0 ** (-np.arange(half, dtype=np.float32) / half)
    ang = np.arange(T, dtype=np.float32)[:, None] * inv_freq[None, :]
    cossin = np.concatenate([np.cos(ang), np.sin(ang)], axis=1).astype(np.float32)
    sel = np.zeros((32, TB, 128), np.float32)
    for tl in range(TB):
        for m in range(128):
            sel[(m // 64) * TB + tl, tl, m] = 1.0
    gam = np.zeros((128, 2, 4, 64), np.float32)
    for p in range(128):
        for hp in range(4):
            h = 2 * hp + p // 64
            gam[p, :, hp, :] = 1.0 - 2.0 ** (-5.0 - h)
    C = CHK["A"]
    j = (np.arange(128) % C).astype(np.float64)
    gh = 1.0 - 2.0 ** (-5.0 - np.arange(8, dtype=np.float64))
    atab = np.zeros((4, 128, 8, 64), np.float64)
    atab[0] = (gh[None, :] ** (j[:, None] + 1.0))[:, :, None]
    atab[1] = (0.125 * gh[None, :] ** (-(j[:, None] + 1.0)))[:, :, None]
    atab[2] = (0.125 * gh[None, :] ** (C - 1.0 - j[:, None]))[:, :, None]
    atab[3] = (gh ** float(C))[None, :, None]
    idx = np.arange(128)
    def trib(c):
        same = (idx[:, None] // c) == (idx[None, :] // c)
        return (same & (idx[:, None] <= idx[None, :])).astype(np.float32), same.astype(np.float32)
    t64, b64 = trib(64)
    t32, b32 = trib(32)
    trimats = np.stack([t64, b64, t32, b32]).astype(np.float32)
    cmask = (np.arange(64)[:, None] <= np.arange(64)[None, :]).astype(np.float32)
    i32 = np.arange(64) % 32
    dmasks = np.stack([(i32[:, None] < i32[None, :]), (i32[:, None] > i32[None, :]), (i32[:, None] <= i32[None, :])]).astype(
        np.float32)
    return dict(cossin=cossin, sel=sel, gamA=gam.reshape(128, 512), atab=atab.reshape(4, 128, 512).astype(np.float32),
                trimats=trimats, cmask=cmask, dmasks=dmasks,
                ident_b=np.eye(128).astype(ml_dtypes.bfloat16), ident_f=np.eye(128, dtype=np.float32))


def make_shared(inp, L, T):
    f = lambda a: np.ascontiguousarray(np.asarray(a, dtype=np.float32))
    sh = dict(w_in=f(inp["w_in"]), w_out=f(inp["w_out"]), w_ffn_gate=f(inp["w_ffn_gate"]), w_ffn_up=f(inp["w_ffn_up"]),
              w_ffn_down=f(inp["w_ffn_down"]))
    sh["norms"] = np.ascontiguousarray(np.stack([f(inp["norm_pre_mix"]), f(inp["norm_post_mix"]), f(inp["norm_pre_ffn"]),
                                                 f(inp["norm_post_ffn"])], axis=1))
    for nm in SMALL + ["hgrn_lb_logits", "mlstm_conv_w", "mlstm_conv_b", "rwkv_mu"]:
        sh[nm] = f(inp[nm])
    sh["mlstm_if_bias"] = np.ascontiguousarray(np.concatenate([f(inp["mlstm_i_bias"]), f(inp["mlstm_f_bias"])], axis=1))
    up = np.zeros((L, 128, 1024), np.float32)
    up[:, 0:64, 0:512] = f(inp["rwkv_w_up"])
    up[:, 64:128, 512:1024] = f(inp["rwkv_a_up"])
    sh["rwkv_up_pad"] = up
    sh["rwkv_g_up"] = f(inp["rwkv_g_up"])
    if L > 1:
        sh["rwkv_v0"] = f(inp["rwkv_v0"])
        sh["rwkv_v_down"] = f(inp["rwkv_v_down"])
        sh["rwkv_v_up"] = f(inp["rwkv_v_up"])
    else:
        sh["rwkv_v0"] = np.zeros((1, G), np.float32)
        sh["rwkv_v_down"] = np.zeros((1, G, 32), np.float32)
        sh["rwkv_v_up"] = np.zeros((1, 32, G), np.float32)
    sh.update(make_consts(T))
    return sh


_CACHE = {}


def kernel(**inp):
    x = np.asarray(inp["x"], dtype=np.float32)
    B, T, _ = x.shape
    L = inp["w_in"].shape[0]
    ncores = 8
    nseq = B // ncores
    cfg = Cfg(T=T, NSEQ=nseq, L=L)
    nc = Builder(cfg).build()
    sh = make_shared(inp, L, T)
    in_maps = []
    for c in range(ncores):
        m = dict(sh)
        m["x"] = np.ascontiguousarray(x[c * nseq:(c + 1) * nseq].reshape(nseq * T, D))
        in_maps.append(m)
    res = run_bass_kernel_spmd(nc, in_maps, core_ids=list(range(ncores)))
    outs = [np.asarray(r["out"]).reshape(nseq, T, D) for r in res.results]
    return np.concatenate(outs, axis=0).astype(np.float32)
```

```python
from contextlib import ExitStack
import numpy as np
import ml_dtypes
import concourse.bass as bass
import concourse.mybir as mybir
from concourse.bass_utils import run_bass_kernel_spmd

F32 = mybir.dt.float32
BF16 = mybir.dt.bfloat16
ALU = mybir.AluOpType
AF = mybir.ActivationFunctionType
AX = mybir.AxisListType

D = 2048
NIN = 7952
DFF = 5632
G = 512
NH = 8
HD = 64
NORM_EPS = 1e-6


class Prog:
    ENG = ("pe", "dve", "act", "pool", "sp")
    NDMASEM = 6

    def __init__(self, nc, stack):
        self.nc = nc
        self.stack = stack
        self.ops = {e: [] for e in self.ENG}
        self.cnt = {e: 0 for e in self.ENG}
        self.known = {e: {} for e in self.ENG}
        self.sem = {e: stack.enter_context(nc.semaphore("s_" + e)) for e in self.ENG if e != "sp"}
        self.semname = {}
        for e, s in self.sem.items():
            self.semname[id(s)] = e
        self.dq = {}
        for q in ("sp", "pool", "act"):
            self.dq[q] = {"sems": [stack.enter_context(nc.semaphore(f"d_{q}{i}")) for i in range(self.NDMASEM)],
                          "n": 0}
        self.bufs = {}
        self.nops = 0

    def _buf(self, k):
        b = self.bufs.get(k)
        if b is None:
            b = [None, {}]
            self.bufs[k] = b
        return b

    def _deps(self, e, reads, writes, extra=()):
        toks = list(extra)
        for k in reads:
            b = self._buf(k)
            if b[0] is not None:
                toks.append((b[0], False))
        for k in writes:
            b = self._buf(k)
            if b[0] is not None:
                toks.append((b[0], False))
            for s, v in b[1].values():
                toks.append(((s, v), True))
        own = self.sem.get(e)
        waits = []
        kn = self.known[e]
        for (s, v), is_reader in toks:
            if s is own and (e == "pe" or is_reader):
                continue
            if kn.get(id(s), 0) >= v:
                continue
            kn[id(s)] = v
            waits.append((s, v))
        return waits

    def _commit(self, tok, reads, writes):
        s, v = tok
        for k in reads:
            b = self._buf(k)
            b[1][id(s)] = (s, v)
        for k in writes:
            b = self._buf(k)
            b[0] = tok
            b[1] = {}

    def op(self, e, fn, reads=(), writes=()):
        reads = tuple(reads)
        writes = tuple(writes)
        waits = self._deps(e, reads, writes)
        self.cnt[e] += 1
        tok = (self.sem[e], self.cnt[e])
        self.ops[e].append((waits, fn, (self.sem[e], 1)))
        self._commit(tok, reads, writes)
        self.nops += 1
        return tok

    def dma(self, q, out, in_, reads=(), writes=(), **kw):
        reads = tuple(reads)
        writes = tuple(writes)
        dq = self.dq[q]
        n = dq["n"]
        dq["n"] += 1
        s = dq["sems"][n % self.NDMASEM]
        prev = 16 * (n // self.NDMASEM)
        extra = [((s, prev), False)] if prev > 0 else []
        waits = self._deps(q, reads, writes, extra)
        tok = (s, prev + 16)
        self.ops[q].append((waits, lambda eng: eng.dma_start(out=out, in_=in_, **kw), (s, 16)))
        self._commit(tok, reads, writes)
        self.nops += 1
        return tok

    def barrier(self):
        toks = [(self.sem[e], self.cnt[e]) for e in self.sem if self.cnt[e] > 0] + self.all_dma_tokens(skip=("pool",))
        for e in self.ENG:
            waits = []
            kn = self.known[e]
            for s, v in toks:
                if e == "pe" and s is self.sem.get("pe"):
                    continue
                if kn.get(id(s), 0) >= v:
                    continue
                kn[id(s)] = v
                waits.append((s, v))
            if waits:
                self.ops[e].append((waits, None, None))
        self.bufs = {k: v for k, v in self.bufs.items() if isinstance(k, tuple) and k and k[0] == "wb"}

    def finish(self, e, toks):
        waits = []
        for s, v in toks:
            waits.append((s, v))
        self.ops[e].append((waits, None, None))

    def all_dma_tokens(self, skip=()):
        toks = []
        for q, dq in self.dq.items():
            if q in skip:
                continue
            n = dq["n"]
            for i in range(self.NDMASEM):
                c = (n - i + self.NDMASEM - 1) // self.NDMASEM if n > i else 0
                if c > 0:
                    toks.append((dq["sems"][i], 16 * c))
        return toks

    def emit(self):
        nc = self.nc
        ops = self.ops

        def run(eng, lst):
            for waits, fn, inc in lst:
                for s, v in waits:
                    eng.wait_ge(s, v)
                if fn is not None:
                    ins = fn(eng)
                    ins.then_inc(inc[0], inc[1])

        with nc.Block() as block:
            @block.tensor
            def _(eng):
                run(eng, ops["pe"])

            @block.vector
            def _(eng):
                run(eng, ops["dve"])

            @block.scalar
            def _(eng):
                run(eng, ops["act"])

            @block.gpsimd
            def _(eng):
                run(eng, ops["pool"])

            @block.sync
            def _(eng):
                run(eng, ops["sp"])


RWC = 1792
OFFS = {"A": 0, "B": 2048, "C": 4096, "D": 6160}
VECS = {"D": ["rD", "kD", "wD", "kkD", "bD"]}
ALLV = VECS["D"]
CHK = {"A": 64, "B": 32, "C": 64}
TB = 16
SMALL = ["hgrn_norm_w", "mlstm_norm_w", "rwkv_w0", "rwkv_a0", "rwkv_k_k", "rwkv_k_a", "rwkv_r_k", "rwkv_ln_w",
         "rwkv_ln_b"]


class Cfg:
    def __init__(self, T=2048, NSEQ=2, L=4):
        self.T = T
        self.NSEQ = NSEQ
        self.L = L
        self.NT = T * NSEQ
        self.TPS = T // 128
        self.NTILE = self.NT // 128


def wb_shape(K, N, CB=512):
    ncb = (N + CB - 1) // CB
    return [ncb, 128, K // 128, CB]


class Builder:
    def __init__(self, cfg):
        self.cfg = cfg
        self.nc = bass.Bass("TRN2", target_bir_lowering=False)
        self.stack = ExitStack()
        self.P = None

    def dram_in(self, name, shape, dt=F32):
        return self.nc.dram_tensor(name, list(shape), dt, kind="ExternalInput").ap()

    def dram_out(self, name, shape, dt=F32):
        return self.nc.dram_tensor(name, list(shape), dt, kind="ExternalOutput").ap()

    def dram_tmp(self, name, shape, dt=F32):
        return self.nc.dram_tensor(name, list(shape), dt, kind="Internal").ap()

    def sb(self, name, shape, dt=F32):
        h = self.stack.enter_context(self.nc.sbuf_tensor(name, list(shape), dt))
        return h[tuple(slice(None) for _ in shape)]

    def ps(self, name, shape, dt=F32):
        return self.stack.enter_context(self.nc.psum_tensor(name, list(shape), dt))

    def carve_reset(self):
        self.cptr = 0

    def carve(self, ncols, dt=F32):
        a = self.arena[:, self.cptr:self.cptr + ncols]
        self.cptr += ncols
        assert self.cptr <= self.NARENA, self.cptr
        if dt == BF16:
            a = a.bitcast(BF16)
        return a

    def tt(self, out, a, b, op, r, w, eng="dve"):
        self.P.op(eng, lambda e: e.tensor_tensor(out=out, in0=a, in1=b, op=op), reads=r, writes=w)

    def ts(self, out, a, s1, s2, op0, op1, r, w, eng="dve"):
        if op1 is None:
            self.P.op(eng, lambda e: e.tensor_scalar(out=out, in0=a, scalar1=s1, scalar2=None, op0=op0), reads=r, writes=w)
        else:
            self.P.op(eng, lambda e: e.tensor_scalar(out=out, in0=a, scalar1=s1, scalar2=s2, op0=op0, op1=op1),
                      reads=r, writes=w)

    def stt(self, out, a, scalar, b, op0, op1, r, w):
        self.P.op("dve", lambda e: e.scalar_tensor_tensor(out=out, in0=a, scalar=scalar, in1=b, op0=op0, op1=op1),
                  reads=r, writes=w)

    def actf(self, out, a, func, r, w, scale=1.0, bias=None):
        if bias is None:
            self.P.op("act", lambda e: e.activation(out=out, in_=a, func=func, scale=scale), reads=r, writes=w)
        else:
            self.P.op("act", lambda e: e.activation(out=out, in_=a, func=func, scale=scale, bias=bias), reads=r, writes=w)

    def red(self, out, a, r, w):
        self.P.op("dve", lambda e: e.tensor_reduce(out=out, in_=a, axis=AX.X, op=ALU.add), reads=r, writes=w)

    def cp(self, out, a, r, w, eng="act"):
        if eng == "act":
            self.P.op("act", lambda e: e.copy(out=out, in_=a), reads=r, writes=w)
        else:
            self.P.op(eng, lambda e: e.tensor_copy(out=out, in_=a), reads=r, writes=w)

    def psbank(self):
        b = self.psel % self.NPS
        self.psel += 1
        return b

    def cast_weight(self, W, Wb, K, N, CB=512):
        P = self.P
        KC = K // 128
        ncb = (N + CB - 1) // CB
        Wv = W.rearrange("(kc p) n -> p kc n", p=128)
        for cb in range(ncb):
            c0 = cb * CB
            nc_ = min(CB, N - c0)
            for k0 in range(0, KC, 16):
                k1 = min(KC, k0 + 16)
                P.dma("pool", Wb[cb, :, k0:k1, 0:nc_], Wv[:, k0:k1, c0:c0 + nc_], reads=(), writes=(("wb", id(Wb), cb),))

    def linear_tm(self, xT, xkeys, ng, KC, Wb, N, consume, CB=512):
        P = self.P
        ncb = (N + CB - 1) // CB
        for cb in range(ncb):
            nc_ = min(CB, N - cb * CB)
            b = self.wsel % 2
            self.wsel += 1
            wbuf = self.wbuf[b][:, 0:KC * CB].rearrange("p (k n) -> p k n", n=CB)
            wkeys = (("wbufh", b, 0), ("wbufh", b, 1))
            P.dma("sp", wbuf[:, :, 0:nc_], Wb[cb, :, 0:KC, 0:nc_], reads=(("wb", id(Wb), cb),), writes=wkeys)
            for g in range(ng):
                bank = self.psbank()
                pst = self.pslin[:, bank, 0:nc_]
                for kc in range(KC):
                    P.op("pe", (lambda e, pst=pst, g=g, kc=kc, wbuf=wbuf, nc_=nc_:
                                e.matmul(pst, lhsT=xT[:, kc, g, :], rhs=wbuf[:, kc, 0:nc_],
                                         start=(kc == 0), stop=(kc == KC - 1))),
                         reads=tuple(xkeys) + wkeys, writes=(("pslin", bank),))
                consume(g, cb, nc_, pst, ("pslin", bank))

    def rstd_of(self, src, src_key, n, eps_ap):
        P = self.P
        ss = self.ss_t
        junk = self.junk
        P.op("act", lambda e: e.activation(out=junk[:, 0:n], in_=src, func=AF.Square, accum_out=ss[:, 0:1]),
             reads=(src_key,), writes=("junk", "ss"))
        P.op("act", lambda e: e.activation(out=ss[:, 1:2], in_=ss[:, 0:1], func=AF.Sqrt, scale=1.0 / n, bias=eps_ap),
             reads=("ss", "eps"), writes=("ss1",))
        P.op("dve", lambda e: e.reciprocal(out=ss[:, 2:3], in_=ss[:, 1:2]), reads=("ss1",), writes=("ss2",))

    def norm_transpose(self, src_tile, src_key, wn_b, xT, g, xkey):
        self.rstd_of(src_tile, src_key, 2048, self.eps_t[:, 0:1])
        ub = self.ub
        self.stt(ub[:, 0:2048], src_tile, self.ss_t[:, 2:3], wn_b, ALU.mult, ALU.mult, (src_key, "ss2", "wnb"), ("ub",))
        self.transpose_to(ub, "ub", xT, g, xkey, 16)

    def transpose_to(self, ub, ubkey, xT, g, xkey, KC):
        P = self.P
        for k0 in range(0, KC, 4):
            k1 = min(KC, k0 + 4)
            bank = self.tsel % 2
            self.tsel += 1
            pt = self.pstr[:, bank, :]
            for kc in range(k0, k1):
                P.op("pe", (lambda e, kc=kc, pt=pt, k0=k0:
                            e.transpose(pt[:, (kc - k0) * 128:(kc - k0 + 1) * 128], ub[:, kc * 128:(kc + 1) * 128],
                                        self.ident_b[:, :])),
                     reads=(ubkey, "ident"), writes=(("pstr", bank),))
            n = k1 - k0
            P.op("act", (lambda e, pt=pt, k0=k0, n=n:
                         e.copy(out=xT[:, k0:k0 + n, g, :], in_=pt[:, 0:n * 128].rearrange("p (k t) -> p k t", t=128))),
                 reads=(("pstr", bank),), writes=(xkey,))

    def store_T(self, src, src_key, dstT, s, g):
        P = self.P
        vt = self.vt
        for hp in range(4):
            bank = self.psbank()
            pt = self.pslin[:, bank, 0:128]
            P.op("pe", lambda e, pt=pt, hp=hp: e.transpose(pt, src[:, hp * 128:(hp + 1) * 128], self.ident_f[:, :]),
                 reads=(src_key, "identf"), writes=(("pslin", bank),))
            self.cp(vt[:, hp, :], pt, (("pslin", bank),), ("vt",))
        P.dma("sp", dstT[:, :, s, g * 128:(g + 1) * 128].rearrange("hp p t -> p hp t"), vt[:, :, :], reads=("vt",),
              writes=(("T", id(dstT), s, g),))

    def bt_store(self, src_bf, src_key, dstT, s, g):
        P = self.P
        bank = self.tsel % 2
        self.tsel += 1
        pt = self.pstr[:, bank, :]
        for hp in range(4):
            P.op("pe", lambda e, hp=hp, pt=pt: e.transpose(pt[:, hp * 128:(hp + 1) * 128], src_bf[:, hp * 128:(hp + 1) * 128],
                                                          self.ident_b[:, :]),
                 reads=(src_key, "ident"), writes=(("pstr", bank),))
        tb = self.tbt
        P.op("act", lambda e, pt=pt: e.copy(out=tb[:, :, :], in_=pt[:, 0:512].rearrange("p (k t) -> p k t", t=128)),
             reads=(("pstr", bank),), writes=("tbt",))
        P.dma("sp", dstT[:, :, s, g * 128:(g + 1) * 128].rearrange("hp p t -> p hp t"), tb[:, :, :], reads=("tbt",),
              writes=(("T", id(dstT), s, g),))

    def store_v(self, v_src, v_key, v_d, rs):
        self.cp(self.vb[:, :, 0:64], v_src.rearrange("p (h e) -> p h e", e=64), (v_key,), ("vb",), eng="dve")
        self.P.dma("sp", v_d[rs, :], self.vb.rearrange("p h e -> p (h e)"), reads=("vb",), writes=(("vd", id(v_d)),))

    def load_T(self, dst, dst_key, srcT, s, g):
        P = self.P
        vt = self.vt
        P.dma("sp", vt[:, :, :], srcT[:, :, s, g * 128:(g + 1) * 128].rearrange("hp p t -> p hp t"),
              reads=(("T", id(srcT), s, g),), writes=("vt",))
        for hp in range(4):
            bank = self.psbank()
            pt = self.pslin[:, bank, 0:128]
            P.op("pe", lambda e, pt=pt, hp=hp: e.transpose(pt, vt[:, hp, :], self.ident_f[:, :]),
                 reads=("vt", "identf"), writes=(("pslin", bank),))
            self.cp(dst[:, hp * 128:(hp + 1) * 128], pt, (("pslin", bank),), (dst_key,))

    def load_shift(self, dst, key, c0, n, tile, k, extra_keys=()):
        P = self.P
        keys = (key,) + tuple(extra_keys)
        cfg = self.cfg
        g = tile % cfg.TPS
        r0 = tile * 128
        if g == 0:
            P.op("dve", lambda e: e.memset(dst[:, 0:n], 0.0), writes=keys)
            P.dma("sp", dst[k:128, 0:n], self.pbuf[r0:r0 + 128 - k, c0:c0 + n],
                  reads=tuple(("p", tile, cb) for cb in range(16)), writes=keys)
        else:
            P.dma("sp", dst[:, 0:n], self.pbuf[r0 - k:r0 - k + 128, c0:c0 + n],
                  reads=tuple(("p", tile, cb) for cb in range(16)) + tuple(("p", tile - 1, cb) for cb in range(16)),
                  writes=keys)

    def bcast_load(self, dst, src_row, key):
        self.P.dma("sp", dst, src_row.partition_broadcast(128), writes=(key,))

    def build(self, dbg=None, layers=None):
        cfg = self.cfg
        nc = self.nc
        L = cfg.L
        NT = cfg.NT
        T = cfg.T
        NSEQ = cfg.NSEQ
        self.P = P = Prog(nc, self.stack)
        layers = list(range(L)) if layers is None else layers
        I = {}
        x = self.dram_in("x", [NT, D])
        out = self.dram_out("out", [NT, D])
        w_in = self.dram_in("w_in", [L, D, NIN])
        w_out = self.dram_in("w_out", [L, D, D])
        w_g = self.dram_in("w_ffn_gate", [L, D, DFF])
        w_u = self.dram_in("w_ffn_up", [L, D, DFF])
        w_d = self.dram_in("w_ffn_down", [L, DFF, D])
        norms = self.dram_in("norms", [L, 4, D])
        for nm in SMALL:
            I[nm] = self.dram_in(nm, [L, G])
        I["hgrn_lb_logits"] = self.dram_in("hgrn_lb_logits", [L, G])
        I["mlstm_conv_w"] = self.dram_in("mlstm_conv_w", [L, 4, 2 * G])
        I["mlstm_conv_b"] = self.dram_in("mlstm_conv_b", [L, 2 * G])
        I["mlstm_if_bias"] = self.dram_in("mlstm_if_bias", [L, 16])
        I["rwkv_mu"] = self.dram_in("rwkv_mu", [L, RWC])
        I["rwkv_up_pad"] = self.dram_in("rwkv_up_pad", [L, 128, 1024])
        I["rwkv_g_up"] = self.dram_in("rwkv_g_up", [L, 128, G])
        I["rwkv_v0"] = self.dram_in("rwkv_v0", [max(L - 1, 1), G])
        I["rwkv_v_down"] = self.dram_in("rwkv_v_down", [max(L - 1, 1), G, 32])
        I["rwkv_v_up"] = self.dram_in("rwkv_v_up", [max(L - 1, 1), 32, G])
        ident_b_d = self.dram_in("ident_b", [128, 128], BF16)
        ident_f_d = self.dram_in("ident_f", [128, 128])
        cs_d = self.dram_in("cossin", [T, 64])
        sel_d = self.dram_in("sel", [32, TB, 128])
        gam_d = self.dram_in("gamA", [128, 512])
        atab_d = self.dram_in("atab", [4, 128, 512])
        tri_d = self.dram_in("trimats", [4, 128, 128])
        mask_d = self.dram_in("cmask", [64, 64])

        hbuf = self.dram_tmp("hbuf", [NT, D])
        self.pbuf = pbuf = self.dram_tmp("pbuf", [NT, NIN])
        obuf = self.dram_tmp("obuf", [NT, D])
        rows = {v: self.dram_tmp("row_" + v, [NT, G]) for v in ALLV}
        vT = {m: self.dram_tmp("vT_" + m, [4, 128, NSEQ, T]) for m in "D"}
        yT = {m: self.dram_tmp("yT_" + m, [4, 128, NSEQ, T]) for m in "D"}
        ck = dict(
            qT={m: self.dram_tmp("cqT_" + m, [4, 128, NSEQ, T], BF16) for m in "ABC"},
            kT={m: self.dram_tmp("ckT_" + m, [4, 128, NSEQ, T], BF16) for m in "ABC"},
            kh={m: self.dram_tmp("ckh_" + m, [NT, G], BF16) for m in "ABC"},
            v={m: self.dram_tmp("cv_" + m, [NT, 520], BF16) for m in "ABC"},
            dec={m: self.dram_tmp("cdec_" + m, [NT, G]) for m in "ABC"},
            y={m: self.dram_tmp("cy_" + m, [NT, 520]) for m in "ABC"},
            atab=atab_d, tri=tri_d, mask=mask_d)
        dmask_d = self.dram_in("dmasks", [3, 64, 64])
        dk = dict(rT=self.dram_tmp("d_rT", [4, 128, NSEQ, T], BF16), cT=self.dram_tmp("d_cT", [4, 128, NSEQ, T], BF16),
                  kT=self.dram_tmp("d_kT", [4, 128, NSEQ, T], BF16), bT=self.dram_tmp("d_bT", [4, 128, NSEQ, T], BF16),
                  kh=self.dram_tmp("d_kh", [NT, G], BF16), bh=self.dram_tmp("d_bh", [NT, G], BF16),
                  v=self.dram_tmp("d_v", [NT, G], BF16), dec=self.dram_tmp("d_dec", [NT, G]),
                  y=self.dram_tmp("d_y", [NT, G]), masks=dmask_d)
        ck["D"] = dk
        gate = {m: self.dram_tmp("gate_" + m, [NT, G]) for m in "ABCD"}
        bonus = self.dram_tmp("bonus", [NT, G])
        vfirst = self.dram_tmp("vfirst", [NT, G])
        wb_in = [self.dram_tmp(f"wb_in{l}", wb_shape(D, NIN), BF16) for l in range(L)]
        wb_out = [self.dram_tmp(f"wb_out{l}", wb_shape(D, D), BF16) for l in range(L)]
        wb_g = [self.dram_tmp(f"wb_g{l}", wb_shape(D, DFF), BF16) for l in range(L)]
        wb_u = [self.dram_tmp(f"wb_u{l}", wb_shape(D, DFF), BF16) for l in range(L)]
        wb_d = [self.dram_tmp(f"wb_d{l}", wb_shape(DFF, D, 128), BF16) for l in range(L)]
        dbg_t = None
        if dbg is not None and dbg[1] is not None:
            dbg_t = self.dram_out("dbg", dbg[1])

        self.ident_b = self.sb("ident_b_s", [128, 128], BF16)
        self.ident_f = self.sb("ident_f_s", [128, 128])
        self.ss_t = self.sb("ss", [128, 8])
        self.eps_t = self.sb("eps_t", [128, 4])
        gamA = self.sb("gamA_s", [128, 512])
        lbd = self.dram_tmp("lbd", [2, 128, L, G])
        wnb = self.sb("wnb", [128, 4, D])
        self.NARENA = 41000
        self.arena = self.sb("arena", [128, self.NARENA])
        self.NPS = 6
        self.pslin = self.ps("pslin", [128, self.NPS, 512])
        self.psel = 0
        self.pstr = self.ps("pstr", [128, 2, 1024], BF16)
        self.tsel = 0
        self.wsel = 0

        P.dma("sp", self.ident_b[:, :], ident_b_d[:, :], writes=("ident",))
        P.dma("sp", self.ident_f[:, :], ident_f_d[:, :], writes=("identf",))
        P.dma("sp", gamA[:, :], gam_d[:, :], writes=("gamA",))
        P.op("dve", lambda e: e.memset(self.eps_t[:, 0:1], NORM_EPS), writes=("eps",))
        P.op("dve", lambda e: e.memset(self.eps_t[:, 1:2], 64e-5), writes=("eps",))
        P.op("dve", lambda e: e.memset(self.eps_t[:, 2:3], 0.0), writes=("eps",))
        for i in range(0, NT, 512):
            P.dma("sp", hbuf[i:i + 512, :], x[i:i + 512, :], writes=tuple(("h", j) for j in range(i // 128, i // 128 + 4)))
        for l in layers:
            self.cast_weight(w_in[l], wb_in[l], D, NIN)
            self.cast_weight(w_out[l], wb_out[l], D, D)
            self.cast_weight(w_g[l], wb_g[l], D, DFF)
            self.cast_weight(w_u[l], wb_u[l], D, DFF)
            self.cast_weight(w_d[l], wb_d[l], DFF, D, 128)

        self.carve_reset()
        ex = self.carve(L * G).rearrange("p (l g) -> p l g", g=G)
        sm = self.carve(G)
        lbt = self.carve(L * G).rearrange("p (l g) -> p l g", g=G)
        oml = self.carve(L * G).rearrange("p (l g) -> p l g", g=G)
        P.dma("sp", ex, I["hgrn_lb_logits"].partition_broadcast(128), writes=("ex",))
        self.actf(ex, ex, AF.Exp, ("ex",), ("ex",))
        self.cp(sm, ex[:, 0, :], ("ex",), ("sm",), eng="dve")
        for j in range(1, L):
            self.tt(sm, sm, ex[:, j, :], ALU.add, ("sm", "ex"), ("sm",))
        P.op("dve", lambda e: e.reciprocal(out=sm, in_=sm), reads=("sm",), writes=("sm",))
        P.op("dve", lambda e: e.memset(lbt[:, 0, :], 0.0), writes=("lbt",))
        for j in range(1, L):
            self.tt(ex[:, j, :], ex[:, j, :], sm, ALU.mult, ("ex", "sm"), ("ex",))
            self.tt(lbt[:, j, :], lbt[:, j - 1, :], ex[:, j, :], ALU.add, ("lbt", "ex"), ("lbt",))
        self.ts(oml[:, :, :], lbt[:, :, :], -1.0, 1.0, ALU.mult, ALU.add, ("lbt",), ("oml",))
        P.dma("sp", lbd[0], lbt, reads=("lbt",), writes=("lbd",))
        P.dma("sp", lbd[1], oml, reads=("oml",), writes=("lbd",))
        P.barrier()

        for l in layers:
            P.dma("sp", wnb[:, :, :], norms[l].partition_broadcast(128), writes=("wnb",))
            self.phase_p1(l, hbuf, wb_in[l], wnb)
            P.barrier()
            if dbg is not None and dbg[0] == "p" and l == layers[-1]:
                break
            self.phase_prep(l, I, rows, vT, gate, bonus, vfirst, cs_d, lbd, ck)
            P.barrier()
            if dbg is not None and dbg[0] == "prep" and l == layers[-1]:
                break
            self.phase_scan(rows, vT, yT, sel_d, ck, only_abc=(dbg is not None and dbg[0] == "scanabc"))
            P.barrier()
            if dbg is not None and dbg[0] == "scanabc" and l == layers[-1]:
                break
            if dbg is not None and dbg[0] == "scan" and l == layers[-1]:
                break
            self.phase_post(l, I, yT, ck, gate, bonus, obuf)
            P.barrier()
            if dbg is not None and dbg[0] == "post" and l == layers[-1]:
                break
            self.phase_ffn(l, hbuf, obuf, wb_out[l], wb_g[l], wb_u[l], wb_d[l], wnb)
            P.barrier()

        if dbg_t is not None:
            src = {"p": pbuf, "post": obuf, "h": hbuf}.get(dbg[0])
            P.dma("sp", dbg_t, src)
        for i in range(0, NT, 512):
            P.dma("sp", out[i:i + 512, :], hbuf[i:i + 512, :])
        P.finish("sp", P.all_dma_tokens())
        P.emit()
        return nc

    def carve_linear(self, ng):
        self.carve_reset()
        self.xT = self.carve(16 * ng * 64, BF16).rearrange("p (k g t) -> p k g t", g=ng, t=128)
        self.wbuf = [self.carve(4096, BF16) for _ in range(2)]
        self.htile = [self.carve(2048) for _ in range(2)]
        self.junk = self.carve(2048)
        self.ub = self.carve(1024, BF16)
        self.stage = [self.carve(512) for _ in range(4)]
        self.ssel = 0

    def phase_p1(self, l, hbuf, wb, wnb):
        P = self.P
        cfg = self.cfg
        NG = 4
        self.carve_linear(NG)
        for grp in range(cfg.NTILE // NG):
            for g in range(NG):
                tile = grp * NG + g
                ht = self.htile[tile % 2]
                P.dma("sp", ht, hbuf[tile * 128:(tile + 1) * 128, :], reads=(("h", tile),), writes=(("htile", tile % 2),))
                self.norm_transpose(ht, ("htile", tile % 2), wnb[:, 0, :], self.xT, g, "xT")

            def consume(g, cb, nc_, pst, pskey, grp=grp):
                st = self.stage[self.ssel % 4]
                skey = ("stage", self.ssel % 4)
                self.ssel += 1
                self.cp(st[:, 0:nc_], pst, (pskey,), (skey,))
                tile = grp * NG + g
                P.dma("sp", self.pbuf[tile * 128:(tile + 1) * 128, cb * 512:cb * 512 + nc_], st[:, 0:nc_],
                      reads=(skey,), writes=(("p", tile, cb),))

            self.linear_tm(self.xT, ("xT",), NG, 16, wb, NIN, consume)

    def phase_prep(self, l, I, rows, vT, gate, bonus, vfirst, cs_d, lbd, ck):
        P = self.P
        cfg = self.cfg
        self.carve_reset()
        seg = self.carve(2064)
        sh = self.carve(3072)
        Wt = [self.carve(512) for _ in range(10)]
        R = self.carve(1024)
        cst = self.carve(64)
        self.vt = self.carve(512).rearrange("p (a b) -> p a b", b=128)
        cw = self.carve(4096).rearrange("p (j c) -> p j c", c=1024)
        cb_ = self.carve(1024)
        mu = self.carve(RWC)
        sv = {nm: self.carve(512) for nm in SMALL}
        v0b = self.carve(512)
        ifb = self.carve(16)
        upw = self.carve(1024)
        gup = self.carve(512)
        vdn = self.carve(128).rearrange("p (c n) -> p c n", n=32)
        vup = self.carve(512)
        small = self.carve(64)
        small2 = self.carve(64)
        QB = self.carve(256, BF16)
        KB = self.carve(256, BF16)
        KH = self.carve(256, BF16)
        XB1 = self.carve(256, BF16)
        XB2 = self.carve(256, BF16)
        XB3 = self.carve(256, BF16)
        XB4 = self.carve(256, BF16)
        self.tbt = self.carve(256, BF16).rearrange("p (a b) -> p a b", b=128)
        self.vb = self.carve(260, BF16).rearrange("p (h e) -> p h e", e=65)
        AT = self.carve(2048).rearrange("p (a c) -> p a c", c=512)
        tri = self.carve(512).rearrange("p (a c) -> p a c", c=128)
        P.dma("sp", AT, ck["atab"].rearrange("a p c -> p a c"), writes=("AT",))
        P.dma("sp", tri, ck["tri"].rearrange("a p c -> p a c"), writes=("tri",))
        P.op("dve", lambda e: e.memset(self.vb[:, :, 64:65], 1.0), writes=("vb",))
        lbt = self.carve(512)
        oml = self.carve(512)
        P.dma("sp", lbt, lbd[0, :, l, :], writes=("lbt",))
        P.dma("sp", oml, lbd[1, :, l, :], writes=("oml",))
        for nm in SMALL:
            self.bcast_load(sv[nm], I[nm][l], "c_" + nm)
        self.bcast_load(cw, I["mlstm_conv_w"][l], "cw")
        self.bcast_load(cb_, I["mlstm_conv_b"][l], "cb")
        self.bcast_load(mu, I["rwkv_mu"][l], "mu")
        self.bcast_load(ifb, I["mlstm_if_bias"][l], "ifb")
        P.dma("sp", upw, I["rwkv_up_pad"][l], writes=("upw",))
        P.dma("sp", gup, I["rwkv_g_up"][l], writes=("gup",))
        if l > 0:
            self.bcast_load(v0b, I["rwkv_v0"][l - 1], "v0b")
            P.dma("sp", vdn, I["rwkv_v_down"][l - 1].rearrange("(c p) n -> p c n", p=128), writes=("vdn",))
            P.dma("sp", vup[0:32, :], I["rwkv_v_up"][l - 1], writes=("vup",))
        pall = lambda tile: tuple(("p", tile, cb) for cb in range(16))
        W = Wt
        wk = lambda i: ("W", i)

        def h8(ap):
            return ap.rearrange("p (h d) -> p h d", d=64)

        def b8(ap8):
            return ap8.unsqueeze(2).to_broadcast([128, 8, 64])

        for tile in range(cfg.NTILE):
            s, g = tile // cfg.TPS, tile % cfg.TPS
            r0 = tile * 128
            rs = slice(r0, r0 + 128)
            P.dma("sp", seg[:, 0:2048], self.pbuf[rs, 0:2048], reads=pall(tile), writes=("seg",))
            P.dma("sp", cst, cs_d[g * 128:(g + 1) * 128, :], writes=("cst",))
            qk = seg[:, 0:1024].rearrange("p (h two d) -> p h two d", two=2, d=32)
            Rv = R.rearrange("p (h two d) -> p h two d", two=2, d=32)
            t1, t2 = qk[:, :, 0, :], qk[:, :, 1, :]
            cosb = cst[:, 0:32].unsqueeze(1).to_broadcast([128, 16, 32])
            sinb = cst[:, 32:64].unsqueeze(1).to_broadcast([128, 16, 32])
            w0v = W[0].rearrange("p (h d) -> p h d", d=32)
            w1v = W[1].rearrange("p (h d) -> p h d", d=32)
            self.tt(w0v, t1, cosb, ALU.mult, ("seg", "cst"), (wk(0),))
            self.tt(w1v, t2, sinb, ALU.mult, ("seg", "cst"), (wk(1),))
            self.tt(Rv[:, :, 0, :], w0v, w1v, ALU.subtract, (wk(0), wk(1)), ("R",))
            self.tt(w0v, t1, sinb, ALU.mult, ("seg", "cst"), (wk(0),))
            self.tt(w1v, t2, cosb, ALU.mult, ("seg", "cst"), (wk(1),))
            self.tt(Rv[:, :, 1, :], w0v, w1v, ALU.add, (wk(0), wk(1)), ("R",))
            self.tt(QB, R[:, 0:512], AT[:, 0, :], ALU.mult, ("R", "AT"), ("QB",))
            self.bt_store(QB, "QB", ck["qT"]["A"], s, g)
            self.tt(KB, R[:, 512:1024], AT[:, 1, :], ALU.mult, ("R", "AT"), ("KB",))
            self.bt_store(KB, "KB", ck["kT"]["A"], s, g)
            self.tt(KH, R[:, 512:1024], AT[:, 2, :], ALU.mult, ("R", "AT"), ("KH",))
            P.dma("sp", ck["kh"]["A"][rs, :], KH, reads=("KH",), writes=(("kh", "A"),))
            self.store_v(seg[:, 1024:1536], "seg", ck["v"]["A"], rs)
            P.dma("sp", ck["dec"]["A"][rs, :], AT[:, 3, :], reads=("AT",), writes=(("dec", "A"),))
            self.actf(W[2], seg[:, 1536:2048], AF.Silu, ("seg",), (wk(2),))
            P.dma("sp", gate["A"][rs, :], W[2], reads=(wk(2),), writes=(("gate", "A", tile),))
            P.dma("sp", seg[:, 0:2048], self.pbuf[rs, 2048:4096], reads=pall(tile), writes=("seg",))
            self.actf(W[0], seg[:, 0:512], AF.Silu, ("seg",), (wk(0),))
            self.ts(W[0], W[0], 0.125, None, ALU.mult, None, (wk(0),), (wk(0),))
            self.actf(W[1], seg[:, 512:1024], AF.Sigmoid, ("seg",), (wk(1),))
            self.tt(W[1], W[1], oml, ALU.mult, (wk(1), "oml"), (wk(1),))
            self.tt(W[1], W[1], lbt, ALU.add, (wk(1), "lbt"), (wk(1),))
            self.ts(W[3], W[1], -1.0, 1.0, ALU.mult, ALU.add, (wk(1),), (wk(3),))
            self.actf(W[4], W[1], AF.Ln, (wk(1),), (wk(4),))
            ba = self.psbank()
            pc = self.pslin[:, ba, :]
            P.op("pe", lambda e, pc=pc: e.matmul(pc, lhsT=tri[:, 2, :], rhs=W[4], start=True, stop=True),
                 reads=(wk(4), "tri"), writes=(("pslin", ba),))
            bb = self.psbank()
            ptot = self.pslin[:, bb, :]
            P.op("pe", lambda e, ptot=ptot: e.matmul(ptot, lhsT=tri[:, 3, :], rhs=W[4], start=True, stop=True),
                 reads=(wk(4), "tri"), writes=(("pslin", bb),))
            self.actf(W[5], pc, AF.Exp, (("pslin", ba),), (wk(5),))
            self.tt(QB, W[0], W[5], ALU.mult, (wk(0), wk(5)), ("QB",))
            self.bt_store(QB, "QB", ck["qT"]["B"], s, g)
            self.actf(W[5], pc, AF.Exp, (("pslin", ba),), (wk(5),), scale=-1.0)
            self.tt(KB, W[3], W[5], ALU.mult, (wk(3), wk(5)), ("KB",))
            self.bt_store(KB, "KB", ck["kT"]["B"], s, g)
            self.cp(W[6], ptot, (("pslin", bb),), (wk(6),))
            self.actf(W[7], W[6], AF.Exp, (wk(6),), (wk(7),))
            P.dma("sp", ck["dec"]["B"][rs, :], W[7], reads=(wk(7),), writes=(("dec", "B"),))
            self.tt(W[6], W[6], pc, ALU.subtract, (wk(6), ("pslin", ba)), (wk(6),))
            self.actf(W[6], W[6], AF.Exp, (wk(6),), (wk(6),))
            self.tt(KH, W[3], W[6], ALU.mult, (wk(3), wk(6)), ("KH",))
            P.dma("sp", ck["kh"]["B"][rs, :], KH, reads=("KH",), writes=(("kh", "B"),))
            self.store_v(seg[:, 1024:1536], "seg", ck["v"]["B"], rs)
            self.actf(W[2], seg[:, 1536:2048], AF.Sigmoid, ("seg",), (wk(2),))
            P.dma("sp", gate["B"][rs, :], W[2], reads=(wk(2),), writes=(("gate", "B", tile),))
            P.dma("sp", seg[:, 0:2064], self.pbuf[rs, 4096:6160], reads=pall(tile), writes=("seg",))
            shv = sh.rearrange("p (j c) -> p j c", c=1024)
            for j in range(3):
                self.load_shift(shv[:, j, :], ("sh", j), 4096, 1024, tile, 3 - j)
            acc = R
            self.tt(acc, seg[:, 0:1024], cw[:, 3, :], ALU.mult, ("seg", "cw"), ("R",))
            self.tt(acc, acc, cb_, ALU.add, ("R", "cb"), ("R",))
            for j in range(3):
                self.tt(shv[:, j, :], shv[:, j, :], cw[:, j, :], ALU.mult, (("sh", j), "cw"), (("sh", j),))
                self.tt(acc, acc, shv[:, j, :], ALU.add, ("R", ("sh", j)), ("R",))
            self.actf(acc, acc, AF.Silu, ("R",), ("R",))
            self.tt(small[:, 0:16], seg[:, 2048:2064], ifb, ALU.add, ("seg", "ifb"), ("small",))
            self.actf(small[:, 24:32], small[:, 8:16], AF.Sigmoid, ("small",), ("small3",))
            self.actf(small[:, 16:24], small[:, 24:32], AF.Ln, ("small3",), ("small2",))
            ba = self.psbank()
            pc8 = self.pslin[:, ba, 0:8]
            P.op("pe", lambda e, pc8=pc8: e.matmul(pc8, lhsT=tri[:, 0, :], rhs=small[:, 16:24], start=True, stop=True),
                 reads=("small2", "tri"), writes=(("pslin", ba),))
            bb = self.psbank()
            pt8 = self.pslin[:, bb, 0:8]
            P.op("pe", lambda e, pt8=pt8: e.matmul(pt8, lhsT=tri[:, 1, :], rhs=small[:, 16:24], start=True, stop=True),
                 reads=("small2", "tri"), writes=(("pslin", bb),))
            self.actf(small2[:, 0:8], pc8, AF.Exp, (("pslin", ba),), ("s2a",))
            self.tt(h8(QB), h8(acc[:, 0:512]), b8(small2[:, 0:8]), ALU.mult, ("R", "s2a"), ("QB",))
            self.bt_store(QB, "QB", ck["qT"]["C"], s, g)
            self.tt(small2[:, 8:16], small[:, 0:8], pc8, ALU.subtract, ("small", ("pslin", ba)), ("s2b",))
            self.actf(small2[:, 16:24], small2[:, 8:16], AF.Exp, ("s2b",), ("s2c",))
            self.stt(h8(KB), h8(acc[:, 512:1024]), 0.125, b8(small2[:, 16:24]), ALU.mult, ALU.mult, ("R", "s2c"), ("KB",))
            self.bt_store(KB, "KB", ck["kT"]["C"], s, g)
            self.tt(small2[:, 24:32], small2[:, 8:16], pt8, ALU.add, ("s2b", ("pslin", bb)), ("s2d",))
            self.actf(small2[:, 32:40], small2[:, 24:32], AF.Exp, ("s2d",), ("s2e",))
            self.stt(h8(KH), h8(acc[:, 512:1024]), 0.125, b8(small2[:, 32:40]), ALU.mult, ALU.mult, ("R", "s2e"), ("KH",))
            P.dma("sp", ck["kh"]["C"][rs, :], KH, reads=("KH",), writes=(("kh", "C"),))
            self.actf(small2[:, 40:48], pt8, AF.Exp, (("pslin", bb),), ("s2f",))
            self.cp(h8(W[1]), b8(small2[:, 40:48]), ("s2f",), (wk(1),), eng="dve")
            P.dma("sp", ck["dec"]["C"][rs, :], W[1], reads=(wk(1),), writes=(("dec", "C"),))
            self.store_v(seg[:, 1024:1536], "seg", ck["v"]["C"], rs)
            self.actf(W[2], seg[:, 1536:2048], AF.Sigmoid, ("seg",), (wk(2),))
            P.dma("sp", gate["C"][rs, :], W[2], reads=(wk(2),), writes=(("gate", "C", tile),))
            P.dma("sp", seg[:, 0:RWC], self.pbuf[rs, 6160:6160 + RWC], reads=pall(tile), writes=("seg",))
            prev = sh[:, 0:RWC]
            self.load_shift(prev, ("sh", 0), 6160, RWC, tile, 1, extra_keys=(("sh", 1),))
            self.tt(prev, prev, seg[:, 0:RWC], ALU.subtract, (("sh", 0), ("sh", 1), "seg"), (("sh", 0), ("sh", 1)))
            self.tt(prev, prev, mu, ALU.mult, (("sh", 0), ("sh", 1), "mu"), (("sh", 0), ("sh", 1)))
            self.tt(seg[:, 0:RWC], seg[:, 0:RWC], prev, ALU.add, ("seg", ("sh", 0), ("sh", 1)), ("seg",))
            r_, k_, v_ = seg[:, 0:512], seg[:, 512:1024], seg[:, 1024:1536]
            Lt = W[0][:, 0:256]
            self.actf(Lt[:, 0:64], seg[:, 1536:1600], AF.Tanh, ("seg",), (wk(0),))
            self.cp(Lt[:, 64:128], seg[:, 1600:1664], ("seg",), (wk(0),))
            self.actf(Lt[:, 128:256], seg[:, 1664:1792], AF.Sigmoid, ("seg",), (wk(0),))
            LT = W[1][:, 0:256]
            for c in range(2):
                bank = self.psbank()
                pt = self.pslin[:, bank, 0:128]
                P.op("pe", lambda e, pt=pt, c=c: e.transpose(pt, Lt[:, c * 128:(c + 1) * 128], self.ident_f[:, :]),
                     reads=(wk(0), "identf"), writes=(("pslin", bank),))
                self.cp(LT[:, c * 128:(c + 1) * 128], pt, (("pslin", bank),), (wk(1),))
            bank = self.psbank()
            pw = self.pslin[:, bank, :]
            P.op("pe", lambda e, pw=pw: e.matmul(pw, lhsT=LT[:, 0:128], rhs=upw[:, 0:512], start=True, stop=True),
                 reads=(wk(1), "upw"), writes=(("pslin", bank),))
            self.tt(W[2], pw, sv["rwkv_w0"], ALU.add, (("pslin", bank), "c_rwkv_w0"), (wk(2),))
            self.actf(W[2], W[2], AF.Sigmoid, (wk(2),), (wk(2),))
            self.ts(W[2], W[2], -0.6065306597126334, None, ALU.mult, None, (wk(2),), (wk(2),))
            bank = self.psbank()
            pa = self.pslin[:, bank, :]
            P.op("pe", lambda e, pa=pa: e.matmul(pa, lhsT=LT[:, 0:128], rhs=upw[:, 512:1024], start=True, stop=True),
                 reads=(wk(1), "upw"), writes=(("pslin", bank),))
            self.tt(W[3], pa, sv["rwkv_a0"], ALU.add, (("pslin", bank), "c_rwkv_a0"), (wk(3),))
            self.actf(W[3], W[3], AF.Sigmoid, (wk(3),), (wk(3),))
            bank = self.psbank()
            pg = self.pslin[:, bank, :]
            P.op("pe", lambda e, pg=pg: e.matmul(pg, lhsT=LT[:, 128:256], rhs=gup, start=True, stop=True),
                 reads=(wk(1), "gup"), writes=(("pslin", bank),))
            self.cp(W[4], pg, (("pslin", bank),), (wk(4),))
            P.dma("sp", gate["D"][rs, :], W[4], reads=(wk(4),), writes=(("gate", "D", tile),))
            if l == 0:
                P.dma("sp", vfirst[rs, :], v_, reads=("seg",), writes=(("vfirst", tile),))
            else:
                vtt = self.vt
                for c in range(4):
                    bank = self.psbank()
                    pt = self.pslin[:, bank, 0:128]
                    P.op("pe", lambda e, pt=pt, c=c: e.transpose(pt, v_[:, c * 128:(c + 1) * 128], self.ident_f[:, :]),
                         reads=("seg", "identf"), writes=(("pslin", bank),))
                    self.cp(vtt[:, c, :], pt, (("pslin", bank),), ("vt",))
                bank = self.psbank()
                pv = self.pslin[:, bank, 0:32]
                for c in range(4):
                    P.op("pe", lambda e, pv=pv, c=c: e.matmul(pv, lhsT=vtt[:, c, :], rhs=vdn[:, c, :], start=(c == 0),
                                                              stop=(c == 3)),
                         reads=("vt", "vdn"), writes=(("pslin", bank),))
                self.cp(W[5][:, 0:32], pv, (("pslin", bank),), (wk(5),))
                bank = self.psbank()
                pt = self.pslin[0:32, bank, 0:128]
                P.op("pe", lambda e, pt=pt: e.transpose(pt, W[5][:, 0:32], self.ident_f[:, :]),
                     reads=(wk(5), "identf"), writes=(("pslin", bank),))
                self.cp(W[6][0:32, 0:128], pt, (("pslin", bank),), (wk(6),))
                bank = self.psbank()
                pv2 = self.pslin[:, bank, :]
                P.op("pe", lambda e, pv2=pv2: e.matmul(pv2, lhsT=W[6][0:32, 0:128], rhs=vup[0:32, :], start=True, stop=True),
                     reads=(wk(6), "vup"), writes=(("pslin", bank),))
                self.tt(W[5], pv2, v0b, ALU.add, (("pslin", bank), "v0b"), (wk(5),))
                self.actf(W[5], W[5], AF.Sigmoid, (wk(5),), (wk(5),))
                P.dma("sp", W[6], vfirst[rs, :], reads=(("vfirst", tile),), writes=(wk(6),))
                self.tt(W[6], W[6], v_, ALU.subtract, (wk(6), "seg"), (wk(6),))
                self.tt(W[6], W[6], W[5], ALU.mult, (wk(6), wk(5)), (wk(6),))
                self.tt(v_, v_, W[6], ALU.add, ("seg", wk(6)), ("seg",))
            self.tt(W[5], k_, sv["rwkv_k_k"], ALU.mult, ("seg", "c_rwkv_k_k"), (wk(5),))
            self.tt(W[6], W[5], W[5], ALU.mult, (wk(5),), (wk(6),))
            self.red(small[:, 32:40], h8(W[6]), (wk(6),), ("small4",))
            self.actf(small[:, 40:48], small[:, 32:40], AF.Sqrt, ("small4",), ("small5",))
            self.ts(small[:, 40:48], small[:, 40:48], 1e-12, None, ALU.max, None, ("small5",), ("small5",))
            P.op("dve", lambda e: e.reciprocal(out=small[:, 48:56], in_=small[:, 40:48]), reads=("small5",), writes=("small6",))
            self.tt(h8(W[5]), h8(W[5]), b8(small[:, 48:56]), ALU.mult, (wk(5), "small6"), (wk(5),))
            self.stt(W[7], W[3], -1.0, sv["rwkv_k_a"], ALU.add, ALU.mult, (wk(3), "c_rwkv_k_a"), (wk(7),))
            self.stt(W[7], W[7], 1.0, k_, ALU.add, ALU.mult, (wk(7), "seg"), (wk(7),))
            self.tt(W[8], W[3], W[5], ALU.mult, (wk(3), wk(5)), (wk(8),))
            self.tt(W[9], r_, W[7], ALU.mult, ("seg", wk(7)), (wk(9),))
            self.tt(W[9], W[9], sv["rwkv_r_k"], ALU.mult, (wk(9), "c_rwkv_r_k"), (wk(9),))
            self.red(small[:, 56:64], h8(W[9]), (wk(9),), ("small7",))
            self.tt(h8(W[9]), h8(v_), b8(small[:, 56:64]), ALU.mult, ("seg", "small7"), (wk(9),))
            P.dma("sp", bonus[rs, :], W[9], reads=(wk(9),), writes=(("bonus", tile),))
            dk = ck["D"]
            ba = self.psbank()
            pc = self.pslin[:, ba, :]
            P.op("pe", lambda e, pc=pc: e.matmul(pc, lhsT=tri[:, 2, :], rhs=W[2], start=True, stop=True),
                 reads=(wk(2), "tri"), writes=(("pslin", ba),))
            bb = self.psbank()
            ptot = self.pslin[:, bb, :]
            P.op("pe", lambda e, ptot=ptot: e.matmul(ptot, lhsT=tri[:, 3, :], rhs=W[2], start=True, stop=True),
                 reads=(wk(2), "tri"), writes=(("pslin", bb),))
            self.actf(W[0], pc, AF.Exp, (("pslin", ba),), (wk(0),))
            self.tt(QB, r_, W[0], ALU.mult, ("seg", wk(0)), ("QB",))
            self.bt_store(QB, "QB", dk["rT"], s, g)
            self.tt(W[1], pc, W[2], ALU.subtract, (("pslin", ba), wk(2)), (wk(1),))
            self.actf(W[1], W[1], AF.Exp, (wk(1),), (wk(1),))
            self.tt(KB, W[5], W[1], ALU.mult, (wk(5), wk(1)), ("KB",))
            self.bt_store(KB, "KB", dk["cT"], s, g)
            self.actf(W[0], pc, AF.Exp, (("pslin", ba),), (wk(0),), scale=-1.0)
            self.tt(XB1, W[7], W[0], ALU.mult, (wk(7), wk(0)), ("XB1",))
            self.bt_store(XB1, "XB1", dk["kT"], s, g)
            self.tt(XB2, W[8], W[0], ALU.mult, (wk(8), wk(0)), ("XB2",))
            self.bt_store(XB2, "XB2", dk["bT"], s, g)
            self.cp(W[6], ptot, (("pslin", bb),), (wk(6),))
            self.actf(W[1], W[6], AF.Exp, (wk(6),), (wk(1),))
            P.dma("sp", dk["dec"][rs, :], W[1], reads=(wk(1),), writes=(("dec", "D"),))
            self.tt(W[6], W[6], pc, ALU.subtract, (wk(6), ("pslin", ba)), (wk(6),))
            self.actf(W[6], W[6], AF.Exp, (wk(6),), (wk(6),))
            self.tt(KH, W[7], W[6], ALU.mult, (wk(7), wk(6)), ("KH",))
            P.dma("sp", dk["kh"][rs, :], KH, reads=("KH",), writes=(("kh", "D"),))
            self.stt(XB3, W[8], -1.0, W[6], ALU.mult, ALU.mult, (wk(8), wk(6)), ("XB3",))
            P.dma("sp", dk["bh"][rs, :], XB3, reads=("XB3",), writes=(("bh", "D"),))
            self.cp(XB4, v_, ("seg",), ("XB4",), eng="dve")
            P.dma("sp", dk["v"][rs, :], XB4, reads=("XB4",), writes=(("vd", "D"),))

    def phase_scan(self, rows, vT, yT, sel_d, ck, only_abc=False):
        P = self.P
        cfg = self.cfg
        self.carve_reset()
        self._phase_id = getattr(self, "_phase_id", 0) + 1

        def chain():
            for m in "ABC":
                for _ in self.gen_chunk(m, CHK[m], ck):
                    yield
        ga = chain()
        gd = self.chunk_D(ck["D"])
        na = 5 * sum((cfg.T // CHK[m]) * cfg.NSEQ * 4 for m in "ABC")
        nd = 19 * (cfg.T // 32 + 1) * cfg.NSEQ * 4
        a_done = d_done = False
        ia = idd = 0
        while not (a_done and d_done):
            if not d_done and (a_done or idd * na <= ia * nd):
                try:
                    next(gd)
                    idd += 1
                except StopIteration:
                    d_done = True
            elif not a_done:
                try:
                    next(ga)
                    ia += 1
                except StopIteration:
                    a_done = True

    def chunk_D(self, dk):
        P = self.P
        cfg = self.cfg
        T = cfg.T
        C = 32
        nch = T // C
        XT = self.carve(T // 2, BF16)
        X2 = {n: self.carve(T, BF16).rearrange("p (n h t) -> p n h t", h=2, t=C) for n in ("r", "c", "k", "b")}
        Vt = self.carve(nch * 32, BF16).rearrange("p (n e) -> p n e", e=64)
        KH2 = self.carve(nch * 64, BF16).rearrange("p (n e) -> p n e", e=128)
        BH2 = self.carve(nch * 64, BF16).rearrange("p (n e) -> p n e", e=128)
        YH = 32
        yo = self.carve(YH * 64).rearrange("p (n e) -> p n e", e=64)
        dch = self.carve(128)
        decT = self.carve(64)
        Zf = self.carve(64)
        Zb = self.carve(32, BF16)
        msk = self.carve(192).rearrange("p (a c) -> p a c", c=64)
        ST = [dict(N=[self.carve(64) for _ in range(5)], NT=[self.carve(64) for _ in range(4)], P=self.carve(64),
                   Q=self.carve(64), BTm=self.carve(32, BF16), S1m=self.carve(32, BF16), S2m=self.carve(32, BF16))
              for _ in range(2)]
        RHSs = self.carve(64)
        Ub = self.carve(32, BF16)
        pstr_f = self.pstr[:, :, :].bitcast(F32)
        I64 = self.ident_f[0:64, 0:64]
        P.dma("sp", msk[0:64, :, :], dk["masks"].rearrange("a p c -> p a c"), writes=("msk",))
        for n in X2:
            P.op("dve", lambda e, n=n: e.memset(X2[n], 0.0), writes=(("X2", n),))
        P.op("dve", lambda e: e.memset(KH2, 0.0), writes=("KH2",))
        P.op("dve", lambda e: e.memset(BH2, 0.0), writes=("BH2",))
        Mup, Mlow, Minc = msk[0:64, 0, :], msk[0:64, 1, :], msk[0:64, 2, :]
        srcT = {"r": dk["rT"], "c": dk["cT"], "k": dk["kT"], "b": dk["bT"]}
        pe = lambda fn, r, w: P.op("pe", fn, reads=r, writes=w)
        for s in range(cfg.NSEQ):
            for hp in range(4):
                for n in ("r", "c", "k", "b"):
                    P.dma("sp", XT[:, 0:T], srcT[n][hp, :, s, :], reads=tuple(("T", id(srcT[n]), s, g) for g in range(cfg.TPS)),
                          writes=("XT",))
                    self.cp(X2[n][0:64, :, 0, :], XT[0:64, 0:T].rearrange("p (n t) -> p n t", t=C), ("XT",), (("X2", n),),
                            eng="dve")
                    self.cp(X2[n][64:128, :, 1, :], XT[64:128, 0:T].rearrange("p (n t) -> p n t", t=C), ("XT",), (("X2", n),))
                rsl = slice(s * T, (s + 1) * T)
                for hh in range(2):
                    col = slice((2 * hp + hh) * 64, (2 * hp + hh + 1) * 64)
                    ps_ = slice(hh * 32, (hh + 1) * 32)
                    P.dma("sp", Vt[ps_, 0:nch, :], dk["v"][rsl, col].rearrange("(n c) k -> c n k", c=C), reads=(("vd", "D"),),
                          writes=("Vt",))
                    P.dma("sp", KH2[ps_, 0:nch, hh * 64:(hh + 1) * 64], dk["kh"][rsl, col].rearrange("(n c) k -> c n k", c=C),
                          reads=(("kh", "D"),), writes=("KH2",))
                    P.dma("sp", BH2[ps_, 0:nch, hh * 64:(hh + 1) * 64], dk["bh"][rsl, col].rearrange("(n c) k -> c n k", c=C),
                          reads=(("bh", "D"),), writes=("BH2",))
                P.dma("sp", dch[0:nch, :], dk["dec"][rsl, hp * 128:(hp + 1) * 128].rearrange("(n c) k -> c n k", c=C)[C - 1, :, :],
                      reads=(("dec", "D"),), writes=("dchD",))
                pt = self.pslin[:, 5, 0:nch]
                pe(lambda e, pt=pt: e.transpose(pt, dch[0:nch, :], self.ident_f[0:nch, 0:nch]), ("dchD", "identf"), (("pslin", 5),))
                self.cp(decT[:, 0:nch], pt, (("pslin", 5),), ("decTD",))
                P.op("dve", lambda e: e.memset(Zf, 0.0), writes=("Zf",))
                P.op("dve", lambda e: e.memset(Zb, 0.0), writes=("Zb",))
                r2, c2, k2, b2 = X2["r"], X2["c"], X2["k"], X2["b"]
                xk = lambda n: ("X2", n)

                def front(c):
                    par = c % 2
                    st = ST[par]
                    sk_ = lambda nm: ("ST", par, nm)
                    sc = self.pslin[0:64, par, :]
                    bk = ("pslin", par)
                    fl = lambda ap: ap.rearrange("p h t -> p (h t)")
                    pe(lambda e: e.matmul(sc[:, 0:64], lhsT=fl(b2[:, c]), rhs=fl(c2[:, c]), start=True, stop=True), (xk("b"), xk("c")), (bk,))
                    pe(lambda e: e.matmul(sc[:, 64:128], lhsT=fl(c2[:, c]), rhs=fl(b2[:, c]), start=True, stop=True), (xk("b"), xk("c")), (bk,))
                    pe(lambda e: e.matmul(sc[:, 128:192], lhsT=fl(k2[:, c]), rhs=fl(c2[:, c]), start=True, stop=True), (xk("k"), xk("c")), (bk,))
                    pe(lambda e: e.matmul(sc[:, 192:256], lhsT=fl(k2[:, c]), rhs=fl(r2[:, c]), start=True, stop=True), (xk("k"), xk("r")), (bk,))
                    pe(lambda e: e.matmul(sc[:, 256:320], lhsT=fl(b2[:, c]), rhs=fl(r2[:, c]), start=True, stop=True), (xk("b"), xk("r")), (bk,))
                    yield
                    N, NT, Pm, Qm = st["N"], st["NT"], st["P"][0:64, :], st["Q"][0:64, :]
                    self.stt(N[0][0:64, :], sc[:, 0:64], -1.0, Mup, ALU.mult, ALU.mult, (bk, "msk"), (sk_("N0"),))
                    self.stt(NT[0][0:64, :], sc[:, 64:128], -1.0, Mlow, ALU.mult, ALU.mult, (bk, "msk"), (sk_("NT0"),))
                    self.tt(st["BTm"][0:64, :], sc[:, 128:192], Mup, ALU.mult, (bk, "msk"), (sk_("BTm"),))
                    self.tt(st["S1m"][0:64, :], sc[:, 192:256], Minc, ALU.mult, (bk, "msk"), (sk_("S1m"),))
                    self.stt(st["S2m"][0:64, :], sc[:, 256:320], -1.0, Minc, ALU.mult, ALU.mult, (bk, "msk"), (sk_("S2m"),))
                    self.tt(Pm, N[0][0:64, :], I64, ALU.add, (sk_("N0"), "identf"), (sk_("P"),))
                    self.tt(Qm, NT[0][0:64, :], I64, ALU.add, (sk_("NT0"), "identf"), (sk_("Q"),))
                    yield
                    for k in range(1, 5):
                        last = (k == 4)
                        pn = self.pslin[0:64, 2, 0:128]
                        pe(lambda e, k=k: e.matmul(pn[:, 0:64], lhsT=NT[k - 1][0:64, :], rhs=N[k - 1][0:64, :], start=True, stop=True),
                           (sk_("N%d" % (k - 1)), sk_("NT%d" % (k - 1))), (("pslin", 2),))
                        if not last:
                            pe(lambda e, k=k: e.matmul(pn[:, 64:128], lhsT=N[k - 1][0:64, :], rhs=NT[k - 1][0:64, :], start=True,
                                                       stop=True),
                               (sk_("N%d" % (k - 1)), sk_("NT%d" % (k - 1))), (("pslin", 2),))
                        yield
                        self.cp(N[k][0:64, :], pn[:, 0:64], (("pslin", 2),), (sk_("N%d" % k),))
                        if not last:
                            self.cp(NT[k][0:64, :], pn[:, 64:128], (("pslin", 2),), (sk_("NT%d" % k),))
                        yield
                        pp = self.pslin[0:64, 2, 128:256]
                        pe(lambda e, k=k: e.matmul(pp[:, 0:64], lhsT=Qm, rhs=N[k][0:64, :], start=True, stop=True),
                           (sk_("Q"), sk_("N%d" % k)), (("pslin", 2),))
                        if not last:
                            pe(lambda e, k=k: e.matmul(pp[:, 64:128], lhsT=N[k][0:64, :], rhs=Qm, start=True, stop=True),
                               (sk_("Q"), sk_("N%d" % k)), (("pslin", 2),))
                        yield
                        self.tt(Pm, Pm, pp[:, 0:64], ALU.add, (sk_("P"), ("pslin", 2)), (sk_("P"),))
                        if not last:
                            self.tt(Qm, Qm, pp[:, 64:128], ALU.add, (sk_("Q"), ("pslin", 2)), (sk_("Q"),))
                        yield

                def back(c):
                    par = c % 2
                    st = ST[par]
                    sk_ = lambda nm: ("ST", par, nm)
                    fl = lambda ap: ap.rearrange("p h t -> p (h t)")
                    pr = self.pslin[0:64, 3, 0:64]
                    pe(lambda e: e.matmul(pr, lhsT=fl(c2[:, c]), rhs=Zb[:, 0:64], start=True, stop=False), (xk("c"), "Zb"), (("pslin", 3),))
                    pe(lambda e: e.matmul(pr, lhsT=st["BTm"][0:64, :], rhs=Vt[0:64, c, :], start=False, stop=True),
                       (sk_("BTm"), "Vt"), (("pslin", 3),))
                    yield
                    self.cp(RHSs[0:64, :], pr, (("pslin", 3),), ("RHSs",))
                    yield
                    pu = self.pslin[0:64, 3, 64:128]
                    pe(lambda e: e.matmul(pu, lhsT=st["P"][0:64, :], rhs=RHSs[0:64, :], start=True, stop=True), (sk_("P"), "RHSs"),
                       (("pslin", 3),))
                    yield
                    self.cp(Ub[0:64, :], pu, (("pslin", 3),), ("Ub",))
                    yield
                    pk = self.pslin[:, 5, 0:64]
                    pe(lambda e: e.matmul(pk, lhsT=KH2[0:64, c, :], rhs=Vt[0:64, c, :], start=True, stop=False), ("KH2", "Vt"),
                       (("pslin", 5),))
                    pe(lambda e: e.matmul(pk, lhsT=BH2[0:64, c, :], rhs=Ub[0:64, :], start=False, stop=True), ("BH2", "Ub"),
                       (("pslin", 5),))
                    py = self.pslin[0:64, 3, 128:192]
                    pe(lambda e: e.matmul(py, lhsT=st["S1m"][0:64, :], rhs=Vt[0:64, c, :], start=True, stop=False), (sk_("S1m"), "Vt"),
                       (("pslin", 3),))
                    pe(lambda e: e.matmul(py, lhsT=st["S2m"][0:64, :], rhs=Ub[0:64, :], start=False, stop=False), (sk_("S2m"), "Ub"),
                       (("pslin", 3),))
                    pe(lambda e: e.matmul(py, lhsT=fl(r2[:, c]), rhs=Zb[:, 0:64], start=False, stop=True), (xk("r"), "Zb"),
                       (("pslin", 3),))
                    yield
                    self.stt(Zf, Zf, decT[:, c:c + 1], pk, ALU.mult, ALU.add, ("Zf", "decTD", ("pslin", 5)), ("Zf",))
                    self.cp(yo[0:64, c % YH, :], py, (("pslin", 3),), ("yoD",))
                    yield
                    self.cp(Zb, Zf, ("Zf",), ("Zb",))
                    if c % YH == YH - 1 or c == nch - 1:
                        c0 = c - (c % YH)
                        for hh in range(2):
                            col = slice((2 * hp + hh) * 64, (2 * hp + hh + 1) * 64)
                            P.dma("sp", dk["y"][s * T + c0 * C:s * T + (c + 1) * C, col].rearrange("(n c) k -> c n k", c=C),
                                  yo[hh * 32:(hh + 1) * 32, 0:c - c0 + 1, :], reads=("yoD",), writes=(("cy", "D"),))
                    yield

                import itertools
                for c in range(nch + 1):
                    gf = front(c) if c < nch else iter(())
                    gb = back(c - 1) if c > 0 else iter(())
                    for _ in itertools.zip_longest(gf, gb):
                        yield

    def gen_scan_D(self, rows, vT, yT, sel_d):
        P = self.P
        cfg = self.cfg
        T = cfg.T
        sel = self.carve(TB * 128).rearrange("p (t m) -> p t m", m=128)
        P.dma("sp", sel[0:32, :, :], sel_d[:, :, :], writes=("sel",))
        Rb = [self.carve(5 * 512).rearrange("p (v n) -> p v n", n=512) for _ in range(2)]
        Vb = [self.carve(8 * TB).rearrange("p (g t) -> p g t", t=TB) for _ in range(2)]
        Yb = [self.carve(8 * TB).rearrange("p (g t) -> p g t", t=TB) for _ in range(2)]
        NR = 3
        BR = [self.carve(5 * 512).rearrange("p (v n) -> p v n", n=512) for _ in range(NR)]
        S = self.carve(512)
        T1 = self.carve(512)
        T2 = self.carve(512)
        sk = self.carve(8)
        h8 = lambda ap: ap.rearrange("p (h d) -> p h d", d=64)
        b8 = lambda ap8: ap8.unsqueeze(2).to_broadcast([128, 8, 64])
        m = "D"
        vecs = VECS[m]
        P.op("dve", lambda e: e.memset(S, 0.0), writes=("S",))
        step = 0
        dps = 0
        for bi in range(T // TB):
            t0 = bi * TB
            pb = bi % 2
            for j, vname in enumerate(vecs):
                src = rows[vname].rearrange("(s t) (hp hh d) -> t s hp hh d", s=cfg.NSEQ, hh=2, d=64)
                for hh in range(2):
                    for s in range(cfg.NSEQ):
                        P.dma("sp", Rb[pb][hh * TB:(hh + 1) * TB, j, s * 256:(s + 1) * 256].rearrange(
                            "t (hp d) -> t hp d", d=64),
                            src[t0:t0 + TB, s, :, hh, :],
                            reads=(("row", vname, (s * T + t0) // 128),), writes=(("Rb", pb, j),))
            P.dma("sp", Vb[pb].rearrange("p (s hp) t -> p s hp t", hp=4),
                  vT[m].rearrange("hp p s t -> p s hp t")[:, :, :, t0:t0 + TB],
                  reads=tuple(("T", id(vT[m]), s, t0 // 128) for s in range(cfg.NSEQ)), writes=(("Vb", pb),))
            for tl in range(TB):
                rb = step % NR
                step += 1
                ps = {}
                for j, vname in enumerate(vecs):
                    bank = dps % 3
                    dps += 1
                    pt = self.pslin[:, bank, :]
                    P.op("pe", lambda e, pt=pt, j=j, tl=tl, pb=pb: e.matmul(pt, lhsT=sel[0:32, tl, :], rhs=Rb[pb][0:32, j, :],
                                                                           start=True, stop=True),
                         reads=("sel", ("Rb", pb, j)), writes=(("pslin", bank),))
                    self.cp(BR[rb][:, j, :], pt, (("pslin", bank),), (("BR", rb, j),))
                    ps[j] = (BR[rb][:, j, :], ("BR", rb, j))
                vb = b8(Vb[pb][:, :, tl])
                yo = Yb[pb][:, :, tl]
                (rp, rk_), (kp, kk_), (wp, wk_), (cp_, ck_), (bp, bk_) = ps[0], ps[1], ps[2], ps[3], ps[4]
                self.tt(T1, S, cp_, ALU.mult, ("S", ck_), ("T1",))
                self.red(sk, h8(T1), ("T1",), ("sk",))
                self.tt(h8(T1), h8(bp), b8(sk), ALU.mult, (bk_, "sk"), ("T1",))
                self.tt(S, S, wp, ALU.mult, ("S", wk_), ("S",))
                self.tt(S, S, T1, ALU.subtract, ("S", "T1"), ("S",))
                self.tt(h8(T2), h8(kp), vb, ALU.mult, (kk_, ("Vb", pb)), ("T2",))
                self.tt(S, S, T2, ALU.add, ("S", "T2"), ("S",))
                self.tt(T2, S, rp, ALU.mult, ("S", rk_), ("T2",))
                self.red(yo, h8(T2), ("T2",), (("Yb", pb),))
                yield
            P.dma("sp", yT[m].rearrange("hp p s t -> p s hp t")[:, :, :, t0:t0 + TB],
                  Yb[pb].rearrange("p (s hp) t -> p s hp t", hp=4), reads=(("Yb", pb),),
                  writes=tuple(("T", id(yT[m]), s, t0 // 128) for s in range(cfg.NSEQ)))

    def gen_chunk(self, m, C, ck):
        P = self.P
        cfg = self.cfg
        T = cfg.T
        nch = T // C
        cb = self.chunk_bufs(T)
        qT, kT, q0, q1, kh, vv, yo, dch, decT, st_f, st_b, scm, mask = cb
        YH = 32
        pstr_f = self.pstr[:, :, :].bitcast(F32)
        qT_d, kT_d, kh_d, v_d, dec_d, y_d = ck["qT"][m], ck["kT"][m], ck["kh"][m], ck["v"][m], ck["dec"][m], ck["y"][m]
        P.dma("sp", mask[0:C, 0:C], ck["mask"][0:C, 0:C], writes=("mask",))
        P.dma("sp", mask[0:C, C:2 * C], ck["mask"][0:C, 0:C], writes=("mask",))
        cnt = 0
        qs = [q0, q1]
        for s in range(cfg.NSEQ):
            for hp in range(4):
                allT = tuple(("T", id(qT_d), s, g) for g in range(cfg.TPS))
                P.dma("sp", qT[:, 0:T], qT_d[hp, :, s, :], reads=allT, writes=("c_qT",))
                P.dma("sp", kT[:, 0:T], kT_d[hp, :, s, :], reads=tuple(("T", id(kT_d), s, g) for g in range(cfg.TPS)),
                      writes=("c_kT",))
                P.dma("sp", kh[0:C, 0:nch, :], kh_d[s * T:(s + 1) * T, hp * 128:(hp + 1) * 128].rearrange(
                    "(n c) k -> c n k", c=C), reads=(("kh", m),), writes=("c_kh",))
                P.dma("sp", vv[0:C, 0:nch, :], v_d[s * T:(s + 1) * T, hp * 130:(hp + 1) * 130].rearrange(
                    "(n c) k -> c n k", c=C), reads=(("vd", id(v_d)),), writes=("c_vv",))
                P.dma("sp", dch[0:nch, :], dec_d[s * T:(s + 1) * T, hp * 128:(hp + 1) * 128].rearrange(
                    "(n c) k -> c n k", c=C)[C - 1, :, :], reads=(("dec", m),), writes=("c_dch",))
                pt = pstr_f[:, 1, 0:nch]
                P.op("pe", lambda e, pt=pt: e.transpose(pt, dch[0:nch, :], self.ident_f[0:nch, 0:nch]),
                     reads=("c_dch", "identf"), writes=(("pstr", 1),))
                self.cp(decT[:, 0:nch], pt, (("pstr", 1),), ("c_decT",))
                P.op("dve", lambda e: e.memset(q0[64:128, 0:T], 0.0), writes=("c_q0",))
                P.op("dve", lambda e: e.memset(q1[0:64, 0:T], 0.0), writes=("c_q1",))
                self.cp(q0[0:64, 0:T], qT[0:64, 0:T], ("c_qT",), ("c_q0",), eng="dve")
                self.cp(q1[64:128, 0:T], qT[64:128, 0:T], ("c_qT",), ("c_q1",), eng="dve")
                P.op("dve", lambda e: e.memset(st_f[:, 0:66], 0.0), writes=("st_f",))
                P.op("dve", lambda e: e.memset(st_b[:, 0:66], 0.0), writes=("st_b",))
                for c in range(nch):
                    cs = slice(c * C, (c + 1) * C)
                    par = c % 2
                    psc = self.pslin[0:C, 4, 0:2 * C]
                    for hh in range(2):
                        qh = qs[hh]
                        qk = "c_q%d" % hh
                        P.op("pe", lambda e, cs=cs, qh=qh, hh=hh: e.matmul(psc[:, hh * C:(hh + 1) * C], lhsT=kT[:, cs], rhs=qh[:, cs],
                                                                           start=True, stop=True),
                             reads=("c_kT", qk), writes=(("pslin", 4),))
                    yield
                    sm = scm[par][0:C, 0:2 * C]
                    self.tt(sm, psc, mask[0:C, 0:2 * C], ALU.mult, (("pslin", 4), "mask"), (("scm", par),))
                    yield
                    pso = pstr_f[0:C, 0, 0:130]
                    for hh in range(2):
                        qh = qs[hh]
                        qk = "c_q%d" % hh
                        vsl = vv[0:C, c, hh * 65:(hh + 1) * 65]
                        po = pso[:, hh * 65:(hh + 1) * 65]
                        P.op("pe", lambda e, po=po, vsl=vsl, hh=hh, sm=sm: e.matmul(po, lhsT=sm[:, hh * C:(hh + 1) * C], rhs=vsl,
                                                                                   start=True, stop=False),
                             reads=(("scm", par), "c_vv"), writes=(("pstr", 0),))
                        P.op("pe", lambda e, po=po, cs=cs, qh=qh: e.matmul(po, lhsT=qh[:, cs], rhs=st_b[:, 0:65], start=False, stop=True),
                             reads=(qk, "st_b"), writes=(("pstr", 0),))
                        pkv = pstr_f[hh * 64:(hh + 1) * 64, 1, 0:65]
                        khs = kh[0:C, c, hh * 64:(hh + 1) * 64]
                        P.op("pe", lambda e, pkv=pkv, khs=khs, vsl=vsl: e.matmul(pkv, lhsT=khs, rhs=vsl, start=True, stop=True),
                             reads=("c_kh", "c_vv"), writes=(("pstr", 1),))
                    yield
                    self.stt(st_f[:, 0:65], st_f[:, 0:65], decT[:, c:c + 1], pstr_f[:, 1, 0:65], ALU.mult, ALU.add,
                             ("st_f", "c_decT", ("pstr", 1)), ("st_f",))
                    self.cp(yo[0:C, c % YH, :], pso, (("pstr", 0),), ("c_yo",))
                    yield
                    self.cp(st_b[:, 0:65], st_f[:, 0:65], ("st_f",), ("st_b",))
                    if c % YH == YH - 1 or c == nch - 1:
                        c0 = c - (c % YH)
                        P.dma("sp", y_d[s * T + c0 * C:s * T + (c + 1) * C, hp * 130:(hp + 1) * 130].rearrange(
                            "(n c) k -> c n k", c=C), yo[0:C, 0:c - c0 + 1, :], reads=("c_yo",), writes=(("cy", m),))
                    yield

    def chunk_bufs(self, T):
        if getattr(self, "_cbufs_at", None) == id(self.P.ops["pe"]) and getattr(self, "_cbufs_phase", -1) == self._phase_id:
            return self._cbufs
        qT = self.carve(T // 2, BF16)
        kT = self.carve(T // 2, BF16)
        q0 = self.carve(T // 2, BF16)
        q1 = self.carve(T // 2, BF16)
        nmax = T // 32
        kh = self.carve(nmax * 64, BF16).rearrange("p (n k) -> p n k", k=128)
        vv = self.carve(nmax * 65, BF16).rearrange("p (n k) -> p n k", k=130)
        yo = self.carve(32 * 130).rearrange("p (n k) -> p n k", k=130)
        dch = self.carve(128)
        decT = self.carve(64)
        st_f = self.carve(66)
        st_b = self.carve(33, BF16)
        scm = [self.carve(64, BF16) for _ in range(2)]
        mask = self.carve(128)
        self._cbufs = (qT, kT, q0, q1, kh, vv, yo, dch, decT, st_f, st_b, scm, mask)
        self._cbufs_at = id(self.P.ops["pe"])
        self._cbufs_phase = self._phase_id
        self._mask_loaded = False
        return self._cbufs

    def phase_post(self, l, I, yT, ck, gate, bonus, obuf):
        P = self.P
        cfg = self.cfg
        self.carve_reset()
        o = self.carve(2048)
        y = self.carve(512)
        y65 = self.carve(520).rearrange("p (h e) -> p h e", e=65)
        gt = self.carve(512)
        W = [self.carve(512) for _ in range(3)]
        self.vt = self.carve(512).rearrange("p (a b) -> p a b", b=128)
        sv = {nm: self.carve(512) for nm in ("hgrn_norm_w", "mlstm_norm_w", "rwkv_ln_w", "rwkv_ln_b")}
        small = self.carve(64)
        for nm in sv:
            self.bcast_load(sv[nm], I[nm][l], "c_" + nm)
        h8 = lambda ap: ap.rearrange("p (h d) -> p h d", d=64)
        b8 = lambda ap8: ap8.unsqueeze(2).to_broadcast([128, 8, 64])
        wk = lambda i: ("W", i)
        for tile in range(cfg.NTILE):
            s, g = tile // cfg.TPS, tile % cfg.TPS
            rs = slice(tile * 128, (tile + 1) * 128)
            for mi, m in enumerate("ABCD"):
                if m == "D":
                    P.dma("sp", y, ck["D"]["y"][rs, :], reads=(("cy", "D"),), writes=("y",))
                else:
                    P.dma("sp", y65.rearrange("p h e -> p (h e)"), ck["y"][m][rs, :], reads=(("cy", m),), writes=("y65",))
                    self.cp(h8(y), y65[:, :, 0:64], ("y65",), ("y",), eng="dve")
                P.dma("sp", gt, gate[m][rs, :], reads=(("gate", m, tile),), writes=("gt",))
                osl = o[:, mi * 512:(mi + 1) * 512]
                if m == "C":
                    d8 = y65[:, :, 64]
                    self.ts(small[:, 0:8], d8, -1.0, None, ALU.mult, None, ("y65",), ("sm0",))
                    self.tt(small[:, 0:8], small[:, 0:8], d8, ALU.max, ("sm0", "y65"), ("sm0",))
                    self.ts(small[:, 0:8], small[:, 0:8], 1.0, None, ALU.max, None, ("sm0",), ("sm0",))
                    P.op("dve", lambda e: e.reciprocal(out=small[:, 8:16], in_=small[:, 0:8]), reads=("sm0",), writes=("sm1",))
                    self.tt(h8(y), h8(y), b8(small[:, 8:16]), ALU.mult, ("y", "sm1"), ("y",))
                if m in "ABC":
                    self.tt(W[0], y, y, ALU.mult, ("y",), (wk(0),))
                    self.red(small[:, 16:24], h8(W[0]), (wk(0),), ("sm2",))
                    self.actf(small[:, 24:32], small[:, 16:24], AF.Sqrt, ("sm2", "eps"), ("sm3",), scale=1.0 / 64,
                              bias=self.eps_t[:, 0:1])
                    P.op("dve", lambda e: e.reciprocal(out=small[:, 32:40], in_=small[:, 24:32]), reads=("sm3",), writes=("sm4",))
                    self.tt(h8(y), h8(y), b8(small[:, 32:40]), ALU.mult, ("y", "sm4"), ("y",))
                    if m == "B":
                        self.tt(y, y, sv["hgrn_norm_w"], ALU.mult, ("y", "c_hgrn_norm_w"), ("y",))
                    if m == "C":
                        self.tt(y, y, sv["mlstm_norm_w"], ALU.mult, ("y", "c_mlstm_norm_w"), ("y",))
                    self.tt(osl, y, gt, ALU.mult, ("y", "gt"), ("o",))
                else:
                    self.red(small[:, 16:24], h8(y), ("y",), ("sm2",))
                    self.ts(small[:, 16:24], small[:, 16:24], -1.0 / 64, None, ALU.mult, None, ("sm2",), ("sm2",))
                    self.tt(h8(y), h8(y), b8(small[:, 16:24]), ALU.add, ("y", "sm2"), ("y",))
                    self.tt(W[0], y, y, ALU.mult, ("y",), (wk(0),))
                    self.red(small[:, 24:32], h8(W[0]), (wk(0),), ("sm3",))
                    self.actf(small[:, 32:40], small[:, 24:32], AF.Sqrt, ("sm3", "eps"), ("sm4",), scale=1.0 / 64,
                              bias=self.eps_t[:, 1:2])
                    P.op("dve", lambda e: e.reciprocal(out=small[:, 40:48], in_=small[:, 32:40]), reads=("sm4",), writes=("sm5",))
                    self.tt(h8(y), h8(y), b8(small[:, 40:48]), ALU.mult, ("y", "sm5"), ("y",))
                    self.tt(y, y, sv["rwkv_ln_w"], ALU.mult, ("y", "c_rwkv_ln_w"), ("y",))
                    self.tt(y, y, sv["rwkv_ln_b"], ALU.add, ("y", "c_rwkv_ln_b"), ("y",))
                    P.dma("sp", W[1], bonus[rs, :], reads=(("bonus", tile),), writes=(wk(1),))
                    self.tt(y, y, W[1], ALU.add, ("y", wk(1)), ("y",))
                    self.tt(osl, y, gt, ALU.mult, ("y", "gt"), ("o",))
            P.dma("sp", obuf[rs, :], o, reads=("o",), writes=(("o", tile),))

    def phase_ffn(self, l, hbuf, obuf, wbo, wbg, wbu, wbd, wnb):
        P = self.P
        cfg = self.cfg
        NG = 4
        self.carve_linear(NG)
        actT = self.carve(44 * 256, BF16).rearrange("p (k g t) -> p k g t", g=NG, t=128)
        acc = [self.carve(2048) for _ in range(NG)]
        xT = self.xT
        for grp in range(cfg.NTILE // NG):
            for g in range(NG):
                tile = grp * NG + g
                ht = self.htile[tile % 2]
                P.dma("sp", ht, obuf[tile * 128:(tile + 1) * 128, :], reads=(("o", tile),), writes=(("htile", tile % 2),))
                self.cp(self.ub[:, 0:2048], ht, (("htile", tile % 2),), ("ub",), eng="dve")
                self.transpose_to(self.ub, "ub", xT, g, "xT", 16)

            def consume(g, cb, nc_, pst, pskey):
                self.cp(acc[g][:, cb * 512:cb * 512 + nc_], pst, (pskey,), (("acc", g),))

            self.linear_tm(xT, ("xT",), NG, 16, wbo, D, consume)
            self.resid_update(grp, NG, acc, hbuf, wnb[:, 1, :])
            for g in range(NG):
                tile = grp * NG + g
                ht = self.htile[tile % 2]
                P.dma("sp", ht, hbuf[tile * 128:(tile + 1) * 128, :], reads=(("h", tile),), writes=(("htile", tile % 2),))
                self.norm_transpose(ht, ("htile", tile % 2), wnb[:, 2, :], xT, g, "xT")
            for hb in range(DFF // 256):
                slot = hb % 2
                cb, half = hb // 2, hb % 2
                wg = self.wbuf[0][:, slot * 4096:(slot + 1) * 4096].rearrange("p (k n) -> p k n", n=256)
                wu = self.wbuf[1][:, slot * 4096:(slot + 1) * 4096].rearrange("p (k n) -> p k n", n=256)
                kg, ku = ("wbufh", 0, slot), ("wbufh", 1, slot)
                P.dma("sp", wg, wbg[cb, :, :, half * 256:(half + 1) * 256], reads=(("wb", id(wbg), cb),), writes=(kg,))
                P.dma("sp", wu, wbu[cb, :, :, half * 256:(half + 1) * 256], reads=(("wb", id(wbu), cb),), writes=(ku,))
                for j in range(2):
                    nchunk = hb * 2 + j
                    bg = self.psbank()
                    bu = self.psbank()
                    pg = self.pslin[:, bg, :]
                    pu = self.pslin[:, bu, :]
                    for kc in range(16):
                        P.op("pe", lambda e, pg=pg, kc=kc, j=j, wg=wg: e.matmul(pg, lhsT=wg[:, kc, j * 128:(j + 1) * 128],
                                                                                rhs=xT[:, kc, :, :].rearrange("p g t -> p (g t)"),
                                                                                start=(kc == 0), stop=(kc == 15)),
                             reads=("xT", kg), writes=(("pslin", bg),))
                    for kc in range(16):
                        P.op("pe", lambda e, pu=pu, kc=kc, j=j, wu=wu: e.matmul(pu, lhsT=wu[:, kc, j * 128:(j + 1) * 128],
                                                                                rhs=xT[:, kc, :, :].rearrange("p g t -> p (g t)"),
                                                                                start=(kc == 0), stop=(kc == 15)),
                             reads=("xT", ku), writes=(("pslin", bu),))
                    st = self.stage[self.ssel % 4]
                    skey = ("stage", self.ssel % 4)
                    self.ssel += 1
                    self.actf(st, pg, AF.Silu, (("pslin", bg),), (skey,))
                    self.tt(actT[:, nchunk, :, :].rearrange("p g t -> p (g t)"), st, pu, ALU.mult, (skey, ("pslin", bu)),
                            ("actT",))

            def consume2(g, cb, nc_, pst, pskey):
                self.cp(acc[g][:, cb * 128:cb * 128 + nc_], pst, (pskey,), (("acc", g),))

            self.linear_tm(actT, ("actT",), NG, 44, wbd, D, consume2, CB=128)
            self.resid_update(grp, NG, acc, hbuf, wnb[:, 3, :])

    def resid_update(self, grp, NG, acc, hbuf, wn):
        P = self.P
        for g in range(NG):
            tile = grp * NG + g
            ht = self.htile[tile % 2]
            hk = ("htile", tile % 2)
            P.dma("sp", ht, hbuf[tile * 128:(tile + 1) * 128, :], reads=(("h", tile),), writes=(hk,))
            self.rstd_of(acc[g], ("acc", g), 2048, self.eps_t[:, 0:1])
            self.stt(acc[g], acc[g], self.ss_t[:, 2:3], wn, ALU.mult, ALU.mult, (("acc", g), "ss2", "wnb"), (("acc", g),))
            self.tt(ht, ht, acc[g], ALU.add, (hk, ("acc", g)), (hk,))
            P.dma("sp", hbuf[tile * 128:(tile + 1) * 128, :], ht, reads=(hk,), writes=(("h", tile),))


def make_consts(T):
    half = 32
    inv_freq = 10000.0 ** (-np.arange(half, dtype=np.float32) / half)
    ang = np.arange(T, dtype=np.float32)[:, None] * inv_freq[None, :]
    cossin = np.concatenate([np.cos(ang), np.sin(ang)], axis=1).astype(np.float32)
    sel = np.zeros((32, TB, 128), np.float32)
    for tl in range(TB):
        for m in range(128):
            sel[(m // 64) * TB + tl, tl, m] = 1.0
    gam = np.zeros((128, 2, 4, 64), np.float32)
    for p in range(128):
        for hp in range(4):
            h = 2 * hp + p // 64
            gam[p, :, hp, :] = 1.0 - 2.0 ** (-5.0 - h)
    C = CHK["A"]
    j = (np.arange(128) % C).astype(np.float64)
    gh = 1.0 - 2.0 ** (-5.0 - np.arange(8, dtype=np.float64))
    atab = np.zeros((4, 128, 8, 64), np.float64)
    atab[0] = (gh[None, :] ** (j[:, None] + 1.0))[:, :, None]
    atab[1] = (0.125 * gh[None, :] ** (-(j[:, None] + 1.0)))[:, :, None]
    atab[2] = (0.125 * gh[None, :] ** (C - 1.0 - j[:, None]))[:, :, None]
    atab[3] = (gh ** float(C))[None, :, None]
    idx = np.arange(128)
    def trib(c):
        same = (idx[:, None] // c) == (idx[None, :] // c)
        return (same & (idx[:, None] <= idx[None, :])).astype(np.float32), same.astype(np.float32)
    t64, b64 = trib(64)
    t32, b32 = trib(32)
    trimats = np.stack([t64, b64, t32, b32]).astype(np.float32)
    cmask = (np.arange(64)[:, None] <= np.arange(64)[None, :]).astype(np.float32)
    i32 = np.arange(64) % 32
    dmasks = np.stack([(i32[:, None] < i32[None, :]), (i32[:, None] > i32[None, :]), (i32[:, None] <= i32[None, :])]).astype(
        np.float32)
    return dict(cossin=cossin, sel=sel, gamA=gam.reshape(128, 512), atab=atab.reshape(4, 128, 512).astype(np.float32),
                trimats=trimats, cmask=cmask, dmasks=dmasks,
                ident_b=np.eye(128).astype(ml_dtypes.bfloat16), ident_f=np.eye(128, dtype=np.float32))


def make_shared(inp, L, T):
    f = lambda a: np.ascontiguousarray(np.asarray(a, dtype=np.float32))
    sh = dict(w_in=f(inp["w_in"]), w_out=f(inp["w_out"]), w_ffn_gate=f(inp["w_ffn_gate"]), w_ffn_up=f(inp["w_ffn_up"]),
              w_ffn_down=f(inp["w_ffn_down"]))
    sh["norms"] = np.ascontiguousarray(np.stack([f(inp["norm_pre_mix"]), f(inp["norm_post_mix"]), f(inp["norm_pre_ffn"]),
                                                 f(inp["norm_post_ffn"])], axis=1))
    for nm in SMALL + ["hgrn_lb_logits", "mlstm_conv_w", "mlstm_conv_b", "rwkv_mu"]:
        sh[nm] = f(inp[nm])
    sh["mlstm_if_bias"] = np.ascontiguousarray(np.concatenate([f(inp["mlstm_i_bias"]), f(inp["mlstm_f_bias"])], axis=1))
    up = np.zeros((L, 128, 1024), np.float32)
    up[:, 0:64, 0:512] = f(inp["rwkv_w_up"])
    up[:, 64:128, 512:1024] = f(inp["rwkv_a_up"])
    sh["rwkv_up_pad"] = up
    sh["rwkv_g_up"] = f(inp["rwkv_g_up"])
    if L > 1:
        sh["rwkv_v0"] = f(inp["rwkv_v0"])
        sh["rwkv_v_down"] = f(inp["rwkv_v_down"])
        sh["rwkv_v_up"] = f(inp["rwkv_v_up"])
    else:
        sh["rwkv_v0"] = np.zeros((1, G), np.float32)
        sh["rwkv_v_down"] = np.zeros((1, G, 32), np.float32)
        sh["rwkv_v_up"] = np.zeros((1, 32, G), np.float32)
    sh.update(make_consts(T))
    return sh


_CACHE = {}


def kernel(**inp):
    x = np.asarray(inp["x"], dtype=np.float32)
    B, T, _ = x.shape
    L = inp["w_in"].shape[0]
    ncores = 8
    nseq = B // ncores
    cfg = Cfg(T=T, NSEQ=nseq, L=L)
    nc = Builder(cfg).build()
    sh = make_shared(inp, L, T)
    in_maps = []
    for c in range(ncores):
        m = dict(sh)
        m["x"] = np.ascontiguousarray(x[c * nseq:(c + 1) * nseq].reshape(nseq * T, D))
        in_maps.append(m)
    res = run_bass_kernel_spmd(nc, in_maps, core_ids=list(range(ncores)))
    outs = [np.asarray(r["out"]).reshape(nseq, T, D) for r in res.results]
    return np.concatenate(outs, axis=0).astype(np.float32)
```

```python
from contextlib import ExitStack
import numpy as np
import ml_dtypes
import concourse.bass as bass
import concourse.mybir as mybir
from concourse.bass_utils import run_bass_kernel_spmd

F32 = mybir.dt.float32
BF16 = mybir.dt.bfloat16
ALU = mybir.AluOpType
AF = mybir.ActivationFunctionType
AX = mybir.AxisListType

D = 2048
NIN = 7952
DFF = 5632
G = 512
NH = 8
HD = 64
NORM_EPS = 1e-6


class Prog:
    ENG = ("pe", "dve", "act", "pool", "sp")
    NDMASEM = 12

    def __init__(self, nc, stack):
        self.nc = nc
        self.stack = stack
        self.ops = {e: [] for e in self.ENG}
        self.cnt = {e: 0 for e in self.ENG}
        self.known = {e: {} for e in self.ENG}
        self.sem = {e: stack.enter_context(nc.semaphore("s_" + e)) for e in self.ENG if e != "sp"}
        self.semname = {}
        for e, s in self.sem.items():
            self.semname[id(s)] = e
        self.dq = {}
        for q in ("sp", "pool", "act"):
            self.dq[q] = {"sems": [stack.enter_context(nc.semaphore(f"d_{q}{i}")) for i in range(self.NDMASEM)],
                          "n": 0}
        self.bufs = {}
        self.nops = 0

    def _buf(self, k):
        b = self.bufs.get(k)
        if b is None:
            b = [None, {}]
            self.bufs[k] = b
        return b

    def _deps(self, e, reads, writes, extra=()):
        toks = list(extra)
        for k in reads:
            b = self._buf(k)
            if b[0] is not None:
                toks.append((b[0], False))
        for k in writes:
            b = self._buf(k)
            if b[0] is not None:
                toks.append((b[0], False))
            for s, v in b[1].values():
                toks.append(((s, v), True))
        own = self.sem.get(e)
        waits = []
        kn = self.known[e]
        for (s, v), is_reader in toks:
            if s is own and (e == "pe" or is_reader):
                continue
            if kn.get(id(s), 0) >= v:
                continue
            kn[id(s)] = v
            waits.append((s, v))
        return waits

    def _commit(self, tok, reads, writes):
        s, v = tok
        for k in reads:
            b = self._buf(k)
            b[1][id(s)] = (s, v)
        for k in writes:
            b = self._buf(k)
            b[0] = tok
            b[1] = {}

    def op(self, e, fn, reads=(), writes=()):
        reads = tuple(reads)
        writes = tuple(writes)
        waits = self._deps(e, reads, writes)
        self.cnt[e] += 1
        tok = (self.sem[e], self.cnt[e])
        self.ops[e].append((waits, fn, (self.sem[e], 1)))
        self._commit(tok, reads, writes)
        self.nops += 1
        return tok

    def dma(self, q, out, in_, reads=(), writes=(), **kw):
        reads = tuple(reads)
        writes = tuple(writes)
        dq = self.dq[q]
        n = dq["n"]
        dq["n"] += 1
        s = dq["sems"][n % self.NDMASEM]
        prev = 16 * (n // self.NDMASEM)
        extra = [((s, prev), False)] if prev > 0 else []
        waits = self._deps(q, reads, writes, extra)
        tok = (s, prev + 16)
        self.ops[q].append((waits, lambda eng: eng.dma_start(out=out, in_=in_, **kw), (s, 16)))
        self._commit(tok, reads, writes)
        self.nops += 1
        return tok

    def barrier(self):
        toks = [(self.sem[e], self.cnt[e]) for e in self.sem if self.cnt[e] > 0] + self.all_dma_tokens(skip=("pool",))
        for e in self.ENG:
            waits = []
            kn = self.known[e]
            for s, v in toks:
                if e == "pe" and s is self.sem.get("pe"):
                    continue
                if kn.get(id(s), 0) >= v:
                    continue
                kn[id(s)] = v
                waits.append((s, v))
            if waits:
                self.ops[e].append((waits, None, None))
        self.bufs = {k: v for k, v in self.bufs.items() if isinstance(k, tuple) and k and k[0] == "wb"}

    def finish(self, e, toks):
        waits = []
        for s, v in toks:
            waits.append((s, v))
        self.ops[e].append((waits, None, None))

    def all_dma_tokens(self, skip=()):
        toks = []
        for q, dq in self.dq.items():
            if q in skip:
                continue
            n = dq["n"]
            for i in range(self.NDMASEM):
                c = (n - i + self.NDMASEM - 1) // self.NDMASEM if n > i else 0
                if c > 0:
                    toks.append((dq["sems"][i], 16 * c))
        return toks

    def emit(self):
        nc = self.nc
        ops = self.ops

        def run(eng, lst):
            for waits, fn, inc in lst:
                for s, v in waits:
                    eng.wait_ge(s, v)
                if fn is not None:
                    ins = fn(eng)
                    ins.then_inc(inc[0], inc[1])

        with nc.Block() as block:
            @block.tensor
            def _(eng):
                run(eng, ops["pe"])

            @block.vector
            def _(eng):
                run(eng, ops["dve"])

            @block.scalar
            def _(eng):
                run(eng, ops["act"])

            @block.gpsimd
            def _(eng):
                run(eng, ops["pool"])

            @block.sync
            def _(eng):
                run(eng, ops["sp"])


RWC = 1792
OFFS = {"A": 0, "B": 2048, "C": 4096, "D": 6160}
VECS = {"D": ["rD", "kD", "wD", "kkD", "bD"]}
ALLV = VECS["D"]
CHK = {"A": 64, "B": 32, "C": 64}
TB = 16
SMALL = ["hgrn_norm_w", "mlstm_norm_w", "rwkv_w0", "rwkv_a0", "rwkv_k_k", "rwkv_k_a", "rwkv_r_k", "rwkv_ln_w",
         "rwkv_ln_b"]


class Cfg:
    def __init__(self, T=2048, NSEQ=2, L=4):
        self.T = T
        self.NSEQ = NSEQ
        self.L = L
        self.NT = T * NSEQ
        self.TPS = T // 128
        self.NTILE = self.NT // 128


def wb_shape(K, N, CB=512):
    ncb = (N + CB - 1) // CB
    return [ncb, 128, K // 128, CB]


class Builder:
    def __init__(self, cfg):
        self.cfg = cfg
        self.nc = bass.Bass("TRN2", target_bir_lowering=False)
        self.stack = ExitStack()
        self.P = None

    def dram_in(self, name, shape, dt=F32):
        return self.nc.dram_tensor(name, list(shape), dt, kind="ExternalInput").ap()

    def dram_out(self, name, shape, dt=F32):
        return self.nc.dram_tensor(name, list(shape), dt, kind="ExternalOutput").ap()

    def dram_tmp(self, name, shape, dt=F32):
        return self.nc.dram_tensor(name, list(shape), dt, kind="Internal").ap()

    def sb(self, name, shape, dt=F32):
        h = self.stack.enter_context(self.nc.sbuf_tensor(name, list(shape), dt))
        return h[tuple(slice(None) for _ in shape)]

    def ps(self, name, shape, dt=F32):
        return self.stack.enter_context(self.nc.psum_tensor(name, list(shape), dt))

    def carve_reset(self):
        self.cptr = 0

    def carve(self, ncols, dt=F32):
        a = self.arena[:, self.cptr:self.cptr + ncols]
        self.cptr += ncols
        assert self.cptr <= self.NARENA, self.cptr
        if dt == BF16:
            a = a.bitcast(BF16)
        return a

    def tt(self, out, a, b, op, r, w, eng="dve"):
        self.P.op(eng, lambda e: e.tensor_tensor(out=out, in0=a, in1=b, op=op), reads=r, writes=w)

    def ts(self, out, a, s1, s2, op0, op1, r, w, eng="dve"):
        if op1 is None:
            self.P.op(eng, lambda e: e.tensor_scalar(out=out, in0=a, scalar1=s1, scalar2=None, op0=op0), reads=r, writes=w)
        else:
            self.P.op(eng, lambda e: e.tensor_scalar(out=out, in0=a, scalar1=s1, scalar2=s2, op0=op0, op1=op1),
                      reads=r, writes=w)

    def stt(self, out, a, scalar, b, op0, op1, r, w):
        self.P.op("dve", lambda e: e.scalar_tensor_tensor(out=out, in0=a, scalar=scalar, in1=b, op0=op0, op1=op1),
                  reads=r, writes=w)

    def actf(self, out, a, func, r, w, scale=1.0, bias=None):
        if bias is None:
            self.P.op("act", lambda e: e.activation(out=out, in_=a, func=func, scale=scale), reads=r, writes=w)
        else:
            self.P.op("act", lambda e: e.activation(out=out, in_=a, func=func, scale=scale, bias=bias), reads=r, writes=w)

    def red(self, out, a, r, w):
        self.P.op("dve", lambda e: e.tensor_reduce(out=out, in_=a, axis=AX.X, op=ALU.add), reads=r, writes=w)

    def cp(self, out, a, r, w, eng="act"):
        if eng == "act":
            self.P.op("act", lambda e: e.copy(out=out, in_=a), reads=r, writes=w)
        else:
            self.P.op(eng, lambda e: e.tensor_copy(out=out, in_=a), reads=r, writes=w)

    def psbank(self):
        b = self.psel % self.NPS
        self.psel += 1
        return b

    def cast_weight(self, W, Wb, K, N, CB=512):
        P = self.P
        KC = K // 128
        ncb = (N + CB - 1) // CB
        Wv = W.rearrange("(kc p) n -> p kc n", p=128)
        for cb in range(ncb):
            c0 = cb * CB
            nc_ = min(CB, N - c0)
            for k0 in range(0, KC, 16):
                k1 = min(KC, k0 + 16)
                P.dma("pool", Wb[cb, :, k0:k1, 0:nc_], Wv[:, k0:k1, c0:c0 + nc_], reads=(), writes=(("wb", id(Wb), cb),))

    def linear_tm(self, xT, xkeys, ng, KC, Wb, N, consume, CB=512):
        P = self.P
        ncb = (N + CB - 1) // CB
        for cb in range(ncb):
            nc_ = min(CB, N - cb * CB)
            b = self.wsel % 2
            self.wsel += 1
            wbuf = self.wbuf[b][:, 0:KC * CB].rearrange("p (k n) -> p k n", n=CB)
            wkeys = (("wbufh", b, 0), ("wbufh", b, 1))
            P.dma("sp", wbuf[:, :, 0:nc_], Wb[cb, :, 0:KC, 0:nc_], reads=(("wb", id(Wb), cb),), writes=wkeys)
            for g in range(ng):
                bank = self.psbank()
                pst = self.pslin[:, bank, 0:nc_]
                for kc in range(KC):
                    P.op("pe", (lambda e, pst=pst, g=g, kc=kc, wbuf=wbuf, nc_=nc_:
                                e.matmul(pst, lhsT=xT[:, kc, g, :], rhs=wbuf[:, kc, 0:nc_],
                                         start=(kc == 0), stop=(kc == KC - 1))),
                         reads=tuple(xkeys) + wkeys, writes=(("pslin", bank),))
                consume(g, cb, nc_, pst, ("pslin", bank))

    def rstd_of(self, src, src_key, n, eps_ap):
        P = self.P
        ss = self.ss_t
        junk = self.junk
        P.op("act", lambda e: e.activation(out=junk[:, 0:n], in_=src, func=AF.Square, accum_out=ss[:, 0:1]),
             reads=(src_key,), writes=("junk", "ss"))
        P.op("act", lambda e: e.activation(out=ss[:, 1:2], in_=ss[:, 0:1], func=AF.Sqrt, scale=1.0 / n, bias=eps_ap),
             reads=("ss", "eps"), writes=("ss1",))
        P.op("dve", lambda e: e.reciprocal(out=ss[:, 2:3], in_=ss[:, 1:2]), reads=("ss1",), writes=("ss2",))

    def norm_transpose(self, src_tile, src_key, wn_b, xT, g, xkey):
        self.rstd_of(src_tile, src_key, 2048, self.eps_t[:, 0:1])
        ub = self.ub
        self.stt(ub[:, 0:2048], src_tile, self.ss_t[:, 2:3], wn_b, ALU.mult, ALU.mult, (src_key, "ss2", "wnb"), ("ub",))
        self.transpose_to(ub, "ub", xT, g, xkey, 16)

    def transpose_to(self, ub, ubkey, xT, g, xkey, KC):
        P = self.P
        for k0 in range(0, KC, 4):
            k1 = min(KC, k0 + 4)
            bank = self.tsel % 2
            self.tsel += 1
            pt = self.pstr[:, bank, :]
            for kc in range(k0, k1):
                P.op("pe", (lambda e, kc=kc, pt=pt, k0=k0:
                            e.transpose(pt[:, (kc - k0) * 128:(kc - k0 + 1) * 128], ub[:, kc * 128:(kc + 1) * 128],
                                        self.ident_b[:, :])),
                     reads=(ubkey, "ident"), writes=(("pstr", bank),))
            n = k1 - k0
            P.op("act", (lambda e, pt=pt, k0=k0, n=n:
                         e.copy(out=xT[:, k0:k0 + n, g, :], in_=pt[:, 0:n * 128].rearrange("p (k t) -> p k t", t=128))),
                 reads=(("pstr", bank),), writes=(xkey,))

    def store_T(self, src, src_key, dstT, s, g):
        P = self.P
        vt = self.vt
        for hp in range(4):
            bank = self.psbank()
            pt = self.pslin[:, bank, 0:128]
            P.op("pe", lambda e, pt=pt, hp=hp: e.transpose(pt, src[:, hp * 128:(hp + 1) * 128], self.ident_f[:, :]),
                 reads=(src_key, "identf"), writes=(("pslin", bank),))
            self.cp(vt[:, hp, :], pt, (("pslin", bank),), ("vt",))
        P.dma("sp", dstT[:, :, s, g * 128:(g + 1) * 128].rearrange("hp p t -> p hp t"), vt[:, :, :], reads=("vt",),
              writes=(("T", id(dstT), s, g),))

    def bt_store(self, src_bf, src_key, dstT, s, g):
        P = self.P
        bank = self.tsel % 2
        self.tsel += 1
        pt = self.pstr[:, bank, :]
        for hp in range(4):
            P.op("pe", lambda e, hp=hp, pt=pt: e.transpose(pt[:, hp * 128:(hp + 1) * 128], src_bf[:, hp * 128:(hp + 1) * 128],
                                                          self.ident_b[:, :]),
                 reads=(src_key, "ident"), writes=(("pstr", bank),))
        tb = self.tbt
        P.op("act", lambda e, pt=pt: e.copy(out=tb[:, :, :], in_=pt[:, 0:512].rearrange("p (k t) -> p k t", t=128)),
             reads=(("pstr", bank),), writes=("tbt",))
        P.dma("sp", dstT[:, :, s, g * 128:(g + 1) * 128].rearrange("hp p t -> p hp t"), tb[:, :, :], reads=("tbt",),
              writes=(("T", id(dstT), s, g),))

    def store_v(self, v_src, v_key, v_d, rs):
        self.cp(self.vb[:, :, 0:64], v_src.rearrange("p (h e) -> p h e", e=64), (v_key,), ("vb",), eng="dve")
        self.P.dma("sp", v_d[rs, :], self.vb.rearrange("p h e -> p (h e)"), reads=("vb",), writes=(("vd", id(v_d)),))

    def load_T(self, dst, dst_key, srcT, s, g):
        P = self.P
        vt = self.vt
        P.dma("sp", vt[:, :, :], srcT[:, :, s, g * 128:(g + 1) * 128].rearrange("hp p t -> p hp t"),
              reads=(("T", id(srcT), s, g),), writes=("vt",))
        for hp in range(4):
            bank = self.psbank()
            pt = self.pslin[:, bank, 0:128]
            P.op("pe", lambda e, pt=pt, hp=hp: e.transpose(pt, vt[:, hp, :], self.ident_f[:, :]),
                 reads=("vt", "identf"), writes=(("pslin", bank),))
            self.cp(dst[:, hp * 128:(hp + 1) * 128], pt, (("pslin", bank),), (dst_key,))

    def load_shift(self, dst, key, c0, n, tile, k, extra_keys=()):
        P = self.P
        keys = (key,) + tuple(extra_keys)
        cfg = self.cfg
        g = tile % cfg.TPS
        r0 = tile * 128
        if g == 0:
            P.op("dve", lambda e: e.memset(dst[:, 0:n], 0.0), writes=keys)
            P.dma("sp", dst[k:128, 0:n], self.pbuf[r0:r0 + 128 - k, c0:c0 + n],
                  reads=tuple(("p", tile, cb) for cb in range(16)), writes=keys)
        else:
            P.dma("sp", dst[:, 0:n], self.pbuf[r0 - k:r0 - k + 128, c0:c0 + n],
                  reads=tuple(("p", tile, cb) for cb in range(16)) + tuple(("p", tile - 1, cb) for cb in range(16)),
                  writes=keys)

    def bcast_load(self, dst, src_row, key):
        self.P.dma("sp", dst, src_row.partition_broadcast(128), writes=(key,))

    def build(self, dbg=None, layers=None):
        cfg = self.cfg
        nc = self.nc
        L = cfg.L
        NT = cfg.NT
        T = cfg.T
        NSEQ = cfg.NSEQ
        self.P = P = Prog(nc, self.stack)
        layers = list(range(L)) if layers is None else layers
        I = {}
        x = self.dram_in("x", [NT, D])
        out = self.dram_out("out", [NT, D])
        w_in = self.dram_in("w_in", [L, D, NIN])
        w_out = self.dram_in("w_out", [L, D, D])
        w_g = self.dram_in("w_ffn_gate", [L, D, DFF])
        w_u = self.dram_in("w_ffn_up", [L, D, DFF])
        w_d = self.dram_in("w_ffn_down", [L, DFF, D])
        norms = self.dram_in("norms", [L, 4, D])
        for nm in SMALL:
            I[nm] = self.dram_in(nm, [L, G])
        I["hgrn_lb_logits"] = self.dram_in("hgrn_lb_logits", [L, G])
        I["mlstm_conv_w"] = self.dram_in("mlstm_conv_w", [L, 4, 2 * G])
        I["mlstm_conv_b"] = self.dram_in("mlstm_conv_b", [L, 2 * G])
        I["mlstm_if_bias"] = self.dram_in("mlstm_if_bias", [L, 16])
        I["rwkv_mu"] = self.dram_in("rwkv_mu", [L, RWC])
        I["rwkv_up_pad"] = self.dram_in("rwkv_up_pad", [L, 128, 1024])
        I["rwkv_g_up"] = self.dram_in("rwkv_g_up", [L, 128, G])
        I["rwkv_v0"] = self.dram_in("rwkv_v0", [max(L - 1, 1), G])
        I["rwkv_v_down"] = self.dram_in("rwkv_v_down", [max(L - 1, 1), G, 32])
        I["rwkv_v_up"] = self.dram_in("rwkv_v_up", [max(L - 1, 1), 32, G])
        ident_b_d = self.dram_in("ident_b", [128, 128], BF16)
        ident_f_d = self.dram_in("ident_f", [128, 128])
        cs_d = self.dram_in("cossin", [T, 64])
        sel_d = self.dram_in("sel", [32, TB, 128])
        gam_d = self.dram_in("gamA", [128, 512])
        atab_d = self.dram_in("atab", [4, 128, 512])
        tri_d = self.dram_in("trimats", [4, 128, 128])
        mask_d = self.dram_in("cmask", [64, 64])

        hbuf = self.dram_tmp("hbuf", [NT, D])
        self.pbuf = pbuf = self.dram_tmp("pbuf", [NT, NIN])
        obuf = self.dram_tmp("obuf", [NT, D])
        rows = {v: self.dram_tmp("row_" + v, [NT, G]) for v in ALLV}
        vT = {m: self.dram_tmp("vT_" + m, [4, 128, NSEQ, T]) for m in "D"}
        yT = {m: self.dram_tmp("yT_" + m, [4, 128, NSEQ, T]) for m in "D"}
        ck = dict(
            qT={m: self.dram_tmp("cqT_" + m, [4, 128, NSEQ, T], BF16) for m in "ABC"},
            kT={m: self.dram_tmp("ckT_" + m, [4, 128, NSEQ, T], BF16) for m in "ABC"},
            kh={m: self.dram_tmp("ckh_" + m, [NT, G], BF16) for m in "ABC"},
            v={m: self.dram_tmp("cv_" + m, [NT, 520], BF16) for m in "ABC"},
            dec={m: self.dram_tmp("cdec_" + m, [NT, G]) for m in "ABC"},
            y={m: self.dram_tmp("cy_" + m, [NT, 520]) for m in "ABC"},
            atab=atab_d, tri=tri_d, mask=mask_d)
        dmask_d = self.dram_in("dmasks", [3, 64, 64])
        dk = dict(rT=self.dram_tmp("d_rT", [4, 128, NSEQ, T], BF16), cT=self.dram_tmp("d_cT", [4, 128, NSEQ, T], BF16),
                  kT=self.dram_tmp("d_kT", [4, 128, NSEQ, T], BF16), bT=self.dram_tmp("d_bT", [4, 128, NSEQ, T], BF16),
                  kh=self.dram_tmp("d_kh", [NT, G], BF16), bh=self.dram_tmp("d_bh", [NT, G], BF16),
                  v=self.dram_tmp("d_v", [NT, G], BF16), dec=self.dram_tmp("d_dec", [NT, G]),
                  y=self.dram_tmp("d_y", [NT, G]), masks=dmask_d)
        ck["D"] = dk
        gate = {m: self.dram_tmp("gate_" + m, [NT, G]) for m in "ABCD"}
        bonus = self.dram_tmp("bonus", [NT, G])
        vfirst = self.dram_tmp("vfirst", [NT, G])
        wb_in = [self.dram_tmp(f"wb_in{l}", wb_shape(D, NIN), BF16) for l in range(L)]
        wb_out = [self.dram_tmp(f"wb_out{l}", wb_shape(D, D), BF16) for l in range(L)]
        wb_g = [self.dram_tmp(f"wb_g{l}", wb_shape(D, DFF), BF16) for l in range(L)]
        wb_u = [self.dram_tmp(f"wb_u{l}", wb_shape(D, DFF), BF16) for l in range(L)]
        wb_d = [self.dram_tmp(f"wb_d{l}", wb_shape(DFF, D, 128), BF16) for l in range(L)]
        dbg_t = None
        if dbg is not None and dbg[1] is not None:
            dbg_t = self.dram_out("dbg", dbg[1])

        self.ident_b = self.sb("ident_b_s", [128, 128], BF16)
        self.ident_f = self.sb("ident_f_s", [128, 128])
        self.ss_t = self.sb("ss", [128, 8])
        self.eps_t = self.sb("eps_t", [128, 4])
        gamA = self.sb("gamA_s", [128, 512])
        lbd = self.dram_tmp("lbd", [2, 128, L, G])
        wnb = self.sb("wnb", [128, 4, D])
        self.NARENA = 41000
        self.arena = self.sb("arena", [128, self.NARENA])
        self.NPS = 6
        self.pslin = self.ps("pslin", [128, self.NPS, 512])
        self.psel = 0
        self.pstr = self.ps("pstr", [128, 2, 1024], BF16)
        self.tsel = 0
        self.wsel = 0

        P.dma("sp", self.ident_b[:, :], ident_b_d[:, :], writes=("ident",))
        P.dma("sp", self.ident_f[:, :], ident_f_d[:, :], writes=("identf",))
        P.dma("sp", gamA[:, :], gam_d[:, :], writes=("gamA",))
        P.op("dve", lambda e: e.memset(self.eps_t[:, 0:1], NORM_EPS), writes=("eps",))
        P.op("dve", lambda e: e.memset(self.eps_t[:, 1:2], 64e-5), writes=("eps",))
        P.op("dve", lambda e: e.memset(self.eps_t[:, 2:3], 0.0), writes=("eps",))
        for i in range(0, NT, 512):
            P.dma("sp", hbuf[i:i + 512, :], x[i:i + 512, :], writes=tuple(("h", j) for j in range(i // 128, i // 128 + 4)))
        for l in layers:
            self.cast_weight(w_in[l], wb_in[l], D, NIN)
            self.cast_weight(w_out[l], wb_out[l], D, D)
            self.cast_weight(w_g[l], wb_g[l], D, DFF)
            self.cast_weight(w_u[l], wb_u[l], D, DFF)
            self.cast_weight(w_d[l], wb_d[l], DFF, D, 128)

        self.carve_reset()
        ex = self.carve(L * G).rearrange("p (l g) -> p l g", g=G)
        sm = self.carve(G)
        lbt = self.carve(L * G).rearrange("p (l g) -> p l g", g=G)
        oml = self.carve(L * G).rearrange("p (l g) -> p l g", g=G)
        P.dma("sp", ex, I["hgrn_lb_logits"].partition_broadcast(128), writes=("ex",))
        self.actf(ex, ex, AF.Exp, ("ex",), ("ex",))
        self.cp(sm, ex[:, 0, :], ("ex",), ("sm",), eng="dve")
        for j in range(1, L):
            self.tt(sm, sm, ex[:, j, :], ALU.add, ("sm", "ex"), ("sm",))
        P.op("dve", lambda e: e.reciprocal(out=sm, in_=sm), reads=("sm",), writes=("sm",))
        P.op("dve", lambda e: e.memset(lbt[:, 0, :], 0.0), writes=("lbt",))
        for j in range(1, L):
            self.tt(ex[:, j, :], ex[:, j, :], sm, ALU.mult, ("ex", "sm"), ("ex",))
            self.tt(lbt[:, j, :], lbt[:, j - 1, :], ex[:, j, :], ALU.add, ("lbt", "ex"), ("lbt",))
        self.ts(oml[:, :, :], lbt[:, :, :], -1.0, 1.0, ALU.mult, ALU.add, ("lbt",), ("oml",))
        P.dma("sp", lbd[0], lbt, reads=("lbt",), writes=("lbd",))
        P.dma("sp", lbd[1], oml, reads=("oml",), writes=("lbd",))
        P.barrier()

        for l in layers:
            P.dma("sp", wnb[:, :, :], norms[l].partition_broadcast(128), writes=("wnb",))
            self.phase_p1(l, hbuf, wb_in[l], wnb)
            P.barrier()
            if dbg is not None and dbg[0] == "p" and l == layers[-1]:
                break
            self.phase_prep(l, I, rows, vT, gate, bonus, vfirst, cs_d, lbd, ck)
            P.barrier()
            if dbg is not None and dbg[0] == "prep" and l == layers[-1]:
                break
            self.phase_scan(rows, vT, yT, sel_d, ck, only_abc=(dbg is not None and dbg[0] == "scanabc"))
            P.barrier()
            if dbg is not None and dbg[0] == "scanabc" and l == layers[-1]:
                break
            if dbg is not None and dbg[0] == "scan" and l == layers[-1]:
                break
            self.phase_post(l, I, yT, ck, gate, bonus, obuf)
            P.barrier()
            if dbg is not None and dbg[0] == "post" and l == layers[-1]:
                break
            self.phase_ffn(l, hbuf, obuf, wb_out[l], wb_g[l], wb_u[l], wb_d[l], wnb)
            P.barrier()

        if dbg_t is not None:
            src = {"p": pbuf, "post": obuf, "h": hbuf}.get(dbg[0])
            P.dma("sp", dbg_t, src)
        for i in range(0, NT, 512):
            P.dma("sp", out[i:i + 512, :], hbuf[i:i + 512, :])
        P.finish("sp", P.all_dma_tokens())
        P.emit()
        return nc

    def carve_linear(self, ng):
        self.carve_reset()
        self.xT = self.carve(16 * ng * 64, BF16).rearrange("p (k g t) -> p k g t", g=ng, t=128)
        self.wbuf = [self.carve(4096, BF16) for _ in range(2)]
        self.htile = [self.carve(2048) for _ in range(2)]
        self.junk = self.carve(2048)
        self.ub = self.carve(1024, BF16)
        self.stage = [self.carve(512) for _ in range(4)]
        self.ssel = 0

    def phase_p1(self, l, hbuf, wb, wnb):
        P = self.P
        cfg = self.cfg
        NG = 4
        self.carve_linear(NG)
        for grp in range(cfg.NTILE // NG):
            for g in range(NG):
                tile = grp * NG + g
                ht = self.htile[tile % 2]
                P.dma("sp", ht, hbuf[tile * 128:(tile + 1) * 128, :], reads=(("h", tile),), writes=(("htile", tile % 2),))
                self.norm_transpose(ht, ("htile", tile % 2), wnb[:, 0, :], self.xT, g, "xT")

            def consume(g, cb, nc_, pst, pskey, grp=grp):
                st = self.stage[self.ssel % 4]
                skey = ("stage", self.ssel % 4)
                self.ssel += 1
                self.cp(st[:, 0:nc_], pst, (pskey,), (skey,))
                tile = grp * NG + g
                P.dma("sp", self.pbuf[tile * 128:(tile + 1) * 128, cb * 512:cb * 512 + nc_], st[:, 0:nc_],
                      reads=(skey,), writes=(("p", tile, cb),))

            self.linear_tm(self.xT, ("xT",), NG, 16, wb, NIN, consume)

    def phase_prep(self, l, I, rows, vT, gate, bonus, vfirst, cs_d, lbd, ck):
        P = self.P
        cfg = self.cfg
        self.carve_reset()
        seg = self.carve(2064)
        sh = self.carve(3072)
        Wt = [self.carve(512) for _ in range(10)]
        R = self.carve(1024)
        cst = self.carve(64)
        self.vt = self.carve(512).rearrange("p (a b) -> p a b", b=128)
        cw = self.carve(4096).rearrange("p (j c) -> p j c", c=1024)
        cb_ = self.carve(1024)
        mu = self.carve(RWC)
        sv = {nm: self.carve(512) for nm in SMALL}
        v0b = self.carve(512)
        ifb = self.carve(16)
        upw = self.carve(1024)
        gup = self.carve(512)
        vdn = self.carve(128).rearrange("p (c n) -> p c n", n=32)
        vup = self.carve(512)
        small = self.carve(64)
        small2 = self.carve(64)
        QB = self.carve(256, BF16)
        KB = self.carve(256, BF16)
        KH = self.carve(256, BF16)
        XB1 = self.carve(256, BF16)
        XB2 = self.carve(256, BF16)
        XB3 = self.carve(256, BF16)
        XB4 = self.carve(256, BF16)
        self.tbt = self.carve(256, BF16).rearrange("p (a b) -> p a b", b=128)
        self.vb = self.carve(260, BF16).rearrange("p (h e) -> p h e", e=65)
        AT = self.carve(2048).rearrange("p (a c) -> p a c", c=512)
        tri = self.carve(512).rearrange("p (a c) -> p a c", c=128)
        P.dma("sp", AT, ck["atab"].rearrange("a p c -> p a c"), writes=("AT",))
        P.dma("sp", tri, ck["tri"].rearrange("a p c -> p a c"), writes=("tri",))
        P.op("dve", lambda e: e.memset(self.vb[:, :, 64:65], 1.0), writes=("vb",))
        lbt = self.carve(512)
        oml = self.carve(512)
        P.dma("sp", lbt, lbd[0, :, l, :], writes=("lbt",))
        P.dma("sp", oml, lbd[1, :, l, :], writes=("oml",))
        for nm in SMALL:
            self.bcast_load(sv[nm], I[nm][l], "c_" + nm)
        self.bcast_load(cw, I["mlstm_conv_w"][l], "cw")
        self.bcast_load(cb_, I["mlstm_conv_b"][l], "cb")
        self.bcast_load(mu, I["rwkv_mu"][l], "mu")
        self.bcast_load(ifb, I["mlstm_if_bias"][l], "ifb")
        P.dma("sp", upw, I["rwkv_up_pad"][l], writes=("upw",))
        P.dma("sp", gup, I["rwkv_g_up"][l], writes=("gup",))
        if l > 0:
            self.bcast_load(v0b, I["rwkv_v0"][l - 1], "v0b")
            P.dma("sp", vdn, I["rwkv_v_down"][l - 1].rearrange("(c p) n -> p c n", p=128), writes=("vdn",))
            P.dma("sp", vup[0:32, :], I["rwkv_v_up"][l - 1], writes=("vup",))
        pall = lambda tile: tuple(("p", tile, cb) for cb in range(16))
        W = Wt
        wk = lambda i: ("W", i)

        def h8(ap):
            return ap.rearrange("p (h d) -> p h d", d=64)

        def b8(ap8):
            return ap8.unsqueeze(2).to_broadcast([128, 8, 64])

        for tile in range(cfg.NTILE):
            s, g = tile // cfg.TPS, tile % cfg.TPS
            r0 = tile * 128
            rs = slice(r0, r0 + 128)
            P.dma("sp", seg[:, 0:2048], self.pbuf[rs, 0:2048], reads=pall(tile), writes=("seg",))
            P.dma("sp", cst, cs_d[g * 128:(g + 1) * 128, :], writes=("cst",))
            qk = seg[:, 0:1024].rearrange("p (h two d) -> p h two d", two=2, d=32)
            Rv = R.rearrange("p (h two d) -> p h two d", two=2, d=32)
            t1, t2 = qk[:, :, 0, :], qk[:, :, 1, :]
            cosb = cst[:, 0:32].unsqueeze(1).to_broadcast([128, 16, 32])
            sinb = cst[:, 32:64].unsqueeze(1).to_broadcast([128, 16, 32])
            w0v = W[0].rearrange("p (h d) -> p h d", d=32)
            w1v = W[1].rearrange("p (h d) -> p h d", d=32)
            self.tt(w0v, t1, cosb, ALU.mult, ("seg", "cst"), (wk(0),))
            self.tt(w1v, t2, sinb, ALU.mult, ("seg", "cst"), (wk(1),))
            self.tt(Rv[:, :, 0, :], w0v, w1v, ALU.subtract, (wk(0), wk(1)), ("R",))
            self.tt(w0v, t1, sinb, ALU.mult, ("seg", "cst"), (wk(0),))
            self.tt(w1v, t2, cosb, ALU.mult, ("seg", "cst"), (wk(1),))
            self.tt(Rv[:, :, 1, :], w0v, w1v, ALU.add, (wk(0), wk(1)), ("R",))
            self.tt(QB, R[:, 0:512], AT[:, 0, :], ALU.mult, ("R", "AT"), ("QB",))
            self.bt_store(QB, "QB", ck["qT"]["A"], s, g)
            self.tt(KB, R[:, 512:1024], AT[:, 1, :], ALU.mult, ("R", "AT"), ("KB",))
            self.bt_store(KB, "KB", ck["kT"]["A"], s, g)
            self.tt(KH, R[:, 512:1024], AT[:, 2, :], ALU.mult, ("R", "AT"), ("KH",))
            P.dma("sp", ck["kh"]["A"][rs, :], KH, reads=("KH",), writes=(("kh", "A"),))
            self.store_v(seg[:, 1024:1536], "seg", ck["v"]["A"], rs)
            P.dma("sp", ck["dec"]["A"][rs, :], AT[:, 3, :], reads=("AT",), writes=(("dec", "A"),))
            self.actf(W[2], seg[:, 1536:2048], AF.Silu, ("seg",), (wk(2),))
            P.dma("sp", gate["A"][rs, :], W[2], reads=(wk(2),), writes=(("gate", "A", tile),))
            P.dma("sp", seg[:, 0:2048], self.pbuf[rs, 2048:4096], reads=pall(tile), writes=("seg",))
            self.actf(W[0], seg[:, 0:512], AF.Silu, ("seg",), (wk(0),))
            self.ts(W[0], W[0], 0.125, None, ALU.mult, None, (wk(0),), (wk(0),))
            self.actf(W[1], seg[:, 512:1024], AF.Sigmoid, ("seg",), (wk(1),))
            self.tt(W[1], W[1], oml, ALU.mult, (wk(1), "oml"), (wk(1),))
            self.tt(W[1], W[1], lbt, ALU.add, (wk(1), "lbt"), (wk(1),))
            self.ts(W[3], W[1], -1.0, 1.0, ALU.mult, ALU.add, (wk(1),), (wk(3),))
            self.actf(W[4], W[1], AF.Ln, (wk(1),), (wk(4),))
            ba = self.psbank()
            pc = self.pslin[:, ba, :]
            P.op("pe", lambda e, pc=pc: e.matmul(pc, lhsT=tri[:, 2, :], rhs=W[4], start=True, stop=True),
                 reads=(wk(4), "tri"), writes=(("pslin", ba),))
            bb = self.psbank()
            ptot = self.pslin[:, bb, :]
            P.op("pe", lambda e, ptot=ptot: e.matmul(ptot, lhsT=tri[:, 3, :], rhs=W[4], start=True, stop=True),
                 reads=(wk(4), "tri"), writes=(("pslin", bb),))
            self.actf(W[5], pc, AF.Exp, (("pslin", ba),), (wk(5),))
            self.tt(QB, W[0], W[5], ALU.mult, (wk(0), wk(5)), ("QB",))
            self.bt_store(QB, "QB", ck["qT"]["B"], s, g)
            self.actf(W[5], pc, AF.Exp, (("pslin", ba),), (wk(5),), scale=-1.0)
            self.tt(KB, W[3], W[5], ALU.mult, (wk(3), wk(5)), ("KB",))
            self.bt_store(KB, "KB", ck["kT"]["B"], s, g)
            self.cp(W[6], ptot, (("pslin", bb),), (wk(6),))
            self.actf(W[7], W[6], AF.Exp, (wk(6),), (wk(7),))
            P.dma("sp", ck["dec"]["B"][rs, :], W[7], reads=(wk(7),), writes=(("dec", "B"),))
            self.tt(W[6], W[6], pc, ALU.subtract, (wk(6), ("pslin", ba)), (wk(6),))
            self.actf(W[6], W[6], AF.Exp, (wk(6),), (wk(6),))
            self.tt(KH, W[3], W[6], ALU.mult, (wk(3), wk(6)), ("KH",))
            P.dma("sp", ck["kh"]["B"][rs, :], KH, reads=("KH",), writes=(("kh", "B"),))
            self.store_v(seg[:, 1024:1536], "seg", ck["v"]["B"], rs)
            self.actf(W[2], seg[:, 1536:2048], AF.Sigmoid, ("seg",), (wk(2),))
            P.dma("sp", gate["B"][rs, :], W[2], reads=(wk(2),), writes=(("gate", "B", tile),))
            P.dma("sp", seg[:, 0:2064], self.pbuf[rs, 4096:6160], reads=pall(tile), writes=("seg",))
            shv = sh.rearrange("p (j c) -> p j c", c=1024)
            for j in range(3):
                self.load_shift(shv[:, j, :], ("sh", j), 4096, 1024, tile, 3 - j)
            acc = R
            self.tt(acc, seg[:, 0:1024], cw[:, 3, :], ALU.mult, ("seg", "cw"), ("R",))
            self.tt(acc, acc, cb_, ALU.add, ("R", "cb"), ("R",))
            for j in range(3):
                self.tt(shv[:, j, :], shv[:, j, :], cw[:, j, :], ALU.mult, (("sh", j), "cw"), (("sh", j),))
                self.tt(acc, acc, shv[:, j, :], ALU.add, ("R", ("sh", j)), ("R",))
            self.actf(acc, acc, AF.Silu, ("R",), ("R",))
            self.tt(small[:, 0:16], seg[:, 2048:2064], ifb, ALU.add, ("seg", "ifb"), ("small",))
            self.actf(small[:, 24:32], small[:, 8:16], AF.Sigmoid, ("small",), ("small3",))
            self.actf(small[:, 16:24], small[:, 24:32], AF.Ln, ("small3",), ("small2",))
            ba = self.psbank()
            pc8 = self.pslin[:, ba, 0:8]
            P.op("pe", lambda e, pc8=pc8: e.matmul(pc8, lhsT=tri[:, 0, :], rhs=small[:, 16:24], start=True, stop=True),
                 reads=("small2", "tri"), writes=(("pslin", ba),))
            bb = self.psbank()
            pt8 = self.pslin[:, bb, 0:8]
            P.op("pe", lambda e, pt8=pt8: e.matmul(pt8, lhsT=tri[:, 1, :], rhs=small[:, 16:24], start=True, stop=True),
                 reads=("small2", "tri"), writes=(("pslin", bb),))
            self.actf(small2[:, 0:8], pc8, AF.Exp, (("pslin", ba),), ("s2a",))
            self.tt(h8(QB), h8(acc[:, 0:512]), b8(small2[:, 0:8]), ALU.mult, ("R", "s2a"), ("QB",))
            self.bt_store(QB, "QB", ck["qT"]["C"], s, g)
            self.tt(small2[:, 8:16], small[:, 0:8], pc8, ALU.subtract, ("small", ("pslin", ba)), ("s2b",))
            self.actf(small2[:, 16:24], small2[:, 8:16], AF.Exp, ("s2b",), ("s2c",))
            self.stt(h8(KB), h8(acc[:, 512:1024]), 0.125, b8(small2[:, 16:24]), ALU.mult, ALU.mult, ("R", "s2c"), ("KB",))
            self.bt_store(KB, "KB", ck["kT"]["C"], s, g)
            self.tt(small2[:, 24:32], small2[:, 8:16], pt8, ALU.add, ("s2b", ("pslin", bb)), ("s2d",))
            self.actf(small2[:, 32:40], small2[:, 24:32], AF.Exp, ("s2d",), ("s2e",))
            self.stt(h8(KH), h8(acc[:, 512:1024]), 0.125, b8(small2[:, 32:40]), ALU.mult, ALU.mult, ("R", "s2e"), ("KH",))
            P.dma("sp", ck["kh"]["C"][rs, :], KH, reads=("KH",), writes=(("kh", "C"),))
            self.actf(small2[:, 40:48], pt8, AF.Exp, (("pslin", bb),), ("s2f",))
            self.cp(h8(W[1]), b8(small2[:, 40:48]), ("s2f",), (wk(1),), eng="dve")
            P.dma("sp", ck["dec"]["C"][rs, :], W[1], reads=(wk(1),), writes=(("dec", "C"),))
            self.store_v(seg[:, 1024:1536], "seg", ck["v"]["C"], rs)
            self.actf(W[2], seg[:, 1536:2048], AF.Sigmoid, ("seg",), (wk(2),))
            P.dma("sp", gate["C"][rs, :], W[2], reads=(wk(2),), writes=(("gate", "C", tile),))
            P.dma("sp", seg[:, 0:RWC], self.pbuf[rs, 6160:6160 + RWC], reads=pall(tile), writes=("seg",))
            prev = sh[:, 0:RWC]
            self.load_shift(prev, ("sh", 0), 6160, RWC, tile, 1, extra_keys=(("sh", 1),))
            self.tt(prev, prev, seg[:, 0:RWC], ALU.subtract, (("sh", 0), ("sh", 1), "seg"), (("sh", 0), ("sh", 1)))
            self.tt(prev, prev, mu, ALU.mult, (("sh", 0), ("sh", 1), "mu"), (("sh", 0), ("sh", 1)))
            self.tt(seg[:, 0:RWC], seg[:, 0:RWC], prev, ALU.add, ("seg", ("sh", 0), ("sh", 1)), ("seg",))
            r_, k_, v_ = seg[:, 0:512], seg[:, 512:1024], seg[:, 1024:1536]
            Lt = W[0][:, 0:256]
            self.actf(Lt[:, 0:64], seg[:, 1536:1600], AF.Tanh, ("seg",), (wk(0),))
            self.cp(Lt[:, 64:128], seg[:, 1600:1664], ("seg",), (wk(0),))
            self.actf(Lt[:, 128:256], seg[:, 1664:1792], AF.Sigmoid, ("seg",), (wk(0),))
            LT = W[1][:, 0:256]
            for c in range(2):
                bank = self.psbank()
                pt = self.pslin[:, bank, 0:128]
                P.op("pe", lambda e, pt=pt, c=c: e.transpose(pt, Lt[:, c * 128:(c + 1) * 128], self.ident_f[:, :]),
                     reads=(wk(0), "identf"), writes=(("pslin", bank),))
                self.cp(LT[:, c * 128:(c + 1) * 128], pt, (("pslin", bank),), (wk(1),))
            bank = self.psbank()
            pw = self.pslin[:, bank, :]
            P.op("pe", lambda e, pw=pw: e.matmul(pw, lhsT=LT[:, 0:128], rhs=upw[:, 0:512], start=True, stop=True),
                 reads=(wk(1), "upw"), writes=(("pslin", bank),))
            self.tt(W[2], pw, sv["rwkv_w0"], ALU.add, (("pslin", bank), "c_rwkv_w0"), (wk(2),))
            self.actf(W[2], W[2], AF.Sigmoid, (wk(2),), (wk(2),))
            self.ts(W[2], W[2], -0.6065306597126334, None, ALU.mult, None, (wk(2),), (wk(2),))
            bank = self.psbank()
            pa = self.pslin[:, bank, :]
            P.op("pe", lambda e, pa=pa: e.matmul(pa, lhsT=LT[:, 0:128], rhs=upw[:, 512:1024], start=True, stop=True),
                 reads=(wk(1), "upw"), writes=(("pslin", bank),))
            self.tt(W[3], pa, sv["rwkv_a0"], ALU.add, (("pslin", bank), "c_rwkv_a0"), (wk(3),))
            self.actf(W[3], W[3], AF.Sigmoid, (wk(3),), (wk(3),))
            bank = self.psbank()
            pg = self.pslin[:, bank, :]
            P.op("pe", lambda e, pg=pg: e.matmul(pg, lhsT=LT[:, 128:256], rhs=gup, start=True, stop=True),
                 reads=(wk(1), "gup"), writes=(("pslin", bank),))
            self.cp(W[4], pg, (("pslin", bank),), (wk(4),))
            P.dma("sp", gate["D"][rs, :], W[4], reads=(wk(4),), writes=(("gate", "D", tile),))
            if l == 0:
                P.dma("sp", vfirst[rs, :], v_, reads=("seg",), writes=(("vfirst", tile),))
            else:
                vtt = self.vt
                for c in range(4):
                    bank = self.psbank()
                    pt = self.pslin[:, bank, 0:128]
                    P.op("pe", lambda e, pt=pt, c=c: e.transpose(pt, v_[:, c * 128:(c + 1) * 128], self.ident_f[:, :]),
                         reads=("seg", "identf"), writes=(("pslin", bank),))
                    self.cp(vtt[:, c, :], pt, (("pslin", bank),), ("vt",))
                bank = self.psbank()
                pv = self.pslin[:, bank, 0:32]
                for c in range(4):
                    P.op("pe", lambda e, pv=pv, c=c: e.matmul(pv, lhsT=vtt[:, c, :], rhs=vdn[:, c, :], start=(c == 0),
                                                              stop=(c == 3)),
                         reads=("vt", "vdn"), writes=(("pslin", bank),))
                self.cp(W[5][:, 0:32], pv, (("pslin", bank),), (wk(5),))
                bank = self.psbank()
                pt = self.pslin[0:32, bank, 0:128]
                P.op("pe", lambda e, pt=pt: e.transpose(pt, W[5][:, 0:32], self.ident_f[:, :]),
                     reads=(wk(5), "identf"), writes=(("pslin", bank),))
                self.cp(W[6][0:32, 0:128], pt, (("pslin", bank),), (wk(6),))
                bank = self.psbank()
                pv2 = self.pslin[:, bank, :]
                P.op("pe", lambda e, pv2=pv2: e.matmul(pv2, lhsT=W[6][0:32, 0:128], rhs=vup[0:32, :], start=True, stop=True),
                     reads=(wk(6), "vup"), writes=(("pslin", bank),))
                self.tt(W[5], pv2, v0b, ALU.add, (("pslin", bank), "v0b"), (wk(5),))
                self.actf(W[5], W[5], AF.Sigmoid, (wk(5),), (wk(5),))
                P.dma("sp", W[6], vfirst[rs, :], reads=(("vfirst", tile),), writes=(wk(6),))
                self.tt(W[6], W[6], v_, ALU.subtract, (wk(6), "seg"), (wk(6),))
                self.tt(W[6], W[6], W[5], ALU.mult, (wk(6), wk(5)), (wk(6),))
                self.tt(v_, v_, W[6], ALU.add, ("seg", wk(6)), ("seg",))
            self.tt(W[5], k_, sv["rwkv_k_k"], ALU.mult, ("seg", "c_rwkv_k_k"), (wk(5),))
            self.tt(W[6], W[5], W[5], ALU.mult, (wk(5),), (wk(6),))
            self.red(small[:, 32:40], h8(W[6]), (wk(6),), ("small4",))
            self.actf(small[:, 40:48], small[:, 32:40], AF.Sqrt, ("small4",), ("small5",))
            self.ts(small[:, 40:48], small[:, 40:48], 1e-12, None, ALU.max, None, ("small5",), ("small5",))
            P.op("dve", lambda e: e.reciprocal(out=small[:, 48:56], in_=small[:, 40:48]), reads=("small5",), writes=("small6",))
            self.tt(h8(W[5]), h8(W[5]), b8(small[:, 48:56]), ALU.mult, (wk(5), "small6"), (wk(5),))
            self.stt(W[7], W[3], -1.0, sv["rwkv_k_a"], ALU.add, ALU.mult, (wk(3), "c_rwkv_k_a"), (wk(7),))
            self.stt(W[7], W[7], 1.0, k_, ALU.add, ALU.mult, (wk(7), "seg"), (wk(7),))
            self.tt(W[8], W[3], W[5], ALU.mult, (wk(3), wk(5)), (wk(8),))
            self.tt(W[9], r_, W[7], ALU.mult, ("seg", wk(7)), (wk(9),))
            self.tt(W[9], W[9], sv["rwkv_r_k"], ALU.mult, (wk(9), "c_rwkv_r_k"), (wk(9),))
            self.red(small[:, 56:64], h8(W[9]), (wk(9),), ("small7",))
            self.tt(h8(W[9]), h8(v_), b8(small[:, 56:64]), ALU.mult, ("seg", "small7"), (wk(9),))
            P.dma("sp", bonus[rs, :], W[9], reads=(wk(9),), writes=(("bonus", tile),))
            dk = ck["D"]
            ba = self.psbank()
            pc = self.pslin[:, ba, :]
            P.op("pe", lambda e, pc=pc: e.matmul(pc, lhsT=tri[:, 2, :], rhs=W[2], start=True, stop=True),
                 reads=(wk(2), "tri"), writes=(("pslin", ba),))
            bb = self.psbank()
            ptot = self.pslin[:, bb, :]
            P.op("pe", lambda e, ptot=ptot: e.matmul(ptot, lhsT=tri[:, 3, :], rhs=W[2], start=True, stop=True),
                 reads=(wk(2), "tri"), writes=(("pslin", bb),))
            self.actf(W[0], pc, AF.Exp, (("pslin", ba),), (wk(0),))
            self.tt(QB, r_, W[0], ALU.mult, ("seg", wk(0)), ("QB",))
            self.bt_store(QB, "QB", dk["rT"], s, g)
            self.tt(W[1], pc, W[2], ALU.subtract, (("pslin", ba), wk(2)), (wk(1),))
            self.actf(W[1], W[1], AF.Exp, (wk(1),), (wk(1),))
            self.tt(KB, W[5], W[1], ALU.mult, (wk(5), wk(1)), ("KB",))
            self.bt_store(KB, "KB", dk["cT"], s, g)
            self.actf(W[0], pc, AF.Exp, (("pslin", ba),), (wk(0),), scale=-1.0)
            self.tt(XB1, W[7], W[0], ALU.mult, (wk(7), wk(0)), ("XB1",))
            self.bt_store(XB1, "XB1", dk["kT"], s, g)
            self.tt(XB2, W[8], W[0], ALU.mult, (wk(8), wk(0)), ("XB2",))
            self.bt_store(XB2, "XB2", dk["bT"], s, g)
            self.cp(W[6], ptot, (("pslin", bb),), (wk(6),))
            self.actf(W[1], W[6], AF.Exp, (wk(6),), (wk(1),))
            P.dma("sp", dk["dec"][rs, :], W[1], reads=(wk(1),), writes=(("dec", "D"),))
            self.tt(W[6], W[6], pc, ALU.subtract, (wk(6), ("pslin", ba)), (wk(6),))
            self.actf(W[6], W[6], AF.Exp, (wk(6),), (wk(6),))
            self.tt(KH, W[7], W[6], ALU.mult, (wk(7), wk(6)), ("KH",))
            P.dma("sp", dk["kh"][rs, :], KH, reads=("KH",), writes=(("kh", "D"),))
            self.stt(XB3, W[8], -1.0, W[6], ALU.mult, ALU.mult, (wk(8), wk(6)), ("XB3",))
            P.dma("sp", dk["bh"][rs, :], XB3, reads=("XB3",), writes=(("bh", "D"),))
            self.cp(XB4, v_, ("seg",), ("XB4",), eng="dve")
            P.dma("sp", dk["v"][rs, :], XB4, reads=("XB4",), writes=(("vd", "D"),))

    def phase_scan(self, rows, vT, yT, sel_d, ck, only_abc=False):
        P = self.P
        cfg = self.cfg
        self.carve_reset()
        self._phase_id = getattr(self, "_phase_id", 0) + 1

        def chain():
            for m in "ABC":
                for _ in self.gen_chunk(m, CHK[m], ck):
                    yield
        ga = chain()
        gd = self.chunk_D(ck["D"])
        na = 5 * sum((cfg.T // CHK[m]) * cfg.NSEQ * 4 for m in "ABC")
        nd = 19 * (cfg.T // 32 + 1) * cfg.NSEQ * 4
        a_done = d_done = False
        ia = idd = 0
        while not (a_done and d_done):
            if not d_done and (a_done or idd * na <= ia * nd):
                try:
                    next(gd)
                    idd += 1
                except StopIteration:
                    d_done = True
            elif not a_done:
                try:
                    next(ga)
                    ia += 1
                except StopIteration:
                    a_done = True

    def chunk_D(self, dk):
        P = self.P
        cfg = self.cfg
        T = cfg.T
        C = 32
        nch = T // C
        XT = self.carve(T // 2, BF16)
        X2 = {n: self.carve(T, BF16).rearrange("p (n h t) -> p n h t", h=2, t=C) for n in ("r", "c", "k", "b")}
        Vt = self.carve(nch * 32, BF16).rearrange("p (n e) -> p n e", e=64)
        KH2 = self.carve(nch * 64, BF16).rearrange("p (n e) -> p n e", e=128)
        BH2 = self.carve(nch * 64, BF16).rearrange("p (n e) -> p n e", e=128)
        YH = 32
        yo = self.carve(YH * 64).rearrange("p (n e) -> p n e", e=64)
        dch = self.carve(128)
        decT = self.carve(64)
        Zf = self.carve(64)
        Zb = self.carve(32, BF16)
        msk = self.carve(192).rearrange("p (a c) -> p a c", c=64)
        ST = [dict(N=[self.carve(64) for _ in range(5)], NT=[self.carve(64) for _ in range(4)], P=self.carve(64),
                   Q=self.carve(64), BTm=self.carve(32, BF16), S1m=self.carve(32, BF16), S2m=self.carve(32, BF16))
              for _ in range(2)]
        RHSs = self.carve(64)
        Ub = self.carve(32, BF16)
        pstr_f = self.pstr[:, :, :].bitcast(F32)
        I64 = self.ident_f[0:64, 0:64]
        P.dma("sp", msk[0:64, :, :], dk["masks"].rearrange("a p c -> p a c"), writes=("msk",))
        for n in X2:
            P.op("dve", lambda e, n=n: e.memset(X2[n], 0.0), writes=(("X2", n),))
        P.op("dve", lambda e: e.memset(KH2, 0.0), writes=("KH2",))
        P.op("dve", lambda e: e.memset(BH2, 0.0), writes=("BH2",))
        Mup, Mlow, Minc = msk[0:64, 0, :], msk[0:64, 1, :], msk[0:64, 2, :]
        srcT = {"r": dk["rT"], "c": dk["cT"], "k": dk["kT"], "b": dk["bT"]}
        pe = lambda fn, r, w: P.op("pe", fn, reads=r, writes=w)
        for s in range(cfg.NSEQ):
            for hp in range(4):
                for n in ("r", "c", "k", "b"):
                    P.dma("sp", XT[:, 0:T], srcT[n][hp, :, s, :], reads=tuple(("T", id(srcT[n]), s, g) for g in range(cfg.TPS)),
                          writes=("XT",))
                    self.cp(X2[n][0:64, :, 0, :], XT[0:64, 0:T].rearrange("p (n t) -> p n t", t=C), ("XT",), (("X2", n),),
                            eng="dve")
                    self.cp(X2[n][64:128, :, 1, :], XT[64:128, 0:T].rearrange("p (n t) -> p n t", t=C), ("XT",), (("X2", n),))
                rsl = slice(s * T, (s + 1) * T)
                for hh in range(2):
                    col = slice((2 * hp + hh) * 64, (2 * hp + hh + 1) * 64)
                    ps_ = slice(hh * 32, (hh + 1) * 32)
                    P.dma("sp", Vt[ps_, 0:nch, :], dk["v"][rsl, col].rearrange("(n c) k -> c n k", c=C), reads=(("vd", "D"),),
                          writes=("Vt",))
                    P.dma("sp", KH2[ps_, 0:nch, hh * 64:(hh + 1) * 64], dk["kh"][rsl, col].rearrange("(n c) k -> c n k", c=C),
                          reads=(("kh", "D"),), writes=("KH2",))
                    P.dma("sp", BH2[ps_, 0:nch, hh * 64:(hh + 1) * 64], dk["bh"][rsl, col].rearrange("(n c) k -> c n k", c=C),
                          reads=(("bh", "D"),), writes=("BH2",))
                P.dma("sp", dch[0:nch, :], dk["dec"][rsl, hp * 128:(hp + 1) * 128].rearrange("(n c) k -> c n k", c=C)[C - 1, :, :],
                      reads=(("dec", "D"),), writes=("dchD",))
                pt = self.pslin[:, 5, 0:nch]
                pe(lambda e, pt=pt: e.transpose(pt, dch[0:nch, :], self.ident_f[0:nch, 0:nch]), ("dchD", "identf"), (("pslin", 5),))
                self.cp(decT[:, 0:nch], pt, (("pslin", 5),), ("decTD",))
                P.op("dve", lambda e: e.memset(Zf, 0.0), writes=("Zf",))
                P.op("dve", lambda e: e.memset(Zb, 0.0), writes=("Zb",))
                r2, c2, k2, b2 = X2["r"], X2["c"], X2["k"], X2["b"]
                xk = lambda n: ("X2", n)

                def front(c):
                    par = c % 2
                    st = ST[par]
                    sk_ = lambda nm: ("ST", par, nm)
                    sc = self.pslin[0:64, par, :]
                    bk = ("pslin", par)
                    fl = lambda ap: ap.rearrange("p h t -> p (h t)")
                    pe(lambda e: e.matmul(sc[:, 0:64], lhsT=fl(b2[:, c]), rhs=fl(c2[:, c]), start=True, stop=True), (xk("b"), xk("c")), (bk,))
                    pe(lambda e: e.matmul(sc[:, 64:128], lhsT=fl(c2[:, c]), rhs=fl(b2[:, c]), start=True, stop=True), (xk("b"), xk("c")), (bk,))
                    pe(lambda e: e.matmul(sc[:, 128:192], lhsT=fl(k2[:, c]), rhs=fl(c2[:, c]), start=True, stop=True), (xk("k"), xk("c")), (bk,))
                    pe(lambda e: e.matmul(sc[:, 192:256], lhsT=fl(k2[:, c]), rhs=fl(r2[:, c]), start=True, stop=True), (xk("k"), xk("r")), (bk,))
                    pe(lambda e: e.matmul(sc[:, 256:320], lhsT=fl(b2[:, c]), rhs=fl(r2[:, c]), start=True, stop=True), (xk("b"), xk("r")), (bk,))
                    yield
                    N, NT, Pm, Qm = st["N"], st["NT"], st["P"][0:64, :], st["Q"][0:64, :]
                    self.stt(N[0][0:64, :], sc[:, 0:64], -1.0, Mup, ALU.mult, ALU.mult, (bk, "msk"), (sk_("N0"),))
                    self.stt(NT[0][0:64, :], sc[:, 64:128], -1.0, Mlow, ALU.mult, ALU.mult, (bk, "msk"), (sk_("NT0"),))
                    self.tt(st["BTm"][0:64, :], sc[:, 128:192], Mup, ALU.mult, (bk, "msk"), (sk_("BTm"),))
                    self.tt(st["S1m"][0:64, :], sc[:, 192:256], Minc, ALU.mult, (bk, "msk"), (sk_("S1m"),))
                    self.stt(st["S2m"][0:64, :], sc[:, 256:320], -1.0, Minc, ALU.mult, ALU.mult, (bk, "msk"), (sk_("S2m"),))
                    self.tt(Pm, N[0][0:64, :], I64, ALU.add, (sk_("N0"), "identf"), (sk_("P"),))
                    self.tt(Qm, NT[0][0:64, :], I64, ALU.add, (sk_("NT0"), "identf"), (sk_("Q"),))
                    yield
                    for k in range(1, 5):
                        last = (k == 4)
                        pn = self.pslin[0:64, 2, 0:128]
                        pe(lambda e, k=k: e.matmul(pn[:, 0:64], lhsT=NT[k - 1][0:64, :], rhs=N[k - 1][0:64, :], start=True, stop=True),
                           (sk_("N%d" % (k - 1)), sk_("NT%d" % (k - 1))), (("pslin", 2),))
                        if not last:
                            pe(lambda e, k=k: e.matmul(pn[:, 64:128], lhsT=N[k - 1][0:64, :], rhs=NT[k - 1][0:64, :], start=True,
                                                       stop=True),
                               (sk_("N%d" % (k - 1)), sk_("NT%d" % (k - 1))), (("pslin", 2),))
                        yield
                        self.cp(N[k][0:64, :], pn[:, 0:64], (("pslin", 2),), (sk_("N%d" % k),))
                        if not last:
                            self.cp(NT[k][0:64, :], pn[:, 64:128], (("pslin", 2),), (sk_("NT%d" % k),))
                        yield
                        pp = self.pslin[0:64, 2, 128:256]
                        pe(lambda e, k=k: e.matmul(pp[:, 0:64], lhsT=Qm, rhs=N[k][0:64, :], start=True, stop=True),
                           (sk_("Q"), sk_("N%d" % k)), (("pslin", 2),))
                        if not last:
                            pe(lambda e, k=k: e.matmul(pp[:, 64:128], lhsT=N[k][0:64, :], rhs=Qm, start=True, stop=True),
                               (sk_("Q"), sk_("N%d" % k)), (("pslin", 2),))
                        yield
                        self.tt(Pm, Pm, pp[:, 0:64], ALU.add, (sk_("P"), ("pslin", 2)), (sk_("P"),))
                        if not last:
                            self.tt(Qm, Qm, pp[:, 64:128], ALU.add, (sk_("Q"), ("pslin", 2)), (sk_("Q"),))
                        yield

                def back(c):
                    par = c % 2
                    st = ST[par]
                    sk_ = lambda nm: ("ST", par, nm)
                    fl = lambda ap: ap.rearrange("p h t -> p (h t)")
                    pr = self.pslin[0:64, 3, 0:64]
                    pe(lambda e: e.matmul(pr, lhsT=fl(c2[:, c]), rhs=Zb[:, 0:64], start=True, stop=False), (xk("c"), "Zb"), (("pslin", 3),))
                    pe(lambda e: e.matmul(pr, lhsT=st["BTm"][0:64, :], rhs=Vt[0:64, c, :], start=False, stop=True),
                       (sk_("BTm"), "Vt"), (("pslin", 3),))
                    yield
                    self.cp(RHSs[0:64, :], pr, (("pslin", 3),), ("RHSs",))
                    yield
                    pu = self.pslin[0:64, 3, 64:128]
                    pe(lambda e: e.matmul(pu, lhsT=st["P"][0:64, :], rhs=RHSs[0:64, :], start=True, stop=True), (sk_("P"), "RHSs"),
                       (("pslin", 3),))
                    yield
                    self.cp(Ub[0:64, :], pu, (("pslin", 3),), ("Ub",))
                    yield
                    pk = self.pslin[:, 5, 0:64]
                    pe(lambda e: e.matmul(pk, lhsT=KH2[0:64, c, :], rhs=Vt[0:64, c, :], start=True, stop=False), ("KH2", "Vt"),
                       (("pslin", 5),))
                    pe(lambda e: e.matmul(pk, lhsT=BH2[0:64, c, :], rhs=Ub[0:64, :], start=False, stop=True), ("BH2", "Ub"),
                       (("pslin", 5),))
                    py = self.pslin[0:64, 3, 128:192]
                    pe(lambda e: e.matmul(py, lhsT=st["S1m"][0:64, :], rhs=Vt[0:64, c, :], start=True, stop=False), (sk_("S1m"), "Vt"),
                       (("pslin", 3),))
                    pe(lambda e: e.matmul(py, lhsT=st["S2m"][0:64, :], rhs=Ub[0:64, :], start=False, stop=False), (sk_("S2m"), "Ub"),
                       (("pslin", 3),))
                    pe(lambda e: e.matmul(py, lhsT=fl(r2[:, c]), rhs=Zb[:, 0:64], start=False, stop=True), (xk("r"), "Zb"),
                       (("pslin", 3),))
                    yield
                    self.stt(Zf, Zf, decT[:, c:c + 1], pk, ALU.mult, ALU.add, ("Zf", "decTD", ("pslin", 5)), ("Zf",))
                    self.cp(yo[0:64, c % YH, :], py, (("pslin", 3),), ("yoD",))
                    yield
                    self.cp(Zb, Zf, ("Zf",), ("Zb",))
                    if c % YH == YH - 1 or c == nch - 1:
                        c0 = c - (c % YH)
                        for hh in range(2):
                            col = slice((2 * hp + hh) * 64, (2 * hp + hh + 1) * 64)
                            P.dma("sp", dk["y"][s * T + c0 * C:s * T + (c + 1) * C, col].rearrange("(n c) k -> c n k", c=C),
                                  yo[hh * 32:(hh + 1) * 32, 0:c - c0 + 1, :], reads=("yoD",), writes=(("cy", "D"),))
                    yield

                import itertools
                for c in range(nch + 1):
                    gf = front(c) if c < nch else iter(())
                    gb = back(c - 1) if c > 0 else iter(())
                    for _ in itertools.zip_longest(gf, gb):
                        yield

    def gen_scan_D(self, rows, vT, yT, sel_d):
        P = self.P
        cfg = self.cfg
        T = cfg.T
        sel = self.carve(TB * 128).rearrange("p (t m) -> p t m", m=128)
        P.dma("sp", sel[0:32, :, :], sel_d[:, :, :], writes=("sel",))
        Rb = [self.carve(5 * 512).rearrange("p (v n) -> p v n", n=512) for _ in range(2)]
        Vb = [self.carve(8 * TB).rearrange("p (g t) -> p g t", t=TB) for _ in range(2)]
        Yb = [self.carve(8 * TB).rearrange("p (g t) -> p g t", t=TB) for _ in range(2)]
        NR = 3
        BR = [self.carve(5 * 512).rearrange("p (v n) -> p v n", n=512) for _ in range(NR)]
        S = self.carve(512)
        T1 = self.carve(512)
        T2 = self.carve(512)
        sk = self.carve(8)
        h8 = lambda ap: ap.rearrange("p (h d) -> p h d", d=64)
        b8 = lambda ap8: ap8.unsqueeze(2).to_broadcast([128, 8, 64])
        m = "D"
        vecs = VECS[m]
        P.op("dve", lambda e: e.memset(S, 0.0), writes=("S",))
        step = 0
        dps = 0
        for bi in range(T // TB):
            t0 = bi * TB
            pb = bi % 2
            for j, vname in enumerate(vecs):
                src = rows[vname].rearrange("(s t) (hp hh d) -> t s hp hh d", s=cfg.NSEQ, hh=2, d=64)
                for hh in range(2):
                    for s in range(cfg.NSEQ):
                        P.dma("sp", Rb[pb][hh * TB:(hh + 1) * TB, j, s * 256:(s + 1) * 256].rearrange(
                            "t (hp d) -> t hp d", d=64),
                            src[t0:t0 + TB, s, :, hh, :],
                            reads=(("row", vname, (s * T + t0) // 128),), writes=(("Rb", pb, j),))
            P.dma("sp", Vb[pb].rearrange("p (s hp) t -> p s hp t", hp=4),
                  vT[m].rearrange("hp p s t -> p s hp t")[:, :, :, t0:t0 + TB],
                  reads=tuple(("T", id(vT[m]), s, t0 // 128) for s in range(cfg.NSEQ)), writes=(("Vb", pb),))
            for tl in range(TB):
                rb = step % NR
                step += 1
                ps = {}
                for j, vname in enumerate(vecs):
                    bank = dps % 3
                    dps += 1
                    pt = self.pslin[:, bank, :]
                    P.op("pe", lambda e, pt=pt, j=j, tl=tl, pb=pb: e.matmul(pt, lhsT=sel[0:32, tl, :], rhs=Rb[pb][0:32, j, :],
                                                                           start=True, stop=True),
                         reads=("sel", ("Rb", pb, j)), writes=(("pslin", bank),))
                    self.cp(BR[rb][:, j, :], pt, (("pslin", bank),), (("BR", rb, j),))
                    ps[j] = (BR[rb][:, j, :], ("BR", rb, j))
                vb = b8(Vb[pb][:, :, tl])
                yo = Yb[pb][:, :, tl]
                (rp, rk_), (kp, kk_), (wp, wk_), (cp_, ck_), (bp, bk_) = ps[0], ps[1], ps[2], ps[3], ps[4]
                self.tt(T1, S, cp_, ALU.mult, ("S", ck_), ("T1",))
                self.red(sk, h8(T1), ("T1",), ("sk",))
                self.tt(h8(T1), h8(bp), b8(sk), ALU.mult, (bk_, "sk"), ("T1",))
                self.tt(S, S, wp, ALU.mult, ("S", wk_), ("S",))
                self.tt(S, S, T1, ALU.subtract, ("S", "T1"), ("S",))
                self.tt(h8(T2), h8(kp), vb, ALU.mult, (kk_, ("Vb", pb)), ("T2",))
                self.tt(S, S, T2, ALU.add, ("S", "T2"), ("S",))
                self.tt(T2, S, rp, ALU.mult, ("S", rk_), ("T2",))
                self.red(yo, h8(T2), ("T2",), (("Yb", pb),))
                yield
            P.dma("sp", yT[m].rearrange("hp p s t -> p s hp t")[:, :, :, t0:t0 + TB],
                  Yb[pb].rearrange("p (s hp) t -> p s hp t", hp=4), reads=(("Yb", pb),),
                  writes=tuple(("T", id(yT[m]), s, t0 // 128) for s in range(cfg.NSEQ)))

    def gen_chunk(self, m, C, ck):
        P = self.P
        cfg = self.cfg
        T = cfg.T
        nch = T // C
        cb = self.chunk_bufs(T)
        qT, kT, q0, q1, kh, vv, yo, dch, decT, st_f, st_b, scm, mask = cb
        YH = 32
        pstr_f = self.pstr[:, :, :].bitcast(F32)
        qT_d, kT_d, kh_d, v_d, dec_d, y_d = ck["qT"][m], ck["kT"][m], ck["kh"][m], ck["v"][m], ck["dec"][m], ck["y"][m]
        P.dma("sp", mask[0:C, 0:C], ck["mask"][0:C, 0:C], writes=("mask",))
        P.dma("sp", mask[0:C, C:2 * C], ck["mask"][0:C, 0:C], writes=("mask",))
        cnt = 0
        qs = [q0, q1]
        for s in range(cfg.NSEQ):
            for hp in range(4):
                allT = tuple(("T", id(qT_d), s, g) for g in range(cfg.TPS))
                P.dma("sp", qT[:, 0:T], qT_d[hp, :, s, :], reads=allT, writes=("c_qT",))
                P.dma("sp", kT[:, 0:T], kT_d[hp, :, s, :], reads=tuple(("T", id(kT_d), s, g) for g in range(cfg.TPS)),
                      writes=("c_kT",))
                P.dma("sp", kh[0:C, 0:nch, :], kh_d[s * T:(s + 1) * T, hp * 128:(hp + 1) * 128].rearrange(
                    "(n c) k -> c n k", c=C), reads=(("kh", m),), writes=("c_kh",))
                P.dma("sp", vv[0:C, 0:nch, :], v_d[s * T:(s + 1) * T, hp * 130:(hp + 1) * 130].rearrange(
                    "(n c) k -> c n k", c=C), reads=(("vd", id(v_d)),), writes=("c_vv",))
                P.dma("sp", dch[0:nch, :], dec_d[s * T:(s + 1) * T, hp * 128:(hp + 1) * 128].rearrange(
                    "(n c) k -> c n k", c=C)[C - 1, :, :], reads=(("dec", m),), writes=("c_dch",))
                pt = pstr_f[:, 1, 0:nch]
                P.op("pe", lambda e, pt=pt: e.transpose(pt, dch[0:nch, :], self.ident_f[0:nch, 0:nch]),
                     reads=("c_dch", "identf"), writes=(("pstr", 1),))
                self.cp(decT[:, 0:nch], pt, (("pstr", 1),), ("c_decT",))
                P.op("dve", lambda e: e.memset(q0[64:128, 0:T], 0.0), writes=("c_q0",))
                P.op("dve", lambda e: e.memset(q1[0:64, 0:T], 0.0), writes=("c_q1",))
                self.cp(q0[0:64, 0:T], qT[0:64, 0:T], ("c_qT",), ("c_q0",), eng="dve")
                self.cp(q1[64:128, 0:T], qT[64:128, 0:T], ("c_qT",), ("c_q1",), eng="dve")
                P.op("dve", lambda e: e.memset(st_f[:, 0:66], 0.0), writes=("st_f",))
                P.op("dve", lambda e: e.memset(st_b[:, 0:66], 0.0), writes=("st_b",))
                for c in range(nch):
                    cs = slice(c * C, (c + 1) * C)
                    par = c % 2
                    psc = self.pslin[0:C, 4, 0:2 * C]
                    for hh in range(2):
                        qh = qs[hh]
                        qk = "c_q%d" % hh
                        P.op("pe", lambda e, cs=cs, qh=qh, hh=hh: e.matmul(psc[:, hh * C:(hh + 1) * C], lhsT=kT[:, cs], rhs=qh[:, cs],
                                                                           start=True, stop=True),
                             reads=("c_kT", qk), writes=(("pslin", 4),))
                    yield
                    sm = scm[par][0:C, 0:2 * C]
                    self.tt(sm, psc, mask[0:C, 0:2 * C], ALU.mult, (("pslin", 4), "mask"), (("scm", par),))
                    yield
                    pso = pstr_f[0:C, 0, 0:130]
                    for hh in range(2):
                        qh = qs[hh]
                        qk = "c_q%d" % hh
                        vsl = vv[0:C, c, hh * 65:(hh + 1) * 65]
                        po = pso[:, hh * 65:(hh + 1) * 65]
                        P.op("pe", lambda e, po=po, vsl=vsl, hh=hh, sm=sm: e.matmul(po, lhsT=sm[:, hh * C:(hh + 1) * C], rhs=vsl,
                                                                                   start=True, stop=False),
                             reads=(("scm", par), "c_vv"), writes=(("pstr", 0),))
                        P.op("pe", lambda e, po=po, cs=cs, qh=qh: e.matmul(po, lhsT=qh[:, cs], rhs=st_b[:, 0:65], start=False, stop=True),
                             reads=(qk, "st_b"), writes=(("pstr", 0),))
                        pkv = pstr_f[hh * 64:(hh + 1) * 64, 1, 0:65]
                        khs = kh[0:C, c, hh * 64:(hh + 1) * 64]
                        P.op("pe", lambda e, pkv=pkv, khs=khs, vsl=vsl: e.matmul(pkv, lhsT=khs, rhs=vsl, start=True, stop=True),
                             reads=("c_kh", "c_vv"), writes=(("pstr", 1),))
                    yield
                    self.stt(st_f[:, 0:65], st_f[:, 0:65], decT[:, c:c + 1], pstr_f[:, 1, 0:65], ALU.mult, ALU.add,
                             ("st_f", "c_decT", ("pstr", 1)), ("st_f",))
                    self.cp(yo[0:C, c % YH, :], pso, (("pstr", 0),), ("c_yo",))
                    yield
                    self.cp(st_b[:, 0:65], st_f[:, 0:65], ("st_f",), ("st_b",))
                    if c % YH == YH - 1 or c == nch - 1:
                        c0 = c - (c % YH)
                        P.dma("sp", y_d[s * T + c0 * C:s * T + (c + 1) * C, hp * 130:(hp + 1) * 130].rearrange(
                            "(n c) k -> c n k", c=C), yo[0:C, 0:c - c0 + 1, :], reads=("c_yo",), writes=(("cy", m),))
                    yield

    def chunk_bufs(self, T):
        if getattr(self, "_cbufs_at", None) == id(self.P.ops["pe"]) and getattr(self, "_cbufs_phase", -1) == self._phase_id:
            return self._cbufs
        qT = self.carve(T // 2, BF16)
        kT = self.carve(T // 2, BF16)
        q0 = self.carve(T // 2, BF16)
        q1 = self.carve(T // 2, BF16)
        nmax = T // 32
        kh = self.carve(nmax * 64, BF16).rearrange("p (n k) -> p n k", k=128)
        vv = self.carve(nmax * 65, BF16).rearrange("p (n k) -> p n k", k=130)
        yo = self.carve(32 * 130).rearrange("p (n k) -> p n k", k=130)
        dch = self.carve(128)
        decT = self.carve(64)
        st_f = self.carve(66)
        st_b = self.carve(33, BF16)
        scm = [self.carve(64, BF16) for _ in range(2)]
        mask = self.carve(128)
        self._cbufs = (qT, kT, q0, q1, kh, vv, yo, dch, decT, st_f, st_b, scm, mask)
        self._cbufs_at = id(self.P.ops["pe"])
        self._cbufs_phase = self._phase_id
        self._mask_loaded = False
        return self._cbufs

    def phase_post(self, l, I, yT, ck, gate, bonus, obuf):
        P = self.P
        cfg = self.cfg
        self.carve_reset()
        o = self.carve(2048)
        y = self.carve(512)
        y65 = self.carve(520).rearrange("p (h e) -> p h e", e=65)
        gt = self.carve(512)
        W = [self.carve(512) for _ in range(3)]
        self.vt = self.carve(512).rearrange("p (a b) -> p a b", b=128)
        sv = {nm: self.carve(512) for nm in ("hgrn_norm_w", "mlstm_norm_w", "rwkv_ln_w", "rwkv_ln_b")}
        small = self.carve(64)
        for nm in sv:
            self.bcast_load(sv[nm], I[nm][l], "c_" + nm)
        h8 = lambda ap: ap.rearrange("p (h d) -> p h d", d=64)
        b8 = lambda ap8: ap8.unsqueeze(2).to_broadcast([128, 8, 64])
        wk = lambda i: ("W", i)
        for tile in range(cfg.NTILE):
            s, g = tile // cfg.TPS, tile % cfg.TPS
            rs = slice(tile * 128, (tile + 1) * 128)
            for mi, m in enumerate("ABCD"):
                if m == "D":
                    P.dma("sp", y, ck["D"]["y"][rs, :], reads=(("cy", "D"),), writes=("y",))
                else:
                    P.dma("sp", y65.rearrange("p h e -> p (h e)"), ck["y"][m][rs, :], reads=(("cy", m),), writes=("y65",))
                    self.cp(h8(y), y65[:, :, 0:64], ("y65",), ("y",), eng="dve")
                P.dma("sp", gt, gate[m][rs, :], reads=(("gate", m, tile),), writes=("gt",))
                osl = o[:, mi * 512:(mi + 1) * 512]
                if m == "C":
                    d8 = y65[:, :, 64]
                    self.ts(small[:, 0:8], d8, -1.0, None, ALU.mult, None, ("y65",), ("sm0",))
                    self.tt(small[:, 0:8], small[:, 0:8], d8, ALU.max, ("sm0", "y65"), ("sm0",))
                    self.ts(small[:, 0:8], small[:, 0:8], 1.0, None, ALU.max, None, ("sm0",), ("sm0",))
                    P.op("dve", lambda e: e.reciprocal(out=small[:, 8:16], in_=small[:, 0:8]), reads=("sm0",), writes=("sm1",))
                    self.tt(h8(y), h8(y), b8(small[:, 8:16]), ALU.mult, ("y", "sm1"), ("y",))
                if m in "ABC":
                    self.tt(W[0], y, y, ALU.mult, ("y",), (wk(0),))
                    self.red(small[:, 16:24], h8(W[0]), (wk(0),), ("sm2",))
                    self.actf(small[:, 24:32], small[:, 16:24], AF.Sqrt, ("sm2", "eps"), ("sm3",), scale=1.0 / 64,
                              bias=self.eps_t[:, 0:1])
                    P.op("dve", lambda e: e.reciprocal(out=small[:, 32:40], in_=small[:, 24:32]), reads=("sm3",), writes=("sm4",))
                    self.tt(h8(y), h8(y), b8(small[:, 32:40]), ALU.mult, ("y", "sm4"), ("y",))
                    if m == "B":
                        self.tt(y, y, sv["hgrn_norm_w"], ALU.mult, ("y", "c_hgrn_norm_w"), ("y",))
                    if m == "C":
                        self.tt(y, y, sv["mlstm_norm_w"], ALU.mult, ("y", "c_mlstm_norm_w"), ("y",))
                    self.tt(osl, y, gt, ALU.mult, ("y", "gt"), ("o",))
                else:
                    self.red(small[:, 16:24], h8(y), ("y",), ("sm2",))
                    self.ts(small[:, 16:24], small[:, 16:24], -1.0 / 64, None, ALU.mult, None, ("sm2",), ("sm2",))
                    self.tt(h8(y), h8(y), b8(small[:, 16:24]), ALU.add, ("y", "sm2"), ("y",))
                    self.tt(W[0], y, y, ALU.mult, ("y",), (wk(0),))
                    self.red(small[:, 24:32], h8(W[0]), (wk(0),), ("sm3",))
                    self.actf(small[:, 32:40], small[:, 24:32], AF.Sqrt, ("sm3", "eps"), ("sm4",), scale=1.0 / 64,
                              bias=self.eps_t[:, 1:2])
                    P.op("dve", lambda e: e.reciprocal(out=small[:, 40:48], in_=small[:, 32:40]), reads=("sm4",), writes=("sm5",))
                    self.tt(h8(y), h8(y), b8(small[:, 40:48]), ALU.mult, ("y", "sm5"), ("y",))
                    self.tt(y, y, sv["rwkv_ln_w"], ALU.mult, ("y", "c_rwkv_ln_w"), ("y",))
                    self.tt(y, y, sv["rwkv_ln_b"], ALU.add, ("y", "c_rwkv_ln_b"), ("y",))
                    P.dma("sp", W[1], bonus[rs, :], reads=(("bonus", tile),), writes=(wk(1),))
                    self.tt(y, y, W[1], ALU.add, ("y", wk(1)), ("y",))
                    self.tt(osl, y, gt, ALU.mult, ("y", "gt"), ("o",))
            P.dma("sp", obuf[rs, :], o, reads=("o",), writes=(("o", tile),))

    def phase_ffn(self, l, hbuf, obuf, wbo, wbg, wbu, wbd, wnb):
        P = self.P
        cfg = self.cfg
        NG = 4
        self.carve_linear(NG)
        actT = self.carve(44 * 256, BF16).rearrange("p (k g t) -> p k g t", g=NG, t=128)
        acc = [self.carve(2048) for _ in range(NG)]
        xT = self.xT
        for grp in range(cfg.NTILE // NG):
            for g in range(NG):
                tile = grp * NG + g
                ht = self.htile[tile % 2]
                P.dma("sp", ht, obuf[tile * 128:(tile + 1) * 128, :], reads=(("o", tile),), writes=(("htile", tile % 2),))
                self.cp(self.ub[:, 0:2048], ht, (("htile", tile % 2),), ("ub",), eng="dve")
                self.transpose_to(self.ub, "ub", xT, g, "xT", 16)

            def consume(g, cb, nc_, pst, pskey):
                self.cp(acc[g][:, cb * 512:cb * 512 + nc_], pst, (pskey,), (("acc", g),))

            self.linear_tm(xT, ("xT",), NG, 16, wbo, D, consume)
            self.resid_update(grp, NG, acc, hbuf, wnb[:, 1, :])
            for g in range(NG):
                tile = grp * NG + g
                ht = self.htile[tile % 2]
                P.dma("sp", ht, hbuf[tile * 128:(tile + 1) * 128, :], reads=(("h", tile),), writes=(("htile", tile % 2),))
                self.norm_transpose(ht, ("htile", tile % 2), wnb[:, 2, :], xT, g, "xT")
            for hb in range(DFF // 256):
                slot = hb % 2
                cb, half = hb // 2, hb % 2
                wg = self.wbuf[0][:, slot * 4096:(slot + 1) * 4096].rearrange("p (k n) -> p k n", n=256)
                wu = self.wbuf[1][:, slot * 4096:(slot + 1) * 4096].rearrange("p (k n) -> p k n", n=256)
                kg, ku = ("wbufh", 0, slot), ("wbufh", 1, slot)
                P.dma("sp", wg, wbg[cb, :, :, half * 256:(half + 1) * 256], reads=(("wb", id(wbg), cb),), writes=(kg,))
                P.dma("sp", wu, wbu[cb, :, :, half * 256:(half + 1) * 256], reads=(("wb", id(wbu), cb),), writes=(ku,))
                for j in range(2):
                    nchunk = hb * 2 + j
                    bg = self.psbank()
                    bu = self.psbank()
                    pg = self.pslin[:, bg, :]
                    pu = self.pslin[:, bu, :]
                    for kc in range(16):
                        P.op("pe", lambda e, pg=pg, kc=kc, j=j, wg=wg: e.matmul(pg, lhsT=wg[:, kc, j * 128:(j + 1) * 128],
                                                                                rhs=xT[:, kc, :, :].rearrange("p g t -> p (g t)"),
                                                                                start=(kc == 0), stop=(kc == 15)),
                             reads=("xT", kg), writes=(("pslin", bg),))
                    for kc in range(16):
                        P.op("pe", lambda e, pu=pu, kc=kc, j=j, wu=wu: e.matmul(pu, lhsT=wu[:, kc, j * 128:(j + 1) * 128],
                                                                                rhs=xT[:, kc, :, :].rearrange("p g t -> p (g t)"),
                                                                                start=(kc == 0), stop=(kc == 15)),
                             reads=("xT", ku), writes=(("pslin", bu),))
                    st = self.stage[self.ssel % 4]
                    skey = ("stage", self.ssel % 4)
                    self.ssel += 1
                    self.actf(st, pg, AF.Silu, (("pslin", bg),), (skey,))
                    self.tt(actT[:, nchunk, :, :].rearrange("p g t -> p (g t)"), st, pu, ALU.mult, (skey, ("pslin", bu)),
                            ("actT",))

            def consume2(g, cb, nc_, pst, pskey):
                self.cp(acc[g][:, cb * 128:cb * 128 + nc_], pst, (pskey,), (("acc", g),))

            self.linear_tm(actT, ("actT",), NG, 44, wbd, D, consume2, CB=128)
            self.resid_update(grp, NG, acc, hbuf, wnb[:, 3, :])

    def resid_update(self, grp, NG, acc, hbuf, wn):
        P = self.P
        for g in range(NG):
            tile = grp * NG + g
            ht = self.htile[tile % 2]
            hk = ("htile", tile % 2)
            P.dma("sp", ht, hbuf[tile * 128:(tile + 1) * 128, :], reads=(("h", tile),), writes=(hk,))
            self.rstd_of(acc[g], ("acc", g), 2048, self.eps_t[:, 0:1])
            self.stt(acc[g], acc[g], self.ss_t[:, 2:3], wn, ALU.mult, ALU.mult, (("acc", g), "ss2", "wnb"), (("acc", g),))
            self.tt(ht, ht, acc[g], ALU.add, (hk, ("acc", g)), (hk,))
            P.dma("sp", hbuf[tile * 128:(tile + 1) * 128, :], ht, reads=(hk,), writes=(("h", tile),))


def make_consts(T):
    half = 32
    inv_freq = 10000.0 ** (-np.arange(half, dtype=np.float32) / half)
    ang = np.arange(T, dtype=np.float32)[:, None] * inv_freq[None, :]
    cossin = np.concatenate([np.cos(ang), np.sin(ang)], axis=1).astype(np.float32)
    sel = np.zeros((32, TB, 128), np.float32)
    for tl in range(TB):
        for m in range(128):
            sel[(m // 64) * TB + tl, tl, m] = 1.0
    gam = np.zeros((128, 2, 4, 64), np.float32)
    for p in range(128):
        for hp in range(4):
            h = 2 * hp + p // 64
            gam[p, :, hp, :] = 1.0 - 2.0 ** (-5.0 - h)
    C = CHK["A"]
    j = (np.arange(128) % C).astype(np.float64)
    gh = 1.0 - 2.0 ** (-5.0 - np.arange(8, dtype=np.float64))
    atab = np.zeros((4, 128, 8, 64), np.float64)
    atab[0] = (gh[None, :] ** (j[:, None] + 1.0))[:, :, None]
    atab[1] = (0.125 * gh[None, :] ** (-(j[:, None] + 1.0)))[:, :, None]
    atab[2] = (0.125 * gh[None, :] ** (C - 1.0 - j[:, None]))[:, :, None]
    atab[3] = (gh ** float(C))[None, :, None]
    idx = np.arange(128)
    def trib(c):
        same = (idx[:, None] // c) == (idx[None, :] // c)
        return (same & (idx[:, None] <= idx[None, :])).astype(np.float32), same.astype(np.float32)
    t64, b64 = trib(64)
    t32, b32 = trib(32)
    trimats = np.stack([t64, b64, t32, b32]).astype(np.float32)
    cmask = (np.arange(64)[:, None] <= np.arange(64)[None, :]).astype(np.float32)
    i32 = np.arange(64) % 32
    dmasks = np.stack([(i32[:, None] < i32[None, :]), (i32[:, None] > i32[None, :]), (i32[:, None] <= i32[None, :])]).astype(
        np.float32)
    return dict(cossin=cossin, sel=sel, gamA=gam.reshape(128, 512), atab=atab.reshape(4, 128, 512).astype(np.float32),
                trimats=trimats, cmask=cmask, dmasks=dmasks,
                ident_b=np.eye(128).astype(ml_dtypes.bfloat16), ident_f=np.eye(128, dtype=np.float32))


def make_shared(inp, L, T):
    f = lambda a: np.ascontiguousarray(np.asarray(a, dtype=np.float32))
    sh = dict(w_in=f(inp["w_in"]), w_out=f(inp["w_out"]), w_ffn_gate=f(inp["w_ffn_gate"]), w_ffn_up=f(inp["w_ffn_up"]),
              w_ffn_down=f(inp["w_ffn_down"]))
    sh["norms"] = np.ascontiguousarray(np.stack([f(inp["norm_pre_mix"]), f(inp["norm_post_mix"]), f(inp["norm_pre_ffn"]),
                                                 f(inp["norm_post_ffn"])], axis=1))
    for nm in SMALL + ["hgrn_lb_logits", "mlstm_conv_w", "mlstm_conv_b", "rwkv_mu"]:
        sh[nm] = f(inp[nm])
    sh["mlstm_if_bias"] = np.ascontiguousarray(np.concatenate([f(inp["mlstm_i_bias"]), f(inp["mlstm_f_bias"])], axis=1))
    up = np.zeros((L, 128, 1024), np.float32)
    up[:, 0:64, 0:512] = f(inp["rwkv_w_up"])
    up[:, 64:128, 512:1024] = f(inp["rwkv_a_up"])
    sh["rwkv_up_pad"] = up
    sh["rwkv_g_up"] = f(inp["rwkv_g_up"])
    if L > 1:
        sh["rwkv_v0"] = f(inp["rwkv_v0"])
        sh["rwkv_v_down"] = f(inp["rwkv_v_down"])
        sh["rwkv_v_up"] = f(inp["rwkv_v_up"])
    else:
        sh["rwkv_v0"] = np.zeros((1, G), np.float32)
        sh["rwkv_v_down"] = np.zeros((1, G, 32), np.float32)
        sh["rwkv_v_up"] = np.zeros((1, 32, G), np.float32)
    sh.update(make_consts(T))
    return sh


_CACHE = {}


def kernel(**inp):
    x = np.asarray(inp["x"], dtype=np.float32)
    B, T, _ = x.shape
    L = inp["w_in"].shape[0]
    ncores = 8
    nseq = B // ncores
    cfg = Cfg(T=T, NSEQ=nseq, L=L)
    nc = Builder(cfg).build()
    sh = make_shared(inp, L, T)
    in_maps = []
    for c in range(ncores):
        m = dict(sh)
        m["x"] = np.ascontiguousarray(x[c * nseq:(c + 1) * nseq].reshape(nseq * T, D))
        in_maps.append(m)
    res = run_bass_kernel_spmd(nc, in_maps, core_ids=list(range(ncores)))
    outs = [np.asarray(r["out"]).reshape(nseq, T, D) for r in res.results]
    return np.concatenate(outs, axis=0).astype(np.float32)
```
